# Optimizing a Trainium2 kernel written in Bass

```python
import math
import jax
import jax.numpy as jnp
from jax import lax
import numpy as np

D_MODEL = 1024
BATCH = 8
SEQ = 2048
DEPTH = 2

N_META = 16
RMS_EPS = 1e-6
CONV_WIDTH = 4
SSD_HEADS = 16
SSD_HEAD_DIM = 64
SSD_WIDTH = SSD_HEADS * SSD_HEAD_DIM
SSD_GROUPS = 2
SSD_HEADS_PER_GROUP = SSD_HEADS // SSD_GROUPS
SSD_STATE = 128
SSD_CHUNK = 128
SSD_CONV_DIM = SSD_WIDTH + 2 * SSD_GROUPS * SSD_STATE
LRU_WIDTH = D_MODEL
LRU_HEADS = 16
LRU_BLOCK = LRU_WIDTH // LRU_HEADS
LRU_C = 8.0
PROJ_COLS = SSD_WIDTH + SSD_CONV_DIM + SSD_HEADS + 2 * LRU_WIDTH
MIX_WIDTH = SSD_WIDTH + LRU_WIDTH
POOL_WINDOWS = (2, 4, 8, 16)
POOL_GROUPS = len(POOL_WINDOWS)
POOL_GROUP_DIM = D_MODEL // POOL_GROUPS
POOL_MAX_WINDOW = 16
MOE_GROUPS = 4
MOE_EXPERTS_PER_GROUP = 4
MOE_EXPERTS = MOE_GROUPS * MOE_EXPERTS_PER_GROUP
MOE_TOP_K = 2
D_EXPERT = D_MODEL // 2
N_EVEN = (DEPTH + 1) // 2
N_ODD = DEPTH // 2

kernel_name = "hybrid_ssd_rglru_pool_hmoe_meta"


def rms_norm(x, gain):
    xf = x.astype(jnp.float32)
    y = xf * lax.rsqrt(jnp.mean(xf * xf, axis=-1, keepdims=True) + RMS_EPS)
    return (y * gain.astype(jnp.float32)).astype(x.dtype)


def causal_depthwise_conv(x, w, b):
    c = x.shape[-1]
    y = lax.conv_general_dilated(
        x, w[:, None, :].astype(x.dtype), (1,), [(CONV_WIDTH - 1, 0)],
        dimension_numbers=("NWC", "WIO", "NWC"), feature_group_count=c)
    return y + b.astype(x.dtype)


def ssd_chunked_scan(xdt, adt, b_mat, c_mat):
    bsz, t = xdt.shape[:2]
    nc, l = t // SSD_CHUNK, SSD_CHUNK
    g, e, p, n = SSD_GROUPS, SSD_HEADS_PER_GROUP, SSD_HEAD_DIM, SSD_STATE
    xc = xdt.reshape(bsz, nc, l, g, e, p)
    bc = b_mat.reshape(bsz, nc, l, g, n)
    cc = c_mat.reshape(bsz, nc, l, g, n)
    ac = adt.reshape(bsz, nc, l, g, e).transpose(0, 3, 4, 1, 2)
    a_cs = jnp.cumsum(ac, axis=-1)
    causal = jnp.tril(jnp.ones((l, l), dtype=bool))
    seg = a_cs[..., :, None] - a_cs[..., None, :]
    decay = jnp.exp(jnp.where(causal, seg, -jnp.inf))
    cb = jnp.einsum("bclgn,bcsgn->bgcls", cc, bc)
    y_diag = jnp.einsum("bgecls,bcsgep->bclgep", cb[:, :, None] * decay, xc)
    decay_to_end = jnp.exp(a_cs[..., -1:] - a_cs)
    states = jnp.einsum("bclgn,bgecl,bclgep->bcgepn", bc, decay_to_end, xc)
    chunk_decay = jnp.pad(a_cs[..., -1], [(0, 0)] * 3 + [(1, 0)])
    ccs = jnp.cumsum(chunk_decay, axis=-1)
    cseg = ccs[..., :, None] - ccs[..., None, :]
    ccausal = jnp.tril(jnp.ones((nc + 1, nc + 1), dtype=bool))
    chunk_trans = jnp.exp(jnp.where(ccausal, cseg, -jnp.inf))
    states = jnp.concatenate([jnp.zeros_like(states[:, :1]), states], axis=1)
    states_in = jnp.einsum("bgezc,bcgepn->bzgepn", chunk_trans, states)[:, :-1]
    in_decay = jnp.exp(a_cs).transpose(0, 3, 4, 1, 2)[..., None]
    y_off = jnp.einsum("bclgn,bcgepn->bclgep", cc, states_in) * in_decay
    return (y_diag + y_off).reshape(bsz, t, g, e, p)


def ssd_mixer(z, xbc, dt_raw, conv_w, conv_b, dt_bias, a_log, d_skip, norm_gain):
    f32 = jnp.float32
    bsz, l, _ = xbc.shape
    g, e, p, n = SSD_GROUPS, SSD_HEADS_PER_GROUP, SSD_HEAD_DIM, SSD_STATE
    xbc = jax.nn.silu(causal_depthwise_conv(xbc, conv_w, conv_b)).astype(f32)
    xh = xbc[..., :SSD_WIDTH].reshape(bsz, l, g, e, p)
    b_mat = xbc[..., SSD_WIDTH:SSD_WIDTH + g * n].reshape(bsz, l, g, n)
    c_mat = xbc[..., SSD_WIDTH + g * n:].reshape(bsz, l, g, n)
    dt = jax.nn.softplus(dt_raw.astype(f32) + dt_bias.astype(f32)).reshape(bsz, l, g, e)
    a = -jnp.exp(a_log.astype(f32)).reshape(g, e)
    pad = SSD_CHUNK - N_META
    def front(t):
        return jnp.pad(t, [(0, 0), (pad, 0)] + [(0, 0)] * (t.ndim - 2))
    y = ssd_chunked_scan(front(xh * dt[..., None]), front(dt * a), front(b_mat), front(c_mat))[:, pad:]
    y = y + d_skip.astype(f32).reshape(g, e)[..., None] * xh
    y = y.reshape(bsz, l, SSD_WIDTH) * jax.nn.silu(z.astype(f32))
    yg = y.reshape(bsz, l, g, SSD_WIDTH // g)
    yg = yg * lax.rsqrt(jnp.mean(yg * yg, axis=-1, keepdims=True) + RMS_EPS)
    return (yg.reshape(bsz, l, SSD_WIDTH) * norm_gain.astype(f32)).astype(z.dtype)


def rg_lru(xb, w_a, b_a, w_x, b_x, lam):
    f32 = jnp.float32
    bsz, l, _ = xb.shape
    xf = xb.astype(f32)
    xh = xf.reshape(bsz, l, LRU_HEADS, LRU_BLOCK)
    r = jax.nn.sigmoid(jnp.einsum("blhi,hij->blhj", xh, w_a.astype(f32)).reshape(bsz, l, LRU_WIDTH) + b_a.astype(f32))
    i = jax.nn.sigmoid(jnp.einsum("blhi,hij->blhj", xh, w_x.astype(f32)).reshape(bsz, l, LRU_WIDTH) + b_x.astype(f32))
    log_a = -LRU_C * r * jax.nn.softplus(-lam.astype(f32))
    a = jnp.exp(log_a)
    mult = jnp.sqrt(-jnp.expm1(2.0 * log_a))
    first = (jnp.arange(l) == 0)[None, :, None]
    mult = jnp.where(first, 1.0, mult)
    u = mult * (i * xf)

    def combine(lhs, rhs):
        a1, b1 = lhs
        a2, b2 = rhs
        return a1 * a2, a2 * b1 + b2

    _, h = lax.associative_scan(combine, (a, u), axis=1)
    return h.astype(xb.dtype)


def ssd_lru_mixer(hn, w_in, ssd_conv_w, ssd_conv_b, ssd_dt_bias, ssd_a_log, ssd_d, ssd_norm,
                  lru_conv_w, lru_conv_b, lru_w_a, lru_b_a, lru_w_x, lru_b_x, lru_lambda, w_out):
    proj = hn @ w_in
    i1 = SSD_WIDTH
    i2 = i1 + SSD_CONV_DIM
    i3 = i2 + SSD_HEADS
    i4 = i3 + LRU_WIDTH
    z, xbc, dt_raw, lru_gate, lru_in = jnp.split(proj, [i1, i2, i3, i4], axis=-1)
    y_ssd = ssd_mixer(z, xbc, dt_raw, ssd_conv_w, ssd_conv_b, ssd_dt_bias, ssd_a_log, ssd_d, ssd_norm)
    xb = causal_depthwise_conv(lru_in, lru_conv_w, lru_conv_b)
    y_lru = rg_lru(xb, lru_w_a, lru_b_a, lru_w_x, lru_b_x, lru_lambda) * jax.nn.gelu(lru_gate, approximate=True)
    return jnp.concatenate([y_ssd, y_lru.astype(y_ssd.dtype)], axis=-1) @ w_out


def multiscale_pool_mixer(hn, pool_w, pool_b, pool_scale):
    f32 = jnp.float32
    bsz, l, d = hn.shape
    hf = hn.astype(f32)
    csum = jnp.concatenate([jnp.zeros((bsz, POOL_MAX_WINDOW, d), f32), jnp.cumsum(hf, axis=1)], axis=1)
    pos = jnp.arange(l)
    outs = []
    for g, w in enumerate(POOL_WINDOWS):
        lo, hi = g * POOL_GROUP_DIM, (g + 1) * POOL_GROUP_DIM
        wsum = csum[:, POOL_MAX_WINDOW:, lo:hi] - csum[:, POOL_MAX_WINDOW - w:POOL_MAX_WINDOW - w + l, lo:hi]
        count = jnp.minimum(pos + 1, w).astype(f32)[None, :, None]
        outs.append(wsum / count - hf[..., lo:hi])
    pooled = jnp.stack(outs, axis=2)
    y = jnp.einsum("blgi,gij->blgj", pooled, pool_w.astype(f32)).reshape(bsz, l, d) + pool_b.astype(f32)
    return (y * pool_scale.astype(f32)).astype(hn.dtype)


def hierarchical_moe(hn, w_group, b_group, w_expert, b_expert, w_gate, w_up, w_down):
    f32 = jnp.float32
    bsz, l, d = hn.shape
    tok = hn.reshape(-1, d)
    tf = tok.astype(f32)
    p_group = jax.nn.softmax(tf @ w_group.astype(f32) + b_group.astype(f32), axis=-1)
    g_sel = jnp.argmax(p_group, axis=-1)
    p_g = jnp.take_along_axis(p_group, g_sel[:, None], axis=-1)
    fine = (tf @ w_expert.astype(f32) + b_expert.astype(f32)).reshape(-1, MOE_GROUPS, MOE_EXPERTS_PER_GROUP)
    fine = jnp.take_along_axis(fine, g_sel[:, None, None], axis=1)[:, 0]
    top_p, top_i = lax.top_k(jax.nn.softmax(fine, axis=-1), MOE_TOP_K)
    top_p = top_p / jnp.sum(top_p, axis=-1, keepdims=True)
    expert_idx = g_sel[:, None] * MOE_EXPERTS_PER_GROUP + top_i
    gates = jnp.sum(jax.nn.one_hot(expert_idx, MOE_EXPERTS, dtype=f32) * (p_g * top_p)[..., None], axis=1)
    out = jnp.zeros_like(tf)
    for e in range(MOE_EXPERTS):
        hid = jax.nn.silu(tok @ w_gate[e]) * (tok @ w_up[e])
        out = out + gates[:, e:e + 1] * (hid @ w_down[e]).astype(f32)
    return out.astype(hn.dtype).reshape(bsz, l, d)


def setup_inputs(seed: int = 0) -> dict:
    key = jax.random.key(seed)
    ks = iter(jax.random.split(key, 48))
    f32 = jnp.float32
    ne, no = N_EVEN, N_ODD

    def nrm(shape, scale):
        return scale * jax.random.normal(next(ks), shape, f32)

    def gain(shape):
        return 1.0 + 0.05 * jax.random.normal(next(ks), shape, f32)

    x = nrm((BATCH, SEQ, D_MODEL), 1.0)
    meta_tokens = nrm((N_META, D_MODEL), 1.0)
    norm_final = gain((D_MODEL,))
    mix_norm_even = gain((ne, D_MODEL))
    w_in = nrm((ne, D_MODEL, PROJ_COLS), D_MODEL ** -0.5)
    ssd_conv_w = nrm((ne, CONV_WIDTH, SSD_CONV_DIM), CONV_WIDTH ** -0.5)
    ssd_conv_b = nrm((ne, SSD_CONV_DIM), 0.02)
    dt0 = jnp.exp(jax.random.uniform(next(ks), (ne, SSD_HEADS), f32, math.log(1e-3), math.log(0.1)))
    ssd_dt_bias = dt0 + jnp.log(-jnp.expm1(-dt0))
    ssd_a_log = jnp.log(jax.random.uniform(next(ks), (ne, SSD_HEADS), f32, 1.0, 16.0))
    ssd_d = gain((ne, SSD_HEADS))
    ssd_norm = gain((ne, SSD_WIDTH))
    lru_conv_w = nrm((ne, CONV_WIDTH, LRU_WIDTH), CONV_WIDTH ** -0.5)
    lru_conv_b = nrm((ne, LRU_WIDTH), 0.02)
    lru_w_a = nrm((ne, LRU_HEADS, LRU_BLOCK, LRU_BLOCK), LRU_BLOCK ** -0.5)
    lru_b_a = nrm((ne, LRU_WIDTH), 0.02)
    lru_w_x = nrm((ne, LRU_HEADS, LRU_BLOCK, LRU_BLOCK), LRU_BLOCK ** -0.5)
    lru_b_x = nrm((ne, LRU_WIDTH), 0.02)
    a0 = jax.random.uniform(next(ks), (ne, LRU_WIDTH), f32, 0.9, 0.999)
    s0 = a0 ** (1.0 / LRU_C)
    lru_lambda = jnp.log(s0) - jnp.log1p(-s0)
    w_out = nrm((ne, MIX_WIDTH, D_MODEL), MIX_WIDTH ** -0.5)
    mix_norm_odd = gain((no, D_MODEL))
    pool_w = nrm((no, POOL_GROUPS, POOL_GROUP_DIM, POOL_GROUP_DIM), POOL_GROUP_DIM ** -0.5)
    pool_b = nrm((no, D_MODEL), 0.02)
    pool_scale = gain((no, D_MODEL))
    ffn_norm = gain((DEPTH, D_MODEL))
    router_group_w = nrm((DEPTH, D_MODEL, MOE_GROUPS), D_MODEL ** -0.5)
    router_group_b = nrm((DEPTH, MOE_GROUPS), 0.01)
    router_expert_w = nrm((DEPTH, D_MODEL, MOE_EXPERTS), D_MODEL ** -0.5)
    router_expert_b = nrm((DEPTH, MOE_EXPERTS), 0.01)
    expert_w_gate = nrm((DEPTH, MOE_EXPERTS, D_MODEL, D_EXPERT), D_MODEL ** -0.5)
    expert_w_up = nrm((DEPTH, MOE_EXPERTS, D_MODEL, D_EXPERT), D_MODEL ** -0.5)
    expert_w_down = nrm((DEPTH, MOE_EXPERTS, D_EXPERT, D_MODEL), D_EXPERT ** -0.5)
    return {
        "x": x, "meta_tokens": meta_tokens, "norm_final": norm_final,
        "mix_norm_even": mix_norm_even, "w_in": w_in,
        "ssd_conv_w": ssd_conv_w, "ssd_conv_b": ssd_conv_b, "ssd_dt_bias": ssd_dt_bias,
        "ssd_a_log": ssd_a_log, "ssd_d": ssd_d, "ssd_norm": ssd_norm,
        "lru_conv_w": lru_conv_w, "lru_conv_b": lru_conv_b, "lru_w_a": lru_w_a, "lru_b_a": lru_b_a,
        "lru_w_x": lru_w_x, "lru_b_x": lru_b_x, "lru_lambda": lru_lambda, "w_out": w_out,
        "mix_norm_odd": mix_norm_odd, "pool_w": pool_w, "pool_b": pool_b, "pool_scale": pool_scale,
        "ffn_norm": ffn_norm, "router_group_w": router_group_w, "router_group_b": router_group_b,
        "router_expert_w": router_expert_w, "router_expert_b": router_expert_b,
        "expert_w_gate": expert_w_gate, "expert_w_up": expert_w_up, "expert_w_down": expert_w_down,
    }


def reference(x, meta_tokens, norm_final, mix_norm_even, w_in, ssd_conv_w, ssd_conv_b, ssd_dt_bias,
              ssd_a_log, ssd_d, ssd_norm, lru_conv_w, lru_conv_b, lru_w_a, lru_b_a, lru_w_x, lru_b_x,
              lru_lambda, w_out, mix_norm_odd, pool_w, pool_b, pool_scale, ffn_norm, router_group_w,
              router_group_b, router_expert_w, router_expert_b, expert_w_gate, expert_w_up, expert_w_down):
    bsz = x.shape[0]
    meta = jnp.broadcast_to(meta_tokens.astype(x.dtype)[None], (bsz, N_META, D_MODEL))
    h = jnp.concatenate([meta, x], axis=1)
    for layer in range(DEPTH):
        j = layer // 2
        if layer % 2 == 0:
            h = h + ssd_lru_mixer(
                rms_norm(h, mix_norm_even[j]), w_in[j], ssd_conv_w[j], ssd_conv_b[j], ssd_dt_bias[j],
                ssd_a_log[j], ssd_d[j], ssd_norm[j], lru_conv_w[j], lru_conv_b[j], lru_w_a[j], lru_b_a[j],
                lru_w_x[j], lru_b_x[j], lru_lambda[j], w_out[j])
        else:
            h = h + multiscale_pool_mixer(rms_norm(h, mix_norm_odd[j]), pool_w[j], pool_b[j], pool_scale[j])
        h = h + hierarchical_moe(
            rms_norm(h, ffn_norm[layer]), router_group_w[layer], router_group_b[layer],
            router_expert_w[layer], router_expert_b[layer], expert_w_gate[layer], expert_w_up[layer],
            expert_w_down[layer])
    return rms_norm(h[:, N_META:], norm_final)
```

```python
from contextlib import ExitStack
import numpy as np
import concourse.bass as bass
import concourse.mybir as mybir
from concourse.bass_utils import run_bass_kernel_spmd

F32 = mybir.dt.float32
BF16 = mybir.dt.bfloat16
AF = mybir.ActivationFunctionType
ALU = mybir.AluOpType
AX = mybir.AxisListType

COMPUTE = ("pe", "act", "dve", "pool")
NDSEM = 16
SAME_ENGINE_GAP = 1 << 30

T = 2176
NT = 17
PADN = 112
D = 1024
TB = [(0, 512), (512, 512), (1024, 512), (1536, 512), (2048, 128)]
EPS = 1e-6
MIX0_PARTS = ("lru", "ssd")
SSD_STOP = None
REORDER = True


class Sched:
    def __init__(self, nc):
        self.nc = nc
        self.ops = {e: [] for e in ("pe", "act", "dve", "pool", "sp")}
        self.ccount = {e: 0 for e in COMPUTE}
        self.dcount = {"sp": 0, "pool": 0}
        self.last_w = {}
        self.readers = {}
        self.sig = {e: set() for e in COMPUTE}
        self.out_dmas = []
        self.pending_bar = {}
        self.ps_last = {}
        self.epoch = 0

    def _deps(self, tok, r, w, eng):
        deps = set()
        for k in r:
            t = self.last_w.get(k)
            if t is not None:
                deps.add(t)
        for k in w:
            t = self.last_w.get(k)
            if t is not None:
                deps.add(t)
            for t in self.readers.get(k, ()):
                deps.add(t)
        for k in w:
            self.last_w[k] = tok
            self.readers[k] = []
        for k in r:
            if k in w:
                continue
            self.readers.setdefault(k, []).append(tok)
        for k in set(r) | set(w):
            if isinstance(k, tuple) and k[0] == "ps":
                d = self.ps_last.setdefault(k, {})
                for oe in list(d):
                    if oe != eng:
                        deps.update(d[oe])
                        d[oe] = []
                d.setdefault(eng, []).append(tok)
        bar = self.pending_bar.pop(eng) if eng in self.pending_bar else set()
        deps.discard(tok)
        return deps, bar

    def barrier(self):
        toks = set()
        for e in COMPUTE:
            if self.ccount[e] > 0:
                toks.add(("c", e, self.ccount[e] - 1))
        for q in ("sp", "pool"):
            for k in range(max(0, self.dcount[q] - NDSEM), self.dcount[q]):
                toks.add(("d", q, k))
        for e in ("pe", "act", "dve", "pool", "sp"):
            self.pending_bar[e] = set(toks) | self.pending_bar.get(e, set())
        self.epoch += 1

    def add(self, eng, fn, r=(), w=(), cost=0.3):
        seq = self.ccount[eng]
        self.ccount[eng] += 1
        tok = ("c", eng, seq)
        deps, bar = self._deps(tok, tuple(r), tuple(w), eng)
        self.ops[eng].append(["c", fn, deps, seq, cost, self.epoch, bar])
        return tok

    def reorder(self, window=96, lat=0.35):
        RE = ("pe", "act", "dve")
        fin = {}
        etime = {e: 0.0 for e in self.ops}
        new_order = {e: [] for e in RE}
        ptr = {e: 0 for e in self.ops}
        fence = 0.0
        tokof = lambda e, op: ("c", e, op[3]) if op[0] == "c" else ("d", e, op[3])
        for ep in range(self.epoch + 1):
            seg = {}
            for e in self.ops:
                lst = self.ops[e]
                i = ptr[e]
                j = i
                while j < len(lst) and lst[j][5] == ep:
                    j += 1
                seg[e] = lst[i:j]
                ptr[e] = j
            for e in seg:
                etime[e] = max(etime[e], fence)
            left = {e: list(seg[e]) for e in seg}
            total = sum(len(v) for v in left.values())
            while total:
                best = None
                for e, lst in left.items():
                    if not lst:
                        continue
                    cands = lst[:window] if e in RE else lst[:1]
                    for pos, op in enumerate(cands):
                        ready = 0.0
                        ok = True
                        for t in op[2]:
                            f = fin.get(t)
                            if f is None:
                                ok = False
                                break
                            if t[1] != e:
                                f += lat
                            if f > ready:
                                ready = f
                        if not ok:
                            continue
                        start = max(etime[e], ready)
                        key = (start, pos)
                        if best is None or key < best[0]:
                            best = (key, e, pos, op, start)
                        if start <= etime[e]:
                            break
                assert best is not None, "scheduler stuck"
                _, e, pos, op, start = best
                left[e].pop(pos)
                total -= 1
                if op[0] == "c":
                    etime[e] = start + op[4]
                    fin[tokof(e, op)] = etime[e]
                else:
                    etime[e] = start + 0.5
                    fin[tokof(e, op)] = start + op[4]
                if e in RE:
                    new_order[e].append(op)
            fence = max([fence] + list(etime.values()) + [fin[tokof(e, op)] for e in seg for op in seg[e]])
        remap = {}
        for e in RE:
            bars = {}
            for op in new_order[e]:
                if op[6]:
                    bars.setdefault(op[5], set()).update(op[6])
                    op[6] = set()
            seen_ep = set()
            for i, op in enumerate(new_order[e]):
                if op[5] not in seen_ep:
                    seen_ep.add(op[5])
                    op[6] = bars.get(op[5], set())
                remap[("c", e, op[3])] = ("c", e, i)
                op[3] = i
            self.ops[e] = new_order[e]
        for e, lst in self.ops.items():
            for op in lst:
                op[2] = {remap.get(t, t) for t in op[2]}
                op[6] = {remap.get(t, t) for t in op[6]}
        self.model_time = max(etime.values())

    def dma(self, q, fn, r=(), w=(), is_output=False, cost=3.0):
        k = self.dcount[q]
        self.dcount[q] += 1
        tok = ("d", q, k)
        deps, bar = self._deps(tok, tuple(r), tuple(w), q)
        self.ops[q].append(["d", fn, deps, k, cost, self.epoch, bar])
        if is_output:
            self.out_dmas.append(tok)
        return tok

    def emit(self):
        nc = self.nc
        for e, lst in self.ops.items():
            for op in lst:
                op[2] = set(op[2]) | set(op[6])
        for e, lst in self.ops.items():
            seen_c = {f: -1 for f in COMPUTE}
            for kind, fn, deps, seq, _c, _e, _b in lst:
                cw = {}
                for t in deps:
                    if t[0] != "c":
                        continue
                    f, s = t[1], t[2]
                    if f == e and kind == "c":
                        if e == "pe":
                            continue
                        if seq - s > SAME_ENGINE_GAP:
                            continue
                    if s <= seen_c[f]:
                        continue
                    cw[f] = max(cw.get(f, -1), s)
                for f, s in cw.items():
                    seen_c[f] = s
                    self.sig[f].add(s)
        sigidx = {}
        for e in COMPUTE:
            s = sorted(self.sig[e])
            sigidx[e] = {seq: i + 1 for i, seq in enumerate(s)}
        with ExitStack() as st:
            csem = {e: st.enter_context(nc.semaphore("c_" + e)) for e in COMPUTE}
            dsem = {q: [st.enter_context(nc.semaphore(f"d_{q}{i}")) for i in range(NDSEM)]
                    for q in ("sp", "pool")}
            block = st.enter_context(nc.Block())

            def run(e, eng):
                seen_c = {f: -1 for f in COMPUTE}
                seen_d = {}
                for kind, fn, deps, seq, _c, _e, _b in self.ops[e]:
                    cw = {}
                    for t in deps:
                        if t[0] == "c":
                            f, s = t[1], t[2]
                            if f == e and kind == "c":
                                if e == "pe":
                                    continue
                                if seq - s > SAME_ENGINE_GAP:
                                    continue
                            if s <= seen_c[f]:
                                continue
                            cw[f] = max(cw.get(f, -1), s)
                        else:
                            q, k = t[1], t[2]
                            key = (q, k % NDSEM)
                            val = 16 * (k // NDSEM + 1)
                            if seen_d.get(key, 0) >= val:
                                continue
                            seen_d[key] = val
                            eng.wait_ge(dsem[q][k % NDSEM], val)
                    for f, s in cw.items():
                        seen_c[f] = s
                        eng.wait_ge(csem[f], sigidx[f][s])
                    if kind == "c":
                        ins = fn(eng)
                        if seq in sigidx[e]:
                            ins.then_inc(csem[e], 1)
                    else:
                        k = seq
                        if k >= NDSEM:
                            key = (e, k % NDSEM)
                            val = 16 * (k // NDSEM)
                            if seen_d.get(key, 0) < val:
                                seen_d[key] = val
                                eng.wait_ge(dsem[e][k % NDSEM], val)
                        ins = fn(eng)
                        ins.then_inc(dsem[e][k % NDSEM], 16)
                if e == "sp":
                    for t in self.out_dmas:
                        q, k = t[1], t[2]
                        eng.wait_ge(dsem[q][k % NDSEM], 16 * (k // NDSEM + 1))

            @block.tensor
            def _(eng):
                run("pe", eng)

            @block.scalar
            def _(eng):
                run("act", eng)

            @block.vector
            def _(eng):
                run("dve", eng)

            @block.gpsimd
            def _(eng):
                run("pool", eng)

            @block.sync
            def _(eng):
                run("sp", eng)


COLP = {}
_o = 0
for _n, _c in [("mix_even", 8), ("ffn0", 8), ("ffn1", 8), ("mix_odd", 8), ("ssd_cw", 48), ("ssd_cb", 12),
               ("ssd_norm", 8), ("lru_cw", 32), ("lru_cb", 8), ("lru_ba", 8), ("lru_bx", 8), ("lru_lam", 8)]:
    COLP[_n] = (_o, _c)
    _o += _c
NCOL = _o
ROWP = {}
_o = 0
for _n, _c in [("dt_bias", 16), ("a_log", 16), ("ssd_d", 16), ("rb0", 20), ("rb1", 20), ("rc", 64)]:
    ROWP[_n] = (_o, _c)
    _o += _c
NROW = _o


def _fm(v):
    v = np.asarray(v, np.float32).reshape(-1, 128)
    return np.ascontiguousarray(v.T)


def pack_inputs(inp):
    f = lambda a: np.ascontiguousarray(np.asarray(a, np.float32))
    colp = np.zeros((128, NCOL), np.float32)

    def put(name, arr):
        o, c = COLP[name]
        assert arr.shape == (128, c), (name, arr.shape)
        colp[:, o:o + c] = arr

    put("mix_even", _fm(inp["mix_norm_even"][0]))
    put("ffn0", _fm(inp["ffn_norm"][0]))
    put("ffn1", _fm(inp["ffn_norm"][1]))
    put("mix_odd", _fm(inp["mix_norm_odd"][0]))
    cw = np.asarray(inp["ssd_conv_w"][0], np.float32)
    put("ssd_cw", np.concatenate([_fm(cw[k]) for k in range(4)], axis=1).reshape(128, 4, 12).transpose(0, 2, 1).reshape(128, 48))
    put("ssd_cb", _fm(inp["ssd_conv_b"][0]))
    put("ssd_norm", _fm(inp["ssd_norm"][0]))
    lw = np.asarray(inp["lru_conv_w"][0], np.float32)
    put("lru_cw", np.concatenate([_fm(lw[k]) for k in range(4)], axis=1).reshape(128, 4, 8).transpose(0, 2, 1).reshape(128, 32))
    put("lru_cb", _fm(inp["lru_conv_b"][0]))
    put("lru_ba", _fm(inp["lru_b_a"][0]))
    put("lru_bx", _fm(inp["lru_b_x"][0]))
    put("lru_lam", _fm(inp["lru_lambda"][0]))

    rowp = np.zeros((1, NROW), np.float32)

    def putr(name, arr):
        o, c = ROWP[name]
        rowp[0, o:o + c] = np.asarray(arr, np.float32).reshape(-1)

    putr("dt_bias", inp["ssd_dt_bias"][0])
    putr("a_log", inp["ssd_a_log"][0])
    putr("ssd_d", inp["ssd_d"][0])
    putr("rb0", np.concatenate([np.asarray(inp["router_group_b"][0]), np.asarray(inp["router_expert_b"][0])]))
    putr("rb1", np.concatenate([np.asarray(inp["router_group_b"][1]), np.asarray(inp["router_expert_b"][1])]))
    rc = np.zeros((4, 16), np.float32)
    for g, w in enumerate((2, 4, 8, 16)):
        rc[g] = 1.0 / np.minimum(np.arange(16) + 1, w)
    putr("rc", rc)

    w_in = f(inp["w_in"][0])
    cols = np.concatenate([np.arange(0, 2560), np.arange(2576, 4624)])
    w_in_r = np.ascontiguousarray(w_in[:, cols].reshape(8, 128, 36, 128).transpose(2, 1, 0, 3))
    w_dt = np.ascontiguousarray(w_in[:, 2560:2576].reshape(8, 128, 16).transpose(1, 0, 2))
    wr = np.stack([np.concatenate([f(inp["router_group_w"][l]), f(inp["router_expert_w"][l])], axis=1)
                   .reshape(8, 128, 20).transpose(1, 0, 2) for l in range(2)])
    k_ = np.arange(128)[:, None]
    s_ = np.arange(128)[None, :]
    cst = np.stack([np.eye(128, dtype=np.float32), (k_ <= s_).astype(np.float32), (k_ > s_).astype(np.float32),
                    np.ones((128, 128), np.float32)], axis=1)
    shared = {
        "meta": f(inp["meta_tokens"]), "colp": colp, "rowp": rowp, "cst": np.ascontiguousarray(cst),
        "w_in_r": w_in_r, "w_dt": w_dt, "w_out": f(inp["w_out"][0]),
        "lru_wa": f(inp["lru_w_a"][0]), "lru_wx": f(inp["lru_w_x"][0]),
        "pool_w": f(inp["pool_w"][0]), "wr": np.ascontiguousarray(wr),
        "bigrow": np.ascontiguousarray(np.stack([f(inp["norm_final"]), f(inp["pool_b"][0]), f(inp["pool_scale"][0])])[None]),
        "wg": f(inp["expert_w_gate"]), "wu": f(inp["expert_w_up"]), "wd": f(inp["expert_w_down"]),
    }
    return shared


def build(phases=("mix0", "moe0", "mix1", "moe1"), dbg=False):
    nc = bass.Bass("TRN2", target_bir_lowering=False)
    dram = lambda n, s, kind="ExternalInput": nc.dram_tensor(n, list(s), F32, kind=kind).ap()
    x_d = dram("x", [2048, D])
    meta_d = dram("meta", [16, D])
    colp_d = dram("colp", [128, NCOL])
    rowp_d = dram("rowp", [1, NROW])
    cst_d = dram("cst", [128, 4, 128])
    w_in_d = dram("w_in_r", [36, 128, 8, 128])
    w_dt_d = dram("w_dt", [128, 8, 16])
    w_out_d = dram("w_out", [2048, D])
    lwa_d = dram("lru_wa", [16, 64, 64])
    lwx_d = dram("lru_wx", [16, 64, 64])
    pw_d = dram("pool_w", [4, 256, 256])
    wr_d = dram("wr", [2, 128, 8, 20])
    big_d = dram("bigrow", [1, 3, D])
    wg_d = dram("wg", [2, 16, D, 512])
    wu_d = dram("wu", [2, 16, D, 512])
    wd_d = dram("wd", [2, 16, 512, D])
    if dbg:
        out_d = dram("out", [T, D], kind="ExternalOutput")
    else:
        out_d = dram("out", [2048, D], kind="ExternalOutput")

    S = Sched(nc)
    with ExitStack() as st:
        sb = lambda n, s, dt=F32: st.enter_context(nc.sbuf_tensor(n, list(s), dt))
        h = sb("h", [128, NT, D])
        hnT = sb("hnT", [128, 8, T], BF16)
        colp = sb("colp_s", [128, NCOL])
        rowp = sb("rowp_s", [128, NROW])
        cst = sb("cst_s", [128, 4, 128])
        cst16 = sb("cst16", [128, 4, 128], BF16)
        stat = sb("stat", [128, 64])
        AW = 25900
        arena = sb("arena", [128, AW])
        ps = [st.enter_context(nc.psum_tensor(f"ps{i}", [128, 512], F32)) for i in range(8)]
        ident = cst[:, 0, :]
        tri_le = cst[:, 1, :]
        u_gt = cst[:, 2, :]
        ones32 = cst[:, 3, :]
        ident16 = cst16[:, 0, :]
        ss = stat[:, 0:17]
        sq = stat[:, 17:34]
        rstd = stat[:, 34:51]

        class Arena:
            def __init__(self):
                self.off = 0

            def reset(self):
                self.off = 0
                S.barrier()

            def a(self, shape, dt=F32):
                n = int(np.prod(shape[1:]))
                words = n if dt == F32 else (n + 1) // 2
                assert self.off + words <= AW, ("arena overflow", self.off, words)
                v = arena[:, self.off:self.off + words]
                self.off += words
                if dt != F32:
                    v = v.bitcast(dt)
                    if v.shape[1] != n:
                        v = v[:, 0:n]
                if len(shape) == 3:
                    v = v.rearrange("p (a b) -> p a b", a=shape[1])
                elif len(shape) == 4:
                    v = v.rearrange("p (a b c) -> p a b c", a=shape[1], b=shape[2])
                return v

        A = Arena()
        colv = lambda name, i=0, n=None: colp[:, COLP[name][0] + i:COLP[name][0] + i + (n if n else 1)]
        rowv = lambda name: rowp[:, ROWP[name][0]:ROWP[name][0] + ROWP[name][1]]

        def fsz(ap):
            n = 1
            for d_ in ap.shape[1:]:
                n *= int(d_)
            return n

        def ecost(eng, n, mult=1.0):
            if eng == "dve":
                return 0.12 + mult * n / 960.0
            if eng == "act":
                return 0.25 + n / 1400.0
            return 2.0 + 0.015 * n

        def mm(out, lhsT, rhs, start, stop, r, w):
            c = max(fsz(rhs), 64) / 2400.0 + 0.03
            if lhsT.dtype == F32:
                c *= 4
            S.add("pe", lambda e: e.matmul(out, lhsT, rhs, start=start, stop=stop), r=r, w=w, cost=c)

        def tr(out, in_, idn, r, w):
            S.add("pe", lambda e: e.transpose(out, in_, idn), r=r, w=w, cost=0.3 if in_.dtype == F32 else 0.12)

        def act(out, in_, func, r, w, scale=1.0, bias=0.0, accum=None):
            c = ecost("act", fsz(in_)) + (0.1 if accum is not None else 0.0)
            if accum is None:
                S.add("act", lambda e: e.activation(out=out, in_=in_, func=func, scale=scale, bias=bias), r=r, w=w, cost=c)
            else:
                S.add("act", lambda e: e.activation(out=out, in_=in_, func=func, scale=scale, bias=bias, accum_out=accum), r=r, w=w, cost=c)

        def tt(eng, out, in0, in1, op, r, w):
            S.add(eng, lambda e: e.tensor_tensor(out=out, in0=in0, in1=in1, op=op), r=r, w=w, cost=ecost(eng, fsz(out)))

        def ts(eng, out, in0, s1, op0, r, w, s2=None, op1=None):
            c = ecost(eng, fsz(out))
            if op1 is None:
                S.add(eng, lambda e: e.tensor_scalar(out=out, in0=in0, scalar1=s1, scalar2=None, op0=op0), r=r, w=w, cost=c)
            else:
                S.add(eng, lambda e: e.tensor_scalar(out=out, in0=in0, scalar1=s1, scalar2=s2, op0=op0, op1=op1), r=r, w=w, cost=c)

        def stt(out, in0, scalar, in1, op0, op1, r, w):
            S.add("dve", lambda e: e.scalar_tensor_tensor(out=out, in0=in0, scalar=scalar, in1=in1, op0=op0, op1=op1), r=r, w=w,
                  cost=ecost("dve", fsz(out)))

        def cp(eng, out, in_, r, w):
            c = ecost(eng, fsz(out))
            if eng == "act":
                S.add("act", lambda e: e.copy(out, in_), r=r, w=w, cost=c)
            else:
                S.add(eng, lambda e: e.tensor_copy(out, in_), r=r, w=w, cost=c)

        def memset(eng, ap, val, w):
            S.add(eng, lambda e: e.memset(ap, val), w=w, cost=ecost(eng, fsz(ap)))

        def dma(q, out, in_, r=(), w=(), is_output=False):
            nbytes = fsz(out) * int(out.shape[0]) * 4
            S.dma(q, lambda e: e.dma_start(out=out, in_=in_), r=r, w=w, is_output=is_output, cost=2.5 + nbytes / 150e3)

        HK = lambda t: [("h", t, 0), ("h", t, 1)]

        dma("sp", colp[:], colp_d, w=["colp"])
        dma("sp", rowp[:], rowp_d.partition_broadcast(128), w=["rowp"])
        dma("sp", cst[:], cst_d, w=["cst"])
        dma("pool", cst16[:], cst_d, w=["cst16"])
        memset("dve", h[:, 0, :], 0.0, w=HK(0))
        dma("sp", h[PADN:128, 0, :], meta_d, w=HK(0))
        xr = x_d.rearrange("(t p) d -> p t d", p=128)
        for i in range(4):
            dma("sp", h[:, 1 + 4 * i:5 + 4 * i, :], xr[:, 4 * i:4 * i + 4, :], w=[k for t in range(1 + 4 * i, 5 + 4 * i) for k in HK(t)])

        def rms_stats(tiles):
            junk = A.a([128, D], BF16)
            for t in tiles:
                act(junk, h[:, t, :], AF.Square, r=HK(t), w=[("ss", t), "junk"], accum=ss[:, t:t + 1])
                act(sq[:, t:t + 1], ss[:, t:t + 1], AF.Sqrt, r=[("ss", t)], w=[("sq", t)], scale=1.0 / D, bias=colv_eps)
                S.add("dve", lambda e, t=t: e.reciprocal(rstd[:, t:t + 1], sq[:, t:t + 1]), r=[("sq", t)], w=[("rstd", t)], cost=0.15)

        def normT(gname, router_l=None, logits=None, wr_s=None, consume=None):
            xsb = [A.a([128, D]) for _ in range(2)]
            t32 = [A.a([128, 4, 128]) for _ in range(4)]
            pend = None
            for t in range(NT):
                xs = xsb[t % 2]
                act(xs, h[:, t, :], AF.Identity, r=HK(t) + [("rstd", t)], w=[("xs", t % 2)], scale=rstd[:, t:t + 1])
                for half in range(2):
                    i2 = (2 * t + half) % 2
                    i4 = (2 * t + half) % 4
                    bank = ps[6 + i2]
                    for kk in range(4):
                        k = half * 4 + kk
                        tr(bank[:, kk * 128:(kk + 1) * 128], xs[:, k * 128:(k + 1) * 128], ident, r=[("xs", t % 2), "cst"], w=[("ps", 6 + i2)])
                    g0 = COLP[gname][0] + half * 4
                    tt("dve", t32[i4], bank[:, :].rearrange("p (a b) -> p a b", a=4),
                       colp[:, g0:g0 + 4].unsqueeze(2).to_broadcast([128, 4, 128]), ALU.mult,
                       r=[("ps", 6 + i2), "colp"], w=[("t32", i4)])
                    cp("act", hnT[:, half * 4:half * 4 + 4, t * 128:(t + 1) * 128], t32[i4], r=[("t32", i4)], w=[("hnT", t)])
                if router_l is not None:
                    if pend is not None:
                        pend()

                    def mk(t=t):
                        def go():
                            for k in range(8):
                                i4 = (2 * t + k // 4) % 4
                                mm(ps[5][:, 0:20], t32[i4][:, k % 4, :], wr_s[:, k, :], k == 0, k == 7,
                                   r=[("t32", i4), "wr"], w=[("ps", 5)])
                            cp("act", logits[:, t, :], ps[5][:, 0:20], r=[("ps", 5)], w=["logits"])
                        return go
                    pend = mk()
            if pend is not None:
                pend()

        memset("pool", stat[:, 60:61], EPS, w=["eps"])
        colv_eps = stat[:, 60:61]

        def moe(l):
            A.reset()
            wr_s = A.a([128, 8, 20])
            logits = A.a([128, NT, 20])
            gates = A.a([128, NT, 16])
            dma("sp", wr_s, wr_d[l], w=["wr"])
            Wg = [A.a([128, 8, 512], BF16) for _ in range(2)]
            Wu = [A.a([128, 8, 512], BF16) for _ in range(2)]
            Wd = [A.a([128, 4, D], BF16) for _ in range(2)]

            def load_w(e):
                b = e % 2
                if e == 0:
                    for fc in range(4):
                        fs = slice(fc * 128, (fc + 1) * 128)
                        dma("pool", Wg[b][:, :, fs], wg_d[l, e][:, fs].rearrange("(k p) f -> p k f", p=128), w=[("Wg", b, fc)])
                        dma("pool", Wu[b][:, :, fs], wu_d[l, e][:, fs].rearrange("(k p) f -> p k f", p=128), w=[("Wu", b, fc)])
                else:
                    dma("pool", Wg[b], wg_d[l, e].rearrange("(k p) f -> p k f", p=128), w=[("Wg", b, fc) for fc in range(4)])
                    dma("pool", Wu[b], wu_d[l, e].rearrange("(k p) f -> p k f", p=128), w=[("Wu", b, fc) for fc in range(4)])
                dma("pool", Wd[b], wd_d[l, e].rearrange("(k p) f -> p k f", p=128), w=[("Wd", b)])

            load_w(0)
            load_w(1)
            mark = A.off
            rms_stats(list(range(NT)))
            normT("ffn%d" % l, router_l=l, logits=logits, wr_s=wr_s)
            R = lambda shape: A.a(shape)
            rb = rowv("rb%d" % l)
            lg = R([128, NT, 20])
            tt("dve", lg, logits, rb.unsqueeze(1).to_broadcast([128, NT, 20]), ALU.add, r=["logits", "rowp"], w=["lg"])
            m4 = R([128, NT])
            S.add("dve", lambda e: e.tensor_reduce(out=m4, in_=lg[:, :, 0:4], axis=AX.X, op=ALU.max), r=["lg"], w=["m4"])
            d4 = R([128, NT, 4])
            tt("dve", d4, lg[:, :, 0:4], m4.unsqueeze(2).to_broadcast([128, NT, 4]), ALU.subtract, r=["lg", "m4"], w=["d4"])
            mg = R([128, NT, 4])
            ts("dve", mg, d4, 0.0, ALU.is_ge, r=["d4"], w=["mg"])
            e4 = R([128, NT, 4])
            act(e4, d4, AF.Exp, r=["d4"], w=["e4"])
            s4 = R([128, NT])
            S.add("dve", lambda e: e.tensor_reduce(out=s4, in_=e4, axis=AX.X, op=ALU.add), r=["e4"], w=["s4"])
            le = lg[:, :, 4:20].rearrange("p t (g j) -> p t g j", g=4)
            ml = R([128, NT, 4, 4])
            tt("dve", ml, le, mg.unsqueeze(3).to_broadcast([128, NT, 4, 4]), ALU.mult, r=["lg", "mg"], w=["ml"])
            sel = R([128, NT, 4])
            tt("dve", sel, ml[:, :, 0, :], ml[:, :, 1, :], ALU.add, r=["ml"], w=["sel"])
            tt("dve", sel, sel, ml[:, :, 2, :], ALU.add, r=["ml", "sel"], w=["sel"])
            tt("dve", sel, sel, ml[:, :, 3, :], ALU.add, r=["ml", "sel"], w=["sel"])
            m1 = R([128, NT])
            S.add("dve", lambda e: e.tensor_reduce(out=m1, in_=sel, axis=AX.X, op=ALU.max), r=["sel"], w=["m1"])
            k1 = R([128, NT, 4])
            tt("dve", k1, sel, m1.unsqueeze(2).to_broadcast([128, NT, 4]), ALU.is_ge, r=["sel", "m1"], w=["k1"])
            sel2 = R([128, NT, 4])
            stt(sel2, k1, -1e30, sel, ALU.mult, ALU.add, r=["k1", "sel"], w=["sel2"])
            m2 = R([128, NT])
            S.add("dve", lambda e: e.tensor_reduce(out=m2, in_=sel2, axis=AX.X, op=ALU.max), r=["sel2"], w=["m2"])
            k2 = R([128, NT, 4])
            tt("dve", k2, sel2, m2.unsqueeze(2).to_broadcast([128, NT, 4]), ALU.is_ge, r=["sel2", "m2"], w=["k2"])
            dd = R([128, NT])
            tt("dve", dd, m2, m1, ALU.subtract, r=["m1", "m2"], w=["dd"])
            w2 = R([128, NT])
            act(w2, dd, AF.Exp, r=["dd"], w=["w2"])
            den = R([128, NT])
            stt(den, w2, 1.0, s4, ALU.add, ALU.mult, r=["w2", "s4"], w=["den"])
            g1 = R([128, NT])
            S.add("dve", lambda e: e.reciprocal(g1, den), r=["den"], w=["g1"])
            g2 = R([128, NT])
            tt("dve", g2, g1, w2, ALU.mult, r=["g1", "w2"], w=["g2"])
            gs = R([128, NT, 4])
            tt("dve", gs, k1, g1.unsqueeze(2).to_broadcast([128, NT, 4]), ALU.mult, r=["k1", "g1"], w=["gs"])
            gs2 = R([128, NT, 4])
            tt("dve", gs2, k2, g2.unsqueeze(2).to_broadcast([128, NT, 4]), ALU.mult, r=["k2", "g2"], w=["gs2"])
            tt("dve", gs, gs, gs2, ALU.add, r=["gs", "gs2"], w=["gs"])
            g4 = gates.rearrange("p t (g j) -> p t g j", g=4)
            for g in range(4):
                tt("dve", g4[:, :, g, :], gs, mg[:, :, g:g + 1].to_broadcast([128, NT, 4]), ALU.mult, r=["gs", "mg"], w=["gates"])

            hid = [A.a([128, 4, 512], BF16) for _ in range(2)]
            sg = [A.a([128, 512]) for _ in range(2)]
            cnt = 0
            cnt_o = [0]
            blk = 0
            pend = None
            for e in range(16):
                b = e % 2
                if e >= 2:
                    load_w(e)
                for (t0, n) in TB:
                    hb = blk % 2
                    blk += 1
                    hk = [("hnT", tt_) for tt_ in range(t0 // 128, (t0 + n) // 128)]
                    for fc in range(4):
                        i = cnt % 2
                        cnt += 1
                        pg, pu = ps[i], ps[2 + i]
                        for k in range(8):
                            mm(pg[:, 0:n], Wg[b][:, k, fc * 128:(fc + 1) * 128], hnT[:, k, t0:t0 + n], k == 0, k == 7,
                               r=[("Wg", b, fc)] + hk, w=[("ps", i)])
                        for k in range(8):
                            mm(pu[:, 0:n], Wu[b][:, k, fc * 128:(fc + 1) * 128], hnT[:, k, t0:t0 + n], k == 0, k == 7,
                               r=[("Wu", b, fc)] + hk, w=[("ps", 2 + i)])
                        act(sg[i][:, 0:n], pg[:, 0:n], AF.Silu, r=[("ps", i)], w=[("sg", i)])
                        tt("dve", hid[hb][:, fc, 0:n], sg[i][:, 0:n], pu[:, 0:n], ALU.mult, r=[("sg", i), ("ps", 2 + i)], w=[("hid", hb)])
                    if pend is not None:
                        pend()

                    def mk(e=e, b=b, hb=hb, t0=t0, n=n):
                        def go():
                            for tl in range(n // 128):
                                t = t0 // 128 + tl
                                for dh in range(2):
                                    io = 4 + cnt_o[0] % 2
                                    cnt_o[0] += 1
                                    for fc in range(4):
                                        mm(ps[io][:, :], hid[hb][:, fc, tl * 128:(tl + 1) * 128], Wd[b][:, fc, dh * 512:(dh + 1) * 512],
                                           fc == 0, fc == 3, r=[("hid", hb), ("Wd", b)], w=[("ps", io)])
                                    hv = h[:, t, dh * 512:(dh + 1) * 512]
                                    stt(hv, ps[io][:, :], gates[:, t, e:e + 1], hv,
                                        ALU.mult, ALU.add, r=[("ps", io), "gates", ("h", t, dh)], w=[("h", t, dh)])
                        return go
                    pend = mk()
            pend()

        def mix1():
            A.reset()
            memset("pool", h[0:PADN, 0, :], 0.0, w=HK(0))
            rms_stats(list(range(NT)))
            pw = A.a([128, 4, 2, 256], BF16)
            for g in range(4):
                dma("pool", pw[:, g, :, :], pw_d[g].rearrange("(c p) j -> p c j", p=128), w=["pw"])
            pb_bc = A.a([128, D])
            sc_bc = A.a([128, D])
            dma("sp", pb_bc, big_d[:, 1, :].partition_broadcast(128), w=["pb_bc"])
            dma("sp", sc_bc, big_d[:, 2, :].partition_broadcast(128), w=["sc_bc"])
            bs_bc = A.a([128, D])
            tt("dve", bs_bc, pb_bc, sc_bc, ALU.mult, r=["pb_bc", "sc_bc"], w=["bs_bc"])
            PF = 16
            hn32 = A.a([128, PF + T])
            sA = A.a([128, PF + T])
            sB = A.a([128, PF + T])
            pooledT = A.a([128, 8, T], BF16)
            for buf, nm in ((hn32, "hn32"), (sA, "sA"), (sB, "sB")):
                memset("pool", buf[:, 0:PF], 0.0, w=[nm])
            xsn = A.a([128, NT, 128])
            fix = A.a([128, 16])
            rcv = rowv("rc")
            for k in range(8):
                g = k // 2
                w = (2, 4, 8, 16)[g]
                tt("dve", xsn, h[:, :, k * 128:(k + 1) * 128], rstd[:, 0:NT].unsqueeze(2).to_broadcast([128, NT, 128]), ALU.mult,
                   r=[("h", t, k // 4) for t in range(NT)] + [("rstd", t) for t in range(NT)], w=["xsn"])
                for q in range(5):
                    tiles = list(range(4 * q, min(4 * q + 4, NT)))
                    bank = ps[6 + q % 2]
                    for i, t in enumerate(tiles):
                        tr(bank[:, i * 128:(i + 1) * 128], xsn[:, t, :], ident, r=["xsn", "cst"], w=[("ps", 6 + q % 2)])
                    nn = len(tiles) * 128
                    ts("dve", hn32[:, PF + q * 512:PF + q * 512 + nn], bank[:, 0:nn], colv("mix_odd", k), ALU.mult,
                       r=[("ps", 6 + q % 2), "colp"], w=["hn32"])
                cur, curk = hn32, "hn32"
                step = 1
                bufs = [(sA, "sA"), (sB, "sB")]
                bi = 0
                while step < w:
                    nxt, nk = bufs[bi % 2]
                    bi += 1
                    tt("dve", nxt[:, PF:PF + T], cur[:, PF:PF + T], cur[:, PF - step:PF + T - step], ALU.add,
                       r=[curk], w=[nk])
                    cur, curk = nxt, nk
                    step *= 2
                stt(pooledT[:, k, :], cur[:, PF:PF + T], 1.0 / w, hn32[:, PF:PF + T], ALU.mult, ALU.subtract,
                    r=[curk, "hn32"], w=[("pooledT", k)])
                tt("dve", fix, cur[:, PF + PADN:PF + 128], rcv[:, g * 16:(g + 1) * 16], ALU.mult, r=[curk, "rowp"], w=["fix"])
                tt("dve", pooledT[:, k, PADN:128], fix, hn32[:, PF + PADN:PF + 128], ALU.subtract, r=["fix", "hn32"], w=[("pooledT", k)])
            t1 = [A.a([128, 512]) for _ in range(2)]
            ci = 0
            for t in range(NT):
                for dh in range(2):
                    bi_ = dh + 2 * (t % 2)
                    bank = ps[bi_]
                    for gg in range(2):
                        g = dh * 2 + gg
                        for ic in range(2):
                            mm(bank[:, gg * 256:(gg + 1) * 256], pooledT[:, 2 * g + ic, t * 128:(t + 1) * 128], pw[:, g, ic, :], ic == 0, ic == 1,
                               r=[("pooledT", 2 * g + ic), "pw"], w=[("ps", bi_)])
                    tb_ = t1[ci % 2]
                    tk = ("t1", ci % 2)
                    ci += 1
                    tt("dve", tb_, bank[:, :], sc_bc[:, dh * 512:(dh + 1) * 512], ALU.mult, r=[("ps", bi_), "sc_bc"], w=[tk])
                    tt("dve", tb_, tb_, bs_bc[:, dh * 512:(dh + 1) * 512], ALU.add, r=[tk, "bs_bc"], w=[tk])
                    hv = h[:, t, dh * 512:(dh + 1) * 512]
                    tt("dve", hv, hv, tb_, ALU.add, r=[tk, ("h", t, dh)], w=[("h", t, dh)])

        def mix0():
            A.reset()
            rms_stats(list(range(NT)))
            normT("mix_even")
            A.reset()
            hnk = [("hnT", t) for t in range(NT)]
            wbuf = [A.a([128, 8, 128], BF16) for _ in range(3)]
            wcnt = [0]

            def load_wchunk(cc):
                i = wcnt[0] % 3
                wcnt[0] += 1
                dma("pool", wbuf[i], w_in_d[cc], w=[("wbuf", i)])
                return i

            def proj_blk(wi, bi, t0, n):
                bk = ("ps", bi % 2)
                bank = ps[bi % 2]
                for k in range(8):
                    mm(bank[:, 0:n], wbuf[wi][:, k, :], hnT[:, k, t0:t0 + n], k == 0, k == 7, r=[("wbuf", wi)] + hnk, w=[bk])
                return bank, bk

            xinb = [A.a([128, 3 + 512]) for _ in range(2)]
            ctb = [A.a([128, 512]) for _ in range(2)]
            cvc = [0]

            def conv_blk(bank, bk, bi, n, wname, bname, cidx):
                i = cvc[0] % 2
                cvc[0] += 1
                xi, xk = xinb[i], ("xinb", i)
                if bi == 0:
                    memset("dve", xi[:, 0:3], 0.0, w=[xk])
                else:
                    pv = xinb[1 - i]
                    cp("dve", xi[:, 0:3], pv[:, 512:515], r=[("xinb", 1 - i)], w=[xk])
                cp("act", xi[:, 3:3 + n], bank[:, 0:n], r=[bk], w=[xk])
                ct, ck = ctb[i], ("ctb", i)
                o4 = COLP[wname][0] + 4 * cidx
                ts("dve", ct[:, 0:n], xi[:, 0:n], colp[:, o4:o4 + 1], ALU.mult, r=[xk, "colp"], w=[ck], s2=colv(bname, cidx), op1=ALU.add)
                for k in range(1, 4):
                    stt(ct[:, 0:n], xi[:, k:k + n], colp[:, o4 + k:o4 + k + 1], ct[:, 0:n], ALU.mult, ALU.add, r=[xk, ck, "colp"], w=[ck])
                return ct, ck

            wo = A.a([128, 4, D], BF16)
            ybuf = A.a([128, 4, T], BF16)

            def out_proj(kc0, scale_ap_fn):
                dma("pool", wo, w_out_d[kc0 * 128:(kc0 + 4) * 128, :].rearrange("(k p) d -> p k d", p=128), w=["wo"])
                cnt = 0
                for t in range(NT):
                    for dh in range(2):
                        io = 4 + cnt % 2
                        cnt += 1
                        for kk in range(4):
                            mm(ps[io][:, :], ybuf[:, kk, t * 128:(t + 1) * 128], wo[:, kk, dh * 512:(dh + 1) * 512], kk == 0, kk == 3,
                               r=[("ybuf", kk), "wo"], w=[("ps", io)])
                        hv = h[:, t, dh * 512:(dh + 1) * 512]
                        if scale_ap_fn is None:
                            tt("dve", hv, hv, ps[io][:, :], ALU.add, r=[("ps", io), ("h", t, dh)], w=[("h", t, dh)])
                        else:
                            stt(hv, ps[io][:, :], scale_ap_fn(t), hv, ALU.mult, ALU.add, r=[("ps", io), ("h", t, dh), "rstdg"], w=[("h", t, dh)])

            mark = A.off

            def lru():
                bdA = A.a([128, 8, 128], BF16)
                bdX = A.a([128, 8, 128], BF16)
                memset("pool", bdA, 0.0, w=["bdA"])
                memset("pool", bdX, 0.0, w=["bdX"])
                for src, dst, nm in ((lwa_d, bdA, "bdA"), (lwx_d, bdX, "bdX")):
                    v = src.rearrange("(j two) i o -> two i j o", two=2)
                    dma("pool", dst[0:64, :, 0:64], v[0], w=[nm])
                    dma("pool", dst[64:128, :, 64:128], v[1], w=[nm])
                c1 = A.a([128, 8])
                tmpc = A.a([128, 8])
                act(tmpc, colv("lru_lam", 0, 8), AF.Exp, r=["colp"], w=["tmpc"], scale=-1.0)
                act(tmpc, tmpc, AF.Ln, r=["tmpc"], w=["tmpc"], bias=1.0)
                ts("dve", c1, tmpc, -8.0, ALU.mult, r=["tmpc"], w=["c1"])
                Bn = lambda n_: [A.a([128, 512]) for _ in range(n_)]
                gl, rr, ii, av, uv, hl = Bn(4), Bn(4), Bn(4), Bn(4), Bn(2), Bn(2)
                xb16 = [A.a([128, 512], BF16) for _ in range(4)]
                NB = len(TB)
                items = [(j, bi) for j in range(8) for bi in range(NB)]
                st1 = {}

                def S1(idx):
                    j, bi = items[idx]
                    t0, n = TB[bi]
                    if bi == 0:
                        st1[j] = (load_wchunk(28 + j), load_wchunk(20 + j))
                    wi_in, wi_gt = st1[j]
                    bank, bk = proj_blk(wi_in, 2 * idx, t0, n)
                    xb, xbk = conv_blk(bank, bk, bi, n, "lru_cw", "lru_cb", j)
                    bank2, bk2 = proj_blk(wi_gt, 2 * idx + 1, t0, n)
                    g3 = idx % 4
                    act(gl[g3][:, 0:n], bank2[:, 0:n], AF.Gelu_apprx_tanh, r=[bk2], w=[("gl", g3)])
                    cp("act", xb16[g3][:, 0:n], xb[:, 0:n], r=[xbk], w=[("xb16", g3)])
                    st1[idx, "xb"] = (xb, xbk)

                def S2(idx):
                    j, bi = items[idx]
                    t0, n = TB[bi]
                    ip = idx % 2
                    q = idx % 4
                    K = lambda nm: (nm, q)
                    xb, xbk = st1.pop((idx, "xb"))
                    pa, px = ps[2 + ip], ps[4 + ip]
                    mm(pa[:, 0:n], bdA[:, j, :], xb16[q][:, 0:n], True, True, r=["bdA", K("xb16")], w=[("ps", 2 + ip)])
                    mm(px[:, 0:n], bdX[:, j, :], xb16[q][:, 0:n], True, True, r=["bdX", K("xb16")], w=[("ps", 4 + ip)])
                    r_, i_ = rr[q], ii[q]
                    act(r_[:, 0:n], pa[:, 0:n], AF.Sigmoid, r=[("ps", 2 + ip), "colp"], w=[K("rr")], bias=colv("lru_ba", j))
                    act(i_[:, 0:n], px[:, 0:n], AF.Sigmoid, r=[("ps", 4 + ip), "colp"], w=[K("ii")], bias=colv("lru_bx", j))
                    act(av[q][:, 0:n], r_[:, 0:n], AF.Exp, r=[K("rr"), "c1"], w=[K("av")], scale=c1[:, j:j + 1])
                    stt(r_[:, 0:n], av[q][:, 0:n], 0.9999998, av[q][:, 0:n], ALU.min, ALU.mult, r=[K("av")], w=[K("rr")])
                    act(r_[:, 0:n], r_[:, 0:n], AF.Sqrt, r=[K("rr")], w=[K("rr")], scale=-1.0, bias=1.0)
                    tt("dve", i_[:, 0:n], i_[:, 0:n], xb[:, 0:n], ALU.mult, r=[K("ii"), xbk], w=[K("ii")])
                    if bi == 0:
                        memset("dve", r_[:, PADN:PADN + 1], 1.0, w=[K("rr")])

                def S3(idx):
                    j, bi = items[idx]
                    t0, n = TB[bi]
                    i = idx % 2
                    q = idx % 4
                    jj = j % 4
                    K = lambda nm: (nm, q)
                    tt("dve", uv[i][:, 0:n], rr[q][:, 0:n], ii[q][:, 0:n], ALU.mult, r=[K("rr"), K("ii")], w=[("uv", i)])
                    if bi == 0:
                        memset("dve", hl[i][:, 0:PADN], 0.0, w=[("hl", i)])
                        S.add("dve", lambda e, i=i, q=q, n=n: e.tensor_tensor_scan(out=hl[i][:, PADN:n], data0=av[q][:, PADN:n], data1=uv[i][:, PADN:n],
                                                                             initial=0.0, op0=ALU.mult, op1=ALU.add),
                              r=[K("av"), ("uv", i)], w=[("hl", i)], cost=1.2)
                    else:
                        S.add("dve", lambda e, i=i, q=q, n=n: e.tensor_tensor_scan(out=hl[i][:, 0:n], data0=av[q][:, 0:n], data1=uv[i][:, 0:n],
                                                                             initial=hl[1 - i][:, 511:512], op0=ALU.mult, op1=ALU.add),
                              r=[K("av"), ("uv", i), ("hl", 1 - i)], w=[("hl", i)], cost=1.2)
                    tt("dve", ybuf[:, jj, t0:t0 + n], hl[i][:, 0:n], gl[q][:, 0:n], ALU.mult, r=[("hl", i), ("gl", q)], w=[("ybuf", jj)])
                    if bi == NB - 1 and jj == 3:
                        out_proj(8 + (j // 4) * 4, None)

                NI = len(items)
                for step in range(NI + 2):
                    if step < NI:
                        S1(step)
                    if 0 <= step - 1 < NI:
                        S2(step - 1)
                    if 0 <= step - 2 < NI:
                        S3(step - 2)

            def ssd():
                dt = A.a([128, NT, 16])
                adt = A.a([128, NT, 16])
                ea = A.a([128, NT, 16])
                eatot = A.a([128, NT, 16])
                dte = A.a([128, NT, 16])
                Aneg = A.a([128, 16])
                wdt = A.a([128, 8, 16], BF16)
                dma("pool", wdt, w_dt_d, w=["wdt"])
                for t in range(NT):
                    for k in range(8):
                        mm(ps[7][:, t * 16:(t + 1) * 16], hnT[:, k, t * 128:(t + 1) * 128], wdt[:, k, :], k == 0, k == 7,
                           r=["wdt", ("hnT", t)], w=[("ps", 7)])
                p7 = ps[7][:, 0:NT * 16].rearrange("p (t h) -> p t h", t=NT)
                tt("dve", dt, p7, rowv("dt_bias").unsqueeze(1).to_broadcast([128, NT, 16]), ALU.add, r=[("ps", 7), "rowp"], w=["dt"])
                act(dte, dt, AF.Abs, r=["dt"], w=["dte"])
                act(dte, dte, AF.Exp, r=["dte"], w=["dte"], scale=-1.0)
                act(dte, dte, AF.Ln, r=["dte"], w=["dte"], bias=1.0)
                stt(dt, dt, 0.0, dte, ALU.max, ALU.add, r=["dt", "dte"], w=["dt"])
                memset("dve", dt[0:PADN, 0, :], 0.0, w=["dt"])
                act(Aneg, rowv("a_log"), AF.Exp, r=["rowp"], w=["Aneg"])
                stt(adt, dt, -1.0, Aneg.unsqueeze(1).to_broadcast([128, NT, 16]), ALU.mult, ALU.mult, r=["dt", "Aneg"], w=["adt"])
                for t in range(NT):
                    mm(ps[6][:, t * 16:(t + 1) * 16], tri_le, adt[:, t, :], True, True, r=["cst", "adt"], w=[("ps", 6)])
                for t in range(NT):
                    mm(ps[7][:, t * 16:(t + 1) * 16], ones32, adt[:, t, :], True, True, r=["cst", "adt"], w=[("ps", 7)])
                p6 = ps[6][:, 0:NT * 16].rearrange("p (t h) -> p t h", t=NT)
                cp("dve", ea, p6, r=[("ps", 6)], w=["ea"])
                cp("dve", eatot, p7, r=[("ps", 7)], w=["eatot"])
                tt("dve", dte, eatot, ea, ALU.subtract, r=["eatot", "ea"], w=["dte"])
                act(dte, dte, AF.Exp, r=["dte"], w=["dte"])
                act(ea, ea, AF.Exp, r=["ea", "dte"], w=["ea"])
                act(eatot, eatot, AF.Exp, r=["eatot", "dte"], w=["eatot"])
                Dbc = rowv("ssd_d")
                DI = A.a([128, 2, 128], BF16)
                if SSD_STOP == "dt":
                    return

                BT = A.a([128, T], BF16)
                CT = A.a([128, T], BF16)
                Btok = A.a([128, NT, 128], BF16)
                CBm = A.a([128, NT, 128], BF16)
                xT16 = [A.a([128, 512], BF16) for _ in range(2)]
                xtok = A.a([128, NT, 128], BF16)
                xdt = A.a([128, NT, 128], BF16)
                sz = A.a([128, NT, 128], BF16)
                MT_all = A.a([128, NT, 256], BF16)
                S_all = A.a([128, NT, 128], BF16)
                ssp = A.a([128, NT, 4])
                rstdg = A.a([128, NT])
                Sst = A.a([128, 128])
                Lb = [A.a([128, 256]) for _ in range(2)]
                Db = [A.a([128, 256]) for _ in range(2)]
                xdte = [A.a([128, 128], BF16) for _ in range(4)]
                ytmp = [A.a([128, 128]) for _ in range(4)]
                yg16 = [A.a([128, 128], BF16) for _ in range(4)]
                junk = A.a([128, 128], BF16)

                def bank16(i):
                    return ps[i][:, 0:256].bitcast(BF16)

                def fm_chunk(cc, cidx, sink):
                    wi = load_wchunk(cc)
                    for bi, (t0, n) in enumerate(TB):
                        bank, bk = proj_blk(wi, bi, t0, n)
                        ct, ck = conv_blk(bank, bk, bi, n, "ssd_cw", "ssd_cb", cidx)
                        sink(bi, t0, n, ct, ck)

                for g in range(2):
                    S.barrier()
                    fm_chunk(8 + 8 + g, 8 + g, lambda bi, t0, n, ct, ck: act(BT[:, t0:t0 + n], ct[:, 0:n], AF.Silu, r=[ck], w=["BT"]))
                    fm_chunk(8 + 10 + g, 10 + g, lambda bi, t0, n, ct, ck: act(CT[:, t0:t0 + n], ct[:, 0:n], AF.Silu, r=[ck], w=["CT"]))
                    for q in range(5):
                        tiles = list(range(4 * q, min(4 * q + 4, NT)))
                        nn = len(tiles)
                        bk = bank16(6 + q % 2)
                        for i, t in enumerate(tiles):
                            tr(bk[:, i * 128:(i + 1) * 128], BT[:, t * 128:(t + 1) * 128], ident16, r=["BT", "cst16"], w=[("ps", 6 + q % 2)])
                        cp("dve", Btok[:, 4 * q:4 * q + nn, :], bk[:, 0:nn * 128].rearrange("p (a b) -> p a b", a=nn), r=[("ps", 6 + q % 2)], w=["Btok"])
                        bank = ps[2 + q % 2]
                        for i, t in enumerate(tiles):
                            mm(bank[:, i * 128:(i + 1) * 128], BT[:, t * 128:(t + 1) * 128], CT[:, t * 128:(t + 1) * 128], True, True,
                               r=["BT", "CT"], w=[("ps", 2 + q % 2)])
                        tt("dve", CBm[:, 4 * q:4 * q + nn, :], bank[:, 0:nn * 128].rearrange("p (a b) -> p a b", a=nn),
                           tri_le.unsqueeze(1).to_broadcast([128, nn, 128]), ALU.mult, r=[("ps", 2 + q % 2), "cst"], w=["CBm"])
                    S.barrier()
                    if SSD_STOP == "bc":
                        return
                    for jj in range(4):
                        j = g * 4 + jj
                        hh0 = 2 * j

                        def xsink(bi, t0, n, ct, ck, hh0=hh0):
                            xt = xT16[bi % 2]
                            xk = ("xT16", bi % 2)
                            act(xt[:, 0:n], ct[:, 0:n], AF.Silu, r=[ck], w=[xk])
                            nn = n // 128
                            bk = bank16(6 + bi % 2)
                            for i in range(nn):
                                tr(bk[:, i * 128:(i + 1) * 128], xt[:, i * 128:(i + 1) * 128], ident16, r=[xk, "cst16"], w=[("ps", 6 + bi % 2)])
                            a0 = t0 // 128
                            cp("act", xtok.rearrange("p a b -> p (a b)")[:, a0 * 128:(a0 + nn) * 128], bk[:, 0:nn * 128], r=[("ps", 6 + bi % 2)], w=[("xtok", bi)])
                            for i in range(nn):
                                t = a0 + i
                                tt("dve", xdt[:, t, :].rearrange("p (h c) -> p h c", h=2), xtok[:, t, :].rearrange("p (h c) -> p h c", h=2),
                                   dt[:, t, hh0:hh0 + 2].unsqueeze(2).to_broadcast([128, 2, 64]), ALU.mult, r=[("xtok", bi), "dt"], w=[("xdt", t)])
                        fm_chunk(8 + j, j, xsink)
                        if SSD_STOP == "x":
                            return
                        wz = load_wchunk(j)
                        for bi, (t0, n) in enumerate(TB):
                            bank, bk_ = proj_blk(wz, bi, t0, n)
                            zt = xT16[bi % 2]
                            zk = ("xT16", bi % 2)
                            act(zt[:, 0:n], bank[:, 0:n], AF.Silu, r=[bk_], w=[zk])
                            nn = n // 128
                            bk = bank16(6 + bi % 2)
                            for i in range(nn):
                                tr(bk[:, i * 128:(i + 1) * 128], zt[:, i * 128:(i + 1) * 128], ident16, r=[zk, "cst16"], w=[("ps", 6 + bi % 2)])
                            a0 = t0 // 128
                            cp("dve", sz.rearrange("p a b -> p (a b)")[:, a0 * 128:(a0 + nn) * 128], bk[:, 0:nn * 128], r=[("ps", 6 + bi % 2)], w=[("sz", bi)])
                        if SSD_STOP == "z":
                            return
                        memset("dve", Sst, 0.0, w=["Sst"])
                        memset("dve", S_all[:, 0, :], 0.0, w=[("S_all", 0)])
                        for hh in range(2):
                            act(DI[:, hh, :], ident, AF.Identity, r=["cst", "rowp"], w=["DI"], scale=Dbc[:, hh0 + hh:hh0 + hh + 1])
                        for c in range(NT):
                            i2 = c % 2
                            i4 = c % 4
                            for hh in range(2):
                                act(Lb[i2][:, hh * 128:(hh + 1) * 128], u_gt, AF.Identity, r=["cst", "adt"], w=[("L", i2, hh)], scale=adt[:, c, hh0 + hh:hh0 + hh + 1])
                                mm(ps[i2][:, hh * 128:(hh + 1) * 128], Lb[i2][:, hh * 128:(hh + 1) * 128], tri_le, True, True, r=[("L", i2, hh), "cst"], w=[("ps", i2)])
                            act(Db[i2], ps[i2][:, 0:256], AF.Exp, r=[("ps", i2)], w=[("D", i2)])
                            tt("dve", MT_all[:, c, :].rearrange("p (h l) -> p h l", h=2), Db[i2].rearrange("p (h l) -> p h l", h=2),
                               CBm[:, c, :].unsqueeze(1).to_broadcast([128, 2, 128]), ALU.mult, r=[("D", i2), "CBm"], w=[("MT", c)])
                            if c + 1 < NT:
                                tt("dve", xdte[i4].rearrange("p (h c) -> p h c", h=2), xdt[:, c, :].rearrange("p (h c) -> p h c", h=2),
                                   dte[:, c, hh0:hh0 + 2].unsqueeze(2).to_broadcast([128, 2, 64]), ALU.mult, r=[("xdt", c), "dte"], w=[("xdte", i4)])
                                mm(ps[2 + i2][:, 0:128], Btok[:, c, :], xdte[i4], True, True, r=["Btok", ("xdte", i4)], w=[("ps", 2 + i2)])
                                tt("dve", Sst.rearrange("p (h c) -> p h c", h=2), Sst.rearrange("p (h c) -> p h c", h=2),
                                   eatot[:, c, hh0:hh0 + 2].unsqueeze(2).to_broadcast([128, 2, 64]), ALU.mult, r=["Sst", "eatot"], w=["Sst"])
                                tt("dve", Sst, Sst, ps[2 + i2][:, 0:128], ALU.add, r=[("ps", 2 + i2), "Sst"], w=["Sst"])
                                cp("act", S_all[:, c + 1, :], Sst, r=["Sst"], w=[("S_all", c + 1)])
                        def p3_mm(c):
                            b = 4 + c % 2
                            pa = ps[b]
                            mm(pa[:, 0:128], CT[:, c * 128:(c + 1) * 128], S_all[:, c, :], True, True, r=["CT", ("S_all", c)], w=[("ps", b)])
                            for hh in range(2):
                                o = pa[:, 128 + hh * 64:128 + (hh + 1) * 64]
                                mm(o, MT_all[:, c, hh * 128:(hh + 1) * 128], xdt[:, c, hh * 64:(hh + 1) * 64], True, False, r=[("MT", c), ("xdt", c)], w=[("ps", b)])
                                mm(o, DI[:, hh, :], xtok[:, c, hh * 64:(hh + 1) * 64], False, True, r=["DI"] + [("xtok", bb) for bb in range(5)], w=[("ps", b)])

                        def p3_ev(c, jj=jj, j=j, hh0=hh0):
                            b = 4 + c % 2
                            pa = ps[b]
                            i4 = c % 4
                            y, yk = ytmp[i4], ("ytmp", i4)
                            tt("dve", y.rearrange("p (h c) -> p h c", h=2), pa[:, 0:128].rearrange("p (h c) -> p h c", h=2),
                               ea[:, c, hh0:hh0 + 2].unsqueeze(2).to_broadcast([128, 2, 64]), ALU.mult, r=[("ps", b), "ea"], w=[yk])
                            tt("dve", y, y, pa[:, 128:256], ALU.add, r=[("ps", b), yk], w=[yk])
                            yt, ytk = yg16[i4], ("yg", i4)
                            tt("dve", yt, y, sz[:, c, :], ALU.mult, r=[yk] + [("sz", bb) for bb in range(5)], w=[ytk])
                            act(junk, yt, AF.Square, r=[ytk], w=[("ssp", c), "junk"], accum=ssp[:, c, jj:jj + 1])
                            bk = bank16(6 + c % 2)
                            tr(bk[:, 0:128], yt, ident16, r=[ytk, "cst16"], w=[("ps", 6 + c % 2)])
                            ts("dve", ybuf[:, jj, c * 128:(c + 1) * 128], bk[:, 0:128], colv("ssd_norm", j), ALU.mult,
                               r=[("ps", 6 + c % 2), "colp"], w=[("ybuf", jj)])

                        p3_mm(0)
                        for c in range(NT):
                            if c + 1 < NT:
                                p3_mm(c + 1)
                            p3_ev(c)
                    if SSD_STOP == "rec":
                        return
                    S.barrier()
                    S.add("dve", lambda e: e.tensor_reduce(out=rstdg, in_=ssp, axis=AX.X, op=ALU.add), r=[("ssp", c) for c in range(NT)], w=["rstdg"])
                    act(rstdg, rstdg, AF.Sqrt, r=["rstdg"], w=["rstdg"], scale=1.0 / 512, bias=colv_eps)
                    S.add("dve", lambda e: e.reciprocal(rstdg, rstdg), r=["rstdg"], w=["rstdg"])
                    out_proj(g * 4, lambda t: rstdg[:, t:t + 1])

            if "lru" in MIX0_PARTS:
                lru()
            S.barrier()
            A.off = mark
            if "ssd" in MIX0_PARTS:
                ssd()

        for ph in phases:
            {"mix0": mix0, "moe0": lambda: moe(0), "mix1": mix1, "moe1": lambda: moe(1)}[ph]()

        if dbg or not phases or not phases[-1].startswith("moe"):
            A.reset()
        if dbg:
            for t in range(NT):
                dma("sp", out_d[t * 128:(t + 1) * 128, :], h[:, t, :], r=HK(t), is_output=True)
        else:
            rms_stats(list(range(1, NT)))
            nf = A.a([128, D])
            dma("sp", nf, big_d[:, 0, :].partition_broadcast(128), w=["nf"])
            ob = [A.a([128, D]) for _ in range(2)]
            for t in range(1, NT):
                o = ob[t % 2]
                stt(o, h[:, t, :], rstd[:, t:t + 1], nf, ALU.mult, ALU.mult, r=HK(t) + [("rstd", t), "nf"], w=[("ob", t % 2)])
                dma("sp", out_d[(t - 1) * 128:t * 128, :], o, r=[("ob", t % 2)], is_output=True)
        if REORDER:
            S.reorder()
        S.emit()
    return nc


_CACHE = {}


def kernel(**inputs):
    shared = pack_inputs(inputs)
    x = np.ascontiguousarray(np.asarray(inputs["x"], np.float32))
    nb = x.shape[0]
    if "nc" not in _CACHE:
        _CACHE["nc"] = build()
    nc = _CACHE["nc"]
    in_maps = [dict(shared, x=x[b]) for b in range(nb)]
    res = run_bass_kernel_spmd(nc, in_maps, core_ids=list(range(nb)))
    return np.stack([np.asarray(r["out"], np.float32) for r in res.results], axis=0)
```

```python
from contextlib import ExitStack
import numpy as np
import concourse.bass as bass
import concourse.mybir as mybir
from concourse.bass_utils import run_bass_kernel_spmd

F32 = mybir.dt.float32
BF16 = mybir.dt.bfloat16
AF = mybir.ActivationFunctionType
ALU = mybir.AluOpType
AX = mybir.AxisListType

COMPUTE = ("pe", "act", "dve", "pool")
NDSEM = 16
SAME_ENGINE_GAP = 1 << 30

T = 2176
NT = 17
PADN = 112
D = 1024
TB = [(0, 512), (512, 512), (1024, 512), (1536, 512), (2048, 128)]
EPS = 1e-6
MIX0_PARTS = ("lru", "ssd")
SSD_STOP = None
REORDER = True


class Sched:
    def __init__(self, nc):
        self.nc = nc
        self.ops = {e: [] for e in ("pe", "act", "dve", "pool", "sp")}
        self.ccount = {e: 0 for e in COMPUTE}
        self.dcount = {"sp": 0, "pool": 0}
        self.last_w = {}
        self.readers = {}
        self.sig = {e: set() for e in COMPUTE}
        self.out_dmas = []
        self.pending_bar = {}
        self.ps_last = {}
        self.epoch = 0

    def _deps(self, tok, r, w, eng):
        deps = set()
        for k in r:
            t = self.last_w.get(k)
            if t is not None:
                deps.add(t)
        for k in w:
            t = self.last_w.get(k)
            if t is not None:
                deps.add(t)
            for t in self.readers.get(k, ()):
                deps.add(t)
        for k in w:
            self.last_w[k] = tok
            self.readers[k] = []
        for k in r:
            if k in w:
                continue
            self.readers.setdefault(k, []).append(tok)
        for k in set(r) | set(w):
            if isinstance(k, tuple) and k[0] == "ps":
                d = self.ps_last.setdefault(k, {})
                for oe in list(d):
                    if oe != eng:
                        deps.update(d[oe])
                        d[oe] = []
                d.setdefault(eng, []).append(tok)
        bar = self.pending_bar.pop(eng) if eng in self.pending_bar else set()
        deps.discard(tok)
        return deps, bar

    def barrier(self):
        toks = set()
        for e in COMPUTE:
            if self.ccount[e] > 0:
                toks.add(("c", e, self.ccount[e] - 1))
        for q in ("sp", "pool"):
            for k in range(max(0, self.dcount[q] - NDSEM), self.dcount[q]):
                toks.add(("d", q, k))
        for e in ("pe", "act", "dve", "pool", "sp"):
            self.pending_bar[e] = set(toks) | self.pending_bar.get(e, set())
        self.epoch += 1

    def add(self, eng, fn, r=(), w=(), cost=0.3):
        seq = self.ccount[eng]
        self.ccount[eng] += 1
        tok = ("c", eng, seq)
        deps, bar = self._deps(tok, tuple(r), tuple(w), eng)
        self.ops[eng].append(["c", fn, deps, seq, cost, self.epoch, bar])
        return tok

    def reorder(self, window=96, lat=0.35):
        RE = ("pe", "act", "dve")
        fin = {}
        etime = {e: 0.0 for e in self.ops}
        new_order = {e: [] for e in RE}
        ptr = {e: 0 for e in self.ops}
        fence = 0.0
        tokof = lambda e, op: ("c", e, op[3]) if op[0] == "c" else ("d", e, op[3])
        for ep in range(self.epoch + 1):
            seg = {}
            for e in self.ops:
                lst = self.ops[e]
                i = ptr[e]
                j = i
                while j < len(lst) and lst[j][5] == ep:
                    j += 1
                seg[e] = lst[i:j]
                ptr[e] = j
            for e in seg:
                etime[e] = max(etime[e], fence)
            left = {e: list(seg[e]) for e in seg}
            total = sum(len(v) for v in left.values())
            while total:
                best = None
                for e, lst in left.items():
                    if not lst:
                        continue
                    cands = lst[:window] if e in RE else lst[:1]
                    for pos, op in enumerate(cands):
                        ready = 0.0
                        ok = True
                        for t in op[2]:
                            f = fin.get(t)
                            if f is None:
                                ok = False
                                break
                            if t[1] != e:
                                f += lat
                            if f > ready:
                                ready = f
                        if not ok:
                            continue
                        start = max(etime[e], ready)
                        key = (start, pos)
                        if best is None or key < best[0]:
                            best = (key, e, pos, op, start)
                        if start <= etime[e]:
                            break
                assert best is not None, "scheduler stuck"
                _, e, pos, op, start = best
                left[e].pop(pos)
                total -= 1
                if op[0] == "c":
                    etime[e] = start + op[4]
                    fin[tokof(e, op)] = etime[e]
                else:
                    etime[e] = start + 0.5
                    fin[tokof(e, op)] = start + op[4]
                if e in RE:
                    new_order[e].append(op)
            fence = max([fence] + list(etime.values()) + [fin[tokof(e, op)] for e in seg for op in seg[e]])
        remap = {}
        for e in RE:
            bars = {}
            for op in new_order[e]:
                if op[6]:
                    bars.setdefault(op[5], set()).update(op[6])
                    op[6] = set()
            seen_ep = set()
            for i, op in enumerate(new_order[e]):
                if op[5] not in seen_ep:
                    seen_ep.add(op[5])
                    op[6] = bars.get(op[5], set())
                remap[("c", e, op[3])] = ("c", e, i)
                op[3] = i
            self.ops[e] = new_order[e]
        for e, lst in self.ops.items():
            for op in lst:
                op[2] = {remap.get(t, t) for t in op[2]}
                op[6] = {remap.get(t, t) for t in op[6]}
        self.model_time = max(etime.values())

    def dma(self, q, fn, r=(), w=(), is_output=False, cost=3.0):
        k = self.dcount[q]
        self.dcount[q] += 1
        tok = ("d", q, k)
        deps, bar = self._deps(tok, tuple(r), tuple(w), q)
        self.ops[q].append(["d", fn, deps, k, cost, self.epoch, bar])
        if is_output:
            self.out_dmas.append(tok)
        return tok

    def emit(self):
        nc = self.nc
        for e, lst in self.ops.items():
            for op in lst:
                op[2] = set(op[2]) | set(op[6])
        for e, lst in self.ops.items():
            seen_c = {f: -1 for f in COMPUTE}
            for kind, fn, deps, seq, _c, _e, _b in lst:
                cw = {}
                for t in deps:
                    if t[0] != "c":
                        continue
                    f, s = t[1], t[2]
                    if f == e and kind == "c":
                        if e == "pe":
                            continue
                        if seq - s > SAME_ENGINE_GAP:
                            continue
                    if s <= seen_c[f]:
                        continue
                    cw[f] = max(cw.get(f, -1), s)
                for f, s in cw.items():
                    seen_c[f] = s
                    self.sig[f].add(s)
        sigidx = {}
        for e in COMPUTE:
            s = sorted(self.sig[e])
            sigidx[e] = {seq: i + 1 for i, seq in enumerate(s)}
        with ExitStack() as st:
            csem = {e: st.enter_context(nc.semaphore("c_" + e)) for e in COMPUTE}
            dsem = {q: [st.enter_context(nc.semaphore(f"d_{q}{i}")) for i in range(NDSEM)]
                    for q in ("sp", "pool")}
            block = st.enter_context(nc.Block())

            def run(e, eng):
                seen_c = {f: -1 for f in COMPUTE}
                seen_d = {}
                for kind, fn, deps, seq, _c, _e, _b in self.ops[e]:
                    cw = {}
                    for t in deps:
                        if t[0] == "c":
                            f, s = t[1], t[2]
                            if f == e and kind == "c":
                                if e == "pe":
                                    continue
                                if seq - s > SAME_ENGINE_GAP:
                                    continue
                            if s <= seen_c[f]:
                                continue
                            cw[f] = max(cw.get(f, -1), s)
                        else:
                            q, k = t[1], t[2]
                            key = (q, k % NDSEM)
                            val = 16 * (k // NDSEM + 1)
                            if seen_d.get(key, 0) >= val:
                                continue
                            seen_d[key] = val
                            eng.wait_ge(dsem[q][k % NDSEM], val)
                    for f, s in cw.items():
                        seen_c[f] = s
                        eng.wait_ge(csem[f], sigidx[f][s])
                    if kind == "c":
                        ins = fn(eng)
                        if seq in sigidx[e]:
                            ins.then_inc(csem[e], 1)
                    else:
                        k = seq
                        if k >= NDSEM:
                            key = (e, k % NDSEM)
                            val = 16 * (k // NDSEM)
                            if seen_d.get(key, 0) < val:
                                seen_d[key] = val
                                eng.wait_ge(dsem[e][k % NDSEM], val)
                        ins = fn(eng)
                        ins.then_inc(dsem[e][k % NDSEM], 16)
                if e == "sp":
                    for t in self.out_dmas:
                        q, k = t[1], t[2]
                        eng.wait_ge(dsem[q][k % NDSEM], 16 * (k // NDSEM + 1))

            @block.tensor
            def _(eng):
                run("pe", eng)

            @block.scalar
            def _(eng):
                run("act", eng)

            @block.vector
            def _(eng):
                run("dve", eng)

            @block.gpsimd
            def _(eng):
                run("pool", eng)

            @block.sync
            def _(eng):
                run("sp", eng)


COLP = {}
_o = 0
for _n, _c in [("mix_even", 8), ("ffn0", 8), ("ffn1", 8), ("mix_odd", 8), ("ssd_cw", 48), ("ssd_cb", 12),
               ("ssd_norm", 8), ("lru_cw", 32), ("lru_cb", 8), ("lru_ba", 8), ("lru_bx", 8), ("lru_lam", 8)]:
    COLP[_n] = (_o, _c)
    _o += _c
NCOL = _o
ROWP = {}
_o = 0
for _n, _c in [("dt_bias", 16), ("a_log", 16), ("ssd_d", 16), ("rb0", 20), ("rb1", 20), ("rc", 64)]:
    ROWP[_n] = (_o, _c)
    _o += _c
NROW = _o


def _fm(v):
    v = np.asarray(v, np.float32).reshape(-1, 128)
    return np.ascontiguousarray(v.T)


def pack_inputs(inp):
    f = lambda a: np.ascontiguousarray(np.asarray(a, np.float32))
    colp = np.zeros((128, NCOL), np.float32)

    def put(name, arr):
        o, c = COLP[name]
        assert arr.shape == (128, c), (name, arr.shape)
        colp[:, o:o + c] = arr

    put("mix_even", _fm(inp["mix_norm_even"][0]))
    put("ffn0", _fm(inp["ffn_norm"][0]))
    put("ffn1", _fm(inp["ffn_norm"][1]))
    put("mix_odd", _fm(inp["mix_norm_odd"][0]))
    cw = np.asarray(inp["ssd_conv_w"][0], np.float32)
    put("ssd_cw", np.concatenate([_fm(cw[k]) for k in range(4)], axis=1).reshape(128, 4, 12).transpose(0, 2, 1).reshape(128, 48))
    put("ssd_cb", _fm(inp["ssd_conv_b"][0]))
    put("ssd_norm", _fm(inp["ssd_norm"][0]))
    lw = np.asarray(inp["lru_conv_w"][0], np.float32)
    put("lru_cw", np.concatenate([_fm(lw[k]) for k in range(4)], axis=1).reshape(128, 4, 8).transpose(0, 2, 1).reshape(128, 32))
    put("lru_cb", _fm(inp["lru_conv_b"][0]))
    put("lru_ba", _fm(inp["lru_b_a"][0]))
    put("lru_bx", _fm(inp["lru_b_x"][0]))
    put("lru_lam", _fm(inp["lru_lambda"][0]))

    rowp = np.zeros((1, NROW), np.float32)

    def putr(name, arr):
        o, c = ROWP[name]
        rowp[0, o:o + c] = np.asarray(arr, np.float32).reshape(-1)

    putr("dt_bias", inp["ssd_dt_bias"][0])
    putr("a_log", inp["ssd_a_log"][0])
    putr("ssd_d", inp["ssd_d"][0])
    putr("rb0", np.concatenate([np.asarray(inp["router_group_b"][0]), np.asarray(inp["router_expert_b"][0])]))
    putr("rb1", np.concatenate([np.asarray(inp["router_group_b"][1]), np.asarray(inp["router_expert_b"][1])]))
    rc = np.zeros((4, 16), np.float32)
    for g, w in enumerate((2, 4, 8, 16)):
        rc[g] = 1.0 / np.minimum(np.arange(16) + 1, w)
    putr("rc", rc)

    w_in = f(inp["w_in"][0])
    cols = np.concatenate([np.arange(0, 2560), np.arange(2576, 4624)])
    w_in_r = np.ascontiguousarray(w_in[:, cols].reshape(8, 128, 36, 128).transpose(2, 1, 0, 3))
    w_dt = np.ascontiguousarray(w_in[:, 2560:2576].reshape(8, 128, 16).transpose(1, 0, 2))
    wr = np.stack([np.concatenate([f(inp["router_group_w"][l]), f(inp["router_expert_w"][l])], axis=1)
                   .reshape(8, 128, 20).transpose(1, 0, 2) for l in range(2)])
    k_ = np.arange(128)[:, None]
    s_ = np.arange(128)[None, :]
    cst = np.stack([np.eye(128, dtype=np.float32), (k_ <= s_).astype(np.float32), (k_ > s_).astype(np.float32),
                    np.ones((128, 128), np.float32)], axis=1)
    shared = {
        "meta": f(inp["meta_tokens"]), "colp": colp, "rowp": rowp, "cst": np.ascontiguousarray(cst),
        "w_in_r": w_in_r, "w_dt": w_dt, "w_out": f(inp["w_out"][0]),
        "lru_wa": f(inp["lru_w_a"][0]), "lru_wx": f(inp["lru_w_x"][0]),
        "pool_w": f(inp["pool_w"][0]), "wr": np.ascontiguousarray(wr),
        "bigrow": np.ascontiguousarray(np.stack([f(inp["norm_final"]), f(inp["pool_b"][0]), f(inp["pool_scale"][0])])[None]),
        "wg": f(inp["expert_w_gate"]), "wu": f(inp["expert_w_up"]), "wd": f(inp["expert_w_down"]),
    }
    return shared


def build(phases=("mix0", "moe0", "mix1", "moe1"), dbg=False):
    nc = bass.Bass("TRN2", target_bir_lowering=False)
    dram = lambda n, s, kind="ExternalInput": nc.dram_tensor(n, list(s), F32, kind=kind).ap()
    x_d = dram("x", [2048, D])
    meta_d = dram("meta", [16, D])
    colp_d = dram("colp", [128, NCOL])
    rowp_d = dram("rowp", [1, NROW])
    cst_d = dram("cst", [128, 4, 128])
    w_in_d = dram("w_in_r", [36, 128, 8, 128])
    w_dt_d = dram("w_dt", [128, 8, 16])
    w_out_d = dram("w_out", [2048, D])
    lwa_d = dram("lru_wa", [16, 64, 64])
    lwx_d = dram("lru_wx", [16, 64, 64])
    pw_d = dram("pool_w", [4, 256, 256])
    wr_d = dram("wr", [2, 128, 8, 20])
    big_d = dram("bigrow", [1, 3, D])
    wg_d = dram("wg", [2, 16, D, 512])
    wu_d = dram("wu", [2, 16, D, 512])
    wd_d = dram("wd", [2, 16, 512, D])
    if dbg:
        out_d = dram("out", [T, D], kind="ExternalOutput")
    else:
        out_d = dram("out", [2048, D], kind="ExternalOutput")

    S = Sched(nc)
    with ExitStack() as st:
        sb = lambda n, s, dt=F32: st.enter_context(nc.sbuf_tensor(n, list(s), dt))
        h = sb("h", [128, NT, D])
        hnT = sb("hnT", [128, 8, T], BF16)
        colp = sb("colp_s", [128, NCOL])
        rowp = sb("rowp_s", [128, NROW])
        cst = sb("cst_s", [128, 4, 128])
        cst16 = sb("cst16", [128, 4, 128], BF16)
        stat = sb("stat", [128, 64])
        AW = 25900
        arena = sb("arena", [128, AW])
        ps = [st.enter_context(nc.psum_tensor(f"ps{i}", [128, 512], F32)) for i in range(8)]
        ident = cst[:, 0, :]
        tri_le = cst[:, 1, :]
        u_gt = cst[:, 2, :]
        ones32 = cst[:, 3, :]
        ident16 = cst16[:, 0, :]
        ss = stat[:, 0:17]
        sq = stat[:, 17:34]
        rstd = stat[:, 34:51]

        class Arena:
            def __init__(self):
                self.off = 0

            def reset(self):
                self.off = 0
                S.barrier()

            def a(self, shape, dt=F32):
                n = int(np.prod(shape[1:]))
                words = n if dt == F32 else (n + 1) // 2
                assert self.off + words <= AW, ("arena overflow", self.off, words)
                v = arena[:, self.off:self.off + words]
                self.off += words
                if dt != F32:
                    v = v.bitcast(dt)
                    if v.shape[1] != n:
                        v = v[:, 0:n]
                if len(shape) == 3:
                    v = v.rearrange("p (a b) -> p a b", a=shape[1])
                elif len(shape) == 4:
                    v = v.rearrange("p (a b c) -> p a b c", a=shape[1], b=shape[2])
                return v

        A = Arena()
        colv = lambda name, i=0, n=None: colp[:, COLP[name][0] + i:COLP[name][0] + i + (n if n else 1)]
        rowv = lambda name: rowp[:, ROWP[name][0]:ROWP[name][0] + ROWP[name][1]]

        def fsz(ap):
            n = 1
            for d_ in ap.shape[1:]:
                n *= int(d_)
            return n

        def ecost(eng, n, mult=1.0):
            if eng == "dve":
                return 0.12 + mult * n / 960.0
            if eng == "act":
                return 0.25 + n / 1400.0
            return 2.0 + 0.015 * n

        def mm(out, lhsT, rhs, start, stop, r, w):
            c = max(fsz(rhs), 64) / 2400.0 + 0.03
            if lhsT.dtype == F32:
                c *= 4
            S.add("pe", lambda e: e.matmul(out, lhsT, rhs, start=start, stop=stop), r=r, w=w, cost=c)

        def tr(out, in_, idn, r, w):
            S.add("pe", lambda e: e.transpose(out, in_, idn), r=r, w=w, cost=0.3 if in_.dtype == F32 else 0.12)

        def act(out, in_, func, r, w, scale=1.0, bias=0.0, accum=None):
            c = ecost("act", fsz(in_)) + (0.1 if accum is not None else 0.0)
            if accum is None:
                S.add("act", lambda e: e.activation(out=out, in_=in_, func=func, scale=scale, bias=bias), r=r, w=w, cost=c)
            else:
                S.add("act", lambda e: e.activation(out=out, in_=in_, func=func, scale=scale, bias=bias, accum_out=accum), r=r, w=w, cost=c)

        def tt(eng, out, in0, in1, op, r, w):
            S.add(eng, lambda e: e.tensor_tensor(out=out, in0=in0, in1=in1, op=op), r=r, w=w, cost=ecost(eng, fsz(out)))

        def ts(eng, out, in0, s1, op0, r, w, s2=None, op1=None):
            c = ecost(eng, fsz(out))
            if op1 is None:
                S.add(eng, lambda e: e.tensor_scalar(out=out, in0=in0, scalar1=s1, scalar2=None, op0=op0), r=r, w=w, cost=c)
            else:
                S.add(eng, lambda e: e.tensor_scalar(out=out, in0=in0, scalar1=s1, scalar2=s2, op0=op0, op1=op1), r=r, w=w, cost=c)

        def stt(out, in0, scalar, in1, op0, op1, r, w):
            S.add("dve", lambda e: e.scalar_tensor_tensor(out=out, in0=in0, scalar=scalar, in1=in1, op0=op0, op1=op1), r=r, w=w,
                  cost=ecost("dve", fsz(out)))

        def cp(eng, out, in_, r, w):
            c = ecost(eng, fsz(out))
            if eng == "act":
                S.add("act", lambda e: e.copy(out, in_), r=r, w=w, cost=c)
            else:
                S.add(eng, lambda e: e.tensor_copy(out, in_), r=r, w=w, cost=c)

        def memset(eng, ap, val, w):
            S.add(eng, lambda e: e.memset(ap, val), w=w, cost=ecost(eng, fsz(ap)))

        def dma(q, out, in_, r=(), w=(), is_output=False):
            nbytes = fsz(out) * int(out.shape[0]) * 4
            S.dma(q, lambda e: e.dma_start(out=out, in_=in_), r=r, w=w, is_output=is_output, cost=2.5 + nbytes / 150e3)

        HK = lambda t: [("h", t, 0), ("h", t, 1)]

        dma("sp", colp[:], colp_d, w=["colp"])
        dma("sp", rowp[:], rowp_d.partition_broadcast(128), w=["rowp"])
        dma("sp", cst[:], cst_d, w=["cst"])
        dma("pool", cst16[:], cst_d, w=["cst16"])
        memset("dve", h[:, 0, :], 0.0, w=HK(0))
        dma("sp", h[PADN:128, 0, :], meta_d, w=HK(0))
        xr = x_d.rearrange("(t p) d -> p t d", p=128)
        for i in range(4):
            dma("sp", h[:, 1 + 4 * i:5 + 4 * i, :], xr[:, 4 * i:4 * i + 4, :], w=[k for t in range(1 + 4 * i, 5 + 4 * i) for k in HK(t)])

        def rms_stats(tiles):
            junk = A.a([128, D], BF16)
            for t in tiles:
                act(junk, h[:, t, :], AF.Square, r=HK(t), w=[("ss", t), "junk"], accum=ss[:, t:t + 1])
                act(sq[:, t:t + 1], ss[:, t:t + 1], AF.Sqrt, r=[("ss", t)], w=[("sq", t)], scale=1.0 / D, bias=colv_eps)
                S.add("dve", lambda e, t=t: e.reciprocal(rstd[:, t:t + 1], sq[:, t:t + 1]), r=[("sq", t)], w=[("rstd", t)], cost=0.15)

        def normT(gname, router_l=None, logits=None, wr_s=None, consume=None):
            xsb = [A.a([128, D]) for _ in range(2)]
            t32 = [A.a([128, 4, 128]) for _ in range(4)]
            pend = None
            for t in range(NT):
                xs = xsb[t % 2]
                act(xs, h[:, t, :], AF.Identity, r=HK(t) + [("rstd", t)], w=[("xs", t % 2)], scale=rstd[:, t:t + 1])
                for half in range(2):
                    i2 = (2 * t + half) % 2
                    i4 = (2 * t + half) % 4
                    bank = ps[6 + i2]
                    for kk in range(4):
                        k = half * 4 + kk
                        tr(bank[:, kk * 128:(kk + 1) * 128], xs[:, k * 128:(k + 1) * 128], ident, r=[("xs", t % 2), "cst"], w=[("ps", 6 + i2)])
                    g0 = COLP[gname][0] + half * 4
                    tt("dve", t32[i4], bank[:, :].rearrange("p (a b) -> p a b", a=4),
                       colp[:, g0:g0 + 4].unsqueeze(2).to_broadcast([128, 4, 128]), ALU.mult,
                       r=[("ps", 6 + i2), "colp"], w=[("t32", i4)])
                    cp("act", hnT[:, half * 4:half * 4 + 4, t * 128:(t + 1) * 128], t32[i4], r=[("t32", i4)], w=[("hnT", t)])
                if router_l is not None:
                    if pend is not None:
                        pend()

                    def mk(t=t):
                        def go():
                            for k in range(8):
                                i4 = (2 * t + k // 4) % 4
                                mm(ps[5][:, 0:20], t32[i4][:, k % 4, :], wr_s[:, k, :], k == 0, k == 7,
                                   r=[("t32", i4), "wr"], w=[("ps", 5)])
                            cp("act", logits[:, t, :], ps[5][:, 0:20], r=[("ps", 5)], w=["logits"])
                        return go
                    pend = mk()
            if pend is not None:
                pend()

        memset("pool", stat[:, 60:61], EPS, w=["eps"])
        colv_eps = stat[:, 60:61]

        def moe(l):
            A.reset()
            wr_s = A.a([128, 8, 20])
            logits = A.a([128, NT, 20])
            gates = A.a([128, NT, 16])
            dma("sp", wr_s, wr_d[l], w=["wr"])
            Wg = [A.a([128, 8, 512], BF16) for _ in range(2)]
            Wu = [A.a([128, 8, 512], BF16) for _ in range(2)]
            Wd = [A.a([128, 4, D], BF16) for _ in range(2)]

            def load_w(e):
                b = e % 2
                if e == 0:
                    for fc in range(4):
                        fs = slice(fc * 128, (fc + 1) * 128)
                        dma("pool", Wg[b][:, :, fs], wg_d[l, e][:, fs].rearrange("(k p) f -> p k f", p=128), w=[("Wg", b, fc)])
                        dma("pool", Wu[b][:, :, fs], wu_d[l, e][:, fs].rearrange("(k p) f -> p k f", p=128), w=[("Wu", b, fc)])
                else:
                    dma("pool", Wg[b], wg_d[l, e].rearrange("(k p) f -> p k f", p=128), w=[("Wg", b, fc) for fc in range(4)])
                    dma("pool", Wu[b], wu_d[l, e].rearrange("(k p) f -> p k f", p=128), w=[("Wu", b, fc) for fc in range(4)])
                dma("pool", Wd[b], wd_d[l, e].rearrange("(k p) f -> p k f", p=128), w=[("Wd", b)])

            load_w(0)
            load_w(1)
            mark = A.off
            rms_stats(list(range(NT)))
            normT("ffn%d" % l, router_l=l, logits=logits, wr_s=wr_s)
            R = lambda shape: A.a(shape)
            rb = rowv("rb%d" % l)
            lg = R([128, NT, 20])
            tt("dve", lg, logits, rb.unsqueeze(1).to_broadcast([128, NT, 20]), ALU.add, r=["logits", "rowp"], w=["lg"])
            m4 = R([128, NT])
            S.add("dve", lambda e: e.tensor_reduce(out=m4, in_=lg[:, :, 0:4], axis=AX.X, op=ALU.max), r=["lg"], w=["m4"])
            d4 = R([128, NT, 4])
            tt("dve", d4, lg[:, :, 0:4], m4.unsqueeze(2).to_broadcast([128, NT, 4]), ALU.subtract, r=["lg", "m4"], w=["d4"])
            mg = R([128, NT, 4])
            ts("dve", mg, d4, 0.0, ALU.is_ge, r=["d4"], w=["mg"])
            e4 = R([128, NT, 4])
            act(e4, d4, AF.Exp, r=["d4"], w=["e4"])
            s4 = R([128, NT])
            S.add("dve", lambda e: e.tensor_reduce(out=s4, in_=e4, axis=AX.X, op=ALU.add), r=["e4"], w=["s4"])
            le = lg[:, :, 4:20].rearrange("p t (g j) -> p t g j", g=4)
            ml = R([128, NT, 4, 4])
            tt("dve", ml, le, mg.unsqueeze(3).to_broadcast([128, NT, 4, 4]), ALU.mult, r=["lg", "mg"], w=["ml"])
            sel = R([128, NT, 4])
            tt("dve", sel, ml[:, :, 0, :], ml[:, :, 1, :], ALU.add, r=["ml"], w=["sel"])
            tt("dve", sel, sel, ml[:, :, 2, :], ALU.add, r=["ml", "sel"], w=["sel"])
            tt("dve", sel, sel, ml[:, :, 3, :], ALU.add, r=["ml", "sel"], w=["sel"])
            m1 = R([128, NT])
            S.add("dve", lambda e: e.tensor_reduce(out=m1, in_=sel, axis=AX.X, op=ALU.max), r=["sel"], w=["m1"])
            k1 = R([128, NT, 4])
            tt("dve", k1, sel, m1.unsqueeze(2).to_broadcast([128, NT, 4]), ALU.is_ge, r=["sel", "m1"], w=["k1"])
            sel2 = R([128, NT, 4])
            stt(sel2, k1, -1e30, sel, ALU.mult, ALU.add, r=["k1", "sel"], w=["sel2"])
            m2 = R([128, NT])
            S.add("dve", lambda e: e.tensor_reduce(out=m2, in_=sel2, axis=AX.X, op=ALU.max), r=["sel2"], w=["m2"])
            k2 = R([128, NT, 4])
            tt("dve", k2, sel2, m2.unsqueeze(2).to_broadcast([128, NT, 4]), ALU.is_ge, r=["sel2", "m2"], w=["k2"])
            dd = R([128, NT])
            tt("dve", dd, m2, m1, ALU.subtract, r=["m1", "m2"], w=["dd"])
            w2 = R([128, NT])
            act(w2, dd, AF.Exp, r=["dd"], w=["w2"])
            den = R([128, NT])
            stt(den, w2, 1.0, s4, ALU.add, ALU.mult, r=["w2", "s4"], w=["den"])
            g1 = R([128, NT])
            S.add("dve", lambda e: e.reciprocal(g1, den), r=["den"], w=["g1"])
            g2 = R([128, NT])
            tt("dve", g2, g1, w2, ALU.mult, r=["g1", "w2"], w=["g2"])
            gs = R([128, NT, 4])
            tt("dve", gs, k1, g1.unsqueeze(2).to_broadcast([128, NT, 4]), ALU.mult, r=["k1", "g1"], w=["gs"])
            gs2 = R([128, NT, 4])
            tt("dve", gs2, k2, g2.unsqueeze(2).to_broadcast([128, NT, 4]), ALU.mult, r=["k2", "g2"], w=["gs2"])
            tt("dve", gs, gs, gs2, ALU.add, r=["gs", "gs2"], w=["gs"])
            g4 = gates.rearrange("p t (g j) -> p t g j", g=4)
            for g in range(4):
                tt("dve", g4[:, :, g, :], gs, mg[:, :, g:g + 1].to_broadcast([128, NT, 4]), ALU.mult, r=["gs", "mg"], w=["gates"])

            hid = [A.a([128, 4, 512], BF16) for _ in range(2)]
            sg = [A.a([128, 512]) for _ in range(2)]
            cnt = 0
            cnt_o = [0]
            blk = 0
            pend = None
            for e in range(16):
                b = e % 2
                if e >= 2:
                    load_w(e)
                for (t0, n) in TB:
                    hb = blk % 2
                    blk += 1
                    hk = [("hnT", tt_) for tt_ in range(t0 // 128, (t0 + n) // 128)]
                    for fc in range(4):
                        i = cnt % 2
                        cnt += 1
                        pg, pu = ps[i], ps[2 + i]
                        for k in range(8):
                            mm(pg[:, 0:n], Wg[b][:, k, fc * 128:(fc + 1) * 128], hnT[:, k, t0:t0 + n], k == 0, k == 7,
                               r=[("Wg", b, fc)] + hk, w=[("ps", i)])
                        for k in range(8):
                            mm(pu[:, 0:n], Wu[b][:, k, fc * 128:(fc + 1) * 128], hnT[:, k, t0:t0 + n], k == 0, k == 7,
                               r=[("Wu", b, fc)] + hk, w=[("ps", 2 + i)])
                        act(sg[i][:, 0:n], pg[:, 0:n], AF.Silu, r=[("ps", i)], w=[("sg", i)])
                        tt("dve", hid[hb][:, fc, 0:n], sg[i][:, 0:n], pu[:, 0:n], ALU.mult, r=[("sg", i), ("ps", 2 + i)], w=[("hid", hb)])
                    if pend is not None:
                        pend()

                    def mk(e=e, b=b, hb=hb, t0=t0, n=n):
                        def go():
                            for tl in range(n // 128):
                                t = t0 // 128 + tl
                                for dh in range(2):
                                    io = 4 + cnt_o[0] % 2
                                    cnt_o[0] += 1
                                    for fc in range(4):
                                        mm(ps[io][:, :], hid[hb][:, fc, tl * 128:(tl + 1) * 128], Wd[b][:, fc, dh * 512:(dh + 1) * 512],
                                           fc == 0, fc == 3, r=[("hid", hb), ("Wd", b)], w=[("ps", io)])
                                    hv = h[:, t, dh * 512:(dh + 1) * 512]
                                    stt(hv, ps[io][:, :], gates[:, t, e:e + 1], hv,
                                        ALU.mult, ALU.add, r=[("ps", io), "gates", ("h", t, dh)], w=[("h", t, dh)])
                        return go
                    pend = mk()
            pend()

        def mix1():
            A.reset()
            memset("pool", h[0:PADN, 0, :], 0.0, w=HK(0))
            rms_stats(list(range(NT)))
            pw = A.a([128, 4, 2, 256], BF16)
            for g in range(4):
                dma("pool", pw[:, g, :, :], pw_d[g].rearrange("(c p) j -> p c j", p=128), w=["pw"])
            pb_bc = A.a([128, D])
            sc_bc = A.a([128, D])
            dma("sp", pb_bc, big_d[:, 1, :].partition_broadcast(128), w=["pb_bc"])
            dma("sp", sc_bc, big_d[:, 2, :].partition_broadcast(128), w=["sc_bc"])
            bs_bc = A.a([128, D])
            tt("dve", bs_bc, pb_bc, sc_bc, ALU.mult, r=["pb_bc", "sc_bc"], w=["bs_bc"])
            PF = 16
            hn32 = A.a([128, PF + T])
            sA = A.a([128, PF + T])
            sB = A.a([128, PF + T])
            pooledT = A.a([128, 8, T], BF16)
            for buf, nm in ((hn32, "hn32"), (sA, "sA"), (sB, "sB")):
                memset("pool", buf[:, 0:PF], 0.0, w=[nm])
            xsn = A.a([128, NT, 128])
            fix = A.a([128, 16])
            rcv = rowv("rc")
            for k in range(8):
                g = k // 2
                w = (2, 4, 8, 16)[g]
                tt("dve", xsn, h[:, :, k * 128:(k + 1) * 128], rstd[:, 0:NT].unsqueeze(2).to_broadcast([128, NT, 128]), ALU.mult,
                   r=[("h", t, k // 4) for t in range(NT)] + [("rstd", t) for t in range(NT)], w=["xsn"])
                for q in range(5):
                    tiles = list(range(4 * q, min(4 * q + 4, NT)))
                    bank = ps[6 + q % 2]
                    for i, t in enumerate(tiles):
                        tr(bank[:, i * 128:(i + 1) * 128], xsn[:, t, :], ident, r=["xsn", "cst"], w=[("ps", 6 + q % 2)])
                    nn = len(tiles) * 128
                    ts("dve", hn32[:, PF + q * 512:PF + q * 512 + nn], bank[:, 0:nn], colv("mix_odd", k), ALU.mult,
                       r=[("ps", 6 + q % 2), "colp"], w=["hn32"])
                cur, curk = hn32, "hn32"
                step = 1
                bufs = [(sA, "sA"), (sB, "sB")]
                bi = 0
                while step < w:
                    nxt, nk = bufs[bi % 2]
                    bi += 1
                    tt("dve", nxt[:, PF:PF + T], cur[:, PF:PF + T], cur[:, PF - step:PF + T - step], ALU.add,
                       r=[curk], w=[nk])
                    cur, curk = nxt, nk
                    step *= 2
                stt(pooledT[:, k, :], cur[:, PF:PF + T], 1.0 / w, hn32[:, PF:PF + T], ALU.mult, ALU.subtract,
                    r=[curk, "hn32"], w=[("pooledT", k)])
                tt("dve", fix, cur[:, PF + PADN:PF + 128], rcv[:, g * 16:(g + 1) * 16], ALU.mult, r=[curk, "rowp"], w=["fix"])
                tt("dve", pooledT[:, k, PADN:128], fix, hn32[:, PF + PADN:PF + 128], ALU.subtract, r=["fix", "hn32"], w=[("pooledT", k)])
            t1 = [A.a([128, 512]) for _ in range(2)]
            ci = 0
            for t in range(NT):
                for dh in range(2):
                    bi_ = dh + 2 * (t % 2)
                    bank = ps[bi_]
                    for gg in range(2):
                        g = dh * 2 + gg
                        for ic in range(2):
                            mm(bank[:, gg * 256:(gg + 1) * 256], pooledT[:, 2 * g + ic, t * 128:(t + 1) * 128], pw[:, g, ic, :], ic == 0, ic == 1,
                               r=[("pooledT", 2 * g + ic), "pw"], w=[("ps", bi_)])
                    tb_ = t1[ci % 2]
                    tk = ("t1", ci % 2)
                    ci += 1
                    tt("dve", tb_, bank[:, :], sc_bc[:, dh * 512:(dh + 1) * 512], ALU.mult, r=[("ps", bi_), "sc_bc"], w=[tk])
                    tt("dve", tb_, tb_, bs_bc[:, dh * 512:(dh + 1) * 512], ALU.add, r=[tk, "bs_bc"], w=[tk])
                    hv = h[:, t, dh * 512:(dh + 1) * 512]
                    tt("dve", hv, hv, tb_, ALU.add, r=[tk, ("h", t, dh)], w=[("h", t, dh)])

        def mix0():
            A.reset()
            rms_stats(list(range(NT)))
            normT("mix_even")
            A.reset()
            hnk = [("hnT", t) for t in range(NT)]
            wbuf = [A.a([128, 8, 128], BF16) for _ in range(3)]
            wcnt = [0]

            def load_wchunk(cc):
                i = wcnt[0] % 3
                wcnt[0] += 1
                dma("pool", wbuf[i], w_in_d[cc], w=[("wbuf", i)])
                return i

            def proj_blk(wi, bi, t0, n):
                bk = ("ps", bi % 2)
                bank = ps[bi % 2]
                for k in range(8):
                    mm(bank[:, 0:n], wbuf[wi][:, k, :], hnT[:, k, t0:t0 + n], k == 0, k == 7, r=[("wbuf", wi)] + hnk, w=[bk])
                return bank, bk

            xinb = [A.a([128, 3 + 512]) for _ in range(2)]
            ctb = [A.a([128, 512]) for _ in range(2)]
            cvc = [0]

            def conv_blk(bank, bk, bi, n, wname, bname, cidx, tap0_act=False, copy_eng="act"):
                i = cvc[0] % 2
                cvc[0] += 1
                xi, xk = xinb[i], ("xinb", i)
                if bi == 0:
                    memset("dve", xi[:, 0:3], 0.0, w=[xk])
                else:
                    pv = xinb[1 - i]
                    cp("dve", xi[:, 0:3], pv[:, 512:515], r=[("xinb", 1 - i)], w=[xk])
                cp(copy_eng, xi[:, 3:3 + n], bank[:, 0:n], r=[bk], w=[xk])
                ct, ck = ctb[i], ("ctb", i)
                o4 = COLP[wname][0] + 4 * cidx
                if tap0_act:
                    act(ct[:, 0:n], xi[:, 0:n], AF.Identity, r=[xk, "colp"], w=[ck], scale=colp[:, o4:o4 + 1], bias=colv(bname, cidx))
                else:
                    ts("dve", ct[:, 0:n], xi[:, 0:n], colp[:, o4:o4 + 1], ALU.mult, r=[xk, "colp"], w=[ck], s2=colv(bname, cidx), op1=ALU.add)
                for k in range(1, 4):
                    stt(ct[:, 0:n], xi[:, k:k + n], colp[:, o4 + k:o4 + k + 1], ct[:, 0:n], ALU.mult, ALU.add, r=[xk, ck, "colp"], w=[ck])
                return ct, ck

            wo = A.a([128, 4, D], BF16)
            ybuf = A.a([128, 4, T], BF16)

            def out_proj(kc0, scale_ap_fn):
                dma("pool", wo, w_out_d[kc0 * 128:(kc0 + 4) * 128, :].rearrange("(k p) d -> p k d", p=128), w=["wo"])
                cnt = 0
                for t in range(NT):
                    for dh in range(2):
                        io = 4 + cnt % 2
                        cnt += 1
                        for kk in range(4):
                            mm(ps[io][:, :], ybuf[:, kk, t * 128:(t + 1) * 128], wo[:, kk, dh * 512:(dh + 1) * 512], kk == 0, kk == 3,
                               r=[("ybuf", kk), "wo"], w=[("ps", io)])
                        hv = h[:, t, dh * 512:(dh + 1) * 512]
                        if scale_ap_fn is None:
                            tt("dve", hv, hv, ps[io][:, :], ALU.add, r=[("ps", io), ("h", t, dh)], w=[("h", t, dh)])
                        else:
                            stt(hv, ps[io][:, :], scale_ap_fn(t), hv, ALU.mult, ALU.add, r=[("ps", io), ("h", t, dh), "rstdg"], w=[("h", t, dh)])

            mark = A.off

            def lru():
                bdA = A.a([128, 8, 128], BF16)
                bdX = A.a([128, 8, 128], BF16)
                memset("dve", bdA, 0.0, w=["bdA"])
                memset("dve", bdX, 0.0, w=["bdX"])
                for src, dst, nm in ((lwa_d, bdA, "bdA"), (lwx_d, bdX, "bdX")):
                    v = src.rearrange("(j two) i o -> two i j o", two=2)
                    dma("pool", dst[0:64, :, 0:64], v[0], w=[nm])
                    dma("pool", dst[64:128, :, 64:128], v[1], w=[nm])
                c1 = A.a([128, 8])
                tmpc = A.a([128, 8])
                act(tmpc, colv("lru_lam", 0, 8), AF.Exp, r=["colp"], w=["tmpc"], scale=-1.0)
                act(tmpc, tmpc, AF.Ln, r=["tmpc"], w=["tmpc"], bias=1.0)
                ts("dve", c1, tmpc, -8.0, ALU.mult, r=["tmpc"], w=["c1"])
                Bn = lambda n_: [A.a([128, 512]) for _ in range(n_)]
                gl, rr, ii, av, uv, hl = Bn(4), Bn(4), Bn(4), Bn(4), Bn(2), Bn(2)
                xb16 = [A.a([128, 512], BF16) for _ in range(4)]
                NB = len(TB)
                items = [(j, bi) for j in range(8) for bi in range(NB)]
                st1 = {}

                def S1(idx):
                    j, bi = items[idx]
                    t0, n = TB[bi]
                    if bi == 0:
                        st1[j] = (load_wchunk(28 + j), load_wchunk(20 + j))
                    wi_in, wi_gt = st1[j]
                    bank, bk = proj_blk(wi_in, 2 * idx, t0, n)
                    xb, xbk = conv_blk(bank, bk, bi, n, "lru_cw", "lru_cb", j, copy_eng="dve")
                    bank2, bk2 = proj_blk(wi_gt, 2 * idx + 1, t0, n)
                    g3 = idx % 4
                    act(gl[g3][:, 0:n], bank2[:, 0:n], AF.Gelu_apprx_tanh, r=[bk2], w=[("gl", g3)])
                    cp("act", xb16[g3][:, 0:n], xb[:, 0:n], r=[xbk], w=[("xb16", g3)])
                    st1[idx, "xb"] = (xb, xbk)

                def S2(idx):
                    j, bi = items[idx]
                    t0, n = TB[bi]
                    ip = idx % 2
                    q = idx % 4
                    K = lambda nm: (nm, q)
                    xb, xbk = st1.pop((idx, "xb"))
                    pa, px = ps[2 + ip], ps[4 + ip]
                    mm(pa[:, 0:n], bdA[:, j, :], xb16[q][:, 0:n], True, True, r=["bdA", K("xb16")], w=[("ps", 2 + ip)])
                    mm(px[:, 0:n], bdX[:, j, :], xb16[q][:, 0:n], True, True, r=["bdX", K("xb16")], w=[("ps", 4 + ip)])
                    r_, i_ = rr[q], ii[q]
                    act(r_[:, 0:n], pa[:, 0:n], AF.Sigmoid, r=[("ps", 2 + ip), "colp"], w=[K("rr")], bias=colv("lru_ba", j))
                    act(i_[:, 0:n], px[:, 0:n], AF.Sigmoid, r=[("ps", 4 + ip), "colp"], w=[K("ii")], bias=colv("lru_bx", j))
                    act(av[q][:, 0:n], r_[:, 0:n], AF.Exp, r=[K("rr"), "c1"], w=[K("av")], scale=c1[:, j:j + 1])
                    stt(r_[:, 0:n], av[q][:, 0:n], 0.9999998, av[q][:, 0:n], ALU.min, ALU.mult, r=[K("av")], w=[K("rr")])
                    act(r_[:, 0:n], r_[:, 0:n], AF.Sqrt, r=[K("rr")], w=[K("rr")], scale=-1.0, bias=1.0)
                    tt("dve", i_[:, 0:n], i_[:, 0:n], xb[:, 0:n], ALU.mult, r=[K("ii"), xbk], w=[K("ii")])
                    if bi == 0:
                        memset("dve", r_[:, PADN:PADN + 1], 1.0, w=[K("rr")])

                def S3(idx):
                    j, bi = items[idx]
                    t0, n = TB[bi]
                    i = idx % 2
                    q = idx % 4
                    jj = j % 4
                    K = lambda nm: (nm, q)
                    tt("dve", uv[i][:, 0:n], rr[q][:, 0:n], ii[q][:, 0:n], ALU.mult, r=[K("rr"), K("ii")], w=[("uv", i)])
                    if bi == 0:
                        memset("dve", hl[i][:, 0:PADN], 0.0, w=[("hl", i)])
                        S.add("dve", lambda e, i=i, q=q, n=n: e.tensor_tensor_scan(out=hl[i][:, PADN:n], data0=av[q][:, PADN:n], data1=uv[i][:, PADN:n],
                                                                             initial=0.0, op0=ALU.mult, op1=ALU.add),
                              r=[K("av"), ("uv", i)], w=[("hl", i)], cost=1.2)
                    else:
                        S.add("dve", lambda e, i=i, q=q, n=n: e.tensor_tensor_scan(out=hl[i][:, 0:n], data0=av[q][:, 0:n], data1=uv[i][:, 0:n],
                                                                             initial=hl[1 - i][:, 511:512], op0=ALU.mult, op1=ALU.add),
                              r=[K("av"), ("uv", i), ("hl", 1 - i)], w=[("hl", i)], cost=1.2)
                    tt("dve", ybuf[:, jj, t0:t0 + n], hl[i][:, 0:n], gl[q][:, 0:n], ALU.mult, r=[("hl", i), ("gl", q)], w=[("ybuf", jj)])
                    if bi == NB - 1 and jj == 3:
                        out_proj(8 + (j // 4) * 4, None)

                NI = len(items)
                for step in range(NI + 2):
                    if step < NI:
                        S1(step)
                    if 0 <= step - 1 < NI:
                        S2(step - 1)
                    if 0 <= step - 2 < NI:
                        S3(step - 2)

            def ssd():
                dt = A.a([128, NT, 16])
                adt = A.a([128, NT, 16])
                ea = A.a([128, NT, 16])
                eatot = A.a([128, NT, 16])
                dte = A.a([128, NT, 16])
                Aneg = A.a([128, 16])
                wdt = A.a([128, 8, 16], BF16)
                dma("pool", wdt, w_dt_d, w=["wdt"])
                for t in range(NT):
                    for k in range(8):
                        mm(ps[7][:, t * 16:(t + 1) * 16], hnT[:, k, t * 128:(t + 1) * 128], wdt[:, k, :], k == 0, k == 7,
                           r=["wdt", ("hnT", t)], w=[("ps", 7)])
                p7 = ps[7][:, 0:NT * 16].rearrange("p (t h) -> p t h", t=NT)
                tt("dve", dt, p7, rowv("dt_bias").unsqueeze(1).to_broadcast([128, NT, 16]), ALU.add, r=[("ps", 7), "rowp"], w=["dt"])
                act(dte, dt, AF.Abs, r=["dt"], w=["dte"])
                act(dte, dte, AF.Exp, r=["dte"], w=["dte"], scale=-1.0)
                act(dte, dte, AF.Ln, r=["dte"], w=["dte"], bias=1.0)
                stt(dt, dt, 0.0, dte, ALU.max, ALU.add, r=["dt", "dte"], w=["dt"])
                memset("dve", dt[0:PADN, 0, :], 0.0, w=["dt"])
                act(Aneg, rowv("a_log"), AF.Exp, r=["rowp"], w=["Aneg"])
                stt(adt, dt, -1.0, Aneg.unsqueeze(1).to_broadcast([128, NT, 16]), ALU.mult, ALU.mult, r=["dt", "Aneg"], w=["adt"])
                for t in range(NT):
                    mm(ps[6][:, t * 16:(t + 1) * 16], tri_le, adt[:, t, :], True, True, r=["cst", "adt"], w=[("ps", 6)])
                for t in range(NT):
                    mm(ps[7][:, t * 16:(t + 1) * 16], ones32, adt[:, t, :], True, True, r=["cst", "adt"], w=[("ps", 7)])
                p6 = ps[6][:, 0:NT * 16].rearrange("p (t h) -> p t h", t=NT)
                cp("dve", ea, p6, r=[("ps", 6)], w=["ea"])
                cp("dve", eatot, p7, r=[("ps", 7)], w=["eatot"])
                tt("dve", dte, eatot, ea, ALU.subtract, r=["eatot", "ea"], w=["dte"])
                act(dte, dte, AF.Exp, r=["dte"], w=["dte"])
                act(ea, ea, AF.Exp, r=["ea", "dte"], w=["ea"])
                act(eatot, eatot, AF.Exp, r=["eatot", "dte"], w=["eatot"])
                Dbc = rowv("ssd_d")
                DI = A.a([128, 2, 128], BF16)
                if SSD_STOP == "dt":
                    return

                BT = A.a([128, T], BF16)
                CT = A.a([128, T], BF16)
                Btok = A.a([128, NT, 128], BF16)
                CBm = A.a([128, NT, 128], BF16)
                xT16 = [A.a([128, 512], BF16) for _ in range(2)]
                xtok = A.a([128, NT, 128], BF16)
                xdt = A.a([128, NT, 128], BF16)
                sz = A.a([128, NT, 128], BF16)
                MT_all = A.a([128, NT, 256], BF16)
                S_all = A.a([128, NT, 128], BF16)
                ssp = A.a([128, NT, 4])
                rstdg = A.a([128, NT])
                Sst = A.a([128, 128])
                Lb = [A.a([128, 256]) for _ in range(2)]
                Db = [A.a([128, 256]) for _ in range(2)]
                xdte = [A.a([128, 128], BF16) for _ in range(4)]
                ytmp = [A.a([128, 128]) for _ in range(4)]
                yg16 = [A.a([128, 128], BF16) for _ in range(4)]
                junk = A.a([128, 128], BF16)

                def bank16(i):
                    return ps[i][:, 0:256].bitcast(BF16)

                def fm_chunk(cc, cidx, sink):
                    wi = load_wchunk(cc)
                    for bi, (t0, n) in enumerate(TB):
                        bank, bk = proj_blk(wi, bi, t0, n)
                        ct, ck = conv_blk(bank, bk, bi, n, "ssd_cw", "ssd_cb", cidx, tap0_act=True)
                        sink(bi, t0, n, ct, ck)

                for g in range(2):
                    pass
                    fm_chunk(8 + 8 + g, 8 + g, lambda bi, t0, n, ct, ck: act(BT[:, t0:t0 + n], ct[:, 0:n], AF.Silu, r=[ck], w=["BT"]))
                    fm_chunk(8 + 10 + g, 10 + g, lambda bi, t0, n, ct, ck: act(CT[:, t0:t0 + n], ct[:, 0:n], AF.Silu, r=[ck], w=["CT"]))
                    for q in range(5):
                        tiles = list(range(4 * q, min(4 * q + 4, NT)))
                        nn = len(tiles)
                        bk = bank16(6 + q % 2)
                        for i, t in enumerate(tiles):
                            tr(bk[:, i * 128:(i + 1) * 128], BT[:, t * 128:(t + 1) * 128], ident16, r=["BT", "cst16"], w=[("ps", 6 + q % 2)])
                        cp("dve", Btok[:, 4 * q:4 * q + nn, :], bk[:, 0:nn * 128].rearrange("p (a b) -> p a b", a=nn), r=[("ps", 6 + q % 2)], w=["Btok"])
                        bank = ps[2 + q % 2]
                        for i, t in enumerate(tiles):
                            mm(bank[:, i * 128:(i + 1) * 128], BT[:, t * 128:(t + 1) * 128], CT[:, t * 128:(t + 1) * 128], True, True,
                               r=["BT", "CT"], w=[("ps", 2 + q % 2)])
                        tt("dve", CBm[:, 4 * q:4 * q + nn, :], bank[:, 0:nn * 128].rearrange("p (a b) -> p a b", a=nn),
                           tri_le.unsqueeze(1).to_broadcast([128, nn, 128]), ALU.mult, r=[("ps", 2 + q % 2), "cst"], w=["CBm"])
                    pass
                    if SSD_STOP == "bc":
                        return
                    for jj in range(4):
                        j = g * 4 + jj
                        hh0 = 2 * j

                        def xsink(bi, t0, n, ct, ck, hh0=hh0):
                            xt = xT16[bi % 2]
                            xk = ("xT16", bi % 2)
                            act(xt[:, 0:n], ct[:, 0:n], AF.Silu, r=[ck], w=[xk])
                            nn = n // 128
                            bk = bank16(6 + bi % 2)
                            for i in range(nn):
                                tr(bk[:, i * 128:(i + 1) * 128], xt[:, i * 128:(i + 1) * 128], ident16, r=[xk, "cst16"], w=[("ps", 6 + bi % 2)])
                            a0 = t0 // 128
                            cp("act", xtok.rearrange("p a b -> p (a b)")[:, a0 * 128:(a0 + nn) * 128], bk[:, 0:nn * 128], r=[("ps", 6 + bi % 2)], w=[("xtok", bi)])
                            for i in range(nn):
                                t = a0 + i
                                tt("dve", xdt[:, t, :].rearrange("p (h c) -> p h c", h=2), xtok[:, t, :].rearrange("p (h c) -> p h c", h=2),
                                   dt[:, t, hh0:hh0 + 2].unsqueeze(2).to_broadcast([128, 2, 64]), ALU.mult, r=[("xtok", bi), "dt"], w=[("xdt", t)])
                        fm_chunk(8 + j, j, xsink)
                        if SSD_STOP == "x":
                            return
                        wz = load_wchunk(j)
                        for bi, (t0, n) in enumerate(TB):
                            bank, bk_ = proj_blk(wz, bi, t0, n)
                            zt = xT16[bi % 2]
                            zk = ("xT16", bi % 2)
                            act(zt[:, 0:n], bank[:, 0:n], AF.Silu, r=[bk_], w=[zk])
                            nn = n // 128
                            bk = bank16(6 + bi % 2)
                            for i in range(nn):
                                tr(bk[:, i * 128:(i + 1) * 128], zt[:, i * 128:(i + 1) * 128], ident16, r=[zk, "cst16"], w=[("ps", 6 + bi % 2)])
                            a0 = t0 // 128
                            cp("dve", sz.rearrange("p a b -> p (a b)")[:, a0 * 128:(a0 + nn) * 128], bk[:, 0:nn * 128], r=[("ps", 6 + bi % 2)], w=[("sz", bi)])
                        if SSD_STOP == "z":
                            return
                        memset("dve", Sst, 0.0, w=["Sst"])
                        memset("dve", S_all[:, 0, :], 0.0, w=[("S_all", 0)])
                        for hh in range(2):
                            act(DI[:, hh, :], ident, AF.Identity, r=["cst", "rowp"], w=["DI"], scale=Dbc[:, hh0 + hh:hh0 + hh + 1])
                        for c in range(NT):
                            i2 = c % 2
                            i4 = c % 4
                            for hh in range(2):
                                act(Lb[i2][:, hh * 128:(hh + 1) * 128], u_gt, AF.Identity, r=["cst", "adt"], w=[("L", i2, hh)], scale=adt[:, c, hh0 + hh:hh0 + hh + 1])
                                mm(ps[i2][:, hh * 128:(hh + 1) * 128], Lb[i2][:, hh * 128:(hh + 1) * 128], tri_le, True, True, r=[("L", i2, hh), "cst"], w=[("ps", i2)])
                            act(Db[i2], ps[i2][:, 0:256], AF.Exp, r=[("ps", i2)], w=[("D", i2)])
                            tt("dve", MT_all[:, c, :].rearrange("p (h l) -> p h l", h=2), Db[i2].rearrange("p (h l) -> p h l", h=2),
                               CBm[:, c, :].unsqueeze(1).to_broadcast([128, 2, 128]), ALU.mult, r=[("D", i2), "CBm"], w=[("MT", c)])
                            if c + 1 < NT:
                                tt("dve", xdte[i4].rearrange("p (h c) -> p h c", h=2), xdt[:, c, :].rearrange("p (h c) -> p h c", h=2),
                                   dte[:, c, hh0:hh0 + 2].unsqueeze(2).to_broadcast([128, 2, 64]), ALU.mult, r=[("xdt", c), "dte"], w=[("xdte", i4)])
                                mm(ps[2 + i2][:, 0:128], Btok[:, c, :], xdte[i4], True, True, r=["Btok", ("xdte", i4)], w=[("ps", 2 + i2)])
                                tt("dve", Sst.rearrange("p (h c) -> p h c", h=2), Sst.rearrange("p (h c) -> p h c", h=2),
                                   eatot[:, c, hh0:hh0 + 2].unsqueeze(2).to_broadcast([128, 2, 64]), ALU.mult, r=["Sst", "eatot"], w=["Sst"])
                                tt("dve", Sst, Sst, ps[2 + i2][:, 0:128], ALU.add, r=[("ps", 2 + i2), "Sst"], w=["Sst"])
                                cp("act", S_all[:, c + 1, :], Sst, r=["Sst"], w=[("S_all", c + 1)])
                        def p3_mm(c):
                            b = 4 + c % 2
                            pa = ps[b]
                            mm(pa[:, 0:128], CT[:, c * 128:(c + 1) * 128], S_all[:, c, :], True, True, r=["CT", ("S_all", c)], w=[("ps", b)])
                            for hh in range(2):
                                o = pa[:, 128 + hh * 64:128 + (hh + 1) * 64]
                                mm(o, MT_all[:, c, hh * 128:(hh + 1) * 128], xdt[:, c, hh * 64:(hh + 1) * 64], True, False, r=[("MT", c), ("xdt", c)], w=[("ps", b)])
                                mm(o, DI[:, hh, :], xtok[:, c, hh * 64:(hh + 1) * 64], False, True, r=["DI"] + [("xtok", bb) for bb in range(5)], w=[("ps", b)])

                        def p3_ev(c, jj=jj, j=j, hh0=hh0):
                            b = 4 + c % 2
                            pa = ps[b]
                            i4 = c % 4
                            y, yk = ytmp[i4], ("ytmp", i4)
                            tt("dve", y.rearrange("p (h c) -> p h c", h=2), pa[:, 0:128].rearrange("p (h c) -> p h c", h=2),
                               ea[:, c, hh0:hh0 + 2].unsqueeze(2).to_broadcast([128, 2, 64]), ALU.mult, r=[("ps", b), "ea"], w=[yk])
                            tt("dve", y, y, pa[:, 128:256], ALU.add, r=[("ps", b), yk], w=[yk])
                            yt, ytk = yg16[i4], ("yg", i4)
                            tt("dve", yt, y, sz[:, c, :], ALU.mult, r=[yk] + [("sz", bb) for bb in range(5)], w=[ytk])
                            act(junk, yt, AF.Square, r=[ytk], w=[("ssp", c), "junk"], accum=ssp[:, c, jj:jj + 1])
                            bk = bank16(6 + c % 2)
                            tr(bk[:, 0:128], yt, ident16, r=[ytk, "cst16"], w=[("ps", 6 + c % 2)])
                            act(ybuf[:, jj, c * 128:(c + 1) * 128], bk[:, 0:128], AF.Identity, r=[("ps", 6 + c % 2), "colp"], w=[("ybuf", jj)],
                                scale=colv("ssd_norm", j))

                        p3_mm(0)
                        for c in range(NT):
                            if c + 1 < NT:
                                p3_mm(c + 1)
                            p3_ev(c)
                    if SSD_STOP == "rec":
                        return
                    pass
                    S.add("dve", lambda e: e.tensor_reduce(out=rstdg, in_=ssp, axis=AX.X, op=ALU.add), r=[("ssp", c) for c in range(NT)], w=["rstdg"])
                    act(rstdg, rstdg, AF.Sqrt, r=["rstdg"], w=["rstdg"], scale=1.0 / 512, bias=colv_eps)
                    S.add("dve", lambda e: e.reciprocal(rstdg, rstdg), r=["rstdg"], w=["rstdg"])
                    out_proj(g * 4, lambda t: rstdg[:, t:t + 1])

            if "lru" in MIX0_PARTS:
                lru()
            S.barrier()
            A.off = mark
            if "ssd" in MIX0_PARTS:
                ssd()

        for ph in phases:
            {"mix0": mix0, "moe0": lambda: moe(0), "mix1": mix1, "moe1": lambda: moe(1)}[ph]()

        if dbg or not phases or not phases[-1].startswith("moe"):
            A.reset()
        if dbg:
            for t in range(NT):
                dma("sp", out_d[t * 128:(t + 1) * 128, :], h[:, t, :], r=HK(t), is_output=True)
        else:
            rms_stats(list(range(1, NT)))
            nf = A.a([128, D])
            dma("sp", nf, big_d[:, 0, :].partition_broadcast(128), w=["nf"])
            ob = [A.a([128, D]) for _ in range(2)]
            for t in range(1, NT):
                o = ob[t % 2]
                stt(o, h[:, t, :], rstd[:, t:t + 1], nf, ALU.mult, ALU.mult, r=HK(t) + [("rstd", t), "nf"], w=[("ob", t % 2)])
                dma("sp", out_d[(t - 1) * 128:t * 128, :], o, r=[("ob", t % 2)], is_output=True)
        if REORDER:
            S.reorder()
        S.emit()
    return nc


_CACHE = {}


def kernel(**inputs):
    shared = pack_inputs(inputs)
    x = np.ascontiguousarray(np.asarray(inputs["x"], np.float32))
    nb = x.shape[0]
    if "nc" not in _CACHE:
        _CACHE["nc"] = build()
    nc = _CACHE["nc"]
    in_maps = [dict(shared, x=x[b]) for b in range(nb)]
    res = run_bass_kernel_spmd(nc, in_maps, core_ids=list(range(nb)))
    return np.stack([np.asarray(r["out"], np.float32) for r in res.results], axis=0)
```

```python
from contextlib import ExitStack
import numpy as np
import concourse.bass as bass
import concourse.mybir as mybir
from concourse.bass_utils import run_bass_kernel_spmd

F32 = mybir.dt.float32
BF16 = mybir.dt.bfloat16
AF = mybir.ActivationFunctionType
ALU = mybir.AluOpType
AX = mybir.AxisListType

COMPUTE = ("pe", "act", "dve", "pool")
NDSEM = 16
SAME_ENGINE_GAP = 1 << 30

T = 2176
NT = 17
PADN = 112
D = 1024
TB = [(0, 512), (512, 512), (1024, 512), (1536, 512), (2048, 128)]
EPS = 1e-6
MIX0_PARTS = ("lru", "ssd")
SSD_STOP = None
REORDER = True


class Sched:
    def __init__(self, nc):
        self.nc = nc
        self.ops = {e: [] for e in ("pe", "act", "dve", "pool", "sp")}
        self.ccount = {e: 0 for e in COMPUTE}
        self.dcount = {"sp": 0, "pool": 0}
        self.last_w = {}
        self.readers = {}
        self.sig = {e: set() for e in COMPUTE}
        self.out_dmas = []
        self.pending_bar = {}
        self.ps_last = {}
        self.epoch = 0

    def _deps(self, tok, r, w, eng):
        deps = set()
        for k in r:
            t = self.last_w.get(k)
            if t is not None:
                deps.add(t)
        for k in w:
            t = self.last_w.get(k)
            if t is not None:
                deps.add(t)
            for t in self.readers.get(k, ()):
                deps.add(t)
        for k in w:
            self.last_w[k] = tok
            self.readers[k] = []
        for k in r:
            if k in w:
                continue
            self.readers.setdefault(k, []).append(tok)
        for k in set(r) | set(w):
            if isinstance(k, tuple) and k[0] == "ps":
                d = self.ps_last.setdefault(k, {})
                for oe in list(d):
                    if oe != eng:
                        deps.update(d[oe])
                        d[oe] = []
                d.setdefault(eng, []).append(tok)
        bar = self.pending_bar.pop(eng) if eng in self.pending_bar else set()
        deps.discard(tok)
        return deps, bar

    def barrier(self):
        toks = set()
        for e in COMPUTE:
            if self.ccount[e] > 0:
                toks.add(("c", e, self.ccount[e] - 1))
        for q in ("sp", "pool"):
            for k in range(max(0, self.dcount[q] - NDSEM), self.dcount[q]):
                toks.add(("d", q, k))
        for e in ("pe", "act", "dve", "pool", "sp"):
            self.pending_bar[e] = set(toks) | self.pending_bar.get(e, set())
        self.epoch += 1

    def add(self, eng, fn, r=(), w=(), cost=0.3):
        seq = self.ccount[eng]
        self.ccount[eng] += 1
        tok = ("c", eng, seq)
        deps, bar = self._deps(tok, tuple(r), tuple(w), eng)
        self.ops[eng].append(["c", fn, deps, seq, cost, self.epoch, bar])
        return tok

    def reorder(self, window=96, lat=0.35):
        RE = ("pe", "act", "dve")
        fin = {}
        etime = {e: 0.0 for e in self.ops}
        new_order = {e: [] for e in RE}
        ptr = {e: 0 for e in self.ops}
        fence = 0.0
        tokof = lambda e, op: ("c", e, op[3]) if op[0] == "c" else ("d", e, op[3])
        for ep in range(self.epoch + 1):
            seg = {}
            for e in self.ops:
                lst = self.ops[e]
                i = ptr[e]
                j = i
                while j < len(lst) and lst[j][5] == ep:
                    j += 1
                seg[e] = lst[i:j]
                ptr[e] = j
            for e in seg:
                etime[e] = max(etime[e], fence)
            left = {e: list(seg[e]) for e in seg}
            total = sum(len(v) for v in left.values())
            while total:
                best = None
                for e, lst in left.items():
                    if not lst:
                        continue
                    cands = lst[:window] if e in RE else lst[:1]
                    for pos, op in enumerate(cands):
                        ready = 0.0
                        ok = True
                        for t in op[2]:
                            f = fin.get(t)
                            if f is None:
                                ok = False
                                break
                            if t[1] != e:
                                f += lat
                            if f > ready:
                                ready = f
                        if not ok:
                            continue
                        start = max(etime[e], ready)
                        key = (start, pos)
                        if best is None or key < best[0]:
                            best = (key, e, pos, op, start)
                        if start <= etime[e]:
                            break
                assert best is not None, "scheduler stuck"
                _, e, pos, op, start = best
                left[e].pop(pos)
                total -= 1
                if op[0] == "c":
                    etime[e] = start + op[4]
                    fin[tokof(e, op)] = etime[e]
                else:
                    etime[e] = start + 0.5
                    fin[tokof(e, op)] = start + op[4]
                if e in RE:
                    new_order[e].append(op)
            fence = max([fence] + list(etime.values()) + [fin[tokof(e, op)] for e in seg for op in seg[e]])
        remap = {}
        for e in RE:
            bars = {}
            for op in new_order[e]:
                if op[6]:
                    bars.setdefault(op[5], set()).update(op[6])
                    op[6] = set()
            seen_ep = set()
            for i, op in enumerate(new_order[e]):
                if op[5] not in seen_ep:
                    seen_ep.add(op[5])
                    op[6] = bars.get(op[5], set())
                remap[("c", e, op[3])] = ("c", e, i)
                op[3] = i
            self.ops[e] = new_order[e]
        for e, lst in self.ops.items():
            for op in lst:
                op[2] = {remap.get(t, t) for t in op[2]}
                op[6] = {remap.get(t, t) for t in op[6]}
        self.model_time = max(etime.values())

    def dma(self, q, fn, r=(), w=(), is_output=False, cost=3.0):
        k = self.dcount[q]
        self.dcount[q] += 1
        tok = ("d", q, k)
        deps, bar = self._deps(tok, tuple(r), tuple(w), q)
        self.ops[q].append(["d", fn, deps, k, cost, self.epoch, bar])
        if is_output:
            self.out_dmas.append(tok)
        return tok

    def emit(self):
        nc = self.nc
        for e, lst in self.ops.items():
            for op in lst:
                op[2] = set(op[2]) | set(op[6])
        for e, lst in self.ops.items():
            seen_c = {f: -1 for f in COMPUTE}
            for kind, fn, deps, seq, _c, _e, _b in lst:
                cw = {}
                for t in deps:
                    if t[0] != "c":
                        continue
                    f, s = t[1], t[2]
                    if f == e and kind == "c":
                        if e == "pe":
                            continue
                        if seq - s > SAME_ENGINE_GAP:
                            continue
                    if s <= seen_c[f]:
                        continue
                    cw[f] = max(cw.get(f, -1), s)
                for f, s in cw.items():
                    seen_c[f] = s
                    self.sig[f].add(s)
        sigidx = {}
        for e in COMPUTE:
            s = sorted(self.sig[e])
            sigidx[e] = {seq: i + 1 for i, seq in enumerate(s)}
        with ExitStack() as st:
            csem = {e: st.enter_context(nc.semaphore("c_" + e)) for e in COMPUTE}
            dsem = {q: [st.enter_context(nc.semaphore(f"d_{q}{i}")) for i in range(NDSEM)]
                    for q in ("sp", "pool")}
            block = st.enter_context(nc.Block())

            def run(e, eng):
                seen_c = {f: -1 for f in COMPUTE}
                seen_d = {}
                for kind, fn, deps, seq, _c, _e, _b in self.ops[e]:
                    cw = {}
                    for t in deps:
                        if t[0] == "c":
                            f, s = t[1], t[2]
                            if f == e and kind == "c":
                                if e == "pe":
                                    continue
                                if seq - s > SAME_ENGINE_GAP:
                                    continue
                            if s <= seen_c[f]:
                                continue
                            cw[f] = max(cw.get(f, -1), s)
                        else:
                            q, k = t[1], t[2]
                            key = (q, k % NDSEM)
                            val = 16 * (k // NDSEM + 1)
                            if seen_d.get(key, 0) >= val:
                                continue
                            seen_d[key] = val
                            eng.wait_ge(dsem[q][k % NDSEM], val)
                    for f, s in cw.items():
                        seen_c[f] = s
                        eng.wait_ge(csem[f], sigidx[f][s])
                    if kind == "c":
                        ins = fn(eng)
                        if seq in sigidx[e]:
                            ins.then_inc(csem[e], 1)
                    else:
                        k = seq
                        if k >= NDSEM:
                            key = (e, k % NDSEM)
                            val = 16 * (k // NDSEM)
                            if seen_d.get(key, 0) < val:
                                seen_d[key] = val
                                eng.wait_ge(dsem[e][k % NDSEM], val)
                        ins = fn(eng)
                        ins.then_inc(dsem[e][k % NDSEM], 16)
                if e == "sp":
                    for t in self.out_dmas:
                        q, k = t[1], t[2]
                        eng.wait_ge(dsem[q][k % NDSEM], 16 * (k // NDSEM + 1))

            @block.tensor
            def _(eng):
                run("pe", eng)

            @block.scalar
            def _(eng):
                run("act", eng)

            @block.vector
            def _(eng):
                run("dve", eng)

            @block.gpsimd
            def _(eng):
                run("pool", eng)

            @block.sync
            def _(eng):
                run("sp", eng)


COLP = {}
_o = 0
for _n, _c in [("mix_even", 8), ("ffn0", 8), ("ffn1", 8), ("mix_odd", 8), ("ssd_cw", 48), ("ssd_cb", 12),
               ("ssd_norm", 8), ("lru_cw", 32), ("lru_cb", 8), ("lru_ba", 8), ("lru_bx", 8), ("lru_lam", 8)]:
    COLP[_n] = (_o, _c)
    _o += _c
NCOL = _o
ROWP = {}
_o = 0
for _n, _c in [("dt_bias", 16), ("a_log", 16), ("ssd_d", 16), ("rb0", 20), ("rb1", 20), ("rc", 64)]:
    ROWP[_n] = (_o, _c)
    _o += _c
NROW = _o


def _fm(v):
    v = np.asarray(v, np.float32).reshape(-1, 128)
    return np.ascontiguousarray(v.T)


def pack_inputs(inp):
    f = lambda a: np.ascontiguousarray(np.asarray(a, np.float32))
    colp = np.zeros((128, NCOL), np.float32)

    def put(name, arr):
        o, c = COLP[name]
        assert arr.shape == (128, c), (name, arr.shape)
        colp[:, o:o + c] = arr

    put("mix_even", _fm(inp["mix_norm_even"][0]))
    put("ffn0", _fm(inp["ffn_norm"][0]))
    put("ffn1", _fm(inp["ffn_norm"][1]))
    put("mix_odd", _fm(inp["mix_norm_odd"][0]))
    cw = np.asarray(inp["ssd_conv_w"][0], np.float32)
    put("ssd_cw", np.concatenate([_fm(cw[k]) for k in range(4)], axis=1).reshape(128, 4, 12).transpose(0, 2, 1).reshape(128, 48))
    put("ssd_cb", _fm(inp["ssd_conv_b"][0]))
    put("ssd_norm", _fm(inp["ssd_norm"][0]))
    lw = np.asarray(inp["lru_conv_w"][0], np.float32)
    put("lru_cw", np.concatenate([_fm(lw[k]) for k in range(4)], axis=1).reshape(128, 4, 8).transpose(0, 2, 1).reshape(128, 32))
    put("lru_cb", _fm(inp["lru_conv_b"][0]))
    put("lru_ba", _fm(inp["lru_b_a"][0]))
    put("lru_bx", _fm(inp["lru_b_x"][0]))
    put("lru_lam", _fm(inp["lru_lambda"][0]))

    rowp = np.zeros((1, NROW), np.float32)

    def putr(name, arr):
        o, c = ROWP[name]
        rowp[0, o:o + c] = np.asarray(arr, np.float32).reshape(-1)

    putr("dt_bias", inp["ssd_dt_bias"][0])
    putr("a_log", inp["ssd_a_log"][0])
    putr("ssd_d", inp["ssd_d"][0])
    putr("rb0", np.concatenate([np.asarray(inp["router_group_b"][0]), np.asarray(inp["router_expert_b"][0])]))
    putr("rb1", np.concatenate([np.asarray(inp["router_group_b"][1]), np.asarray(inp["router_expert_b"][1])]))
    rc = np.zeros((4, 16), np.float32)
    for g, w in enumerate((2, 4, 8, 16)):
        rc[g] = 1.0 / np.minimum(np.arange(16) + 1, w)
    putr("rc", rc)

    w_in = f(inp["w_in"][0])
    cols = np.concatenate([np.arange(0, 2560), np.arange(2576, 4624)])
    w_in_r = np.ascontiguousarray(w_in[:, cols].reshape(8, 128, 36, 128).transpose(2, 1, 0, 3))
    w_dt = np.ascontiguousarray(w_in[:, 2560:2576].reshape(8, 128, 16).transpose(1, 0, 2))
    wr = np.stack([np.concatenate([f(inp["router_group_w"][l]), f(inp["router_expert_w"][l])], axis=1)
                   .reshape(8, 128, 20).transpose(1, 0, 2) for l in range(2)])
    k_ = np.arange(128)[:, None]
    s_ = np.arange(128)[None, :]
    cst = np.stack([np.eye(128, dtype=np.float32), (k_ <= s_).astype(np.float32), (k_ > s_).astype(np.float32),
                    np.ones((128, 128), np.float32)], axis=1)
    shared = {
        "meta": f(inp["meta_tokens"]), "colp": colp, "rowp": rowp, "cst": np.ascontiguousarray(cst),
        "w_in_r": w_in_r, "w_dt": w_dt, "w_out": f(inp["w_out"][0]),
        "lru_wa": f(inp["lru_w_a"][0]), "lru_wx": f(inp["lru_w_x"][0]),
        "pool_w": f(inp["pool_w"][0]), "wr": np.ascontiguousarray(wr),
        "bigrow": np.ascontiguousarray(np.stack([f(inp["norm_final"]), f(inp["pool_b"][0]), f(inp["pool_scale"][0])])[None]),
        "wg": f(inp["expert_w_gate"]), "wu": f(inp["expert_w_up"]), "wd": f(inp["expert_w_down"]),
    }
    return shared


def build(phases=("mix0", "moe0", "mix1", "moe1"), dbg=False):
    nc = bass.Bass("TRN2", target_bir_lowering=False)
    dram = lambda n, s, kind="ExternalInput": nc.dram_tensor(n, list(s), F32, kind=kind).ap()
    x_d = dram("x", [2048, D])
    meta_d = dram("meta", [16, D])
    colp_d = dram("colp", [128, NCOL])
    rowp_d = dram("rowp", [1, NROW])
    cst_d = dram("cst", [128, 4, 128])
    w_in_d = dram("w_in_r", [36, 128, 8, 128])
    w_dt_d = dram("w_dt", [128, 8, 16])
    w_out_d = dram("w_out", [2048, D])
    lwa_d = dram("lru_wa", [16, 64, 64])
    lwx_d = dram("lru_wx", [16, 64, 64])
    pw_d = dram("pool_w", [4, 256, 256])
    wr_d = dram("wr", [2, 128, 8, 20])
    big_d = dram("bigrow", [1, 3, D])
    wg_d = dram("wg", [2, 16, D, 512])
    wu_d = dram("wu", [2, 16, D, 512])
    wd_d = dram("wd", [2, 16, 512, D])
    if dbg:
        out_d = dram("out", [T, D], kind="ExternalOutput")
    else:
        out_d = dram("out", [2048, D], kind="ExternalOutput")

    S = Sched(nc)
    with ExitStack() as st:
        sb = lambda n, s, dt=F32: st.enter_context(nc.sbuf_tensor(n, list(s), dt))
        h = sb("h", [128, NT, D])
        hnT = sb("hnT", [128, 8, T], BF16)
        colp = sb("colp_s", [128, NCOL])
        rowp = sb("rowp_s", [128, NROW])
        cst = sb("cst_s", [128, 4, 128])
        cst16 = sb("cst16", [128, 4, 128], BF16)
        stat = sb("stat", [128, 64])
        AW = 25900
        arena = sb("arena", [128, AW])
        ps = [st.enter_context(nc.psum_tensor(f"ps{i}", [128, 512], F32)) for i in range(8)]
        ident = cst[:, 0, :]
        tri_le = cst[:, 1, :]
        u_gt = cst[:, 2, :]
        ones32 = cst[:, 3, :]
        ident16 = cst16[:, 0, :]
        ss = stat[:, 0:17]
        sq = stat[:, 17:34]
        rstd = stat[:, 34:51]

        class Arena:
            def __init__(self):
                self.off = 0

            def reset(self):
                self.off = 0
                S.barrier()

            def a(self, shape, dt=F32):
                n = int(np.prod(shape[1:]))
                words = n if dt == F32 else (n + 1) // 2
                assert self.off + words <= AW, ("arena overflow", self.off, words)
                v = arena[:, self.off:self.off + words]
                self.off += words
                if dt != F32:
                    v = v.bitcast(dt)
                    if v.shape[1] != n:
                        v = v[:, 0:n]
                if len(shape) == 3:
                    v = v.rearrange("p (a b) -> p a b", a=shape[1])
                elif len(shape) == 4:
                    v = v.rearrange("p (a b c) -> p a b c", a=shape[1], b=shape[2])
                return v

        A = Arena()
        colv = lambda name, i=0, n=None: colp[:, COLP[name][0] + i:COLP[name][0] + i + (n if n else 1)]
        rowv = lambda name: rowp[:, ROWP[name][0]:ROWP[name][0] + ROWP[name][1]]

        def fsz(ap):
            n = 1
            for d_ in ap.shape[1:]:
                n *= int(d_)
            return n

        def ecost(eng, n, mult=1.0):
            if eng == "dve":
                return 0.12 + mult * n / 960.0
            if eng == "act":
                return 0.25 + n / 1400.0
            return 2.0 + 0.015 * n

        def mm(out, lhsT, rhs, start, stop, r, w):
            c = max(fsz(rhs), 64) / 2400.0 + 0.03
            if lhsT.dtype == F32:
                c *= 4
            S.add("pe", lambda e: e.matmul(out, lhsT, rhs, start=start, stop=stop), r=r, w=w, cost=c)

        def tr(out, in_, idn, r, w):
            S.add("pe", lambda e: e.transpose(out, in_, idn), r=r, w=w, cost=0.3 if in_.dtype == F32 else 0.12)

        def act(out, in_, func, r, w, scale=1.0, bias=0.0, accum=None):
            c = ecost("act", fsz(in_)) + (0.1 if accum is not None else 0.0)
            if accum is None:
                S.add("act", lambda e: e.activation(out=out, in_=in_, func=func, scale=scale, bias=bias), r=r, w=w, cost=c)
            else:
                S.add("act", lambda e: e.activation(out=out, in_=in_, func=func, scale=scale, bias=bias, accum_out=accum), r=r, w=w, cost=c)

        def tt(eng, out, in0, in1, op, r, w):
            S.add(eng, lambda e: e.tensor_tensor(out=out, in0=in0, in1=in1, op=op), r=r, w=w, cost=ecost(eng, fsz(out)))

        def ts(eng, out, in0, s1, op0, r, w, s2=None, op1=None):
            c = ecost(eng, fsz(out))
            if op1 is None:
                S.add(eng, lambda e: e.tensor_scalar(out=out, in0=in0, scalar1=s1, scalar2=None, op0=op0), r=r, w=w, cost=c)
            else:
                S.add(eng, lambda e: e.tensor_scalar(out=out, in0=in0, scalar1=s1, scalar2=s2, op0=op0, op1=op1), r=r, w=w, cost=c)

        def stt(out, in0, scalar, in1, op0, op1, r, w):
            S.add("dve", lambda e: e.scalar_tensor_tensor(out=out, in0=in0, scalar=scalar, in1=in1, op0=op0, op1=op1), r=r, w=w,
                  cost=ecost("dve", fsz(out)))

        def cp(eng, out, in_, r, w):
            c = ecost(eng, fsz(out))
            if eng == "act":
                S.add("act", lambda e: e.copy(out, in_), r=r, w=w, cost=c)
            else:
                S.add(eng, lambda e: e.tensor_copy(out, in_), r=r, w=w, cost=c)

        def memset(eng, ap, val, w):
            S.add(eng, lambda e: e.memset(ap, val), w=w, cost=ecost(eng, fsz(ap)))

        def dma(q, out, in_, r=(), w=(), is_output=False):
            nbytes = fsz(out) * int(out.shape[0]) * 4
            S.dma(q, lambda e: e.dma_start(out=out, in_=in_), r=r, w=w, is_output=is_output, cost=2.5 + nbytes / 150e3)

        HK = lambda t: [("h", t, 0), ("h", t, 1)]

        dma("sp", colp[:], colp_d, w=["colp"])
        dma("sp", rowp[:], rowp_d.partition_broadcast(128), w=["rowp"])
        dma("sp", cst[:], cst_d, w=["cst"])
        dma("pool", cst16[:], cst_d, w=["cst16"])
        memset("dve", h[:, 0, :], 0.0, w=HK(0))
        dma("sp", h[PADN:128, 0, :], meta_d, w=HK(0))
        xr = x_d.rearrange("(t p) d -> p t d", p=128)
        for i in range(4):
            dma("sp", h[:, 1 + 4 * i:5 + 4 * i, :], xr[:, 4 * i:4 * i + 4, :], w=[k for t in range(1 + 4 * i, 5 + 4 * i) for k in HK(t)])

        def rms_stats(tiles):
            junk = A.a([128, D], BF16)
            for t in tiles:
                act(junk, h[:, t, :], AF.Square, r=HK(t), w=[("ss", t), "junk"], accum=ss[:, t:t + 1])
                act(sq[:, t:t + 1], ss[:, t:t + 1], AF.Sqrt, r=[("ss", t)], w=[("sq", t)], scale=1.0 / D, bias=colv_eps)
                S.add("dve", lambda e, t=t: e.reciprocal(rstd[:, t:t + 1], sq[:, t:t + 1]), r=[("sq", t)], w=[("rstd", t)], cost=0.15)

        def normT(gname, router_l=None, logits=None, wr_s=None, consume=None):
            xsb = [A.a([128, D]) for _ in range(2)]
            t32 = [A.a([128, 4, 128]) for _ in range(4)]
            pend = None
            for t in range(NT):
                xs = xsb[t % 2]
                act(xs, h[:, t, :], AF.Identity, r=HK(t) + [("rstd", t)], w=[("xs", t % 2)], scale=rstd[:, t:t + 1])
                for half in range(2):
                    i2 = (2 * t + half) % 2
                    i4 = (2 * t + half) % 4
                    bank = ps[6 + i2]
                    for kk in range(4):
                        k = half * 4 + kk
                        tr(bank[:, kk * 128:(kk + 1) * 128], xs[:, k * 128:(k + 1) * 128], ident, r=[("xs", t % 2), "cst"], w=[("ps", 6 + i2)])
                    g0 = COLP[gname][0] + half * 4
                    tt("dve", t32[i4], bank[:, :].rearrange("p (a b) -> p a b", a=4),
                       colp[:, g0:g0 + 4].unsqueeze(2).to_broadcast([128, 4, 128]), ALU.mult,
                       r=[("ps", 6 + i2), "colp"], w=[("t32", i4)])
                    cp("act", hnT[:, half * 4:half * 4 + 4, t * 128:(t + 1) * 128], t32[i4], r=[("t32", i4)], w=[("hnT", t)])
                if router_l is not None:
                    if pend is not None:
                        pend()

                    def mk(t=t):
                        def go():
                            for k in range(8):
                                i4 = (2 * t + k // 4) % 4
                                mm(ps[5][:, 0:20], t32[i4][:, k % 4, :], wr_s[:, k, :], k == 0, k == 7,
                                   r=[("t32", i4), "wr"], w=[("ps", 5)])
                            cp("act", logits[:, t, :], ps[5][:, 0:20], r=[("ps", 5)], w=["logits"])
                        return go
                    pend = mk()
            if pend is not None:
                pend()

        memset("pool", stat[:, 60:61], EPS, w=["eps"])
        colv_eps = stat[:, 60:61]

        def moe(l):
            A.reset()
            wr_s = A.a([128, 8, 20])
            logits = A.a([128, NT, 20])
            gates = A.a([128, NT, 16])
            dma("sp", wr_s, wr_d[l], w=["wr"])
            Wg = [A.a([128, 8, 512], BF16) for _ in range(2)]
            Wu = [A.a([128, 8, 512], BF16) for _ in range(2)]
            Wd = [A.a([128, 4, D], BF16) for _ in range(2)]

            def load_w(e):
                b = e % 2
                if e == 0:
                    for fc in range(4):
                        fs = slice(fc * 128, (fc + 1) * 128)
                        dma("pool", Wg[b][:, :, fs], wg_d[l, e][:, fs].rearrange("(k p) f -> p k f", p=128), w=[("Wg", b, fc)])
                        dma("pool", Wu[b][:, :, fs], wu_d[l, e][:, fs].rearrange("(k p) f -> p k f", p=128), w=[("Wu", b, fc)])
                else:
                    dma("pool", Wg[b], wg_d[l, e].rearrange("(k p) f -> p k f", p=128), w=[("Wg", b, fc) for fc in range(4)])
                    dma("pool", Wu[b], wu_d[l, e].rearrange("(k p) f -> p k f", p=128), w=[("Wu", b, fc) for fc in range(4)])
                dma("pool", Wd[b], wd_d[l, e].rearrange("(k p) f -> p k f", p=128), w=[("Wd", b)])

            load_w(0)
            load_w(1)
            mark = A.off
            rms_stats(list(range(NT)))
            normT("ffn%d" % l, router_l=l, logits=logits, wr_s=wr_s)
            R = lambda shape: A.a(shape)
            rb = rowv("rb%d" % l)
            lg = R([128, NT, 20])
            tt("dve", lg, logits, rb.unsqueeze(1).to_broadcast([128, NT, 20]), ALU.add, r=["logits", "rowp"], w=["lg"])
            m4 = R([128, NT])
            S.add("dve", lambda e: e.tensor_reduce(out=m4, in_=lg[:, :, 0:4], axis=AX.X, op=ALU.max), r=["lg"], w=["m4"])
            d4 = R([128, NT, 4])
            tt("dve", d4, lg[:, :, 0:4], m4.unsqueeze(2).to_broadcast([128, NT, 4]), ALU.subtract, r=["lg", "m4"], w=["d4"])
            mg = R([128, NT, 4])
            ts("dve", mg, d4, 0.0, ALU.is_ge, r=["d4"], w=["mg"])
            e4 = R([128, NT, 4])
            act(e4, d4, AF.Exp, r=["d4"], w=["e4"])
            s4 = R([128, NT])
            S.add("dve", lambda e: e.tensor_reduce(out=s4, in_=e4, axis=AX.X, op=ALU.add), r=["e4"], w=["s4"])
            le = lg[:, :, 4:20].rearrange("p t (g j) -> p t g j", g=4)
            ml = R([128, NT, 4, 4])
            tt("dve", ml, le, mg.unsqueeze(3).to_broadcast([128, NT, 4, 4]), ALU.mult, r=["lg", "mg"], w=["ml"])
            sel = R([128, NT, 4])
            tt("dve", sel, ml[:, :, 0, :], ml[:, :, 1, :], ALU.add, r=["ml"], w=["sel"])
            tt("dve", sel, sel, ml[:, :, 2, :], ALU.add, r=["ml", "sel"], w=["sel"])
            tt("dve", sel, sel, ml[:, :, 3, :], ALU.add, r=["ml", "sel"], w=["sel"])
            m1 = R([128, NT])
            S.add("dve", lambda e: e.tensor_reduce(out=m1, in_=sel, axis=AX.X, op=ALU.max), r=["sel"], w=["m1"])
            k1 = R([128, NT, 4])
            tt("dve", k1, sel, m1.unsqueeze(2).to_broadcast([128, NT, 4]), ALU.is_ge, r=["sel", "m1"], w=["k1"])
            sel2 = R([128, NT, 4])
            stt(sel2, k1, -1e30, sel, ALU.mult, ALU.add, r=["k1", "sel"], w=["sel2"])
            m2 = R([128, NT])
            S.add("dve", lambda e: e.tensor_reduce(out=m2, in_=sel2, axis=AX.X, op=ALU.max), r=["sel2"], w=["m2"])
            k2 = R([128, NT, 4])
            tt("dve", k2, sel2, m2.unsqueeze(2).to_broadcast([128, NT, 4]), ALU.is_ge, r=["sel2", "m2"], w=["k2"])
            dd = R([128, NT])
            tt("dve", dd, m2, m1, ALU.subtract, r=["m1", "m2"], w=["dd"])
            w2 = R([128, NT])
            act(w2, dd, AF.Exp, r=["dd"], w=["w2"])
            den = R([128, NT])
            stt(den, w2, 1.0, s4, ALU.add, ALU.mult, r=["w2", "s4"], w=["den"])
            g1 = R([128, NT])
            S.add("dve", lambda e: e.reciprocal(g1, den), r=["den"], w=["g1"])
            g2 = R([128, NT])
            tt("dve", g2, g1, w2, ALU.mult, r=["g1", "w2"], w=["g2"])
            gs = R([128, NT, 4])
            tt("dve", gs, k1, g1.unsqueeze(2).to_broadcast([128, NT, 4]), ALU.mult, r=["k1", "g1"], w=["gs"])
            gs2 = R([128, NT, 4])
            tt("dve", gs2, k2, g2.unsqueeze(2).to_broadcast([128, NT, 4]), ALU.mult, r=["k2", "g2"], w=["gs2"])
            tt("dve", gs, gs, gs2, ALU.add, r=["gs", "gs2"], w=["gs"])
            g4 = gates.rearrange("p t (g j) -> p t g j", g=4)
            for g in range(4):
                tt("dve", g4[:, :, g, :], gs, mg[:, :, g:g + 1].to_broadcast([128, NT, 4]), ALU.mult, r=["gs", "mg"], w=["gates"])

            hid = [A.a([128, 4, 512], BF16) for _ in range(2)]
            sg = [A.a([128, 512]) for _ in range(2)]
            cnt = 0
            cnt_o = [0]
            blk = 0
            pend = None
            for e in range(16):
                b = e % 2
                if e >= 2:
                    load_w(e)
                for (t0, n) in TB:
                    hb = blk % 2
                    blk += 1
                    hk = [("hnT", tt_) for tt_ in range(t0 // 128, (t0 + n) // 128)]
                    for fc in range(4):
                        i = cnt % 2
                        cnt += 1
                        pg, pu = ps[i], ps[2 + i]
                        for k in range(8):
                            mm(pg[:, 0:n], Wg[b][:, k, fc * 128:(fc + 1) * 128], hnT[:, k, t0:t0 + n], k == 0, k == 7,
                               r=[("Wg", b, fc)] + hk, w=[("ps", i)])
                        for k in range(8):
                            mm(pu[:, 0:n], Wu[b][:, k, fc * 128:(fc + 1) * 128], hnT[:, k, t0:t0 + n], k == 0, k == 7,
                               r=[("Wu", b, fc)] + hk, w=[("ps", 2 + i)])
                        act(sg[i][:, 0:n], pg[:, 0:n], AF.Silu, r=[("ps", i)], w=[("sg", i)])
                        tt("dve", hid[hb][:, fc, 0:n], sg[i][:, 0:n], pu[:, 0:n], ALU.mult, r=[("sg", i), ("ps", 2 + i)], w=[("hid", hb)])
                    if pend is not None:
                        pend()

                    def mk(e=e, b=b, hb=hb, t0=t0, n=n):
                        def go():
                            for tl in range(n // 128):
                                t = t0 // 128 + tl
                                for dh in range(2):
                                    io = 4 + cnt_o[0] % 2
                                    cnt_o[0] += 1
                                    for fc in range(4):
                                        mm(ps[io][:, :], hid[hb][:, fc, tl * 128:(tl + 1) * 128], Wd[b][:, fc, dh * 512:(dh + 1) * 512],
                                           fc == 0, fc == 3, r=[("hid", hb), ("Wd", b)], w=[("ps", io)])
                                    hv = h[:, t, dh * 512:(dh + 1) * 512]
                                    stt(hv, ps[io][:, :], gates[:, t, e:e + 1], hv,
                                        ALU.mult, ALU.add, r=[("ps", io), "gates", ("h", t, dh)], w=[("h", t, dh)])
                        return go
                    pend = mk()
            pend()

        def mix1():
            A.reset()
            memset("pool", h[0:PADN, 0, :], 0.0, w=HK(0))
            rms_stats(list(range(NT)))
            pw = A.a([128, 4, 2, 256], BF16)
            for g in range(4):
                dma("pool", pw[:, g, :, :], pw_d[g].rearrange("(c p) j -> p c j", p=128), w=["pw"])
            pb_bc = A.a([128, D])
            sc_bc = A.a([128, D])
            dma("sp", pb_bc, big_d[:, 1, :].partition_broadcast(128), w=["pb_bc"])
            dma("sp", sc_bc, big_d[:, 2, :].partition_broadcast(128), w=["sc_bc"])
            bs_bc = A.a([128, D])
            tt("dve", bs_bc, pb_bc, sc_bc, ALU.mult, r=["pb_bc", "sc_bc"], w=["bs_bc"])
            bs16 = A.a([128, D], BF16)
            cp("dve", bs16, bs_bc, r=["bs_bc"], w=["bs16"])
            pws = A.a([128, 4, 2, 256], BF16)
            for g in range(4):
                tt("dve", pws[:, g, :, :], pw[:, g, :, :], sc_bc[:, g * 256:(g + 1) * 256].unsqueeze(1).to_broadcast([128, 2, 256]), ALU.mult,
                   r=["pw", "sc_bc"], w=["pws"])
            PF = 16
            hn32 = A.a([128, PF + T])
            sA = A.a([128, PF + T])
            sB = A.a([128, PF + T])
            pooledT = A.a([128, 8, T], BF16)
            for buf, nm in ((hn32, "hn32"), (sA, "sA"), (sB, "sB")):
                memset("pool", buf[:, 0:PF], 0.0, w=[nm])
            xsn = A.a([128, NT, 128])
            fix = A.a([128, 16])
            rcv = rowv("rc")
            for k in range(8):
                g = k // 2
                w = (2, 4, 8, 16)[g]
                tt("dve", xsn, h[:, :, k * 128:(k + 1) * 128], rstd[:, 0:NT].unsqueeze(2).to_broadcast([128, NT, 128]), ALU.mult,
                   r=[("h", t, k // 4) for t in range(NT)] + [("rstd", t) for t in range(NT)], w=["xsn"])
                for q in range(5):
                    tiles = list(range(4 * q, min(4 * q + 4, NT)))
                    bank = ps[6 + q % 2]
                    for i, t in enumerate(tiles):
                        tr(bank[:, i * 128:(i + 1) * 128], xsn[:, t, :], ident, r=["xsn", "cst"], w=[("ps", 6 + q % 2)])
                    nn = len(tiles) * 128
                    act(hn32[:, PF + q * 512:PF + q * 512 + nn], bank[:, 0:nn], AF.Identity, r=[("ps", 6 + q % 2), "colp"], w=["hn32"],
                        scale=colv("mix_odd", k))
                cur, curk = hn32, "hn32"
                step = 1
                bufs = [(sA, "sA"), (sB, "sB")]
                bi = 0
                while step < w:
                    nxt, nk = bufs[bi % 2]
                    bi += 1
                    tt("dve", nxt[:, PF:PF + T], cur[:, PF:PF + T], cur[:, PF - step:PF + T - step], ALU.add,
                       r=[curk], w=[nk])
                    cur, curk = nxt, nk
                    step *= 2
                stt(pooledT[:, k, :], cur[:, PF:PF + T], 1.0 / w, hn32[:, PF:PF + T], ALU.mult, ALU.subtract,
                    r=[curk, "hn32"], w=[("pooledT", k)])
                tt("dve", fix, cur[:, PF + PADN:PF + 128], rcv[:, g * 16:(g + 1) * 16], ALU.mult, r=[curk, "rowp"], w=["fix"])
                tt("dve", pooledT[:, k, PADN:128], fix, hn32[:, PF + PADN:PF + 128], ALU.subtract, r=["fix", "hn32"], w=[("pooledT", k)])
            for t in range(NT):
                for dh in range(2):
                    bi_ = dh + 2 * (t % 2)
                    bank = ps[bi_]
                    for gg in range(2):
                        g = dh * 2 + gg
                        o = bank[:, gg * 256:(gg + 1) * 256]
                        for ic in range(2):
                            mm(o, pooledT[:, 2 * g + ic, t * 128:(t + 1) * 128], pws[:, g, ic, :], ic == 0, False,
                               r=[("pooledT", 2 * g + ic), "pws"], w=[("ps", bi_)])
                        mm(o, cst16[0:1, 3, :], bs16[0:1, g * 256:(g + 1) * 256], False, True, r=["cst16", "bs16"], w=[("ps", bi_)])
                    hv = h[:, t, dh * 512:(dh + 1) * 512]
                    tt("dve", hv, hv, bank[:, :], ALU.add, r=[("ps", bi_), ("h", t, dh)], w=[("h", t, dh)])

        def mix0():
            A.reset()
            rms_stats(list(range(NT)))
            normT("mix_even")
            A.reset()
            hnk = [("hnT", t) for t in range(NT)]
            wbuf = [A.a([128, 8, 128], BF16) for _ in range(3)]
            wcnt = [0]

            def load_wchunk(cc):
                i = wcnt[0] % 3
                wcnt[0] += 1
                dma("pool", wbuf[i], w_in_d[cc], w=[("wbuf", i)])
                return i

            def proj_blk(wi, bi, t0, n):
                bk = ("ps", bi % 2)
                bank = ps[bi % 2]
                for k in range(8):
                    mm(bank[:, 0:n], wbuf[wi][:, k, :], hnT[:, k, t0:t0 + n], k == 0, k == 7, r=[("wbuf", wi)] + hnk, w=[bk])
                return bank, bk

            xinb = [A.a([128, 3 + 512]) for _ in range(2)]
            ctb = [A.a([128, 512]) for _ in range(2)]
            cvc = [0]

            def conv_blk(bank, bk, bi, n, wname, bname, cidx, tap0_act=False, copy_eng="act"):
                i = cvc[0] % 2
                cvc[0] += 1
                xi, xk = xinb[i], ("xinb", i)
                if bi == 0:
                    memset("dve", xi[:, 0:3], 0.0, w=[xk])
                else:
                    pv = xinb[1 - i]
                    cp("dve", xi[:, 0:3], pv[:, 512:515], r=[("xinb", 1 - i)], w=[xk])
                cp(copy_eng, xi[:, 3:3 + n], bank[:, 0:n], r=[bk], w=[xk])
                ct, ck = ctb[i], ("ctb", i)
                o4 = COLP[wname][0] + 4 * cidx
                if tap0_act:
                    act(ct[:, 0:n], xi[:, 0:n], AF.Identity, r=[xk, "colp"], w=[ck], scale=colp[:, o4:o4 + 1], bias=colv(bname, cidx))
                else:
                    ts("dve", ct[:, 0:n], xi[:, 0:n], colp[:, o4:o4 + 1], ALU.mult, r=[xk, "colp"], w=[ck], s2=colv(bname, cidx), op1=ALU.add)
                for k in range(1, 4):
                    stt(ct[:, 0:n], xi[:, k:k + n], colp[:, o4 + k:o4 + k + 1], ct[:, 0:n], ALU.mult, ALU.add, r=[xk, ck, "colp"], w=[ck])
                return ct, ck

            wo = A.a([128, 4, D], BF16)
            ybuf = A.a([128, 4, T], BF16)

            def out_proj(kc0, scale_ap_fn):
                dma("pool", wo, w_out_d[kc0 * 128:(kc0 + 4) * 128, :].rearrange("(k p) d -> p k d", p=128), w=["wo"])
                cnt = 0
                for t in range(NT):
                    for dh in range(2):
                        io = 4 + cnt % 2
                        cnt += 1
                        for kk in range(4):
                            mm(ps[io][:, :], ybuf[:, kk, t * 128:(t + 1) * 128], wo[:, kk, dh * 512:(dh + 1) * 512], kk == 0, kk == 3,
                               r=[("ybuf", kk), "wo"], w=[("ps", io)])
                        hv = h[:, t, dh * 512:(dh + 1) * 512]
                        if scale_ap_fn is None:
                            tt("dve", hv, hv, ps[io][:, :], ALU.add, r=[("ps", io), ("h", t, dh)], w=[("h", t, dh)])
                        else:
                            stt(hv, ps[io][:, :], scale_ap_fn(t), hv, ALU.mult, ALU.add, r=[("ps", io), ("h", t, dh), "rstdg"], w=[("h", t, dh)])

            mark = A.off

            def lru():
                bdA = A.a([128, 8, 128], BF16)
                bdX = A.a([128, 8, 128], BF16)
                memset("dve", bdA, 0.0, w=["bdA"])
                memset("dve", bdX, 0.0, w=["bdX"])
                for src, dst, nm in ((lwa_d, bdA, "bdA"), (lwx_d, bdX, "bdX")):
                    v = src.rearrange("(j two) i o -> two i j o", two=2)
                    dma("pool", dst[0:64, :, 0:64], v[0], w=[nm])
                    dma("pool", dst[64:128, :, 64:128], v[1], w=[nm])
                c1 = A.a([128, 8])
                tmpc = A.a([128, 8])
                act(tmpc, colv("lru_lam", 0, 8), AF.Exp, r=["colp"], w=["tmpc"], scale=-1.0)
                act(tmpc, tmpc, AF.Ln, r=["tmpc"], w=["tmpc"], bias=1.0)
                ts("dve", c1, tmpc, -8.0, ALU.mult, r=["tmpc"], w=["c1"])
                Bn = lambda n_: [A.a([128, 512]) for _ in range(n_)]
                gl, rr, ii, av, uv, hl = Bn(4), Bn(4), Bn(4), Bn(4), Bn(2), Bn(2)
                xb16 = [A.a([128, 512], BF16) for _ in range(4)]
                NB = len(TB)
                items = [(j, bi) for j in range(8) for bi in range(NB)]
                st1 = {}

                def S1(idx):
                    j, bi = items[idx]
                    t0, n = TB[bi]
                    if bi == 0:
                        st1[j] = (load_wchunk(28 + j), load_wchunk(20 + j))
                    wi_in, wi_gt = st1[j]
                    bank, bk = proj_blk(wi_in, 2 * idx, t0, n)
                    xb, xbk = conv_blk(bank, bk, bi, n, "lru_cw", "lru_cb", j, copy_eng="dve")
                    bank2, bk2 = proj_blk(wi_gt, 2 * idx + 1, t0, n)
                    g3 = idx % 4
                    act(gl[g3][:, 0:n], bank2[:, 0:n], AF.Gelu_apprx_tanh, r=[bk2], w=[("gl", g3)])
                    cp("act", xb16[g3][:, 0:n], xb[:, 0:n], r=[xbk], w=[("xb16", g3)])
                    st1[idx, "xb"] = (xb, xbk)

                def S2(idx):
                    j, bi = items[idx]
                    t0, n = TB[bi]
                    ip = idx % 2
                    q = idx % 4
                    K = lambda nm: (nm, q)
                    xb, xbk = st1.pop((idx, "xb"))
                    pa, px = ps[2 + ip], ps[4 + ip]
                    mm(pa[:, 0:n], bdA[:, j, :], xb16[q][:, 0:n], True, True, r=["bdA", K("xb16")], w=[("ps", 2 + ip)])
                    mm(px[:, 0:n], bdX[:, j, :], xb16[q][:, 0:n], True, True, r=["bdX", K("xb16")], w=[("ps", 4 + ip)])
                    r_, i_ = rr[q], ii[q]
                    act(r_[:, 0:n], pa[:, 0:n], AF.Sigmoid, r=[("ps", 2 + ip), "colp"], w=[K("rr")], bias=colv("lru_ba", j))
                    act(i_[:, 0:n], px[:, 0:n], AF.Sigmoid, r=[("ps", 4 + ip), "colp"], w=[K("ii")], bias=colv("lru_bx", j))
                    act(av[q][:, 0:n], r_[:, 0:n], AF.Exp, r=[K("rr"), "c1"], w=[K("av")], scale=c1[:, j:j + 1])
                    stt(r_[:, 0:n], av[q][:, 0:n], 0.9999998, av[q][:, 0:n], ALU.min, ALU.mult, r=[K("av")], w=[K("rr")])
                    act(r_[:, 0:n], r_[:, 0:n], AF.Sqrt, r=[K("rr")], w=[K("rr")], scale=-1.0, bias=1.0)
                    tt("dve", i_[:, 0:n], i_[:, 0:n], xb[:, 0:n], ALU.mult, r=[K("ii"), xbk], w=[K("ii")])
                    if bi == 0:
                        memset("dve", r_[:, PADN:PADN + 1], 1.0, w=[K("rr")])

                def S3(idx):
                    j, bi = items[idx]
                    t0, n = TB[bi]
                    i = idx % 2
                    q = idx % 4
                    jj = j % 4
                    K = lambda nm: (nm, q)
                    tt("dve", uv[i][:, 0:n], rr[q][:, 0:n], ii[q][:, 0:n], ALU.mult, r=[K("rr"), K("ii")], w=[("uv", i)])
                    if bi == 0:
                        memset("dve", hl[i][:, 0:PADN], 0.0, w=[("hl", i)])
                        S.add("dve", lambda e, i=i, q=q, n=n: e.tensor_tensor_scan(out=hl[i][:, PADN:n], data0=av[q][:, PADN:n], data1=uv[i][:, PADN:n],
                                                                             initial=0.0, op0=ALU.mult, op1=ALU.add),
                              r=[K("av"), ("uv", i)], w=[("hl", i)], cost=1.2)
                    else:
                        S.add("dve", lambda e, i=i, q=q, n=n: e.tensor_tensor_scan(out=hl[i][:, 0:n], data0=av[q][:, 0:n], data1=uv[i][:, 0:n],
                                                                             initial=hl[1 - i][:, 511:512], op0=ALU.mult, op1=ALU.add),
                              r=[K("av"), ("uv", i), ("hl", 1 - i)], w=[("hl", i)], cost=1.2)
                    tt("dve", ybuf[:, jj, t0:t0 + n], hl[i][:, 0:n], gl[q][:, 0:n], ALU.mult, r=[("hl", i), ("gl", q)], w=[("ybuf", jj)])
                    if bi == NB - 1 and jj == 3:
                        out_proj(8 + (j // 4) * 4, None)

                NI = len(items)
                for step in range(NI + 2):
                    if step < NI:
                        S1(step)
                    if 0 <= step - 1 < NI:
                        S2(step - 1)
                    if 0 <= step - 2 < NI:
                        S3(step - 2)

            def ssd():
                dt = A.a([128, NT, 16])
                adt = A.a([128, NT, 16])
                ea = A.a([128, NT, 16])
                eatot = A.a([128, NT, 16])
                dte = A.a([128, NT, 16])
                Aneg = A.a([128, 16])
                wdt = A.a([128, 8, 16], BF16)
                dma("pool", wdt, w_dt_d, w=["wdt"])
                for t in range(NT):
                    for k in range(8):
                        mm(ps[7][:, t * 16:(t + 1) * 16], hnT[:, k, t * 128:(t + 1) * 128], wdt[:, k, :], k == 0, k == 7,
                           r=["wdt", ("hnT", t)], w=[("ps", 7)])
                p7 = ps[7][:, 0:NT * 16].rearrange("p (t h) -> p t h", t=NT)
                tt("dve", dt, p7, rowv("dt_bias").unsqueeze(1).to_broadcast([128, NT, 16]), ALU.add, r=[("ps", 7), "rowp"], w=["dt"])
                act(dte, dt, AF.Abs, r=["dt"], w=["dte"])
                act(dte, dte, AF.Exp, r=["dte"], w=["dte"], scale=-1.0)
                act(dte, dte, AF.Ln, r=["dte"], w=["dte"], bias=1.0)
                stt(dt, dt, 0.0, dte, ALU.max, ALU.add, r=["dt", "dte"], w=["dt"])
                memset("dve", dt[0:PADN, 0, :], 0.0, w=["dt"])
                act(Aneg, rowv("a_log"), AF.Exp, r=["rowp"], w=["Aneg"])
                stt(adt, dt, -1.0, Aneg.unsqueeze(1).to_broadcast([128, NT, 16]), ALU.mult, ALU.mult, r=["dt", "Aneg"], w=["adt"])
                for t in range(NT):
                    mm(ps[6][:, t * 16:(t + 1) * 16], tri_le, adt[:, t, :], True, True, r=["cst", "adt"], w=[("ps", 6)])
                for t in range(NT):
                    mm(ps[7][:, t * 16:(t + 1) * 16], ones32, adt[:, t, :], True, True, r=["cst", "adt"], w=[("ps", 7)])
                p6 = ps[6][:, 0:NT * 16].rearrange("p (t h) -> p t h", t=NT)
                cp("dve", ea, p6, r=[("ps", 6)], w=["ea"])
                cp("dve", eatot, p7, r=[("ps", 7)], w=["eatot"])
                tt("dve", dte, eatot, ea, ALU.subtract, r=["eatot", "ea"], w=["dte"])
                act(dte, dte, AF.Exp, r=["dte"], w=["dte"])
                act(ea, ea, AF.Exp, r=["ea", "dte"], w=["ea"])
                act(eatot, eatot, AF.Exp, r=["eatot", "dte"], w=["eatot"])
                Dbc = rowv("ssd_d")
                DI = A.a([128, 2, 128], BF16)
                if SSD_STOP == "dt":
                    return

                BT = A.a([128, T], BF16)
                CT = A.a([128, T], BF16)
                Btok = A.a([128, NT, 128], BF16)
                CBm = A.a([128, NT, 128], BF16)
                xT16 = [A.a([128, 512], BF16) for _ in range(2)]
                xtok = A.a([128, NT, 128], BF16)
                xdt = A.a([128, NT, 128], BF16)
                sz = A.a([128, NT, 128], BF16)
                MT_all = A.a([128, NT, 256], BF16)
                S_all = A.a([128, NT, 128], BF16)
                ssp = A.a([128, NT, 4])
                rstdg = A.a([128, NT])
                Sst = A.a([128, 128])
                Lb = [A.a([128, 256]) for _ in range(2)]
                Db = [A.a([128, 256]) for _ in range(2)]
                xdte = [A.a([128, 128], BF16) for _ in range(4)]
                ytmp = [A.a([128, 128]) for _ in range(4)]
                yg16 = [A.a([128, 128], BF16) for _ in range(4)]
                junk = A.a([128, 128], BF16)

                def bank16(i):
                    return ps[i][:, 0:256].bitcast(BF16)

                def fm_chunk(cc, cidx, sink):
                    wi = load_wchunk(cc)
                    for bi, (t0, n) in enumerate(TB):
                        bank, bk = proj_blk(wi, bi, t0, n)
                        ct, ck = conv_blk(bank, bk, bi, n, "ssd_cw", "ssd_cb", cidx, tap0_act=True)
                        sink(bi, t0, n, ct, ck)

                for g in range(2):
                    pass
                    fm_chunk(8 + 8 + g, 8 + g, lambda bi, t0, n, ct, ck: act(BT[:, t0:t0 + n], ct[:, 0:n], AF.Silu, r=[ck], w=["BT"]))
                    fm_chunk(8 + 10 + g, 10 + g, lambda bi, t0, n, ct, ck: act(CT[:, t0:t0 + n], ct[:, 0:n], AF.Silu, r=[ck], w=["CT"]))
                    for q in range(5):
                        tiles = list(range(4 * q, min(4 * q + 4, NT)))
                        nn = len(tiles)
                        bk = bank16(6 + q % 2)
                        for i, t in enumerate(tiles):
                            tr(bk[:, i * 128:(i + 1) * 128], BT[:, t * 128:(t + 1) * 128], ident16, r=["BT", "cst16"], w=[("ps", 6 + q % 2)])
                        cp("dve", Btok[:, 4 * q:4 * q + nn, :], bk[:, 0:nn * 128].rearrange("p (a b) -> p a b", a=nn), r=[("ps", 6 + q % 2)], w=["Btok"])
                        bank = ps[2 + q % 2]
                        for i, t in enumerate(tiles):
                            mm(bank[:, i * 128:(i + 1) * 128], BT[:, t * 128:(t + 1) * 128], CT[:, t * 128:(t + 1) * 128], True, True,
                               r=["BT", "CT"], w=[("ps", 2 + q % 2)])
                        tt("dve", CBm[:, 4 * q:4 * q + nn, :], bank[:, 0:nn * 128].rearrange("p (a b) -> p a b", a=nn),
                           tri_le.unsqueeze(1).to_broadcast([128, nn, 128]), ALU.mult, r=[("ps", 2 + q % 2), "cst"], w=["CBm"])
                    pass
                    if SSD_STOP == "bc":
                        return
                    for jj in range(4):
                        j = g * 4 + jj
                        hh0 = 2 * j

                        def xsink(bi, t0, n, ct, ck, hh0=hh0):
                            xt = xT16[bi % 2]
                            xk = ("xT16", bi % 2)
                            act(xt[:, 0:n], ct[:, 0:n], AF.Silu, r=[ck], w=[xk])
                            nn = n // 128
                            bk = bank16(6 + bi % 2)
                            for i in range(nn):
                                tr(bk[:, i * 128:(i + 1) * 128], xt[:, i * 128:(i + 1) * 128], ident16, r=[xk, "cst16"], w=[("ps", 6 + bi % 2)])
                            a0 = t0 // 128
                            cp("act", xtok.rearrange("p a b -> p (a b)")[:, a0 * 128:(a0 + nn) * 128], bk[:, 0:nn * 128], r=[("ps", 6 + bi % 2)], w=[("xtok", bi)])
                            for i in range(nn):
                                t = a0 + i
                                tt("dve", xdt[:, t, :].rearrange("p (h c) -> p h c", h=2), xtok[:, t, :].rearrange("p (h c) -> p h c", h=2),
                                   dt[:, t, hh0:hh0 + 2].unsqueeze(2).to_broadcast([128, 2, 64]), ALU.mult, r=[("xtok", bi), "dt"], w=[("xdt", t)])
                        fm_chunk(8 + j, j, xsink)
                        if SSD_STOP == "x":
                            return
                        wz = load_wchunk(j)
                        for bi, (t0, n) in enumerate(TB):
                            bank, bk_ = proj_blk(wz, bi, t0, n)
                            zt = xT16[bi % 2]
                            zk = ("xT16", bi % 2)
                            act(zt[:, 0:n], bank[:, 0:n], AF.Silu, r=[bk_], w=[zk])
                            nn = n // 128
                            bk = bank16(6 + bi % 2)
                            for i in range(nn):
                                tr(bk[:, i * 128:(i + 1) * 128], zt[:, i * 128:(i + 1) * 128], ident16, r=[zk, "cst16"], w=[("ps", 6 + bi % 2)])
                            a0 = t0 // 128
                            cp("dve", sz.rearrange("p a b -> p (a b)")[:, a0 * 128:(a0 + nn) * 128], bk[:, 0:nn * 128], r=[("ps", 6 + bi % 2)], w=[("sz", bi)])
                        if SSD_STOP == "z":
                            return
                        memset("dve", Sst, 0.0, w=["Sst"])
                        memset("dve", S_all[:, 0, :], 0.0, w=[("S_all", 0)])
                        for hh in range(2):
                            act(DI[:, hh, :], ident, AF.Identity, r=["cst", "rowp"], w=["DI"], scale=Dbc[:, hh0 + hh:hh0 + hh + 1])
                        for c in range(NT):
                            i2 = c % 2
                            i4 = c % 4
                            for hh in range(2):
                                act(Lb[i2][:, hh * 128:(hh + 1) * 128], u_gt, AF.Identity, r=["cst", "adt"], w=[("L", i2, hh)], scale=adt[:, c, hh0 + hh:hh0 + hh + 1])
                                mm(ps[i2][:, hh * 128:(hh + 1) * 128], Lb[i2][:, hh * 128:(hh + 1) * 128], tri_le, True, True, r=[("L", i2, hh), "cst"], w=[("ps", i2)])
                            act(Db[i2], ps[i2][:, 0:256], AF.Exp, r=[("ps", i2)], w=[("D", i2)])
                            tt("dve", MT_all[:, c, :].rearrange("p (h l) -> p h l", h=2), Db[i2].rearrange("p (h l) -> p h l", h=2),
                               CBm[:, c, :].unsqueeze(1).to_broadcast([128, 2, 128]), ALU.mult, r=[("D", i2), "CBm"], w=[("MT", c)])
                            if c + 1 < NT:
                                tt("dve", xdte[i4].rearrange("p (h c) -> p h c", h=2), xdt[:, c, :].rearrange("p (h c) -> p h c", h=2),
                                   dte[:, c, hh0:hh0 + 2].unsqueeze(2).to_broadcast([128, 2, 64]), ALU.mult, r=[("xdt", c), "dte"], w=[("xdte", i4)])
                                mm(ps[2 + i2][:, 0:128], Btok[:, c, :], xdte[i4], True, True, r=["Btok", ("xdte", i4)], w=[("ps", 2 + i2)])
                                tt("dve", Sst.rearrange("p (h c) -> p h c", h=2), Sst.rearrange("p (h c) -> p h c", h=2),
                                   eatot[:, c, hh0:hh0 + 2].unsqueeze(2).to_broadcast([128, 2, 64]), ALU.mult, r=["Sst", "eatot"], w=["Sst"])
                                tt("dve", Sst, Sst, ps[2 + i2][:, 0:128], ALU.add, r=[("ps", 2 + i2), "Sst"], w=["Sst"])
                                cp("act", S_all[:, c + 1, :], Sst, r=["Sst"], w=[("S_all", c + 1)])
                        def p3_mm(c):
                            b = 4 + c % 2
                            pa = ps[b]
                            mm(pa[:, 0:128], CT[:, c * 128:(c + 1) * 128], S_all[:, c, :], True, True, r=["CT", ("S_all", c)], w=[("ps", b)])
                            for hh in range(2):
                                o = pa[:, 128 + hh * 64:128 + (hh + 1) * 64]
                                mm(o, MT_all[:, c, hh * 128:(hh + 1) * 128], xdt[:, c, hh * 64:(hh + 1) * 64], True, False, r=[("MT", c), ("xdt", c)], w=[("ps", b)])
                                mm(o, DI[:, hh, :], xtok[:, c, hh * 64:(hh + 1) * 64], False, True, r=["DI"] + [("xtok", bb) for bb in range(5)], w=[("ps", b)])

                        def p3_ev(c, jj=jj, j=j, hh0=hh0):
                            b = 4 + c % 2
                            pa = ps[b]
                            i4 = c % 4
                            y, yk = ytmp[i4], ("ytmp", i4)
                            tt("dve", y.rearrange("p (h c) -> p h c", h=2), pa[:, 0:128].rearrange("p (h c) -> p h c", h=2),
                               ea[:, c, hh0:hh0 + 2].unsqueeze(2).to_broadcast([128, 2, 64]), ALU.mult, r=[("ps", b), "ea"], w=[yk])
                            tt("dve", y, y, pa[:, 128:256], ALU.add, r=[("ps", b), yk], w=[yk])
                            yt, ytk = yg16[i4], ("yg", i4)
                            tt("dve", yt, y, sz[:, c, :], ALU.mult, r=[yk] + [("sz", bb) for bb in range(5)], w=[ytk])
                            act(junk, yt, AF.Square, r=[ytk], w=[("ssp", c), "junk"], accum=ssp[:, c, jj:jj + 1])
                            bk = bank16(6 + c % 2)
                            tr(bk[:, 0:128], yt, ident16, r=[ytk, "cst16"], w=[("ps", 6 + c % 2)])
                            act(ybuf[:, jj, c * 128:(c + 1) * 128], bk[:, 0:128], AF.Identity, r=[("ps", 6 + c % 2), "colp"], w=[("ybuf", jj)],
                                scale=colv("ssd_norm", j))

                        p3_mm(0)
                        for c in range(NT):
                            if c + 1 < NT:
                                p3_mm(c + 1)
                            p3_ev(c)
                    if SSD_STOP == "rec":
                        return
                    pass
                    S.add("dve", lambda e: e.tensor_reduce(out=rstdg, in_=ssp, axis=AX.X, op=ALU.add), r=[("ssp", c) for c in range(NT)], w=["rstdg"])
                    act(rstdg, rstdg, AF.Sqrt, r=["rstdg"], w=["rstdg"], scale=1.0 / 512, bias=colv_eps)
                    S.add("dve", lambda e: e.reciprocal(rstdg, rstdg), r=["rstdg"], w=["rstdg"])
                    out_proj(g * 4, lambda t: rstdg[:, t:t + 1])

            if "lru" in MIX0_PARTS:
                lru()
            S.barrier()
            A.off = mark
            if "ssd" in MIX0_PARTS:
                ssd()

        for ph in phases:
            {"mix0": mix0, "moe0": lambda: moe(0), "mix1": mix1, "moe1": lambda: moe(1)}[ph]()

        if dbg or not phases or not phases[-1].startswith("moe"):
            A.reset()
        if dbg:
            for t in range(NT):
                dma("sp", out_d[t * 128:(t + 1) * 128, :], h[:, t, :], r=HK(t), is_output=True)
        else:
            rms_stats(list(range(1, NT)))
            nf = A.a([128, D])
            dma("sp", nf, big_d[:, 0, :].partition_broadcast(128), w=["nf"])
            ob = [A.a([128, D]) for _ in range(2)]
            for t in range(1, NT):
                o = ob[t % 2]
                stt(o, h[:, t, :], rstd[:, t:t + 1], nf, ALU.mult, ALU.mult, r=HK(t) + [("rstd", t), "nf"], w=[("ob", t % 2)])
                dma("sp", out_d[(t - 1) * 128:t * 128, :], o, r=[("ob", t % 2)], is_output=True)
        if REORDER:
            S.reorder()
        S.emit()
    return nc


_CACHE = {}


def kernel(**inputs):
    shared = pack_inputs(inputs)
    x = np.ascontiguousarray(np.asarray(inputs["x"], np.float32))
    nb = x.shape[0]
    if "nc" not in _CACHE:
        _CACHE["nc"] = build()
    nc = _CACHE["nc"]
    in_maps = [dict(shared, x=x[b]) for b in range(nb)]
    res = run_bass_kernel_spmd(nc, in_maps, core_ids=list(range(nb)))
    return np.stack([np.asarray(r["out"], np.float32) for r in res.results], axis=0)
```

```python
from contextlib import ExitStack
import numpy as np
import concourse.bass as bass
import concourse.mybir as mybir
from concourse.bass_utils import run_bass_kernel_spmd

F32 = mybir.dt.float32
BF16 = mybir.dt.bfloat16
AF = mybir.ActivationFunctionType
ALU = mybir.AluOpType
AX = mybir.AxisListType

COMPUTE = ("pe", "act", "dve", "pool")
NDSEM = 16
SAME_ENGINE_GAP = 1 << 30

T = 2176
NT = 17
PADN = 112
D = 1024
TB = [(0, 512), (512, 512), (1024, 512), (1536, 512), (2048, 128)]
EPS = 1e-6
MIX0_PARTS = ("lru", "ssd")
SSD_STOP = None
REORDER = True


class Sched:
    def __init__(self, nc):
        self.nc = nc
        self.ops = {e: [] for e in ("pe", "act", "dve", "pool", "sp")}
        self.ccount = {e: 0 for e in COMPUTE}
        self.dcount = {"sp": 0, "pool": 0}
        self.last_w = {}
        self.readers = {}
        self.sig = {e: set() for e in COMPUTE}
        self.out_dmas = []
        self.pending_bar = {}
        self.ps_last = {}
        self.epoch = 0

    def _deps(self, tok, r, w, eng):
        deps = set()
        for k in r:
            t = self.last_w.get(k)
            if t is not None:
                deps.add(t)
        for k in w:
            t = self.last_w.get(k)
            if t is not None:
                deps.add(t)
            for t in self.readers.get(k, ()):
                deps.add(t)
        for k in w:
            self.last_w[k] = tok
            self.readers[k] = []
        for k in r:
            if k in w:
                continue
            self.readers.setdefault(k, []).append(tok)
        for k in set(r) | set(w):
            if isinstance(k, tuple) and k[0] == "ps":
                d = self.ps_last.setdefault(k, {})
                for oe in list(d):
                    if oe != eng:
                        deps.update(d[oe])
                        d[oe] = []
                d.setdefault(eng, []).append(tok)
        bar = self.pending_bar.pop(eng) if eng in self.pending_bar else set()
        deps.discard(tok)
        return deps, bar

    def barrier(self):
        toks = set()
        for e in COMPUTE:
            if self.ccount[e] > 0:
                toks.add(("c", e, self.ccount[e] - 1))
        for q in ("sp", "pool"):
            for k in range(max(0, self.dcount[q] - NDSEM), self.dcount[q]):
                toks.add(("d", q, k))
        for e in ("pe", "act", "dve", "pool", "sp"):
            self.pending_bar[e] = set(toks) | self.pending_bar.get(e, set())
        self.epoch += 1

    def add(self, eng, fn, r=(), w=(), cost=0.3):
        seq = self.ccount[eng]
        self.ccount[eng] += 1
        tok = ("c", eng, seq)
        deps, bar = self._deps(tok, tuple(r), tuple(w), eng)
        self.ops[eng].append(["c", fn, deps, seq, cost, self.epoch, bar])
        return tok

    def reorder(self, window=192, lat=1.0):
        RE = ("pe", "act", "dve")
        fin = {}
        etime = {e: 0.0 for e in self.ops}
        new_order = {e: [] for e in RE}
        ptr = {e: 0 for e in self.ops}
        fence = 0.0
        tokof = lambda e, op: ("c", e, op[3]) if op[0] == "c" else ("d", e, op[3])
        for ep in range(self.epoch + 1):
            seg = {}
            for e in self.ops:
                lst = self.ops[e]
                i = ptr[e]
                j = i
                while j < len(lst) and lst[j][5] == ep:
                    j += 1
                seg[e] = lst[i:j]
                ptr[e] = j
            for e in seg:
                etime[e] = max(etime[e], fence)
            left = {e: list(seg[e]) for e in seg}
            total = sum(len(v) for v in left.values())
            while total:
                best = None
                for e, lst in left.items():
                    if not lst:
                        continue
                    cands = lst[:window] if e in RE else lst[:1]
                    for pos, op in enumerate(cands):
                        ready = 0.0
                        ok = True
                        for t in op[2]:
                            f = fin.get(t)
                            if f is None:
                                ok = False
                                break
                            if t[1] != e:
                                f += lat
                            if f > ready:
                                ready = f
                        if not ok:
                            continue
                        start = max(etime[e], ready)
                        key = (start, pos)
                        if best is None or key < best[0]:
                            best = (key, e, pos, op, start)
                        if start <= etime[e]:
                            break
                assert best is not None, "scheduler stuck"
                _, e, pos, op, start = best
                left[e].pop(pos)
                total -= 1
                if op[0] == "c":
                    etime[e] = start + op[4]
                    fin[tokof(e, op)] = etime[e]
                else:
                    etime[e] = start + 0.5
                    fin[tokof(e, op)] = start + op[4]
                if e in RE:
                    new_order[e].append(op)
            fence = max([fence] + list(etime.values()) + [fin[tokof(e, op)] for e in seg for op in seg[e]])
        remap = {}
        for e in RE:
            bars = {}
            for op in new_order[e]:
                if op[6]:
                    bars.setdefault(op[5], set()).update(op[6])
                    op[6] = set()
            seen_ep = set()
            for i, op in enumerate(new_order[e]):
                if op[5] not in seen_ep:
                    seen_ep.add(op[5])
                    op[6] = bars.get(op[5], set())
                remap[("c", e, op[3])] = ("c", e, i)
                op[3] = i
            self.ops[e] = new_order[e]
        for e, lst in self.ops.items():
            for op in lst:
                op[2] = {remap.get(t, t) for t in op[2]}
                op[6] = {remap.get(t, t) for t in op[6]}
        self.model_time = max(etime.values())

    def dma(self, q, fn, r=(), w=(), is_output=False, cost=3.0):
        k = self.dcount[q]
        self.dcount[q] += 1
        tok = ("d", q, k)
        deps, bar = self._deps(tok, tuple(r), tuple(w), q)
        self.ops[q].append(["d", fn, deps, k, cost, self.epoch, bar])
        if is_output:
            self.out_dmas.append(tok)
        return tok

    def emit(self):
        nc = self.nc
        for e, lst in self.ops.items():
            for op in lst:
                op[2] = set(op[2]) | set(op[6])
        for e, lst in self.ops.items():
            seen_c = {f: -1 for f in COMPUTE}
            for kind, fn, deps, seq, _c, _e, _b in lst:
                cw = {}
                for t in deps:
                    if t[0] != "c":
                        continue
                    f, s = t[1], t[2]
                    if f == e and kind == "c":
                        if e == "pe":
                            continue
                        if seq - s > SAME_ENGINE_GAP:
                            continue
                    if s <= seen_c[f]:
                        continue
                    cw[f] = max(cw.get(f, -1), s)
                for f, s in cw.items():
                    seen_c[f] = s
                    self.sig[f].add(s)
        sigidx = {}
        for e in COMPUTE:
            s = sorted(self.sig[e])
            sigidx[e] = {seq: i + 1 for i, seq in enumerate(s)}
        with ExitStack() as st:
            csem = {e: st.enter_context(nc.semaphore("c_" + e)) for e in COMPUTE}
            dsem = {q: [st.enter_context(nc.semaphore(f"d_{q}{i}")) for i in range(NDSEM)]
                    for q in ("sp", "pool")}
            block = st.enter_context(nc.Block())

            def run(e, eng):
                seen_c = {f: -1 for f in COMPUTE}
                seen_d = {}
                for kind, fn, deps, seq, _c, _e, _b in self.ops[e]:
                    cw = {}
                    for t in deps:
                        if t[0] == "c":
                            f, s = t[1], t[2]
                            if f == e and kind == "c":
                                if e == "pe":
                                    continue
                                if seq - s > SAME_ENGINE_GAP:
                                    continue
                            if s <= seen_c[f]:
                                continue
                            cw[f] = max(cw.get(f, -1), s)
                        else:
                            q, k = t[1], t[2]
                            key = (q, k % NDSEM)
                            val = 16 * (k // NDSEM + 1)
                            if seen_d.get(key, 0) >= val:
                                continue
                            seen_d[key] = val
                            eng.wait_ge(dsem[q][k % NDSEM], val)
                    for f, s in cw.items():
                        seen_c[f] = s
                        eng.wait_ge(csem[f], sigidx[f][s])
                    if kind == "c":
                        ins = fn(eng)
                        if seq in sigidx[e]:
                            ins.then_inc(csem[e], 1)
                    else:
                        k = seq
                        if k >= NDSEM:
                            key = (e, k % NDSEM)
                            val = 16 * (k // NDSEM)
                            if seen_d.get(key, 0) < val:
                                seen_d[key] = val
                                eng.wait_ge(dsem[e][k % NDSEM], val)
                        ins = fn(eng)
                        ins.then_inc(dsem[e][k % NDSEM], 16)
                if e == "sp":
                    for t in self.out_dmas:
                        q, k = t[1], t[2]
                        eng.wait_ge(dsem[q][k % NDSEM], 16 * (k // NDSEM + 1))

            @block.tensor
            def _(eng):
                run("pe", eng)

            @block.scalar
            def _(eng):
                run("act", eng)

            @block.vector
            def _(eng):
                run("dve", eng)

            @block.gpsimd
            def _(eng):
                run("pool", eng)

            @block.sync
            def _(eng):
                run("sp", eng)


COLP = {}
_o = 0
for _n, _c in [("mix_even", 8), ("ffn0", 8), ("ffn1", 8), ("mix_odd", 8), ("ssd_cw", 48), ("ssd_cb", 12),
               ("ssd_norm", 8), ("lru_cw", 32), ("lru_cb", 8), ("lru_ba", 8), ("lru_bx", 8), ("lru_lam", 8)]:
    COLP[_n] = (_o, _c)
    _o += _c
NCOL = _o
ROWP = {}
_o = 0
for _n, _c in [("dt_bias", 16), ("a_log", 16), ("ssd_d", 16), ("rb0", 20), ("rb1", 20), ("rc", 64)]:
    ROWP[_n] = (_o, _c)
    _o += _c
NROW = _o


def _fm(v):
    v = np.asarray(v, np.float32).reshape(-1, 128)
    return np.ascontiguousarray(v.T)


def pack_inputs(inp):
    f = lambda a: np.ascontiguousarray(np.asarray(a, np.float32))
    colp = np.zeros((128, NCOL), np.float32)

    def put(name, arr):
        o, c = COLP[name]
        assert arr.shape == (128, c), (name, arr.shape)
        colp[:, o:o + c] = arr

    put("mix_even", _fm(inp["mix_norm_even"][0]))
    put("ffn0", _fm(inp["ffn_norm"][0]))
    put("ffn1", _fm(inp["ffn_norm"][1]))
    put("mix_odd", _fm(inp["mix_norm_odd"][0]))
    cw = np.asarray(inp["ssd_conv_w"][0], np.float32)
    put("ssd_cw", np.concatenate([_fm(cw[k]) for k in range(4)], axis=1).reshape(128, 4, 12).transpose(0, 2, 1).reshape(128, 48))
    put("ssd_cb", _fm(inp["ssd_conv_b"][0]))
    put("ssd_norm", _fm(inp["ssd_norm"][0]))
    lw = np.asarray(inp["lru_conv_w"][0], np.float32)
    put("lru_cw", np.concatenate([_fm(lw[k]) for k in range(4)], axis=1).reshape(128, 4, 8).transpose(0, 2, 1).reshape(128, 32))
    put("lru_cb", _fm(inp["lru_conv_b"][0]))
    put("lru_ba", _fm(inp["lru_b_a"][0]))
    put("lru_bx", _fm(inp["lru_b_x"][0]))
    put("lru_lam", _fm(inp["lru_lambda"][0]))

    rowp = np.zeros((1, NROW), np.float32)

    def putr(name, arr):
        o, c = ROWP[name]
        rowp[0, o:o + c] = np.asarray(arr, np.float32).reshape(-1)

    putr("dt_bias", inp["ssd_dt_bias"][0])
    putr("a_log", inp["ssd_a_log"][0])
    putr("ssd_d", inp["ssd_d"][0])
    putr("rb0", np.concatenate([np.asarray(inp["router_group_b"][0]), np.asarray(inp["router_expert_b"][0])]))
    putr("rb1", np.concatenate([np.asarray(inp["router_group_b"][1]), np.asarray(inp["router_expert_b"][1])]))
    rc = np.zeros((4, 16), np.float32)
    for g, w in enumerate((2, 4, 8, 16)):
        rc[g] = 1.0 / np.minimum(np.arange(16) + 1, w)
    putr("rc", rc)

    w_in = f(inp["w_in"][0])
    cols = np.concatenate([np.arange(0, 2560), np.arange(2576, 4624)])
    w_in_r = np.ascontiguousarray(w_in[:, cols].reshape(8, 128, 36, 128).transpose(2, 1, 0, 3))
    w_dt = np.ascontiguousarray(w_in[:, 2560:2576].reshape(8, 128, 16).transpose(1, 0, 2))
    wr = np.stack([np.concatenate([f(inp["router_group_w"][l]), f(inp["router_expert_w"][l])], axis=1)
                   .reshape(8, 128, 20).transpose(1, 0, 2) for l in range(2)])
    k_ = np.arange(128)[:, None]
    s_ = np.arange(128)[None, :]
    cst = np.stack([np.eye(128, dtype=np.float32), (k_ <= s_).astype(np.float32), (k_ > s_).astype(np.float32),
                    np.ones((128, 128), np.float32)], axis=1)
    shared = {
        "meta": f(inp["meta_tokens"]), "colp": colp, "rowp": rowp, "cst": np.ascontiguousarray(cst),
        "w_in_r": w_in_r, "w_dt": w_dt, "w_out": f(inp["w_out"][0]),
        "lru_wa": f(inp["lru_w_a"][0]), "lru_wx": f(inp["lru_w_x"][0]),
        "pool_w": f(inp["pool_w"][0]), "wr": np.ascontiguousarray(wr),
        "bigrow": np.ascontiguousarray(np.stack([f(inp["norm_final"]), f(inp["pool_b"][0]), f(inp["pool_scale"][0])])[None]),
        "wg": f(inp["expert_w_gate"]), "wu": f(inp["expert_w_up"]), "wd": f(inp["expert_w_down"]),
    }
    return shared


def build(phases=("mix0", "moe0", "mix1", "moe1"), dbg=False):
    nc = bass.Bass("TRN2", target_bir_lowering=False)
    dram = lambda n, s, kind="ExternalInput": nc.dram_tensor(n, list(s), F32, kind=kind).ap()
    x_d = dram("x", [2048, D])
    meta_d = dram("meta", [16, D])
    colp_d = dram("colp", [128, NCOL])
    rowp_d = dram("rowp", [1, NROW])
    cst_d = dram("cst", [128, 4, 128])
    w_in_d = dram("w_in_r", [36, 128, 8, 128])
    w_dt_d = dram("w_dt", [128, 8, 16])
    w_out_d = dram("w_out", [2048, D])
    lwa_d = dram("lru_wa", [16, 64, 64])
    lwx_d = dram("lru_wx", [16, 64, 64])
    pw_d = dram("pool_w", [4, 256, 256])
    wr_d = dram("wr", [2, 128, 8, 20])
    big_d = dram("bigrow", [1, 3, D])
    wg_d = dram("wg", [2, 16, D, 512])
    wu_d = dram("wu", [2, 16, D, 512])
    wd_d = dram("wd", [2, 16, 512, D])
    if dbg:
        out_d = dram("out", [T, D], kind="ExternalOutput")
    else:
        out_d = dram("out", [2048, D], kind="ExternalOutput")

    S = Sched(nc)
    with ExitStack() as st:
        sb = lambda n, s, dt=F32: st.enter_context(nc.sbuf_tensor(n, list(s), dt))
        h = sb("h", [128, NT, D])
        hnT = sb("hnT", [128, 8, T], BF16)
        colp = sb("colp_s", [128, NCOL])
        rowp = sb("rowp_s", [128, NROW])
        cst = sb("cst_s", [128, 4, 128])
        cst16 = sb("cst16", [128, 4, 128], BF16)
        stat = sb("stat", [128, 64])
        AW = 25900
        arena = sb("arena", [128, AW])
        ps = [st.enter_context(nc.psum_tensor(f"ps{i}", [128, 512], F32)) for i in range(8)]
        ident = cst[:, 0, :]
        tri_le = cst[:, 1, :]
        u_gt = cst[:, 2, :]
        ones32 = cst[:, 3, :]
        ident16 = cst16[:, 0, :]
        ss = stat[:, 0:17]
        sq = stat[:, 17:34]
        rstd = stat[:, 34:51]

        class Arena:
            def __init__(self):
                self.off = 0

            def reset(self):
                self.off = 0
                S.barrier()

            def a(self, shape, dt=F32):
                n = int(np.prod(shape[1:]))
                words = n if dt == F32 else (n + 1) // 2
                assert self.off + words <= AW, ("arena overflow", self.off, words)
                v = arena[:, self.off:self.off + words]
                self.off += words
                if dt != F32:
                    v = v.bitcast(dt)
                    if v.shape[1] != n:
                        v = v[:, 0:n]
                if len(shape) == 3:
                    v = v.rearrange("p (a b) -> p a b", a=shape[1])
                elif len(shape) == 4:
                    v = v.rearrange("p (a b c) -> p a b c", a=shape[1], b=shape[2])
                return v

        A = Arena()
        colv = lambda name, i=0, n=None: colp[:, COLP[name][0] + i:COLP[name][0] + i + (n if n else 1)]
        rowv = lambda name: rowp[:, ROWP[name][0]:ROWP[name][0] + ROWP[name][1]]

        def fsz(ap):
            n = 1
            for d_ in ap.shape[1:]:
                n *= int(d_)
            return n

        def ecost(eng, n, mult=1.0):
            if eng == "dve":
                return 0.12 + mult * n / 960.0
            if eng == "act":
                return 0.25 + n / 1400.0
            return 2.0 + 0.015 * n

        def mm(out, lhsT, rhs, start, stop, r, w):
            c = max(fsz(rhs), 64) / 2400.0 + 0.03
            if lhsT.dtype == F32:
                c *= 4
            S.add("pe", lambda e: e.matmul(out, lhsT, rhs, start=start, stop=stop), r=r, w=w, cost=c)

        def tr(out, in_, idn, r, w):
            S.add("pe", lambda e: e.transpose(out, in_, idn), r=r, w=w, cost=0.3 if in_.dtype == F32 else 0.12)

        def act(out, in_, func, r, w, scale=1.0, bias=0.0, accum=None):
            c = ecost("act", fsz(in_)) + (0.1 if accum is not None else 0.0)
            if accum is None:
                S.add("act", lambda e: e.activation(out=out, in_=in_, func=func, scale=scale, bias=bias), r=r, w=w, cost=c)
            else:
                S.add("act", lambda e: e.activation(out=out, in_=in_, func=func, scale=scale, bias=bias, accum_out=accum), r=r, w=w, cost=c)

        def tt(eng, out, in0, in1, op, r, w):
            S.add(eng, lambda e: e.tensor_tensor(out=out, in0=in0, in1=in1, op=op), r=r, w=w, cost=ecost(eng, fsz(out)))

        def ts(eng, out, in0, s1, op0, r, w, s2=None, op1=None):
            c = ecost(eng, fsz(out))
            if op1 is None:
                S.add(eng, lambda e: e.tensor_scalar(out=out, in0=in0, scalar1=s1, scalar2=None, op0=op0), r=r, w=w, cost=c)
            else:
                S.add(eng, lambda e: e.tensor_scalar(out=out, in0=in0, scalar1=s1, scalar2=s2, op0=op0, op1=op1), r=r, w=w, cost=c)

        def stt(out, in0, scalar, in1, op0, op1, r, w):
            S.add("dve", lambda e: e.scalar_tensor_tensor(out=out, in0=in0, scalar=scalar, in1=in1, op0=op0, op1=op1), r=r, w=w,
                  cost=ecost("dve", fsz(out)))

        def cp(eng, out, in_, r, w):
            c = ecost(eng, fsz(out))
            if eng == "act":
                S.add("act", lambda e: e.copy(out, in_), r=r, w=w, cost=c)
            else:
                S.add(eng, lambda e: e.tensor_copy(out, in_), r=r, w=w, cost=c)

        def memset(eng, ap, val, w):
            S.add(eng, lambda e: e.memset(ap, val), w=w, cost=ecost(eng, fsz(ap)))

        def dma(q, out, in_, r=(), w=(), is_output=False):
            nbytes = fsz(out) * int(out.shape[0]) * 4
            S.dma(q, lambda e: e.dma_start(out=out, in_=in_), r=r, w=w, is_output=is_output, cost=2.5 + nbytes / 150e3)

        HK = lambda t: [("h", t, 0), ("h", t, 1)]

        dma("sp", colp[:], colp_d, w=["colp"])
        dma("sp", rowp[:], rowp_d.partition_broadcast(128), w=["rowp"])
        dma("sp", cst[:], cst_d, w=["cst"])
        dma("pool", cst16[:], cst_d, w=["cst16"])
        memset("dve", h[:, 0, :], 0.0, w=HK(0))
        dma("sp", h[PADN:128, 0, :], meta_d, w=HK(0))
        xr = x_d.rearrange("(t p) d -> p t d", p=128)
        for i in range(4):
            dma("sp", h[:, 1 + 4 * i:5 + 4 * i, :], xr[:, 4 * i:4 * i + 4, :], w=[k for t in range(1 + 4 * i, 5 + 4 * i) for k in HK(t)])

        def rms_stats(tiles):
            junk = A.a([128, D], BF16)
            for t in tiles:
                act(junk, h[:, t, :], AF.Square, r=HK(t), w=[("ss", t), "junk"], accum=ss[:, t:t + 1])
                act(sq[:, t:t + 1], ss[:, t:t + 1], AF.Sqrt, r=[("ss", t)], w=[("sq", t)], scale=1.0 / D, bias=colv_eps)
                S.add("dve", lambda e, t=t: e.reciprocal(rstd[:, t:t + 1], sq[:, t:t + 1]), r=[("sq", t)], w=[("rstd", t)], cost=0.15)

        def normT(gname, router_l=None, logits=None, wr_s=None, consume=None):
            xsb = [A.a([128, D]) for _ in range(2)]
            t32 = [A.a([128, 4, 128]) for _ in range(4)]
            pend = None
            for t in range(NT):
                xs = xsb[t % 2]
                act(xs, h[:, t, :], AF.Identity, r=HK(t) + [("rstd", t)], w=[("xs", t % 2)], scale=rstd[:, t:t + 1])
                for half in range(2):
                    i2 = (2 * t + half) % 2
                    i4 = (2 * t + half) % 4
                    bank = ps[6 + i2]
                    for kk in range(4):
                        k = half * 4 + kk
                        tr(bank[:, kk * 128:(kk + 1) * 128], xs[:, k * 128:(k + 1) * 128], ident, r=[("xs", t % 2), "cst"], w=[("ps", 6 + i2)])
                    g0 = COLP[gname][0] + half * 4
                    tt("dve", t32[i4], bank[:, :].rearrange("p (a b) -> p a b", a=4),
                       colp[:, g0:g0 + 4].unsqueeze(2).to_broadcast([128, 4, 128]), ALU.mult,
                       r=[("ps", 6 + i2), "colp"], w=[("t32", i4)])
                    cp("act", hnT[:, half * 4:half * 4 + 4, t * 128:(t + 1) * 128], t32[i4], r=[("t32", i4)], w=[("hnT", t)])
                if router_l is not None:
                    if pend is not None:
                        pend()

                    def mk(t=t):
                        def go():
                            for k in range(8):
                                i4 = (2 * t + k // 4) % 4
                                mm(ps[5][:, 0:20], t32[i4][:, k % 4, :], wr_s[:, k, :], k == 0, k == 7,
                                   r=[("t32", i4), "wr"], w=[("ps", 5)])
                            cp("act", logits[:, t, :], ps[5][:, 0:20], r=[("ps", 5)], w=["logits"])
                        return go
                    pend = mk()
            if pend is not None:
                pend()

        memset("pool", stat[:, 60:61], EPS, w=["eps"])
        colv_eps = stat[:, 60:61]

        def moe(l):
            A.reset()
            wr_s = A.a([128, 8, 20])
            logits = A.a([128, NT, 20])
            gates = A.a([128, NT, 16])
            dma("sp", wr_s, wr_d[l], w=["wr"])
            Wg = [A.a([128, 8, 512], BF16) for _ in range(2)]
            Wu = [A.a([128, 8, 512], BF16) for _ in range(2)]
            Wd = [A.a([128, 4, D], BF16) for _ in range(2)]

            def load_w(e):
                b = e % 2
                if e == 0:
                    for fc in range(4):
                        fs = slice(fc * 128, (fc + 1) * 128)
                        dma("pool", Wg[b][:, :, fs], wg_d[l, e][:, fs].rearrange("(k p) f -> p k f", p=128), w=[("Wg", b, fc)])
                        dma("pool", Wu[b][:, :, fs], wu_d[l, e][:, fs].rearrange("(k p) f -> p k f", p=128), w=[("Wu", b, fc)])
                else:
                    dma("pool", Wg[b], wg_d[l, e].rearrange("(k p) f -> p k f", p=128), w=[("Wg", b, fc) for fc in range(4)])
                    dma("pool", Wu[b], wu_d[l, e].rearrange("(k p) f -> p k f", p=128), w=[("Wu", b, fc) for fc in range(4)])
                dma("pool", Wd[b], wd_d[l, e].rearrange("(k p) f -> p k f", p=128), w=[("Wd", b)])

            load_w(0)
            load_w(1)
            mark = A.off
            rms_stats(list(range(NT)))
            normT("ffn%d" % l, router_l=l, logits=logits, wr_s=wr_s)
            R = lambda shape: A.a(shape)
            rb = rowv("rb%d" % l)
            lg = R([128, NT, 20])
            tt("dve", lg, logits, rb.unsqueeze(1).to_broadcast([128, NT, 20]), ALU.add, r=["logits", "rowp"], w=["lg"])
            m4 = R([128, NT])
            S.add("dve", lambda e: e.tensor_reduce(out=m4, in_=lg[:, :, 0:4], axis=AX.X, op=ALU.max), r=["lg"], w=["m4"])
            d4 = R([128, NT, 4])
            tt("dve", d4, lg[:, :, 0:4], m4.unsqueeze(2).to_broadcast([128, NT, 4]), ALU.subtract, r=["lg", "m4"], w=["d4"])
            mg = R([128, NT, 4])
            ts("dve", mg, d4, 0.0, ALU.is_ge, r=["d4"], w=["mg"])
            e4 = R([128, NT, 4])
            act(e4, d4, AF.Exp, r=["d4"], w=["e4"])
            s4 = R([128, NT])
            S.add("dve", lambda e: e.tensor_reduce(out=s4, in_=e4, axis=AX.X, op=ALU.add), r=["e4"], w=["s4"])
            le = lg[:, :, 4:20].rearrange("p t (g j) -> p t g j", g=4)
            ml = R([128, NT, 4, 4])
            tt("dve", ml, le, mg.unsqueeze(3).to_broadcast([128, NT, 4, 4]), ALU.mult, r=["lg", "mg"], w=["ml"])
            sel = R([128, NT, 4])
            tt("dve", sel, ml[:, :, 0, :], ml[:, :, 1, :], ALU.add, r=["ml"], w=["sel"])
            tt("dve", sel, sel, ml[:, :, 2, :], ALU.add, r=["ml", "sel"], w=["sel"])
            tt("dve", sel, sel, ml[:, :, 3, :], ALU.add, r=["ml", "sel"], w=["sel"])
            m1 = R([128, NT])
            S.add("dve", lambda e: e.tensor_reduce(out=m1, in_=sel, axis=AX.X, op=ALU.max), r=["sel"], w=["m1"])
            k1 = R([128, NT, 4])
            tt("dve", k1, sel, m1.unsqueeze(2).to_broadcast([128, NT, 4]), ALU.is_ge, r=["sel", "m1"], w=["k1"])
            sel2 = R([128, NT, 4])
            stt(sel2, k1, -1e30, sel, ALU.mult, ALU.add, r=["k1", "sel"], w=["sel2"])
            m2 = R([128, NT])
            S.add("dve", lambda e: e.tensor_reduce(out=m2, in_=sel2, axis=AX.X, op=ALU.max), r=["sel2"], w=["m2"])
            k2 = R([128, NT, 4])
            tt("dve", k2, sel2, m2.unsqueeze(2).to_broadcast([128, NT, 4]), ALU.is_ge, r=["sel2", "m2"], w=["k2"])
            dd = R([128, NT])
            tt("dve", dd, m2, m1, ALU.subtract, r=["m1", "m2"], w=["dd"])
            w2 = R([128, NT])
            act(w2, dd, AF.Exp, r=["dd"], w=["w2"])
            den = R([128, NT])
            stt(den, w2, 1.0, s4, ALU.add, ALU.mult, r=["w2", "s4"], w=["den"])
            g1 = R([128, NT])
            S.add("dve", lambda e: e.reciprocal(g1, den), r=["den"], w=["g1"])
            g2 = R([128, NT])
            tt("dve", g2, g1, w2, ALU.mult, r=["g1", "w2"], w=["g2"])
            gs = R([128, NT, 4])
            tt("dve", gs, k1, g1.unsqueeze(2).to_broadcast([128, NT, 4]), ALU.mult, r=["k1", "g1"], w=["gs"])
            gs2 = R([128, NT, 4])
            tt("dve", gs2, k2, g2.unsqueeze(2).to_broadcast([128, NT, 4]), ALU.mult, r=["k2", "g2"], w=["gs2"])
            tt("dve", gs, gs, gs2, ALU.add, r=["gs", "gs2"], w=["gs"])
            g4 = gates.rearrange("p t (g j) -> p t g j", g=4)
            for g in range(4):
                tt("dve", g4[:, :, g, :], gs, mg[:, :, g:g + 1].to_broadcast([128, NT, 4]), ALU.mult, r=["gs", "mg"], w=["gates"])

            hid = [A.a([128, 4, 512], BF16) for _ in range(2)]
            sg = [A.a([128, 512]) for _ in range(2)]
            cnt = 0
            cnt_o = [0]
            blk = 0
            pend = None
            for e in range(16):
                b = e % 2
                if e >= 2:
                    load_w(e)
                for (t0, n) in TB:
                    hb = blk % 2
                    blk += 1
                    hk = [("hnT", tt_) for tt_ in range(t0 // 128, (t0 + n) // 128)]
                    for fc in range(4):
                        i = cnt % 2
                        cnt += 1
                        pg, pu = ps[i], ps[2 + i]
                        for k in range(8):
                            mm(pg[:, 0:n], Wg[b][:, k, fc * 128:(fc + 1) * 128], hnT[:, k, t0:t0 + n], k == 0, k == 7,
                               r=[("Wg", b, fc)] + hk, w=[("ps", i)])
                        for k in range(8):
                            mm(pu[:, 0:n], Wu[b][:, k, fc * 128:(fc + 1) * 128], hnT[:, k, t0:t0 + n], k == 0, k == 7,
                               r=[("Wu", b, fc)] + hk, w=[("ps", 2 + i)])
                        act(sg[i][:, 0:n], pg[:, 0:n], AF.Silu, r=[("ps", i)], w=[("sg", i)])
                        tt("dve", hid[hb][:, fc, 0:n], sg[i][:, 0:n], pu[:, 0:n], ALU.mult, r=[("sg", i), ("ps", 2 + i)], w=[("hid", hb)])
                    if pend is not None:
                        pend()

                    def mk(e=e, b=b, hb=hb, t0=t0, n=n):
                        def go():
                            for tl in range(n // 128):
                                t = t0 // 128 + tl
                                for dh in range(2):
                                    io = 4 + cnt_o[0] % 2
                                    cnt_o[0] += 1
                                    for fc in range(4):
                                        mm(ps[io][:, :], hid[hb][:, fc, tl * 128:(tl + 1) * 128], Wd[b][:, fc, dh * 512:(dh + 1) * 512],
                                           fc == 0, fc == 3, r=[("hid", hb), ("Wd", b)], w=[("ps", io)])
                                    hv = h[:, t, dh * 512:(dh + 1) * 512]
                                    stt(hv, ps[io][:, :], gates[:, t, e:e + 1], hv,
                                        ALU.mult, ALU.add, r=[("ps", io), "gates", ("h", t, dh)], w=[("h", t, dh)])
                        return go
                    pend = mk()
            pend()

        def mix1():
            A.reset()
            memset("pool", h[0:PADN, 0, :], 0.0, w=HK(0))
            rms_stats(list(range(NT)))
            pw = A.a([128, 4, 2, 256], BF16)
            for g in range(4):
                dma("pool", pw[:, g, :, :], pw_d[g].rearrange("(c p) j -> p c j", p=128), w=["pw"])
            pb_bc = A.a([128, D])
            sc_bc = A.a([128, D])
            dma("sp", pb_bc, big_d[:, 1, :].partition_broadcast(128), w=["pb_bc"])
            dma("sp", sc_bc, big_d[:, 2, :].partition_broadcast(128), w=["sc_bc"])
            bs_bc = A.a([128, D])
            tt("dve", bs_bc, pb_bc, sc_bc, ALU.mult, r=["pb_bc", "sc_bc"], w=["bs_bc"])
            bs16 = A.a([128, D], BF16)
            cp("dve", bs16, bs_bc, r=["bs_bc"], w=["bs16"])
            pws = A.a([128, 4, 2, 256], BF16)
            for g in range(4):
                tt("dve", pws[:, g, :, :], pw[:, g, :, :], sc_bc[:, g * 256:(g + 1) * 256].unsqueeze(1).to_broadcast([128, 2, 256]), ALU.mult,
                   r=["pw", "sc_bc"], w=["pws"])
            PF = 16
            hn32 = A.a([128, PF + T])
            sA = A.a([128, PF + T])
            sB = A.a([128, PF + T])
            pooledT = A.a([128, 8, T], BF16)
            for buf, nm in ((hn32, "hn32"), (sA, "sA"), (sB, "sB")):
                memset("pool", buf[:, 0:PF], 0.0, w=[nm])
            xsn = A.a([128, NT, 128])
            fix = A.a([128, 16])
            rcv = rowv("rc")
            for k in range(8):
                g = k // 2
                w = (2, 4, 8, 16)[g]
                tt("dve", xsn, h[:, :, k * 128:(k + 1) * 128], rstd[:, 0:NT].unsqueeze(2).to_broadcast([128, NT, 128]), ALU.mult,
                   r=[("h", t, k // 4) for t in range(NT)] + [("rstd", t) for t in range(NT)], w=["xsn"])
                for q in range(5):
                    tiles = list(range(4 * q, min(4 * q + 4, NT)))
                    bank = ps[6 + q % 2]
                    for i, t in enumerate(tiles):
                        tr(bank[:, i * 128:(i + 1) * 128], xsn[:, t, :], ident, r=["xsn", "cst"], w=[("ps", 6 + q % 2)])
                    nn = len(tiles) * 128
                    act(hn32[:, PF + q * 512:PF + q * 512 + nn], bank[:, 0:nn], AF.Identity, r=[("ps", 6 + q % 2), "colp"], w=["hn32"],
                        scale=colv("mix_odd", k))
                cur, curk = hn32, "hn32"
                step = 1
                bufs = [(sA, "sA"), (sB, "sB")]
                bi = 0
                while step < w:
                    nxt, nk = bufs[bi % 2]
                    bi += 1
                    tt("dve", nxt[:, PF:PF + T], cur[:, PF:PF + T], cur[:, PF - step:PF + T - step], ALU.add,
                       r=[curk], w=[nk])
                    cur, curk = nxt, nk
                    step *= 2
                stt(pooledT[:, k, :], cur[:, PF:PF + T], 1.0 / w, hn32[:, PF:PF + T], ALU.mult, ALU.subtract,
                    r=[curk, "hn32"], w=[("pooledT", k)])
                tt("dve", fix, cur[:, PF + PADN:PF + 128], rcv[:, g * 16:(g + 1) * 16], ALU.mult, r=[curk, "rowp"], w=["fix"])
                tt("dve", pooledT[:, k, PADN:128], fix, hn32[:, PF + PADN:PF + 128], ALU.subtract, r=["fix", "hn32"], w=[("pooledT", k)])
            for t in range(NT):
                for dh in range(2):
                    bi_ = dh + 2 * (t % 2)
                    bank = ps[bi_]
                    for gg in range(2):
                        g = dh * 2 + gg
                        o = bank[:, gg * 256:(gg + 1) * 256]
                        for ic in range(2):
                            mm(o, pooledT[:, 2 * g + ic, t * 128:(t + 1) * 128], pws[:, g, ic, :], ic == 0, False,
                               r=[("pooledT", 2 * g + ic), "pws"], w=[("ps", bi_)])
                        mm(o, cst16[0:1, 3, :], bs16[0:1, g * 256:(g + 1) * 256], False, True, r=["cst16", "bs16"], w=[("ps", bi_)])
                    hv = h[:, t, dh * 512:(dh + 1) * 512]
                    tt("dve", hv, hv, bank[:, :], ALU.add, r=[("ps", bi_), ("h", t, dh)], w=[("h", t, dh)])

        def mix0():
            A.reset()
            rms_stats(list(range(NT)))
            normT("mix_even")
            A.reset()
            hnk = [("hnT", t) for t in range(NT)]
            wbuf = [A.a([128, 8, 128], BF16) for _ in range(3)]
            wcnt = [0]

            def load_wchunk(cc):
                i = wcnt[0] % 3
                wcnt[0] += 1
                dma("pool", wbuf[i], w_in_d[cc], w=[("wbuf", i)])
                return i

            def proj_blk(wi, bi, t0, n):
                bk = ("ps", bi % 2)
                bank = ps[bi % 2]
                for k in range(8):
                    mm(bank[:, 0:n], wbuf[wi][:, k, :], hnT[:, k, t0:t0 + n], k == 0, k == 7, r=[("wbuf", wi)] + hnk, w=[bk])
                return bank, bk

            xinb = [A.a([128, 3 + 512]) for _ in range(2)]
            ctb = [A.a([128, 512]) for _ in range(2)]
            cvc = [0]

            def conv_blk(bank, bk, bi, n, wname, bname, cidx, tap0_act=False, copy_eng="act"):
                i = cvc[0] % 2
                cvc[0] += 1
                xi, xk = xinb[i], ("xinb", i)
                if bi == 0:
                    memset("dve", xi[:, 0:3], 0.0, w=[xk])
                else:
                    pv = xinb[1 - i]
                    cp("dve", xi[:, 0:3], pv[:, 512:515], r=[("xinb", 1 - i)], w=[xk])
                cp(copy_eng, xi[:, 3:3 + n], bank[:, 0:n], r=[bk], w=[xk])
                ct, ck = ctb[i], ("ctb", i)
                o4 = COLP[wname][0] + 4 * cidx
                if tap0_act:
                    act(ct[:, 0:n], xi[:, 0:n], AF.Identity, r=[xk, "colp"], w=[ck], scale=colp[:, o4:o4 + 1], bias=colv(bname, cidx))
                else:
                    ts("dve", ct[:, 0:n], xi[:, 0:n], colp[:, o4:o4 + 1], ALU.mult, r=[xk, "colp"], w=[ck], s2=colv(bname, cidx), op1=ALU.add)
                for k in range(1, 4):
                    stt(ct[:, 0:n], xi[:, k:k + n], colp[:, o4 + k:o4 + k + 1], ct[:, 0:n], ALU.mult, ALU.add, r=[xk, ck, "colp"], w=[ck])
                return ct, ck

            wo = A.a([128, 4, D], BF16)
            ybuf = A.a([128, 4, T], BF16)

            def out_proj(kc0, scale_ap_fn):
                dma("pool", wo, w_out_d[kc0 * 128:(kc0 + 4) * 128, :].rearrange("(k p) d -> p k d", p=128), w=["wo"])
                cnt = 0
                for t in range(NT):
                    for dh in range(2):
                        io = 4 + cnt % 2
                        cnt += 1
                        for kk in range(4):
                            mm(ps[io][:, :], ybuf[:, kk, t * 128:(t + 1) * 128], wo[:, kk, dh * 512:(dh + 1) * 512], kk == 0, kk == 3,
                               r=[("ybuf", kk), "wo"], w=[("ps", io)])
                        hv = h[:, t, dh * 512:(dh + 1) * 512]
                        if scale_ap_fn is None:
                            tt("dve", hv, hv, ps[io][:, :], ALU.add, r=[("ps", io), ("h", t, dh)], w=[("h", t, dh)])
                        else:
                            stt(hv, ps[io][:, :], scale_ap_fn(t), hv, ALU.mult, ALU.add, r=[("ps", io), ("h", t, dh), "rstdg"], w=[("h", t, dh)])

            mark = A.off

            def lru():
                bdA = A.a([128, 8, 128], BF16)
                bdX = A.a([128, 8, 128], BF16)
                memset("dve", bdA, 0.0, w=["bdA"])
                memset("dve", bdX, 0.0, w=["bdX"])
                for src, dst, nm in ((lwa_d, bdA, "bdA"), (lwx_d, bdX, "bdX")):
                    v = src.rearrange("(j two) i o -> two i j o", two=2)
                    dma("pool", dst[0:64, :, 0:64], v[0], w=[nm])
                    dma("pool", dst[64:128, :, 64:128], v[1], w=[nm])
                c1 = A.a([128, 8])
                tmpc = A.a([128, 8])
                act(tmpc, colv("lru_lam", 0, 8), AF.Exp, r=["colp"], w=["tmpc"], scale=-1.0)
                act(tmpc, tmpc, AF.Ln, r=["tmpc"], w=["tmpc"], bias=1.0)
                ts("dve", c1, tmpc, -8.0, ALU.mult, r=["tmpc"], w=["c1"])
                Bn = lambda n_: [A.a([128, 512]) for _ in range(n_)]
                gl, rr, ii, av, uv, hl = Bn(4), Bn(4), Bn(4), Bn(4), Bn(2), Bn(2)
                xb16 = [A.a([128, 512], BF16) for _ in range(4)]
                NB = len(TB)
                items = [(j, bi) for j in range(8) for bi in range(NB)]
                st1 = {}

                def S1(idx):
                    j, bi = items[idx]
                    t0, n = TB[bi]
                    if bi == 0:
                        st1[j] = (load_wchunk(28 + j), load_wchunk(20 + j))
                    wi_in, wi_gt = st1[j]
                    bank, bk = proj_blk(wi_in, 2 * idx, t0, n)
                    xb, xbk = conv_blk(bank, bk, bi, n, "lru_cw", "lru_cb", j, copy_eng="dve")
                    bank2, bk2 = proj_blk(wi_gt, 2 * idx + 1, t0, n)
                    g3 = idx % 4
                    act(gl[g3][:, 0:n], bank2[:, 0:n], AF.Gelu_apprx_tanh, r=[bk2], w=[("gl", g3)])
                    cp("act", xb16[g3][:, 0:n], xb[:, 0:n], r=[xbk], w=[("xb16", g3)])
                    st1[idx, "xb"] = (xb, xbk)

                def S2(idx):
                    j, bi = items[idx]
                    t0, n = TB[bi]
                    ip = idx % 2
                    q = idx % 4
                    K = lambda nm: (nm, q)
                    xb, xbk = st1.pop((idx, "xb"))
                    pa, px = ps[2 + ip], ps[4 + ip]
                    mm(pa[:, 0:n], bdA[:, j, :], xb16[q][:, 0:n], True, True, r=["bdA", K("xb16")], w=[("ps", 2 + ip)])
                    mm(px[:, 0:n], bdX[:, j, :], xb16[q][:, 0:n], True, True, r=["bdX", K("xb16")], w=[("ps", 4 + ip)])
                    r_, i_ = rr[q], ii[q]
                    act(r_[:, 0:n], pa[:, 0:n], AF.Sigmoid, r=[("ps", 2 + ip), "colp"], w=[K("rr")], bias=colv("lru_ba", j))
                    act(i_[:, 0:n], px[:, 0:n], AF.Sigmoid, r=[("ps", 4 + ip), "colp"], w=[K("ii")], bias=colv("lru_bx", j))
                    act(av[q][:, 0:n], r_[:, 0:n], AF.Exp, r=[K("rr"), "c1"], w=[K("av")], scale=c1[:, j:j + 1])
                    stt(r_[:, 0:n], av[q][:, 0:n], 0.9999998, av[q][:, 0:n], ALU.min, ALU.mult, r=[K("av")], w=[K("rr")])
                    act(r_[:, 0:n], r_[:, 0:n], AF.Sqrt, r=[K("rr")], w=[K("rr")], scale=-1.0, bias=1.0)
                    tt("dve", i_[:, 0:n], i_[:, 0:n], xb[:, 0:n], ALU.mult, r=[K("ii"), xbk], w=[K("ii")])
                    if bi == 0:
                        memset("dve", r_[:, PADN:PADN + 1], 1.0, w=[K("rr")])

                def S3(idx):
                    j, bi = items[idx]
                    t0, n = TB[bi]
                    i = idx % 2
                    q = idx % 4
                    jj = j % 4
                    K = lambda nm: (nm, q)
                    tt("dve", uv[i][:, 0:n], rr[q][:, 0:n], ii[q][:, 0:n], ALU.mult, r=[K("rr"), K("ii")], w=[("uv", i)])
                    if bi == 0:
                        memset("dve", hl[i][:, 0:PADN], 0.0, w=[("hl", i)])
                        S.add("dve", lambda e, i=i, q=q, n=n: e.tensor_tensor_scan(out=hl[i][:, PADN:n], data0=av[q][:, PADN:n], data1=uv[i][:, PADN:n],
                                                                             initial=0.0, op0=ALU.mult, op1=ALU.add),
                              r=[K("av"), ("uv", i)], w=[("hl", i)], cost=1.2)
                    else:
                        S.add("dve", lambda e, i=i, q=q, n=n: e.tensor_tensor_scan(out=hl[i][:, 0:n], data0=av[q][:, 0:n], data1=uv[i][:, 0:n],
                                                                             initial=hl[1 - i][:, 511:512], op0=ALU.mult, op1=ALU.add),
                              r=[K("av"), ("uv", i), ("hl", 1 - i)], w=[("hl", i)], cost=1.2)
                    tt("dve", ybuf[:, jj, t0:t0 + n], hl[i][:, 0:n], gl[q][:, 0:n], ALU.mult, r=[("hl", i), ("gl", q)], w=[("ybuf", jj)])
                    if bi == NB - 1 and jj == 3:
                        out_proj(8 + (j // 4) * 4, None)

                NI = len(items)
                for step in range(NI + 2):
                    if step < NI:
                        S1(step)
                    if 0 <= step - 1 < NI:
                        S2(step - 1)
                    if 0 <= step - 2 < NI:
                        S3(step - 2)

            def ssd():
                dt = A.a([128, NT, 16])
                adt = A.a([128, NT, 16])
                ea = A.a([128, NT, 16])
                eatot = A.a([128, NT, 16])
                dte = A.a([128, NT, 16])
                Aneg = A.a([128, 16])
                wdt = A.a([128, 8, 16], BF16)
                dma("pool", wdt, w_dt_d, w=["wdt"])
                for t in range(NT):
                    for k in range(8):
                        mm(ps[7][:, t * 16:(t + 1) * 16], hnT[:, k, t * 128:(t + 1) * 128], wdt[:, k, :], k == 0, k == 7,
                           r=["wdt", ("hnT", t)], w=[("ps", 7)])
                p7 = ps[7][:, 0:NT * 16].rearrange("p (t h) -> p t h", t=NT)
                tt("dve", dt, p7, rowv("dt_bias").unsqueeze(1).to_broadcast([128, NT, 16]), ALU.add, r=[("ps", 7), "rowp"], w=["dt"])
                act(dte, dt, AF.Abs, r=["dt"], w=["dte"])
                act(dte, dte, AF.Exp, r=["dte"], w=["dte"], scale=-1.0)
                act(dte, dte, AF.Ln, r=["dte"], w=["dte"], bias=1.0)
                stt(dt, dt, 0.0, dte, ALU.max, ALU.add, r=["dt", "dte"], w=["dt"])
                memset("dve", dt[0:PADN, 0, :], 0.0, w=["dt"])
                act(Aneg, rowv("a_log"), AF.Exp, r=["rowp"], w=["Aneg"])
                stt(adt, dt, -1.0, Aneg.unsqueeze(1).to_broadcast([128, NT, 16]), ALU.mult, ALU.mult, r=["dt", "Aneg"], w=["adt"])
                for t in range(NT):
                    mm(ps[6][:, t * 16:(t + 1) * 16], tri_le, adt[:, t, :], True, True, r=["cst", "adt"], w=[("ps", 6)])
                for t in range(NT):
                    mm(ps[7][:, t * 16:(t + 1) * 16], ones32, adt[:, t, :], True, True, r=["cst", "adt"], w=[("ps", 7)])
                p6 = ps[6][:, 0:NT * 16].rearrange("p (t h) -> p t h", t=NT)
                cp("dve", ea, p6, r=[("ps", 6)], w=["ea"])
                cp("dve", eatot, p7, r=[("ps", 7)], w=["eatot"])
                tt("dve", dte, eatot, ea, ALU.subtract, r=["eatot", "ea"], w=["dte"])
                act(dte, dte, AF.Exp, r=["dte"], w=["dte"])
                act(ea, ea, AF.Exp, r=["ea", "dte"], w=["ea"])
                act(eatot, eatot, AF.Exp, r=["eatot", "dte"], w=["eatot"])
                Dbc = rowv("ssd_d")
                DI = A.a([128, 2, 128], BF16)
                if SSD_STOP == "dt":
                    return

                BT = A.a([128, T], BF16)
                CT = A.a([128, T], BF16)
                Btok = A.a([128, NT, 128], BF16)
                CBm = A.a([128, NT, 128], BF16)
                xT16 = [A.a([128, 512], BF16) for _ in range(2)]
                xtok = A.a([128, NT, 128], BF16)
                xdt = A.a([128, NT, 128], BF16)
                sz = A.a([128, NT, 128], BF16)
                MT_all = A.a([128, NT, 256], BF16)
                S_all = A.a([128, NT, 128], BF16)
                ssp = A.a([128, NT, 4])
                rstdg = A.a([128, NT])
                Sst = A.a([128, 128])
                Lb = [A.a([128, 256]) for _ in range(2)]
                Db = [A.a([128, 256]) for _ in range(2)]
                xdte = [A.a([128, 128], BF16) for _ in range(4)]
                ytmp = [A.a([128, 128]) for _ in range(4)]
                yg16 = [A.a([128, 128], BF16) for _ in range(4)]
                junk = A.a([128, 128], BF16)

                def bank16(i):
                    return ps[i][:, 0:256].bitcast(BF16)

                def fm_chunk(cc, cidx, sink):
                    wi = load_wchunk(cc)
                    for bi, (t0, n) in enumerate(TB):
                        bank, bk = proj_blk(wi, bi, t0, n)
                        ct, ck = conv_blk(bank, bk, bi, n, "ssd_cw", "ssd_cb", cidx, tap0_act=True)
                        sink(bi, t0, n, ct, ck)

                for g in range(2):
                    pass
                    fm_chunk(8 + 8 + g, 8 + g, lambda bi, t0, n, ct, ck: act(BT[:, t0:t0 + n], ct[:, 0:n], AF.Silu, r=[ck], w=["BT"]))
                    fm_chunk(8 + 10 + g, 10 + g, lambda bi, t0, n, ct, ck: act(CT[:, t0:t0 + n], ct[:, 0:n], AF.Silu, r=[ck], w=["CT"]))
                    for q in range(5):
                        tiles = list(range(4 * q, min(4 * q + 4, NT)))
                        nn = len(tiles)
                        bk = bank16(6 + q % 2)
                        for i, t in enumerate(tiles):
                            tr(bk[:, i * 128:(i + 1) * 128], BT[:, t * 128:(t + 1) * 128], ident16, r=["BT", "cst16"], w=[("ps", 6 + q % 2)])
                        cp("dve", Btok[:, 4 * q:4 * q + nn, :], bk[:, 0:nn * 128].rearrange("p (a b) -> p a b", a=nn), r=[("ps", 6 + q % 2)], w=["Btok"])
                        bank = ps[2 + q % 2]
                        for i, t in enumerate(tiles):
                            mm(bank[:, i * 128:(i + 1) * 128], BT[:, t * 128:(t + 1) * 128], CT[:, t * 128:(t + 1) * 128], True, True,
                               r=["BT", "CT"], w=[("ps", 2 + q % 2)])
                        tt("dve", CBm[:, 4 * q:4 * q + nn, :], bank[:, 0:nn * 128].rearrange("p (a b) -> p a b", a=nn),
                           tri_le.unsqueeze(1).to_broadcast([128, nn, 128]), ALU.mult, r=[("ps", 2 + q % 2), "cst"], w=["CBm"])
                    pass
                    if SSD_STOP == "bc":
                        return
                    for jj in range(4):
                        j = g * 4 + jj
                        hh0 = 2 * j

                        def xsink(bi, t0, n, ct, ck, hh0=hh0):
                            xt = xT16[bi % 2]
                            xk = ("xT16", bi % 2)
                            act(xt[:, 0:n], ct[:, 0:n], AF.Silu, r=[ck], w=[xk])
                            nn = n // 128
                            bk = bank16(6 + bi % 2)
                            for i in range(nn):
                                tr(bk[:, i * 128:(i + 1) * 128], xt[:, i * 128:(i + 1) * 128], ident16, r=[xk, "cst16"], w=[("ps", 6 + bi % 2)])
                            a0 = t0 // 128
                            cp("act", xtok.rearrange("p a b -> p (a b)")[:, a0 * 128:(a0 + nn) * 128], bk[:, 0:nn * 128], r=[("ps", 6 + bi % 2)], w=[("xtok", bi)])
                            for i in range(nn):
                                t = a0 + i
                                tt("dve", xdt[:, t, :].rearrange("p (h c) -> p h c", h=2), xtok[:, t, :].rearrange("p (h c) -> p h c", h=2),
                                   dt[:, t, hh0:hh0 + 2].unsqueeze(2).to_broadcast([128, 2, 64]), ALU.mult, r=[("xtok", bi), "dt"], w=[("xdt", t)])
                        fm_chunk(8 + j, j, xsink)
                        if SSD_STOP == "x":
                            return
                        wz = load_wchunk(j)
                        for bi, (t0, n) in enumerate(TB):
                            bank, bk_ = proj_blk(wz, bi, t0, n)
                            zt = xT16[bi % 2]
                            zk = ("xT16", bi % 2)
                            act(zt[:, 0:n], bank[:, 0:n], AF.Silu, r=[bk_], w=[zk])
                            nn = n // 128
                            bk = bank16(6 + bi % 2)
                            for i in range(nn):
                                tr(bk[:, i * 128:(i + 1) * 128], zt[:, i * 128:(i + 1) * 128], ident16, r=[zk, "cst16"], w=[("ps", 6 + bi % 2)])
                            a0 = t0 // 128
                            cp("dve", sz.rearrange("p a b -> p (a b)")[:, a0 * 128:(a0 + nn) * 128], bk[:, 0:nn * 128], r=[("ps", 6 + bi % 2)], w=[("sz", bi)])
                        if SSD_STOP == "z":
                            return
                        memset("dve", Sst, 0.0, w=["Sst"])
                        memset("dve", S_all[:, 0, :], 0.0, w=[("S_all", 0)])
                        for hh in range(2):
                            act(DI[:, hh, :], ident, AF.Identity, r=["cst", "rowp"], w=["DI"], scale=Dbc[:, hh0 + hh:hh0 + hh + 1])
                        for c in range(NT):
                            i2 = c % 2
                            i4 = c % 4
                            for hh in range(2):
                                act(Lb[i2][:, hh * 128:(hh + 1) * 128], u_gt, AF.Identity, r=["cst", "adt"], w=[("L", i2, hh)], scale=adt[:, c, hh0 + hh:hh0 + hh + 1])
                                mm(ps[i2][:, hh * 128:(hh + 1) * 128], Lb[i2][:, hh * 128:(hh + 1) * 128], tri_le, True, True, r=[("L", i2, hh), "cst"], w=[("ps", i2)])
                            act(Db[i2], ps[i2][:, 0:256], AF.Exp, r=[("ps", i2)], w=[("D", i2)])
                            tt("dve", MT_all[:, c, :].rearrange("p (h l) -> p h l", h=2), Db[i2].rearrange("p (h l) -> p h l", h=2),
                               CBm[:, c, :].unsqueeze(1).to_broadcast([128, 2, 128]), ALU.mult, r=[("D", i2), "CBm"], w=[("MT", c)])
                            if c + 1 < NT:
                                tt("dve", xdte[i4].rearrange("p (h c) -> p h c", h=2), xdt[:, c, :].rearrange("p (h c) -> p h c", h=2),
                                   dte[:, c, hh0:hh0 + 2].unsqueeze(2).to_broadcast([128, 2, 64]), ALU.mult, r=[("xdt", c), "dte"], w=[("xdte", i4)])
                                mm(ps[2 + i2][:, 0:128], Btok[:, c, :], xdte[i4], True, True, r=["Btok", ("xdte", i4)], w=[("ps", 2 + i2)])
                                tt("dve", Sst.rearrange("p (h c) -> p h c", h=2), Sst.rearrange("p (h c) -> p h c", h=2),
                                   eatot[:, c, hh0:hh0 + 2].unsqueeze(2).to_broadcast([128, 2, 64]), ALU.mult, r=["Sst", "eatot"], w=["Sst"])
                                tt("dve", Sst, Sst, ps[2 + i2][:, 0:128], ALU.add, r=[("ps", 2 + i2), "Sst"], w=["Sst"])
                                cp("act", S_all[:, c + 1, :], Sst, r=["Sst"], w=[("S_all", c + 1)])
                        def p3_mm(c):
                            b = 4 + c % 2
                            pa = ps[b]
                            mm(pa[:, 0:128], CT[:, c * 128:(c + 1) * 128], S_all[:, c, :], True, True, r=["CT", ("S_all", c)], w=[("ps", b)])
                            for hh in range(2):
                                o = pa[:, 128 + hh * 64:128 + (hh + 1) * 64]
                                mm(o, MT_all[:, c, hh * 128:(hh + 1) * 128], xdt[:, c, hh * 64:(hh + 1) * 64], True, False, r=[("MT", c), ("xdt", c)], w=[("ps", b)])
                                mm(o, DI[:, hh, :], xtok[:, c, hh * 64:(hh + 1) * 64], False, True, r=["DI"] + [("xtok", bb) for bb in range(5)], w=[("ps", b)])

                        def p3_ev(c, jj=jj, j=j, hh0=hh0):
                            b = 4 + c % 2
                            pa = ps[b]
                            i4 = c % 4
                            y, yk = ytmp[i4], ("ytmp", i4)
                            tt("dve", y.rearrange("p (h c) -> p h c", h=2), pa[:, 0:128].rearrange("p (h c) -> p h c", h=2),
                               ea[:, c, hh0:hh0 + 2].unsqueeze(2).to_broadcast([128, 2, 64]), ALU.mult, r=[("ps", b), "ea"], w=[yk])
                            tt("dve", y, y, pa[:, 128:256], ALU.add, r=[("ps", b), yk], w=[yk])
                            yt, ytk = yg16[i4], ("yg", i4)
                            tt("dve", yt, y, sz[:, c, :], ALU.mult, r=[yk] + [("sz", bb) for bb in range(5)], w=[ytk])
                            act(junk, yt, AF.Square, r=[ytk], w=[("ssp", c), "junk"], accum=ssp[:, c, jj:jj + 1])
                            bk = bank16(6 + c % 2)
                            tr(bk[:, 0:128], yt, ident16, r=[ytk, "cst16"], w=[("ps", 6 + c % 2)])
                            act(ybuf[:, jj, c * 128:(c + 1) * 128], bk[:, 0:128], AF.Identity, r=[("ps", 6 + c % 2), "colp"], w=[("ybuf", jj)],
                                scale=colv("ssd_norm", j))

                        p3_mm(0)
                        for c in range(NT):
                            if c + 1 < NT:
                                p3_mm(c + 1)
                            p3_ev(c)
                    if SSD_STOP == "rec":
                        return
                    pass
                    S.add("dve", lambda e: e.tensor_reduce(out=rstdg, in_=ssp, axis=AX.X, op=ALU.add), r=[("ssp", c) for c in range(NT)], w=["rstdg"])
                    act(rstdg, rstdg, AF.Sqrt, r=["rstdg"], w=["rstdg"], scale=1.0 / 512, bias=colv_eps)
                    S.add("dve", lambda e: e.reciprocal(rstdg, rstdg), r=["rstdg"], w=["rstdg"])
                    out_proj(g * 4, lambda t: rstdg[:, t:t + 1])

            if "lru" in MIX0_PARTS:
                lru()
            S.barrier()
            A.off = mark
            if "ssd" in MIX0_PARTS:
                ssd()

        for ph in phases:
            {"mix0": mix0, "moe0": lambda: moe(0), "mix1": mix1, "moe1": lambda: moe(1)}[ph]()

        if dbg or not phases or not phases[-1].startswith("moe"):
            A.reset()
        if dbg:
            for t in range(NT):
                dma("sp", out_d[t * 128:(t + 1) * 128, :], h[:, t, :], r=HK(t), is_output=True)
        else:
            rms_stats(list(range(1, NT)))
            nf = A.a([128, D])
            dma("sp", nf, big_d[:, 0, :].partition_broadcast(128), w=["nf"])
            ob = [A.a([128, D]) for _ in range(2)]
            for t in range(1, NT):
                o = ob[t % 2]
                stt(o, h[:, t, :], rstd[:, t:t + 1], nf, ALU.mult, ALU.mult, r=HK(t) + [("rstd", t), "nf"], w=[("ob", t % 2)])
                dma("sp", out_d[(t - 1) * 128:t * 128, :], o, r=[("ob", t % 2)], is_output=True)
        if REORDER:
            S.reorder()
        S.emit()
    return nc


_CACHE = {}


def kernel(**inputs):
    shared = pack_inputs(inputs)
    x = np.ascontiguousarray(np.asarray(inputs["x"], np.float32))
    nb = x.shape[0]
    if "nc" not in _CACHE:
        _CACHE["nc"] = build()
    nc = _CACHE["nc"]
    in_maps = [dict(shared, x=x[b]) for b in range(nb)]
    res = run_bass_kernel_spmd(nc, in_maps, core_ids=list(range(nb)))
    return np.stack([np.asarray(r["out"], np.float32) for r in res.results], axis=0)
```

```python
from contextlib import ExitStack
import numpy as np
import concourse.bass as bass
import concourse.mybir as mybir
from concourse.bass_utils import run_bass_kernel_spmd

F32 = mybir.dt.float32
BF16 = mybir.dt.bfloat16
AF = mybir.ActivationFunctionType
ALU = mybir.AluOpType
AX = mybir.AxisListType

COMPUTE = ("pe", "act", "dve", "pool")
NDSEM = 16
SAME_ENGINE_GAP = 1 << 30

T = 2176
NT = 17
PADN = 112
D = 1024
TB = [(0, 512), (512, 512), (1024, 512), (1536, 512), (2048, 128)]
EPS = 1e-6
MIX0_PARTS = ("lru", "ssd")
SSD_STOP = None
REORDER = True


class Sched:
    def __init__(self, nc):
        self.nc = nc
        self.ops = {e: [] for e in ("pe", "act", "dve", "pool", "sp")}
        self.ccount = {e: 0 for e in COMPUTE}
        self.dcount = {"sp": 0, "pool": 0}
        self.last_w = {}
        self.readers = {}
        self.sig = {e: set() for e in COMPUTE}
        self.out_dmas = []
        self.pending_bar = {}
        self.ps_last = {}
        self.epoch = 0

    def _deps(self, tok, r, w, eng):
        deps = set()
        for k in r:
            t = self.last_w.get(k)
            if t is not None:
                deps.add(t)
        for k in w:
            t = self.last_w.get(k)
            if t is not None:
                deps.add(t)
            for t in self.readers.get(k, ()):
                deps.add(t)
        for k in w:
            self.last_w[k] = tok
            self.readers[k] = []
        for k in r:
            if k in w:
                continue
            self.readers.setdefault(k, []).append(tok)
        for k in set(r) | set(w):
            if isinstance(k, tuple) and k[0] == "ps":
                d = self.ps_last.setdefault(k, {})
                for oe in list(d):
                    if oe != eng:
                        deps.update(d[oe])
                        d[oe] = []
                d.setdefault(eng, []).append(tok)
        bar = self.pending_bar.pop(eng) if eng in self.pending_bar else set()
        deps.discard(tok)
        return deps, bar

    def barrier(self):
        toks = set()
        for e in COMPUTE:
            if self.ccount[e] > 0:
                toks.add(("c", e, self.ccount[e] - 1))
        for q in ("sp", "pool"):
            for k in range(max(0, self.dcount[q] - NDSEM), self.dcount[q]):
                toks.add(("d", q, k))
        for e in ("pe", "act", "dve", "pool", "sp"):
            self.pending_bar[e] = set(toks) | self.pending_bar.get(e, set())
        self.epoch += 1

    def add(self, eng, fn, r=(), w=(), cost=0.3):
        seq = self.ccount[eng]
        self.ccount[eng] += 1
        tok = ("c", eng, seq)
        deps, bar = self._deps(tok, tuple(r), tuple(w), eng)
        self.ops[eng].append(["c", fn, deps, seq, cost, self.epoch, bar])
        return tok

    def reorder(self, window=192, lat=1.0):
        RE = ("pe", "act", "dve")
        fin = {}
        etime = {e: 0.0 for e in self.ops}
        new_order = {e: [] for e in RE}
        ptr = {e: 0 for e in self.ops}
        fence = 0.0
        tokof = lambda e, op: ("c", e, op[3]) if op[0] == "c" else ("d", e, op[3])
        for ep in range(self.epoch + 1):
            seg = {}
            for e in self.ops:
                lst = self.ops[e]
                i = ptr[e]
                j = i
                while j < len(lst) and lst[j][5] == ep:
                    j += 1
                seg[e] = lst[i:j]
                ptr[e] = j
            for e in seg:
                etime[e] = max(etime[e], fence)
            left = {e: list(seg[e]) for e in seg}
            total = sum(len(v) for v in left.values())
            while total:
                best = None
                for e, lst in left.items():
                    if not lst:
                        continue
                    cands = lst[:window] if e in RE else lst[:1]
                    for pos, op in enumerate(cands):
                        ready = 0.0
                        ok = True
                        for t in op[2]:
                            f = fin.get(t)
                            if f is None:
                                ok = False
                                break
                            if t[1] != e:
                                f += lat
                            if f > ready:
                                ready = f
                        if not ok:
                            continue
                        start = max(etime[e], ready)
                        key = (start, pos)
                        if best is None or key < best[0]:
                            best = (key, e, pos, op, start)
                        if start <= etime[e]:
                            break
                assert best is not None, "scheduler stuck"
                _, e, pos, op, start = best
                left[e].pop(pos)
                total -= 1
                if op[0] == "c":
                    etime[e] = start + op[4]
                    fin[tokof(e, op)] = etime[e]
                else:
                    etime[e] = start + 0.5
                    fin[tokof(e, op)] = start + op[4]
                if e in RE:
                    new_order[e].append(op)
            fence = max([fence] + list(etime.values()) + [fin[tokof(e, op)] for e in seg for op in seg[e]])
        remap = {}
        for e in RE:
            bars = {}
            for op in new_order[e]:
                if op[6]:
                    bars.setdefault(op[5], set()).update(op[6])
                    op[6] = set()
            seen_ep = set()
            for i, op in enumerate(new_order[e]):
                if op[5] not in seen_ep:
                    seen_ep.add(op[5])
                    op[6] = bars.get(op[5], set())
                remap[("c", e, op[3])] = ("c", e, i)
                op[3] = i
            self.ops[e] = new_order[e]
        for e, lst in self.ops.items():
            for op in lst:
                op[2] = {remap.get(t, t) for t in op[2]}
                op[6] = {remap.get(t, t) for t in op[6]}
        self.model_time = max(etime.values())

    def dma(self, q, fn, r=(), w=(), is_output=False, cost=3.0):
        k = self.dcount[q]
        self.dcount[q] += 1
        tok = ("d", q, k)
        deps, bar = self._deps(tok, tuple(r), tuple(w), q)
        self.ops[q].append(["d", fn, deps, k, cost, self.epoch, bar])
        if is_output:
            self.out_dmas.append(tok)
        return tok

    def emit(self):
        nc = self.nc
        for e, lst in self.ops.items():
            for op in lst:
                op[2] = set(op[2]) | set(op[6])
        for e, lst in self.ops.items():
            seen_c = {f: -1 for f in COMPUTE}
            for kind, fn, deps, seq, _c, _e, _b in lst:
                cw = {}
                for t in deps:
                    if t[0] != "c":
                        continue
                    f, s = t[1], t[2]
                    if f == e and kind == "c":
                        if e == "pe":
                            continue
                        if seq - s > SAME_ENGINE_GAP:
                            continue
                    if s <= seen_c[f]:
                        continue
                    cw[f] = max(cw.get(f, -1), s)
                for f, s in cw.items():
                    seen_c[f] = s
                    self.sig[f].add(s)
        sigidx = {}
        for e in COMPUTE:
            s = sorted(self.sig[e])
            sigidx[e] = {seq: i + 1 for i, seq in enumerate(s)}
        with ExitStack() as st:
            csem = {e: st.enter_context(nc.semaphore("c_" + e)) for e in COMPUTE}
            dsem = {q: [st.enter_context(nc.semaphore(f"d_{q}{i}")) for i in range(NDSEM)]
                    for q in ("sp", "pool")}
            block = st.enter_context(nc.Block())

            def run(e, eng):
                seen_c = {f: -1 for f in COMPUTE}
                seen_d = {}
                for kind, fn, deps, seq, _c, _e, _b in self.ops[e]:
                    cw = {}
                    for t in deps:
                        if t[0] == "c":
                            f, s = t[1], t[2]
                            if f == e and kind == "c":
                                if e == "pe":
                                    continue
                                if seq - s > SAME_ENGINE_GAP:
                                    continue
                            if s <= seen_c[f]:
                                continue
                            cw[f] = max(cw.get(f, -1), s)
                        else:
                            q, k = t[1], t[2]
                            key = (q, k % NDSEM)
                            val = 16 * (k // NDSEM + 1)
                            if seen_d.get(key, 0) >= val:
                                continue
                            seen_d[key] = val
                            eng.wait_ge(dsem[q][k % NDSEM], val)
                    for f, s in cw.items():
                        seen_c[f] = s
                        eng.wait_ge(csem[f], sigidx[f][s])
                    if kind == "c":
                        ins = fn(eng)
                        if seq in sigidx[e]:
                            ins.then_inc(csem[e], 1)
                    else:
                        k = seq
                        if k >= NDSEM:
                            key = (e, k % NDSEM)
                            val = 16 * (k // NDSEM)
                            if seen_d.get(key, 0) < val:
                                seen_d[key] = val
                                eng.wait_ge(dsem[e][k % NDSEM], val)
                        ins = fn(eng)
                        ins.then_inc(dsem[e][k % NDSEM], 16)
                if e == "sp":
                    for t in self.out_dmas:
                        q, k = t[1], t[2]
                        eng.wait_ge(dsem[q][k % NDSEM], 16 * (k // NDSEM + 1))

            @block.tensor
            def _(eng):
                run("pe", eng)

            @block.scalar
            def _(eng):
                run("act", eng)

            @block.vector
            def _(eng):
                run("dve", eng)

            @block.gpsimd
            def _(eng):
                run("pool", eng)

            @block.sync
            def _(eng):
                run("sp", eng)


COLP = {}
_o = 0
for _n, _c in [("mix_even", 8), ("ffn0", 8), ("ffn1", 8), ("mix_odd", 8), ("ssd_cw", 48), ("ssd_cb", 12),
               ("ssd_norm", 8), ("lru_cw", 32), ("lru_cb", 8), ("lru_ba", 8), ("lru_bx", 8), ("lru_lam", 8)]:
    COLP[_n] = (_o, _c)
    _o += _c
NCOL = _o
ROWP = {}
_o = 0
for _n, _c in [("dt_bias", 16), ("a_log", 16), ("ssd_d", 16), ("rb0", 20), ("rb1", 20), ("rc", 64)]:
    ROWP[_n] = (_o, _c)
    _o += _c
NROW = _o


def _fm(v):
    v = np.asarray(v, np.float32).reshape(-1, 128)
    return np.ascontiguousarray(v.T)


def _pool_band_consts():
    A = np.zeros((128, 12, 128), np.float32)
    s_ = np.arange(128)[:, None]
    t_ = np.arange(128)[None, :]
    for g, w in enumerate((2, 4, 8, 16)):
        inwin = ((t_ - s_) >= 0) & ((t_ - s_) < w)
        A[:, 3 * g + 0, :] = inwin / float(w) - (s_ == t_)
        A[:, 3 * g + 1, :] = (((t_ + 128 - s_) < w) & ((t_ + 128 - s_) >= 0)) / float(w)
        cnt = np.where(t_ >= PADN, np.minimum(t_ - PADN + 1, w), w).astype(np.float32)
        A[:, 3 * g + 2, :] = inwin / cnt - (s_ == t_)
    return A


def pack_inputs(inp):
    f = lambda a: np.ascontiguousarray(np.asarray(a, np.float32))
    colp = np.zeros((128, NCOL), np.float32)

    def put(name, arr):
        o, c = COLP[name]
        assert arr.shape == (128, c), (name, arr.shape)
        colp[:, o:o + c] = arr

    put("mix_even", _fm(inp["mix_norm_even"][0]))
    put("ffn0", _fm(inp["ffn_norm"][0]))
    put("ffn1", _fm(inp["ffn_norm"][1]))
    put("mix_odd", _fm(inp["mix_norm_odd"][0]))
    cw = np.asarray(inp["ssd_conv_w"][0], np.float32)
    put("ssd_cw", np.concatenate([_fm(cw[k]) for k in range(4)], axis=1).reshape(128, 4, 12).transpose(0, 2, 1).reshape(128, 48))
    put("ssd_cb", _fm(inp["ssd_conv_b"][0]))
    put("ssd_norm", _fm(inp["ssd_norm"][0]))
    lw = np.asarray(inp["lru_conv_w"][0], np.float32)
    put("lru_cw", np.concatenate([_fm(lw[k]) for k in range(4)], axis=1).reshape(128, 4, 8).transpose(0, 2, 1).reshape(128, 32))
    put("lru_cb", _fm(inp["lru_conv_b"][0]))
    put("lru_ba", _fm(inp["lru_b_a"][0]))
    put("lru_bx", _fm(inp["lru_b_x"][0]))
    put("lru_lam", _fm(inp["lru_lambda"][0]))

    rowp = np.zeros((1, NROW), np.float32)

    def putr(name, arr):
        o, c = ROWP[name]
        rowp[0, o:o + c] = np.asarray(arr, np.float32).reshape(-1)

    putr("dt_bias", inp["ssd_dt_bias"][0])
    putr("a_log", inp["ssd_a_log"][0])
    putr("ssd_d", inp["ssd_d"][0])
    putr("rb0", np.concatenate([np.asarray(inp["router_group_b"][0]), np.asarray(inp["router_expert_b"][0])]))
    putr("rb1", np.concatenate([np.asarray(inp["router_group_b"][1]), np.asarray(inp["router_expert_b"][1])]))
    rc = np.zeros((4, 16), np.float32)
    for g, w in enumerate((2, 4, 8, 16)):
        rc[g] = 1.0 / np.minimum(np.arange(16) + 1, w)
    putr("rc", rc)

    w_in = f(inp["w_in"][0])
    cols = np.concatenate([np.arange(0, 2560), np.arange(2576, 4624)])
    w_in_r = np.ascontiguousarray(w_in[:, cols].reshape(8, 128, 36, 128).transpose(2, 1, 0, 3))
    w_dt = np.ascontiguousarray(w_in[:, 2560:2576].reshape(8, 128, 16).transpose(1, 0, 2))
    wr = np.stack([np.concatenate([f(inp["router_group_w"][l]), f(inp["router_expert_w"][l])], axis=1)
                   .reshape(8, 128, 20).transpose(1, 0, 2) for l in range(2)])
    k_ = np.arange(128)[:, None]
    s_ = np.arange(128)[None, :]
    cst = np.stack([np.eye(128, dtype=np.float32), (k_ <= s_).astype(np.float32), (k_ > s_).astype(np.float32),
                    np.ones((128, 128), np.float32)], axis=1)
    shared = {
        "meta": f(inp["meta_tokens"]), "colp": colp, "rowp": rowp, "cst": np.ascontiguousarray(cst),
        "w_in_r": w_in_r, "w_dt": w_dt, "w_out": f(inp["w_out"][0]),
        "lru_wa": f(inp["lru_w_a"][0]), "lru_wx": f(inp["lru_w_x"][0]),
        "pool_w": f(inp["pool_w"][0]), "wr": np.ascontiguousarray(wr),
        "bigrow": np.ascontiguousarray(np.stack([f(inp["norm_final"]), f(inp["pool_b"][0]), f(inp["pool_scale"][0]),
                                                 f(inp["mix_norm_odd"][0])])[None]),
        "poolA": _pool_band_consts(),
        "wg": f(inp["expert_w_gate"]), "wu": f(inp["expert_w_up"]), "wd": f(inp["expert_w_down"]),
    }
    return shared


def build(phases=("mix0", "moe0", "mix1", "moe1"), dbg=False):
    nc = bass.Bass("TRN2", target_bir_lowering=False)
    dram = lambda n, s, kind="ExternalInput": nc.dram_tensor(n, list(s), F32, kind=kind).ap()
    x_d = dram("x", [2048, D])
    meta_d = dram("meta", [16, D])
    colp_d = dram("colp", [128, NCOL])
    rowp_d = dram("rowp", [1, NROW])
    cst_d = dram("cst", [128, 4, 128])
    w_in_d = dram("w_in_r", [36, 128, 8, 128])
    w_dt_d = dram("w_dt", [128, 8, 16])
    w_out_d = dram("w_out", [2048, D])
    lwa_d = dram("lru_wa", [16, 64, 64])
    lwx_d = dram("lru_wx", [16, 64, 64])
    pw_d = dram("pool_w", [4, 256, 256])
    wr_d = dram("wr", [2, 128, 8, 20])
    big_d = dram("bigrow", [1, 4, D])
    pA_d = dram("poolA", [128, 12, 128])
    wg_d = dram("wg", [2, 16, D, 512])
    wu_d = dram("wu", [2, 16, D, 512])
    wd_d = dram("wd", [2, 16, 512, D])
    if dbg:
        out_d = dram("out", [T, D], kind="ExternalOutput")
    else:
        out_d = dram("out", [2048, D], kind="ExternalOutput")

    S = Sched(nc)
    with ExitStack() as st:
        sb = lambda n, s, dt=F32: st.enter_context(nc.sbuf_tensor(n, list(s), dt))
        h = sb("h", [128, NT, D])
        hnT = sb("hnT", [128, 8, T], BF16)
        colp = sb("colp_s", [128, NCOL])
        rowp = sb("rowp_s", [128, NROW])
        cst = sb("cst_s", [128, 4, 128])
        cst16 = sb("cst16", [128, 4, 128], BF16)
        stat = sb("stat", [128, 64])
        AW = 25900
        arena = sb("arena", [128, AW])
        ps = [st.enter_context(nc.psum_tensor(f"ps{i}", [128, 512], F32)) for i in range(8)]
        ident = cst[:, 0, :]
        tri_le = cst[:, 1, :]
        u_gt = cst[:, 2, :]
        ones32 = cst[:, 3, :]
        ident16 = cst16[:, 0, :]
        ss = stat[:, 0:17]
        sq = stat[:, 17:34]
        rstd = stat[:, 34:51]

        class Arena:
            def __init__(self):
                self.off = 0

            def reset(self):
                self.off = 0
                S.barrier()

            def a(self, shape, dt=F32):
                n = int(np.prod(shape[1:]))
                words = n if dt == F32 else (n + 1) // 2
                assert self.off + words <= AW, ("arena overflow", self.off, words)
                v = arena[:, self.off:self.off + words]
                self.off += words
                if dt != F32:
                    v = v.bitcast(dt)
                    if v.shape[1] != n:
                        v = v[:, 0:n]
                if len(shape) == 3:
                    v = v.rearrange("p (a b) -> p a b", a=shape[1])
                elif len(shape) == 4:
                    v = v.rearrange("p (a b c) -> p a b c", a=shape[1], b=shape[2])
                return v

        A = Arena()
        colv = lambda name, i=0, n=None: colp[:, COLP[name][0] + i:COLP[name][0] + i + (n if n else 1)]
        rowv = lambda name: rowp[:, ROWP[name][0]:ROWP[name][0] + ROWP[name][1]]

        def fsz(ap):
            n = 1
            for d_ in ap.shape[1:]:
                n *= int(d_)
            return n

        def ecost(eng, n, mult=1.0):
            if eng == "dve":
                return 0.12 + mult * n / 960.0
            if eng == "act":
                return 0.25 + n / 1400.0
            return 2.0 + 0.015 * n

        def mm(out, lhsT, rhs, start, stop, r, w):
            c = max(fsz(rhs), 64) / 2400.0 + 0.03
            if lhsT.dtype == F32:
                c *= 4
            S.add("pe", lambda e: e.matmul(out, lhsT, rhs, start=start, stop=stop), r=r, w=w, cost=c)

        def tr(out, in_, idn, r, w):
            S.add("pe", lambda e: e.transpose(out, in_, idn), r=r, w=w, cost=0.3 if in_.dtype == F32 else 0.12)

        def act(out, in_, func, r, w, scale=1.0, bias=0.0, accum=None):
            c = ecost("act", fsz(in_)) + (0.1 if accum is not None else 0.0)
            if accum is None:
                S.add("act", lambda e: e.activation(out=out, in_=in_, func=func, scale=scale, bias=bias), r=r, w=w, cost=c)
            else:
                S.add("act", lambda e: e.activation(out=out, in_=in_, func=func, scale=scale, bias=bias, accum_out=accum), r=r, w=w, cost=c)

        def tt(eng, out, in0, in1, op, r, w):
            S.add(eng, lambda e: e.tensor_tensor(out=out, in0=in0, in1=in1, op=op), r=r, w=w, cost=ecost(eng, fsz(out)))

        def ts(eng, out, in0, s1, op0, r, w, s2=None, op1=None):
            c = ecost(eng, fsz(out))
            if op1 is None:
                S.add(eng, lambda e: e.tensor_scalar(out=out, in0=in0, scalar1=s1, scalar2=None, op0=op0), r=r, w=w, cost=c)
            else:
                S.add(eng, lambda e: e.tensor_scalar(out=out, in0=in0, scalar1=s1, scalar2=s2, op0=op0, op1=op1), r=r, w=w, cost=c)

        def stt(out, in0, scalar, in1, op0, op1, r, w):
            S.add("dve", lambda e: e.scalar_tensor_tensor(out=out, in0=in0, scalar=scalar, in1=in1, op0=op0, op1=op1), r=r, w=w,
                  cost=ecost("dve", fsz(out)))

        def cp(eng, out, in_, r, w):
            c = ecost(eng, fsz(out))
            if eng == "act":
                S.add("act", lambda e: e.copy(out, in_), r=r, w=w, cost=c)
            else:
                S.add(eng, lambda e: e.tensor_copy(out, in_), r=r, w=w, cost=c)

        def memset(eng, ap, val, w):
            S.add(eng, lambda e: e.memset(ap, val), w=w, cost=ecost(eng, fsz(ap)))

        def dma(q, out, in_, r=(), w=(), is_output=False):
            nbytes = fsz(out) * int(out.shape[0]) * 4
            S.dma(q, lambda e: e.dma_start(out=out, in_=in_), r=r, w=w, is_output=is_output, cost=2.5 + nbytes / 150e3)

        HK = lambda t: [("h", t, 0), ("h", t, 1)]

        dma("sp", colp[:], colp_d, w=["colp"])
        dma("sp", rowp[:], rowp_d.partition_broadcast(128), w=["rowp"])
        dma("sp", cst[:], cst_d, w=["cst"])
        dma("pool", cst16[:], cst_d, w=["cst16"])
        memset("dve", h[:, 0, :], 0.0, w=HK(0))
        dma("sp", h[PADN:128, 0, :], meta_d, w=HK(0))
        xr = x_d.rearrange("(t p) d -> p t d", p=128)
        for i in range(4):
            dma("sp", h[:, 1 + 4 * i:5 + 4 * i, :], xr[:, 4 * i:4 * i + 4, :], w=[k for t in range(1 + 4 * i, 5 + 4 * i) for k in HK(t)])

        def rms_stats(tiles):
            junk = A.a([128, D], BF16)
            for t in tiles:
                act(junk, h[:, t, :], AF.Square, r=HK(t), w=[("ss", t), "junk"], accum=ss[:, t:t + 1])
                act(sq[:, t:t + 1], ss[:, t:t + 1], AF.Sqrt, r=[("ss", t)], w=[("sq", t)], scale=1.0 / D, bias=colv_eps)
                S.add("dve", lambda e, t=t: e.reciprocal(rstd[:, t:t + 1], sq[:, t:t + 1]), r=[("sq", t)], w=[("rstd", t)], cost=0.15)

        def normT(gname, router_l=None, logits=None, wr_s=None, consume=None):
            xsb = [A.a([128, D]) for _ in range(2)]
            t32 = [A.a([128, 4, 128]) for _ in range(4)]
            pend = None
            for t in range(NT):
                xs = xsb[t % 2]
                act(xs, h[:, t, :], AF.Identity, r=HK(t) + [("rstd", t)], w=[("xs", t % 2)], scale=rstd[:, t:t + 1])
                for half in range(2):
                    i2 = (2 * t + half) % 2
                    i4 = (2 * t + half) % 4
                    bank = ps[6 + i2]
                    for kk in range(4):
                        k = half * 4 + kk
                        tr(bank[:, kk * 128:(kk + 1) * 128], xs[:, k * 128:(k + 1) * 128], ident, r=[("xs", t % 2), "cst"], w=[("ps", 6 + i2)])
                    g0 = COLP[gname][0] + half * 4
                    tt("dve", t32[i4], bank[:, :].rearrange("p (a b) -> p a b", a=4),
                       colp[:, g0:g0 + 4].unsqueeze(2).to_broadcast([128, 4, 128]), ALU.mult,
                       r=[("ps", 6 + i2), "colp"], w=[("t32", i4)])
                    cp("act", hnT[:, half * 4:half * 4 + 4, t * 128:(t + 1) * 128], t32[i4], r=[("t32", i4)], w=[("hnT", t)])
                if router_l is not None:
                    if pend is not None:
                        pend()

                    def mk(t=t):
                        def go():
                            for k in range(8):
                                i4 = (2 * t + k // 4) % 4
                                mm(ps[5][:, 0:20], t32[i4][:, k % 4, :], wr_s[:, k, :], k == 0, k == 7,
                                   r=[("t32", i4), "wr"], w=[("ps", 5)])
                            cp("act", logits[:, t, :], ps[5][:, 0:20], r=[("ps", 5)], w=["logits"])
                        return go
                    pend = mk()
            if pend is not None:
                pend()

        memset("pool", stat[:, 60:61], EPS, w=["eps"])
        colv_eps = stat[:, 60:61]

        def moe(l):
            A.reset()
            wr_s = A.a([128, 8, 20])
            logits = A.a([128, NT, 20])
            gates = A.a([128, NT, 16])
            dma("sp", wr_s, wr_d[l], w=["wr"])
            Wg = [A.a([128, 8, 512], BF16) for _ in range(2)]
            Wu = [A.a([128, 8, 512], BF16) for _ in range(2)]
            Wd = [A.a([128, 4, D], BF16) for _ in range(2)]

            def load_w(e):
                b = e % 2
                if e == 0:
                    for fc in range(4):
                        fs = slice(fc * 128, (fc + 1) * 128)
                        dma("pool", Wg[b][:, :, fs], wg_d[l, e][:, fs].rearrange("(k p) f -> p k f", p=128), w=[("Wg", b, fc)])
                        dma("pool", Wu[b][:, :, fs], wu_d[l, e][:, fs].rearrange("(k p) f -> p k f", p=128), w=[("Wu", b, fc)])
                else:
                    dma("pool", Wg[b], wg_d[l, e].rearrange("(k p) f -> p k f", p=128), w=[("Wg", b, fc) for fc in range(4)])
                    dma("pool", Wu[b], wu_d[l, e].rearrange("(k p) f -> p k f", p=128), w=[("Wu", b, fc) for fc in range(4)])
                dma("pool", Wd[b], wd_d[l, e].rearrange("(k p) f -> p k f", p=128), w=[("Wd", b)])

            load_w(0)
            load_w(1)
            mark = A.off
            rms_stats(list(range(NT)))
            normT("ffn%d" % l, router_l=l, logits=logits, wr_s=wr_s)
            R = lambda shape: A.a(shape)
            rb = rowv("rb%d" % l)
            lg = R([128, NT, 20])
            tt("dve", lg, logits, rb.unsqueeze(1).to_broadcast([128, NT, 20]), ALU.add, r=["logits", "rowp"], w=["lg"])
            m4 = R([128, NT])
            S.add("dve", lambda e: e.tensor_reduce(out=m4, in_=lg[:, :, 0:4], axis=AX.X, op=ALU.max), r=["lg"], w=["m4"])
            d4 = R([128, NT, 4])
            tt("dve", d4, lg[:, :, 0:4], m4.unsqueeze(2).to_broadcast([128, NT, 4]), ALU.subtract, r=["lg", "m4"], w=["d4"])
            mg = R([128, NT, 4])
            ts("dve", mg, d4, 0.0, ALU.is_ge, r=["d4"], w=["mg"])
            e4 = R([128, NT, 4])
            act(e4, d4, AF.Exp, r=["d4"], w=["e4"])
            s4 = R([128, NT])
            S.add("dve", lambda e: e.tensor_reduce(out=s4, in_=e4, axis=AX.X, op=ALU.add), r=["e4"], w=["s4"])
            le = lg[:, :, 4:20].rearrange("p t (g j) -> p t g j", g=4)
            ml = R([128, NT, 4, 4])
            tt("dve", ml, le, mg.unsqueeze(3).to_broadcast([128, NT, 4, 4]), ALU.mult, r=["lg", "mg"], w=["ml"])
            sel = R([128, NT, 4])
            tt("dve", sel, ml[:, :, 0, :], ml[:, :, 1, :], ALU.add, r=["ml"], w=["sel"])
            tt("dve", sel, sel, ml[:, :, 2, :], ALU.add, r=["ml", "sel"], w=["sel"])
            tt("dve", sel, sel, ml[:, :, 3, :], ALU.add, r=["ml", "sel"], w=["sel"])
            m1 = R([128, NT])
            S.add("dve", lambda e: e.tensor_reduce(out=m1, in_=sel, axis=AX.X, op=ALU.max), r=["sel"], w=["m1"])
            k1 = R([128, NT, 4])
            tt("dve", k1, sel, m1.unsqueeze(2).to_broadcast([128, NT, 4]), ALU.is_ge, r=["sel", "m1"], w=["k1"])
            sel2 = R([128, NT, 4])
            stt(sel2, k1, -1e30, sel, ALU.mult, ALU.add, r=["k1", "sel"], w=["sel2"])
            m2 = R([128, NT])
            S.add("dve", lambda e: e.tensor_reduce(out=m2, in_=sel2, axis=AX.X, op=ALU.max), r=["sel2"], w=["m2"])
            k2 = R([128, NT, 4])
            tt("dve", k2, sel2, m2.unsqueeze(2).to_broadcast([128, NT, 4]), ALU.is_ge, r=["sel2", "m2"], w=["k2"])
            dd = R([128, NT])
            tt("dve", dd, m2, m1, ALU.subtract, r=["m1", "m2"], w=["dd"])
            w2 = R([128, NT])
            act(w2, dd, AF.Exp, r=["dd"], w=["w2"])
            den = R([128, NT])
            stt(den, w2, 1.0, s4, ALU.add, ALU.mult, r=["w2", "s4"], w=["den"])
            g1 = R([128, NT])
            S.add("dve", lambda e: e.reciprocal(g1, den), r=["den"], w=["g1"])
            g2 = R([128, NT])
            tt("dve", g2, g1, w2, ALU.mult, r=["g1", "w2"], w=["g2"])
            gs = R([128, NT, 4])
            tt("dve", gs, k1, g1.unsqueeze(2).to_broadcast([128, NT, 4]), ALU.mult, r=["k1", "g1"], w=["gs"])
            gs2 = R([128, NT, 4])
            tt("dve", gs2, k2, g2.unsqueeze(2).to_broadcast([128, NT, 4]), ALU.mult, r=["k2", "g2"], w=["gs2"])
            tt("dve", gs, gs, gs2, ALU.add, r=["gs", "gs2"], w=["gs"])
            g4 = gates.rearrange("p t (g j) -> p t g j", g=4)
            for g in range(4):
                tt("dve", g4[:, :, g, :], gs, mg[:, :, g:g + 1].to_broadcast([128, NT, 4]), ALU.mult, r=["gs", "mg"], w=["gates"])

            hid = [A.a([128, 4, 512], BF16) for _ in range(2)]
            sg = [A.a([128, 512]) for _ in range(2)]
            cnt = 0
            cnt_o = [0]
            blk = 0
            pend = None
            for e in range(16):
                b = e % 2
                if e >= 2:
                    load_w(e)
                for (t0, n) in TB:
                    hb = blk % 2
                    blk += 1
                    hk = [("hnT", tt_) for tt_ in range(t0 // 128, (t0 + n) // 128)]
                    for fc in range(4):
                        i = cnt % 2
                        cnt += 1
                        pg, pu = ps[i], ps[2 + i]
                        for k in range(8):
                            mm(pg[:, 0:n], Wg[b][:, k, fc * 128:(fc + 1) * 128], hnT[:, k, t0:t0 + n], k == 0, k == 7,
                               r=[("Wg", b, fc)] + hk, w=[("ps", i)])
                        for k in range(8):
                            mm(pu[:, 0:n], Wu[b][:, k, fc * 128:(fc + 1) * 128], hnT[:, k, t0:t0 + n], k == 0, k == 7,
                               r=[("Wu", b, fc)] + hk, w=[("ps", 2 + i)])
                        act(sg[i][:, 0:n], pg[:, 0:n], AF.Silu, r=[("ps", i)], w=[("sg", i)])
                        tt("dve", hid[hb][:, fc, 0:n], sg[i][:, 0:n], pu[:, 0:n], ALU.mult, r=[("sg", i), ("ps", 2 + i)], w=[("hid", hb)])
                    if pend is not None:
                        pend()

                    def mk(e=e, b=b, hb=hb, t0=t0, n=n):
                        def go():
                            for tl in range(n // 128):
                                t = t0 // 128 + tl
                                for dh in range(2):
                                    io = 4 + cnt_o[0] % 2
                                    cnt_o[0] += 1
                                    for fc in range(4):
                                        mm(ps[io][:, :], hid[hb][:, fc, tl * 128:(tl + 1) * 128], Wd[b][:, fc, dh * 512:(dh + 1) * 512],
                                           fc == 0, fc == 3, r=[("hid", hb), ("Wd", b)], w=[("ps", io)])
                                    hv = h[:, t, dh * 512:(dh + 1) * 512]
                                    stt(hv, ps[io][:, :], gates[:, t, e:e + 1], hv,
                                        ALU.mult, ALU.add, r=[("ps", io), "gates", ("h", t, dh)], w=[("h", t, dh)])
                        return go
                    pend = mk()
            pend()

        def mix1():
            A.reset()
            memset("pool", h[0:PADN, 0, :], 0.0, w=HK(0))
            rms_stats(list(range(NT)))
            pw = A.a([128, 4, 2, 256], BF16)
            for g in range(4):
                dma("pool", pw[:, g, :, :], pw_d[g].rearrange("(c p) j -> p c j", p=128), w=["pw"])
            pA = A.a([128, 12, 128], BF16)
            dma("pool", pA, pA_d, w=["pA"])
            pb_bc = A.a([128, D])
            sc_bc = A.a([128, D])
            gn_bc = A.a([128, D])
            dma("sp", pb_bc, big_d[:, 1, :].partition_broadcast(128), w=["pb_bc"])
            dma("sp", sc_bc, big_d[:, 2, :].partition_broadcast(128), w=["sc_bc"])
            dma("sp", gn_bc, big_d[:, 3, :].partition_broadcast(128), w=["gn_bc"])
            bs_bc = A.a([128, D])
            tt("dve", bs_bc, pb_bc, sc_bc, ALU.mult, r=["pb_bc", "sc_bc"], w=["bs_bc"])
            bs16 = A.a([128, D], BF16)
            cp("dve", bs16, bs_bc, r=["bs_bc"], w=["bs16"])
            pws = A.a([128, 4, 2, 256], BF16)
            for g in range(4):
                tt("dve", pws[:, g, :, :], pw[:, g, :, :], sc_bc[:, g * 256:(g + 1) * 256].unsqueeze(1).to_broadcast([128, 2, 256]), ALU.mult,
                   r=["pw", "sc_bc"], w=["pws"])
            xsn = A.a([128, NT, D], BF16)
            for t in range(NT):
                stt(xsn[:, t, :], h[:, t, :], rstd[:, t:t + 1], gn_bc, ALU.mult, ALU.mult, r=HK(t) + [("rstd", t), "gn_bc"], w=[("xsn", t)])
            pooledT = A.a([128, 8, T], BF16)
            ev = 0
            for k in range(8):
                g = k // 2
                for q in range(5):
                    tiles = list(range(4 * q, min(4 * q + 4, NT)))
                    bi_ = 6 + (k * 5 + q) % 2
                    bank = ps[bi_]
                    for i, t in enumerate(tiles):
                        o = bank[:, i * 128:(i + 1) * 128]
                        if t == 0:
                            mm(o, xsn[:, 0, k * 128:(k + 1) * 128], pA[:, 3 * g + 2, :], True, True, r=[("xsn", 0), "pA"], w=[("ps", bi_)])
                        else:
                            mm(o, xsn[:, t - 1, k * 128:(k + 1) * 128], pA[:, 3 * g + 1, :], True, False, r=[("xsn", t - 1), "pA"], w=[("ps", bi_)])
                            mm(o, xsn[:, t, k * 128:(k + 1) * 128], pA[:, 3 * g + 0, :], False, True, r=[("xsn", t), "pA"], w=[("ps", bi_)])
                    nn = len(tiles) * 128
                    cp("act" if ev % 2 == 0 else "dve", pooledT[:, k, q * 512:q * 512 + nn], bank[:, 0:nn], r=[("ps", bi_)], w=[("pooledT", k)])
                    ev += 1
            for t in range(NT):
                for dh in range(2):
                    bi_ = dh + 2 * (t % 2)
                    bank = ps[bi_]
                    for gg in range(2):
                        g = dh * 2 + gg
                        o = bank[:, gg * 256:(gg + 1) * 256]
                        for ic in range(2):
                            mm(o, pooledT[:, 2 * g + ic, t * 128:(t + 1) * 128], pws[:, g, ic, :], ic == 0, False,
                               r=[("pooledT", 2 * g + ic), "pws"], w=[("ps", bi_)])
                        mm(o, cst16[0:1, 3, :], bs16[0:1, g * 256:(g + 1) * 256], False, True, r=["cst16", "bs16"], w=[("ps", bi_)])
                    hv = h[:, t, dh * 512:(dh + 1) * 512]
                    tt("dve", hv, hv, bank[:, :], ALU.add, r=[("ps", bi_), ("h", t, dh)], w=[("h", t, dh)])

        def mix0():
            A.reset()
            rms_stats(list(range(NT)))
            normT("mix_even")
            A.reset()
            hnk = [("hnT", t) for t in range(NT)]
            wbuf = [A.a([128, 8, 128], BF16) for _ in range(3)]
            wcnt = [0]

            def load_wchunk(cc):
                i = wcnt[0] % 3
                wcnt[0] += 1
                dma("pool", wbuf[i], w_in_d[cc], w=[("wbuf", i)])
                return i

            def proj_blk(wi, bi, t0, n):
                bk = ("ps", bi % 2)
                bank = ps[bi % 2]
                for k in range(8):
                    mm(bank[:, 0:n], wbuf[wi][:, k, :], hnT[:, k, t0:t0 + n], k == 0, k == 7, r=[("wbuf", wi)] + hnk, w=[bk])
                return bank, bk

            xinb = [A.a([128, 3 + 512]) for _ in range(2)]
            ctb = [A.a([128, 512]) for _ in range(2)]
            cvc = [0]

            def conv_blk(bank, bk, bi, n, wname, bname, cidx, tap0_act=False, copy_eng="act"):
                i = cvc[0] % 2
                cvc[0] += 1
                xi, xk = xinb[i], ("xinb", i)
                if bi == 0:
                    memset("dve", xi[:, 0:3], 0.0, w=[xk])
                else:
                    pv = xinb[1 - i]
                    cp("dve", xi[:, 0:3], pv[:, 512:515], r=[("xinb", 1 - i)], w=[xk])
                cp(copy_eng, xi[:, 3:3 + n], bank[:, 0:n], r=[bk], w=[xk])
                ct, ck = ctb[i], ("ctb", i)
                o4 = COLP[wname][0] + 4 * cidx
                if tap0_act:
                    act(ct[:, 0:n], xi[:, 0:n], AF.Identity, r=[xk, "colp"], w=[ck], scale=colp[:, o4:o4 + 1], bias=colv(bname, cidx))
                else:
                    ts("dve", ct[:, 0:n], xi[:, 0:n], colp[:, o4:o4 + 1], ALU.mult, r=[xk, "colp"], w=[ck], s2=colv(bname, cidx), op1=ALU.add)
                for k in range(1, 4):
                    stt(ct[:, 0:n], xi[:, k:k + n], colp[:, o4 + k:o4 + k + 1], ct[:, 0:n], ALU.mult, ALU.add, r=[xk, ck, "colp"], w=[ck])
                return ct, ck

            wo = A.a([128, 4, D], BF16)
            ybuf = A.a([128, 4, T], BF16)

            def out_proj(kc0, scale_ap_fn):
                dma("pool", wo, w_out_d[kc0 * 128:(kc0 + 4) * 128, :].rearrange("(k p) d -> p k d", p=128), w=["wo"])
                cnt = 0
                for t in range(NT):
                    for dh in range(2):
                        io = 4 + cnt % 2
                        cnt += 1
                        for kk in range(4):
                            mm(ps[io][:, :], ybuf[:, kk, t * 128:(t + 1) * 128], wo[:, kk, dh * 512:(dh + 1) * 512], kk == 0, kk == 3,
                               r=[("ybuf", kk), "wo"], w=[("ps", io)])
                        hv = h[:, t, dh * 512:(dh + 1) * 512]
                        if scale_ap_fn is None:
                            tt("dve", hv, hv, ps[io][:, :], ALU.add, r=[("ps", io), ("h", t, dh)], w=[("h", t, dh)])
                        else:
                            stt(hv, ps[io][:, :], scale_ap_fn(t), hv, ALU.mult, ALU.add, r=[("ps", io), ("h", t, dh), "rstdg"], w=[("h", t, dh)])

            mark = A.off

            def lru():
                bdA = A.a([128, 8, 128], BF16)
                bdX = A.a([128, 8, 128], BF16)
                memset("dve", bdA, 0.0, w=["bdA"])
                memset("dve", bdX, 0.0, w=["bdX"])
                for src, dst, nm in ((lwa_d, bdA, "bdA"), (lwx_d, bdX, "bdX")):
                    v = src.rearrange("(j two) i o -> two i j o", two=2)
                    dma("pool", dst[0:64, :, 0:64], v[0], w=[nm])
                    dma("pool", dst[64:128, :, 64:128], v[1], w=[nm])
                c1 = A.a([128, 8])
                tmpc = A.a([128, 8])
                act(tmpc, colv("lru_lam", 0, 8), AF.Exp, r=["colp"], w=["tmpc"], scale=-1.0)
                act(tmpc, tmpc, AF.Ln, r=["tmpc"], w=["tmpc"], bias=1.0)
                ts("dve", c1, tmpc, -8.0, ALU.mult, r=["tmpc"], w=["c1"])
                Bn = lambda n_: [A.a([128, 512]) for _ in range(n_)]
                gl, rr, ii, av, uv, hl = Bn(4), Bn(4), Bn(4), Bn(4), Bn(2), Bn(2)
                xb16 = [A.a([128, 512], BF16) for _ in range(4)]
                NB = len(TB)
                items = [(j, bi) for j in range(8) for bi in range(NB)]
                st1 = {}

                def S1(idx):
                    j, bi = items[idx]
                    t0, n = TB[bi]
                    if bi == 0:
                        st1[j] = (load_wchunk(28 + j), load_wchunk(20 + j))
                    wi_in, wi_gt = st1[j]
                    bank, bk = proj_blk(wi_in, 2 * idx, t0, n)
                    xb, xbk = conv_blk(bank, bk, bi, n, "lru_cw", "lru_cb", j, copy_eng="dve")
                    bank2, bk2 = proj_blk(wi_gt, 2 * idx + 1, t0, n)
                    g3 = idx % 4
                    act(gl[g3][:, 0:n], bank2[:, 0:n], AF.Gelu_apprx_tanh, r=[bk2], w=[("gl", g3)])
                    cp("act", xb16[g3][:, 0:n], xb[:, 0:n], r=[xbk], w=[("xb16", g3)])
                    st1[idx, "xb"] = (xb, xbk)

                def S2(idx):
                    j, bi = items[idx]
                    t0, n = TB[bi]
                    ip = idx % 2
                    q = idx % 4
                    K = lambda nm: (nm, q)
                    xb, xbk = st1.pop((idx, "xb"))
                    pa, px = ps[2 + ip], ps[4 + ip]
                    mm(pa[:, 0:n], bdA[:, j, :], xb16[q][:, 0:n], True, True, r=["bdA", K("xb16")], w=[("ps", 2 + ip)])
                    mm(px[:, 0:n], bdX[:, j, :], xb16[q][:, 0:n], True, True, r=["bdX", K("xb16")], w=[("ps", 4 + ip)])
                    r_, i_ = rr[q], ii[q]
                    act(r_[:, 0:n], pa[:, 0:n], AF.Sigmoid, r=[("ps", 2 + ip), "colp"], w=[K("rr")], bias=colv("lru_ba", j))
                    act(i_[:, 0:n], px[:, 0:n], AF.Sigmoid, r=[("ps", 4 + ip), "colp"], w=[K("ii")], bias=colv("lru_bx", j))
                    act(av[q][:, 0:n], r_[:, 0:n], AF.Exp, r=[K("rr"), "c1"], w=[K("av")], scale=c1[:, j:j + 1])
                    stt(r_[:, 0:n], av[q][:, 0:n], 0.9999998, av[q][:, 0:n], ALU.min, ALU.mult, r=[K("av")], w=[K("rr")])
                    act(r_[:, 0:n], r_[:, 0:n], AF.Sqrt, r=[K("rr")], w=[K("rr")], scale=-1.0, bias=1.0)
                    tt("dve", i_[:, 0:n], i_[:, 0:n], xb[:, 0:n], ALU.mult, r=[K("ii"), xbk], w=[K("ii")])
                    if bi == 0:
                        memset("dve", r_[:, PADN:PADN + 1], 1.0, w=[K("rr")])

                def S3(idx):
                    j, bi = items[idx]
                    t0, n = TB[bi]
                    i = idx % 2
                    q = idx % 4
                    jj = j % 4
                    K = lambda nm: (nm, q)
                    tt("dve", uv[i][:, 0:n], rr[q][:, 0:n], ii[q][:, 0:n], ALU.mult, r=[K("rr"), K("ii")], w=[("uv", i)])
                    if bi == 0:
                        memset("dve", hl[i][:, 0:PADN], 0.0, w=[("hl", i)])
                        S.add("dve", lambda e, i=i, q=q, n=n: e.tensor_tensor_scan(out=hl[i][:, PADN:n], data0=av[q][:, PADN:n], data1=uv[i][:, PADN:n],
                                                                             initial=0.0, op0=ALU.mult, op1=ALU.add),
                              r=[K("av"), ("uv", i)], w=[("hl", i)], cost=1.2)
                    else:
                        S.add("dve", lambda e, i=i, q=q, n=n: e.tensor_tensor_scan(out=hl[i][:, 0:n], data0=av[q][:, 0:n], data1=uv[i][:, 0:n],
                                                                             initial=hl[1 - i][:, 511:512], op0=ALU.mult, op1=ALU.add),
                              r=[K("av"), ("uv", i), ("hl", 1 - i)], w=[("hl", i)], cost=1.2)
                    tt("dve", ybuf[:, jj, t0:t0 + n], hl[i][:, 0:n], gl[q][:, 0:n], ALU.mult, r=[("hl", i), ("gl", q)], w=[("ybuf", jj)])
                    if bi == NB - 1 and jj == 3:
                        out_proj(8 + (j // 4) * 4, None)

                NI = len(items)
                for step in range(NI + 2):
                    if step < NI:
                        S1(step)
                    if 0 <= step - 1 < NI:
                        S2(step - 1)
                    if 0 <= step - 2 < NI:
                        S3(step - 2)

            def ssd():
                dt = A.a([128, NT, 16])
                adt = A.a([128, NT, 16])
                ea = A.a([128, NT, 16])
                eatot = A.a([128, NT, 16])
                dte = A.a([128, NT, 16])
                Aneg = A.a([128, 16])
                wdt = A.a([128, 8, 16], BF16)
                dma("pool", wdt, w_dt_d, w=["wdt"])
                for t in range(NT):
                    for k in range(8):
                        mm(ps[7][:, t * 16:(t + 1) * 16], hnT[:, k, t * 128:(t + 1) * 128], wdt[:, k, :], k == 0, k == 7,
                           r=["wdt", ("hnT", t)], w=[("ps", 7)])
                p7 = ps[7][:, 0:NT * 16].rearrange("p (t h) -> p t h", t=NT)
                tt("dve", dt, p7, rowv("dt_bias").unsqueeze(1).to_broadcast([128, NT, 16]), ALU.add, r=[("ps", 7), "rowp"], w=["dt"])
                act(dte, dt, AF.Abs, r=["dt"], w=["dte"])
                act(dte, dte, AF.Exp, r=["dte"], w=["dte"], scale=-1.0)
                act(dte, dte, AF.Ln, r=["dte"], w=["dte"], bias=1.0)
                stt(dt, dt, 0.0, dte, ALU.max, ALU.add, r=["dt", "dte"], w=["dt"])
                memset("dve", dt[0:PADN, 0, :], 0.0, w=["dt"])
                act(Aneg, rowv("a_log"), AF.Exp, r=["rowp"], w=["Aneg"])
                stt(adt, dt, -1.0, Aneg.unsqueeze(1).to_broadcast([128, NT, 16]), ALU.mult, ALU.mult, r=["dt", "Aneg"], w=["adt"])
                for t in range(NT):
                    mm(ps[6][:, t * 16:(t + 1) * 16], tri_le, adt[:, t, :], True, True, r=["cst", "adt"], w=[("ps", 6)])
                for t in range(NT):
                    mm(ps[7][:, t * 16:(t + 1) * 16], ones32, adt[:, t, :], True, True, r=["cst", "adt"], w=[("ps", 7)])
                p6 = ps[6][:, 0:NT * 16].rearrange("p (t h) -> p t h", t=NT)
                cp("dve", ea, p6, r=[("ps", 6)], w=["ea"])
                cp("dve", eatot, p7, r=[("ps", 7)], w=["eatot"])
                tt("dve", dte, eatot, ea, ALU.subtract, r=["eatot", "ea"], w=["dte"])
                act(dte, dte, AF.Exp, r=["dte"], w=["dte"])
                act(ea, ea, AF.Exp, r=["ea", "dte"], w=["ea"])
                act(eatot, eatot, AF.Exp, r=["eatot", "dte"], w=["eatot"])
                Dbc = rowv("ssd_d")
                DI = A.a([128, 2, 128], BF16)
                if SSD_STOP == "dt":
                    return

                BT = A.a([128, T], BF16)
                CT = A.a([128, T], BF16)
                Btok = A.a([128, NT, 128], BF16)
                CBm = A.a([128, NT, 128], BF16)
                xT16 = [A.a([128, 512], BF16) for _ in range(2)]
                xtok = A.a([128, NT, 128], BF16)
                xdt = A.a([128, NT, 128], BF16)
                sz = A.a([128, NT, 128], BF16)
                MT_all = A.a([128, NT, 256], BF16)
                S_all = A.a([128, NT, 128], BF16)
                ssp = A.a([128, NT, 4])
                rstdg = A.a([128, NT])
                Sst = A.a([128, 128])
                Lb = [A.a([128, 256]) for _ in range(2)]
                Db = [A.a([128, 256]) for _ in range(2)]
                xdte = [A.a([128, 128], BF16) for _ in range(4)]
                ytmp = [A.a([128, 128]) for _ in range(4)]
                yg16 = [A.a([128, 128], BF16) for _ in range(4)]
                junk = A.a([128, 128], BF16)

                def bank16(i):
                    return ps[i][:, 0:256].bitcast(BF16)

                def fm_chunk(cc, cidx, sink):
                    wi = load_wchunk(cc)
                    for bi, (t0, n) in enumerate(TB):
                        bank, bk = proj_blk(wi, bi, t0, n)
                        ct, ck = conv_blk(bank, bk, bi, n, "ssd_cw", "ssd_cb", cidx, tap0_act=True)
                        sink(bi, t0, n, ct, ck)

                for g in range(2):
                    pass
                    fm_chunk(8 + 8 + g, 8 + g, lambda bi, t0, n, ct, ck: act(BT[:, t0:t0 + n], ct[:, 0:n], AF.Silu, r=[ck], w=["BT"]))
                    fm_chunk(8 + 10 + g, 10 + g, lambda bi, t0, n, ct, ck: act(CT[:, t0:t0 + n], ct[:, 0:n], AF.Silu, r=[ck], w=["CT"]))
                    for q in range(5):
                        tiles = list(range(4 * q, min(4 * q + 4, NT)))
                        nn = len(tiles)
                        bk = bank16(6 + q % 2)
                        for i, t in enumerate(tiles):
                            tr(bk[:, i * 128:(i + 1) * 128], BT[:, t * 128:(t + 1) * 128], ident16, r=["BT", "cst16"], w=[("ps", 6 + q % 2)])
                        cp("dve", Btok[:, 4 * q:4 * q + nn, :], bk[:, 0:nn * 128].rearrange("p (a b) -> p a b", a=nn), r=[("ps", 6 + q % 2)], w=["Btok"])
                        bank = ps[2 + q % 2]
                        for i, t in enumerate(tiles):
                            mm(bank[:, i * 128:(i + 1) * 128], BT[:, t * 128:(t + 1) * 128], CT[:, t * 128:(t + 1) * 128], True, True,
                               r=["BT", "CT"], w=[("ps", 2 + q % 2)])
                        tt("dve", CBm[:, 4 * q:4 * q + nn, :], bank[:, 0:nn * 128].rearrange("p (a b) -> p a b", a=nn),
                           tri_le.unsqueeze(1).to_broadcast([128, nn, 128]), ALU.mult, r=[("ps", 2 + q % 2), "cst"], w=["CBm"])
                    pass
                    if SSD_STOP == "bc":
                        return
                    for jj in range(4):
                        j = g * 4 + jj
                        hh0 = 2 * j

                        def xsink(bi, t0, n, ct, ck, hh0=hh0):
                            xt = xT16[bi % 2]
                            xk = ("xT16", bi % 2)
                            act(xt[:, 0:n], ct[:, 0:n], AF.Silu, r=[ck], w=[xk])
                            nn = n // 128
                            bk = bank16(6 + bi % 2)
                            for i in range(nn):
                                tr(bk[:, i * 128:(i + 1) * 128], xt[:, i * 128:(i + 1) * 128], ident16, r=[xk, "cst16"], w=[("ps", 6 + bi % 2)])
                            a0 = t0 // 128
                            cp("act", xtok.rearrange("p a b -> p (a b)")[:, a0 * 128:(a0 + nn) * 128], bk[:, 0:nn * 128], r=[("ps", 6 + bi % 2)], w=[("xtok", bi)])
                            for i in range(nn):
                                t = a0 + i
                                tt("dve", xdt[:, t, :].rearrange("p (h c) -> p h c", h=2), xtok[:, t, :].rearrange("p (h c) -> p h c", h=2),
                                   dt[:, t, hh0:hh0 + 2].unsqueeze(2).to_broadcast([128, 2, 64]), ALU.mult, r=[("xtok", bi), "dt"], w=[("xdt", t)])
                        fm_chunk(8 + j, j, xsink)
                        if SSD_STOP == "x":
                            return
                        wz = load_wchunk(j)
                        for bi, (t0, n) in enumerate(TB):
                            bank, bk_ = proj_blk(wz, bi, t0, n)
                            zt = xT16[bi % 2]
                            zk = ("xT16", bi % 2)
                            act(zt[:, 0:n], bank[:, 0:n], AF.Silu, r=[bk_], w=[zk])
                            nn = n // 128
                            bk = bank16(6 + bi % 2)
                            for i in range(nn):
                                tr(bk[:, i * 128:(i + 1) * 128], zt[:, i * 128:(i + 1) * 128], ident16, r=[zk, "cst16"], w=[("ps", 6 + bi % 2)])
                            a0 = t0 // 128
                            cp("dve", sz.rearrange("p a b -> p (a b)")[:, a0 * 128:(a0 + nn) * 128], bk[:, 0:nn * 128], r=[("ps", 6 + bi % 2)], w=[("sz", bi)])
                        if SSD_STOP == "z":
                            return
                        memset("dve", Sst, 0.0, w=["Sst"])
                        memset("dve", S_all[:, 0, :], 0.0, w=[("S_all", 0)])
                        for hh in range(2):
                            act(DI[:, hh, :], ident, AF.Identity, r=["cst", "rowp"], w=["DI"], scale=Dbc[:, hh0 + hh:hh0 + hh + 1])
                        for c in range(NT):
                            i2 = c % 2
                            i4 = c % 4
                            for hh in range(2):
                                act(Lb[i2][:, hh * 128:(hh + 1) * 128], u_gt, AF.Identity, r=["cst", "adt"], w=[("L", i2, hh)], scale=adt[:, c, hh0 + hh:hh0 + hh + 1])
                                mm(ps[i2][:, hh * 128:(hh + 1) * 128], Lb[i2][:, hh * 128:(hh + 1) * 128], tri_le, True, True, r=[("L", i2, hh), "cst"], w=[("ps", i2)])
                            act(Db[i2], ps[i2][:, 0:256], AF.Exp, r=[("ps", i2)], w=[("D", i2)])
                            tt("dve", MT_all[:, c, :].rearrange("p (h l) -> p h l", h=2), Db[i2].rearrange("p (h l) -> p h l", h=2),
                               CBm[:, c, :].unsqueeze(1).to_broadcast([128, 2, 128]), ALU.mult, r=[("D", i2), "CBm"], w=[("MT", c)])
                            if c + 1 < NT:
                                tt("dve", xdte[i4].rearrange("p (h c) -> p h c", h=2), xdt[:, c, :].rearrange("p (h c) -> p h c", h=2),
                                   dte[:, c, hh0:hh0 + 2].unsqueeze(2).to_broadcast([128, 2, 64]), ALU.mult, r=[("xdt", c), "dte"], w=[("xdte", i4)])
                                mm(ps[2 + i2][:, 0:128], Btok[:, c, :], xdte[i4], True, True, r=["Btok", ("xdte", i4)], w=[("ps", 2 + i2)])
                                tt("dve", Sst.rearrange("p (h c) -> p h c", h=2), Sst.rearrange("p (h c) -> p h c", h=2),
                                   eatot[:, c, hh0:hh0 + 2].unsqueeze(2).to_broadcast([128, 2, 64]), ALU.mult, r=["Sst", "eatot"], w=["Sst"])
                                tt("dve", Sst, Sst, ps[2 + i2][:, 0:128], ALU.add, r=[("ps", 2 + i2), "Sst"], w=["Sst"])
                                cp("act", S_all[:, c + 1, :], Sst, r=["Sst"], w=[("S_all", c + 1)])
                        def p3_mm(c):
                            b = 4 + c % 2
                            pa = ps[b]
                            mm(pa[:, 0:128], CT[:, c * 128:(c + 1) * 128], S_all[:, c, :], True, True, r=["CT", ("S_all", c)], w=[("ps", b)])
                            for hh in range(2):
                                o = pa[:, 128 + hh * 64:128 + (hh + 1) * 64]
                                mm(o, MT_all[:, c, hh * 128:(hh + 1) * 128], xdt[:, c, hh * 64:(hh + 1) * 64], True, False, r=[("MT", c), ("xdt", c)], w=[("ps", b)])
                                mm(o, DI[:, hh, :], xtok[:, c, hh * 64:(hh + 1) * 64], False, True, r=["DI"] + [("xtok", bb) for bb in range(5)], w=[("ps", b)])

                        def p3_ev(c, jj=jj, j=j, hh0=hh0):
                            b = 4 + c % 2
                            pa = ps[b]
                            i4 = c % 4
                            y, yk = ytmp[i4], ("ytmp", i4)
                            tt("dve", y.rearrange("p (h c) -> p h c", h=2), pa[:, 0:128].rearrange("p (h c) -> p h c", h=2),
                               ea[:, c, hh0:hh0 + 2].unsqueeze(2).to_broadcast([128, 2, 64]), ALU.mult, r=[("ps", b), "ea"], w=[yk])
                            tt("dve", y, y, pa[:, 128:256], ALU.add, r=[("ps", b), yk], w=[yk])
                            yt, ytk = yg16[i4], ("yg", i4)
                            tt("dve", yt, y, sz[:, c, :], ALU.mult, r=[yk] + [("sz", bb) for bb in range(5)], w=[ytk])
                            act(junk, yt, AF.Square, r=[ytk], w=[("ssp", c), "junk"], accum=ssp[:, c, jj:jj + 1])
                            bk = bank16(6 + c % 2)
                            tr(bk[:, 0:128], yt, ident16, r=[ytk, "cst16"], w=[("ps", 6 + c % 2)])
                            act(ybuf[:, jj, c * 128:(c + 1) * 128], bk[:, 0:128], AF.Identity, r=[("ps", 6 + c % 2), "colp"], w=[("ybuf", jj)],
                                scale=colv("ssd_norm", j))

                        p3_mm(0)
                        for c in range(NT):
                            if c + 1 < NT:
                                p3_mm(c + 1)
                            p3_ev(c)
                    if SSD_STOP == "rec":
                        return
                    pass
                    S.add("dve", lambda e: e.tensor_reduce(out=rstdg, in_=ssp, axis=AX.X, op=ALU.add), r=[("ssp", c) for c in range(NT)], w=["rstdg"])
                    act(rstdg, rstdg, AF.Sqrt, r=["rstdg"], w=["rstdg"], scale=1.0 / 512, bias=colv_eps)
                    S.add("dve", lambda e: e.reciprocal(rstdg, rstdg), r=["rstdg"], w=["rstdg"])
                    out_proj(g * 4, lambda t: rstdg[:, t:t + 1])

            if "lru" in MIX0_PARTS:
                lru()
            S.barrier()
            A.off = mark
            if "ssd" in MIX0_PARTS:
                ssd()

        for ph in phases:
            {"mix0": mix0, "moe0": lambda: moe(0), "mix1": mix1, "moe1": lambda: moe(1)}[ph]()

        if dbg or not phases or not phases[-1].startswith("moe"):
            A.reset()
        if dbg:
            for t in range(NT):
                dma("sp", out_d[t * 128:(t + 1) * 128, :], h[:, t, :], r=HK(t), is_output=True)
        else:
            rms_stats(list(range(1, NT)))
            nf = A.a([128, D])
            dma("sp", nf, big_d[:, 0, :].partition_broadcast(128), w=["nf"])
            ob = [A.a([128, D]) for _ in range(2)]
            for t in range(1, NT):
                o = ob[t % 2]
                stt(o, h[:, t, :], rstd[:, t:t + 1], nf, ALU.mult, ALU.mult, r=HK(t) + [("rstd", t), "nf"], w=[("ob", t % 2)])
                dma("sp", out_d[(t - 1) * 128:t * 128, :], o, r=[("ob", t % 2)], is_output=True)
        if REORDER:
            S.reorder()
        S.emit()
    return nc


_CACHE = {}


def kernel(**inputs):
    shared = pack_inputs(inputs)
    x = np.ascontiguousarray(np.asarray(inputs["x"], np.float32))
    nb = x.shape[0]
    if "nc" not in _CACHE:
        _CACHE["nc"] = build()
    nc = _CACHE["nc"]
    in_maps = [dict(shared, x=x[b]) for b in range(nb)]
    res = run_bass_kernel_spmd(nc, in_maps, core_ids=list(range(nb)))
    return np.stack([np.asarray(r["out"], np.float32) for r in res.results], axis=0)
```

```python
from contextlib import ExitStack
import numpy as np
import concourse.bass as bass
import concourse.mybir as mybir
from concourse.bass_utils import run_bass_kernel_spmd

F32 = mybir.dt.float32
BF16 = mybir.dt.bfloat16
AF = mybir.ActivationFunctionType
ALU = mybir.AluOpType
AX = mybir.AxisListType

COMPUTE = ("pe", "act", "dve", "pool")
NDSEM = 16
SAME_ENGINE_GAP = 1 << 30

T = 2176
NT = 17
PADN = 112
D = 1024
TB = [(0, 512), (512, 512), (1024, 512), (1536, 512), (2048, 128)]
EPS = 1e-6
MIX0_PARTS = ("lru", "ssd")
SSD_STOP = None
REORDER = True


class Sched:
    def __init__(self, nc):
        self.nc = nc
        self.ops = {e: [] for e in ("pe", "act", "dve", "pool", "sp")}
        self.ccount = {e: 0 for e in COMPUTE}
        self.dcount = {"sp": 0, "pool": 0}
        self.last_w = {}
        self.readers = {}
        self.sig = {e: set() for e in COMPUTE}
        self.out_dmas = []
        self.pending_bar = {}
        self.ps_last = {}
        self.epoch = 0

    def _deps(self, tok, r, w, eng):
        deps = set()
        for k in r:
            t = self.last_w.get(k)
            if t is not None:
                deps.add(t)
        for k in w:
            t = self.last_w.get(k)
            if t is not None:
                deps.add(t)
            for t in self.readers.get(k, ()):
                deps.add(t)
        for k in w:
            self.last_w[k] = tok
            self.readers[k] = []
        for k in r:
            if k in w:
                continue
            self.readers.setdefault(k, []).append(tok)
        for k in set(r) | set(w):
            if isinstance(k, tuple) and k[0] == "ps":
                d = self.ps_last.setdefault(k, {})
                for oe in list(d):
                    if oe != eng:
                        deps.update(d[oe])
                        d[oe] = []
                d.setdefault(eng, []).append(tok)
        bar = self.pending_bar.pop(eng) if eng in self.pending_bar else set()
        deps.discard(tok)
        return deps, bar

    def barrier(self):
        toks = set()
        for e in COMPUTE:
            if self.ccount[e] > 0:
                toks.add(("c", e, self.ccount[e] - 1))
        for q in ("sp", "pool"):
            for k in range(max(0, self.dcount[q] - NDSEM), self.dcount[q]):
                toks.add(("d", q, k))
        for e in ("pe", "act", "dve", "pool", "sp"):
            self.pending_bar[e] = set(toks) | self.pending_bar.get(e, set())
        self.epoch += 1

    def add(self, eng, fn, r=(), w=(), cost=0.3):
        seq = self.ccount[eng]
        self.ccount[eng] += 1
        tok = ("c", eng, seq)
        deps, bar = self._deps(tok, tuple(r), tuple(w), eng)
        self.ops[eng].append(["c", fn, deps, seq, cost, self.epoch, bar])
        return tok

    def reorder(self, window=192, lat=1.0):
        RE = ("pe", "act", "dve")
        fin = {}
        etime = {e: 0.0 for e in self.ops}
        new_order = {e: [] for e in RE}
        ptr = {e: 0 for e in self.ops}
        fence = 0.0
        tokof = lambda e, op: ("c", e, op[3]) if op[0] == "c" else ("d", e, op[3])
        for ep in range(self.epoch + 1):
            seg = {}
            for e in self.ops:
                lst = self.ops[e]
                i = ptr[e]
                j = i
                while j < len(lst) and lst[j][5] == ep:
                    j += 1
                seg[e] = lst[i:j]
                ptr[e] = j
            for e in seg:
                etime[e] = max(etime[e], fence)
            left = {e: list(seg[e]) for e in seg}
            total = sum(len(v) for v in left.values())
            while total:
                best = None
                for e, lst in left.items():
                    if not lst:
                        continue
                    cands = lst[:window] if e in RE else lst[:1]
                    for pos, op in enumerate(cands):
                        ready = 0.0
                        ok = True
                        for t in op[2]:
                            f = fin.get(t)
                            if f is None:
                                ok = False
                                break
                            if t[1] != e:
                                f += lat
                            if f > ready:
                                ready = f
                        if not ok:
                            continue
                        start = max(etime[e], ready)
                        key = (start, pos)
                        if best is None or key < best[0]:
                            best = (key, e, pos, op, start)
                        if start <= etime[e]:
                            break
                assert best is not None, "scheduler stuck"
                _, e, pos, op, start = best
                left[e].pop(pos)
                total -= 1
                if op[0] == "c":
                    etime[e] = start + op[4]
                    fin[tokof(e, op)] = etime[e]
                else:
                    etime[e] = start + 0.5
                    fin[tokof(e, op)] = start + op[4]
                if e in RE:
                    new_order[e].append(op)
            fence = max([fence] + list(etime.values()) + [fin[tokof(e, op)] for e in seg for op in seg[e]])
        remap = {}
        for e in RE:
            bars = {}
            for op in new_order[e]:
                if op[6]:
                    bars.setdefault(op[5], set()).update(op[6])
                    op[6] = set()
            seen_ep = set()
            for i, op in enumerate(new_order[e]):
                if op[5] not in seen_ep:
                    seen_ep.add(op[5])
                    op[6] = bars.get(op[5], set())
                remap[("c", e, op[3])] = ("c", e, i)
                op[3] = i
            self.ops[e] = new_order[e]
        for e, lst in self.ops.items():
            for op in lst:
                op[2] = {remap.get(t, t) for t in op[2]}
                op[6] = {remap.get(t, t) for t in op[6]}
        self.model_time = max(etime.values())

    def dma(self, q, fn, r=(), w=(), is_output=False, cost=3.0):
        k = self.dcount[q]
        self.dcount[q] += 1
        tok = ("d", q, k)
        deps, bar = self._deps(tok, tuple(r), tuple(w), q)
        self.ops[q].append(["d", fn, deps, k, cost, self.epoch, bar])
        if is_output:
            self.out_dmas.append(tok)
        return tok

    def emit(self):
        nc = self.nc
        for e, lst in self.ops.items():
            for op in lst:
                op[2] = set(op[2]) | set(op[6])
        for e, lst in self.ops.items():
            seen_c = {f: -1 for f in COMPUTE}
            for kind, fn, deps, seq, _c, _e, _b in lst:
                cw = {}
                for t in deps:
                    if t[0] != "c":
                        continue
                    f, s = t[1], t[2]
                    if f == e and kind == "c":
                        if e == "pe":
                            continue
                        if seq - s > SAME_ENGINE_GAP:
                            continue
                    if s <= seen_c[f]:
                        continue
                    cw[f] = max(cw.get(f, -1), s)
                for f, s in cw.items():
                    seen_c[f] = s
                    self.sig[f].add(s)
        sigidx = {}
        for e in COMPUTE:
            s = sorted(self.sig[e])
            sigidx[e] = {seq: i + 1 for i, seq in enumerate(s)}
        with ExitStack() as st:
            csem = {e: st.enter_context(nc.semaphore("c_" + e)) for e in COMPUTE}
            dsem = {q: [st.enter_context(nc.semaphore(f"d_{q}{i}")) for i in range(NDSEM)]
                    for q in ("sp", "pool")}
            block = st.enter_context(nc.Block())

            def run(e, eng):
                seen_c = {f: -1 for f in COMPUTE}
                seen_d = {}
                for kind, fn, deps, seq, _c, _e, _b in self.ops[e]:
                    cw = {}
                    for t in deps:
                        if t[0] == "c":
                            f, s = t[1], t[2]
                            if f == e and kind == "c":
                                if e == "pe":
                                    continue
                                if seq - s > SAME_ENGINE_GAP:
                                    continue
                            if s <= seen_c[f]:
                                continue
                            cw[f] = max(cw.get(f, -1), s)
                        else:
                            q, k = t[1], t[2]
                            key = (q, k % NDSEM)
                            val = 16 * (k // NDSEM + 1)
                            if seen_d.get(key, 0) >= val:
                                continue
                            seen_d[key] = val
                            eng.wait_ge(dsem[q][k % NDSEM], val)
                    for f, s in cw.items():
                        seen_c[f] = s
                        eng.wait_ge(csem[f], sigidx[f][s])
                    if kind == "c":
                        ins = fn(eng)
                        if seq in sigidx[e]:
                            ins.then_inc(csem[e], 1)
                    else:
                        k = seq
                        if k >= NDSEM:
                            key = (e, k % NDSEM)
                            val = 16 * (k // NDSEM)
                            if seen_d.get(key, 0) < val:
                                seen_d[key] = val
                                eng.wait_ge(dsem[e][k % NDSEM], val)
                        ins = fn(eng)
                        ins.then_inc(dsem[e][k % NDSEM], 16)
                if e == "sp":
                    for t in self.out_dmas:
                        q, k = t[1], t[2]
                        eng.wait_ge(dsem[q][k % NDSEM], 16 * (k // NDSEM + 1))

            @block.tensor
            def _(eng):
                run("pe", eng)

            @block.scalar
            def _(eng):
                run("act", eng)

            @block.vector
            def _(eng):
                run("dve", eng)

            @block.gpsimd
            def _(eng):
                run("pool", eng)

            @block.sync
            def _(eng):
                run("sp", eng)


COLP = {}
_o = 0
for _n, _c in [("mix_even", 8), ("ffn0", 8), ("ffn1", 8), ("mix_odd", 8), ("ssd_cw", 48), ("ssd_cb", 12),
               ("ssd_norm", 8), ("lru_cw", 32), ("lru_cb", 8), ("lru_ba", 8), ("lru_bx", 8), ("lru_lam", 8)]:
    COLP[_n] = (_o, _c)
    _o += _c
NCOL = _o
ROWP = {}
_o = 0
for _n, _c in [("dt_bias", 16), ("a_log", 16), ("ssd_d", 16), ("rb0", 20), ("rb1", 20), ("rc", 64)]:
    ROWP[_n] = (_o, _c)
    _o += _c
NROW = _o


def _fm(v):
    v = np.asarray(v, np.float32).reshape(-1, 128)
    return np.ascontiguousarray(v.T)


def _pool_band_consts():
    A = np.zeros((128, 12, 128), np.float32)
    s_ = np.arange(128)[:, None]
    t_ = np.arange(128)[None, :]
    for g, w in enumerate((2, 4, 8, 16)):
        inwin = ((t_ - s_) >= 0) & ((t_ - s_) < w)
        A[:, 3 * g + 0, :] = inwin / float(w) - (s_ == t_)
        A[:, 3 * g + 1, :] = (((t_ + 128 - s_) < w) & ((t_ + 128 - s_) >= 0)) / float(w)
        cnt = np.where(t_ >= PADN, np.minimum(t_ - PADN + 1, w), w).astype(np.float32)
        A[:, 3 * g + 2, :] = inwin / cnt - (s_ == t_)
    return A


def pack_inputs(inp):
    f = lambda a: np.ascontiguousarray(np.asarray(a, np.float32))
    colp = np.zeros((128, NCOL), np.float32)

    def put(name, arr):
        o, c = COLP[name]
        assert arr.shape == (128, c), (name, arr.shape)
        colp[:, o:o + c] = arr

    put("mix_even", _fm(inp["mix_norm_even"][0]))
    put("ffn0", _fm(inp["ffn_norm"][0]))
    put("ffn1", _fm(inp["ffn_norm"][1]))
    put("mix_odd", _fm(inp["mix_norm_odd"][0]))
    cw = np.asarray(inp["ssd_conv_w"][0], np.float32)
    put("ssd_cw", np.concatenate([_fm(cw[k]) for k in range(4)], axis=1).reshape(128, 4, 12).transpose(0, 2, 1).reshape(128, 48))
    put("ssd_cb", _fm(inp["ssd_conv_b"][0]))
    put("ssd_norm", _fm(inp["ssd_norm"][0]))
    lw = np.asarray(inp["lru_conv_w"][0], np.float32)
    put("lru_cw", np.concatenate([_fm(lw[k]) for k in range(4)], axis=1).reshape(128, 4, 8).transpose(0, 2, 1).reshape(128, 32))
    put("lru_cb", _fm(inp["lru_conv_b"][0]))
    put("lru_ba", _fm(inp["lru_b_a"][0]))
    put("lru_bx", _fm(inp["lru_b_x"][0]))
    put("lru_lam", _fm(inp["lru_lambda"][0]))

    rowp = np.zeros((1, NROW), np.float32)

    def putr(name, arr):
        o, c = ROWP[name]
        rowp[0, o:o + c] = np.asarray(arr, np.float32).reshape(-1)

    putr("dt_bias", inp["ssd_dt_bias"][0])
    putr("a_log", inp["ssd_a_log"][0])
    putr("ssd_d", inp["ssd_d"][0])
    putr("rb0", np.concatenate([np.asarray(inp["router_group_b"][0]), np.asarray(inp["router_expert_b"][0])]))
    putr("rb1", np.concatenate([np.asarray(inp["router_group_b"][1]), np.asarray(inp["router_expert_b"][1])]))
    rc = np.zeros((4, 16), np.float32)
    for g, w in enumerate((2, 4, 8, 16)):
        rc[g] = 1.0 / np.minimum(np.arange(16) + 1, w)
    putr("rc", rc)

    w_in = f(inp["w_in"][0])
    cols = np.concatenate([np.arange(0, 2560), np.arange(2576, 4624)])
    w_in_r = np.ascontiguousarray(w_in[:, cols].reshape(8, 128, 36, 128).transpose(2, 1, 0, 3))
    w_dt = np.ascontiguousarray(w_in[:, 2560:2576].reshape(8, 128, 16).transpose(1, 0, 2))
    wr = np.stack([np.concatenate([f(inp["router_group_w"][l]), f(inp["router_expert_w"][l])], axis=1)
                   .reshape(8, 128, 20).transpose(1, 0, 2) for l in range(2)])
    k_ = np.arange(128)[:, None]
    s_ = np.arange(128)[None, :]
    cst = np.stack([np.eye(128, dtype=np.float32), (k_ <= s_).astype(np.float32), (k_ > s_).astype(np.float32),
                    np.ones((128, 128), np.float32)], axis=1)
    shared = {
        "meta": f(inp["meta_tokens"]), "colp": colp, "rowp": rowp, "cst": np.ascontiguousarray(cst),
        "w_in_r": w_in_r, "w_dt": w_dt, "w_out": f(inp["w_out"][0]),
        "lru_wa": f(inp["lru_w_a"][0]), "lru_wx": f(inp["lru_w_x"][0]),
        "pool_w": f(inp["pool_w"][0]), "wr": np.ascontiguousarray(wr),
        "bigrow": np.ascontiguousarray(np.stack([f(inp["norm_final"]), f(inp["pool_b"][0]), f(inp["pool_scale"][0]),
                                                 f(inp["mix_norm_odd"][0])])[None]),
        "poolA": _pool_band_consts(),
        "wg": f(inp["expert_w_gate"]), "wu": f(inp["expert_w_up"]), "wd": f(inp["expert_w_down"]),
    }
    return shared


def build(phases=("mix0", "moe0", "mix1", "moe1"), dbg=False):
    nc = bass.Bass("TRN2", target_bir_lowering=False)
    dram = lambda n, s, kind="ExternalInput": nc.dram_tensor(n, list(s), F32, kind=kind).ap()
    x_d = dram("x", [2048, D])
    meta_d = dram("meta", [16, D])
    colp_d = dram("colp", [128, NCOL])
    rowp_d = dram("rowp", [1, NROW])
    cst_d = dram("cst", [128, 4, 128])
    w_in_d = dram("w_in_r", [36, 128, 8, 128])
    w_dt_d = dram("w_dt", [128, 8, 16])
    w_out_d = dram("w_out", [2048, D])
    lwa_d = dram("lru_wa", [16, 64, 64])
    lwx_d = dram("lru_wx", [16, 64, 64])
    pw_d = dram("pool_w", [4, 256, 256])
    wr_d = dram("wr", [2, 128, 8, 20])
    big_d = dram("bigrow", [1, 4, D])
    pA_d = dram("poolA", [128, 12, 128])
    wg_d = dram("wg", [2, 16, D, 512])
    wu_d = dram("wu", [2, 16, D, 512])
    wd_d = dram("wd", [2, 16, 512, D])
    if dbg:
        out_d = dram("out", [T, D], kind="ExternalOutput")
    else:
        out_d = dram("out", [2048, D], kind="ExternalOutput")

    S = Sched(nc)
    with ExitStack() as st:
        sb = lambda n, s, dt=F32: st.enter_context(nc.sbuf_tensor(n, list(s), dt))
        h = sb("h", [128, NT, D])
        hnT = sb("hnT", [128, 8, T], BF16)
        colp = sb("colp_s", [128, NCOL])
        rowp = sb("rowp_s", [128, NROW])
        cst = sb("cst_s", [128, 4, 128])
        cst16 = sb("cst16", [128, 4, 128], BF16)
        stat = sb("stat", [128, 64])
        AW = 25900
        arena = sb("arena", [128, AW])
        ps = [st.enter_context(nc.psum_tensor(f"ps{i}", [128, 512], F32)) for i in range(8)]
        ident = cst[:, 0, :]
        tri_le = cst[:, 1, :]
        u_gt = cst[:, 2, :]
        ones32 = cst[:, 3, :]
        ident16 = cst16[:, 0, :]
        ss = stat[:, 0:17]
        sq = stat[:, 17:34]
        rstd = stat[:, 34:51]

        class Arena:
            def __init__(self):
                self.off = 0

            def reset(self):
                self.off = 0
                S.barrier()

            def a(self, shape, dt=F32):
                n = int(np.prod(shape[1:]))
                words = n if dt == F32 else (n + 1) // 2
                assert self.off + words <= AW, ("arena overflow", self.off, words)
                v = arena[:, self.off:self.off + words]
                self.off += words
                if dt != F32:
                    v = v.bitcast(dt)
                    if v.shape[1] != n:
                        v = v[:, 0:n]
                if len(shape) == 3:
                    v = v.rearrange("p (a b) -> p a b", a=shape[1])
                elif len(shape) == 4:
                    v = v.rearrange("p (a b c) -> p a b c", a=shape[1], b=shape[2])
                return v

        A = Arena()
        colv = lambda name, i=0, n=None: colp[:, COLP[name][0] + i:COLP[name][0] + i + (n if n else 1)]
        rowv = lambda name: rowp[:, ROWP[name][0]:ROWP[name][0] + ROWP[name][1]]

        def fsz(ap):
            n = 1
            for d_ in ap.shape[1:]:
                n *= int(d_)
            return n

        def ecost(eng, n, mult=1.0):
            if eng == "dve":
                return 0.12 + mult * n / 960.0
            if eng == "act":
                return 0.25 + n / 1400.0
            return 2.0 + 0.015 * n

        def mm(out, lhsT, rhs, start, stop, r, w):
            c = max(fsz(rhs), 64) / 2400.0 + 0.03
            if lhsT.dtype == F32:
                c *= 4
            S.add("pe", lambda e: e.matmul(out, lhsT, rhs, start=start, stop=stop), r=r, w=w, cost=c)

        def tr(out, in_, idn, r, w):
            S.add("pe", lambda e: e.transpose(out, in_, idn), r=r, w=w, cost=0.3 if in_.dtype == F32 else 0.12)

        def act(out, in_, func, r, w, scale=1.0, bias=0.0, accum=None):
            c = ecost("act", fsz(in_)) + (0.1 if accum is not None else 0.0)
            if accum is None:
                S.add("act", lambda e: e.activation(out=out, in_=in_, func=func, scale=scale, bias=bias), r=r, w=w, cost=c)
            else:
                S.add("act", lambda e: e.activation(out=out, in_=in_, func=func, scale=scale, bias=bias, accum_out=accum), r=r, w=w, cost=c)

        def tt(eng, out, in0, in1, op, r, w):
            S.add(eng, lambda e: e.tensor_tensor(out=out, in0=in0, in1=in1, op=op), r=r, w=w, cost=ecost(eng, fsz(out)))

        def ts(eng, out, in0, s1, op0, r, w, s2=None, op1=None):
            c = ecost(eng, fsz(out))
            if op1 is None:
                S.add(eng, lambda e: e.tensor_scalar(out=out, in0=in0, scalar1=s1, scalar2=None, op0=op0), r=r, w=w, cost=c)
            else:
                S.add(eng, lambda e: e.tensor_scalar(out=out, in0=in0, scalar1=s1, scalar2=s2, op0=op0, op1=op1), r=r, w=w, cost=c)

        def stt(out, in0, scalar, in1, op0, op1, r, w):
            S.add("dve", lambda e: e.scalar_tensor_tensor(out=out, in0=in0, scalar=scalar, in1=in1, op0=op0, op1=op1), r=r, w=w,
                  cost=ecost("dve", fsz(out)))

        def cp(eng, out, in_, r, w):
            c = ecost(eng, fsz(out))
            if eng == "act":
                S.add("act", lambda e: e.copy(out, in_), r=r, w=w, cost=c)
            else:
                S.add(eng, lambda e: e.tensor_copy(out, in_), r=r, w=w, cost=c)

        def memset(eng, ap, val, w):
            S.add(eng, lambda e: e.memset(ap, val), w=w, cost=ecost(eng, fsz(ap)))

        def dma(q, out, in_, r=(), w=(), is_output=False):
            nbytes = fsz(out) * int(out.shape[0]) * 4
            S.dma(q, lambda e: e.dma_start(out=out, in_=in_), r=r, w=w, is_output=is_output, cost=2.5 + nbytes / 150e3)

        HK = lambda t: [("h", t, 0), ("h", t, 1)]

        dma("sp", colp[:], colp_d, w=["colp"])
        dma("sp", rowp[:], rowp_d.partition_broadcast(128), w=["rowp"])
        dma("sp", cst[:], cst_d, w=["cst"])
        dma("pool", cst16[:], cst_d, w=["cst16"])
        memset("dve", h[:, 0, :], 0.0, w=HK(0))
        dma("sp", h[PADN:128, 0, :], meta_d, w=HK(0))
        xr = x_d.rearrange("(t p) d -> p t d", p=128)
        for i in range(4):
            dma("sp", h[:, 1 + 4 * i:5 + 4 * i, :], xr[:, 4 * i:4 * i + 4, :], w=[k for t in range(1 + 4 * i, 5 + 4 * i) for k in HK(t)])

        def rms_stats(tiles):
            junk = A.a([128, D], BF16)
            for t in tiles:
                act(junk, h[:, t, :], AF.Square, r=HK(t), w=[("ss", t), "junk"], accum=ss[:, t:t + 1])
                act(sq[:, t:t + 1], ss[:, t:t + 1], AF.Sqrt, r=[("ss", t)], w=[("sq", t)], scale=1.0 / D, bias=colv_eps)
                S.add("dve", lambda e, t=t: e.reciprocal(rstd[:, t:t + 1], sq[:, t:t + 1]), r=[("sq", t)], w=[("rstd", t)], cost=0.15)

        def normT(gname, router_l=None, logits=None, wr_s=None, consume=None):
            xsb = [A.a([128, D]) for _ in range(2)]
            t32 = [A.a([128, 4, 128]) for _ in range(4)]
            pend = None
            for t in range(NT):
                xs = xsb[t % 2]
                act(xs, h[:, t, :], AF.Identity, r=HK(t) + [("rstd", t)], w=[("xs", t % 2)], scale=rstd[:, t:t + 1])
                for half in range(2):
                    i2 = (2 * t + half) % 2
                    i4 = (2 * t + half) % 4
                    bank = ps[6 + i2]
                    for kk in range(4):
                        k = half * 4 + kk
                        tr(bank[:, kk * 128:(kk + 1) * 128], xs[:, k * 128:(k + 1) * 128], ident, r=[("xs", t % 2), "cst"], w=[("ps", 6 + i2)])
                    g0 = COLP[gname][0] + half * 4
                    tt("dve", t32[i4], bank[:, :].rearrange("p (a b) -> p a b", a=4),
                       colp[:, g0:g0 + 4].unsqueeze(2).to_broadcast([128, 4, 128]), ALU.mult,
                       r=[("ps", 6 + i2), "colp"], w=[("t32", i4)])
                    cp("act", hnT[:, half * 4:half * 4 + 4, t * 128:(t + 1) * 128], t32[i4], r=[("t32", i4)], w=[("hnT", t)])
                if router_l is not None:
                    if pend is not None:
                        pend()

                    def mk(t=t):
                        def go():
                            for k in range(8):
                                i4 = (2 * t + k // 4) % 4
                                mm(ps[5][:, 0:20], t32[i4][:, k % 4, :], wr_s[:, k, :], k == 0, k == 7,
                                   r=[("t32", i4), "wr"], w=[("ps", 5)])
                            cp("act", logits[:, t, :], ps[5][:, 0:20], r=[("ps", 5)], w=["logits"])
                        return go
                    pend = mk()
            if pend is not None:
                pend()

        memset("pool", stat[:, 60:61], EPS, w=["eps"])
        colv_eps = stat[:, 60:61]

        def moe(l):
            A.reset()
            wr_s = A.a([128, 8, 20])
            logits = A.a([128, NT, 20])
            gates = A.a([128, NT, 16])
            dma("sp", wr_s, wr_d[l], w=["wr"])
            Wg = [A.a([128, 8, 512], BF16) for _ in range(2)]
            Wu = [A.a([128, 8, 512], BF16) for _ in range(2)]
            Wd = [A.a([128, 4, D], BF16) for _ in range(2)]

            def load_w(e):
                b = e % 2
                if e == 0:
                    for fc in range(4):
                        fs = slice(fc * 128, (fc + 1) * 128)
                        dma("pool", Wg[b][:, :, fs], wg_d[l, e][:, fs].rearrange("(k p) f -> p k f", p=128), w=[("Wg", b, fc)])
                        dma("pool", Wu[b][:, :, fs], wu_d[l, e][:, fs].rearrange("(k p) f -> p k f", p=128), w=[("Wu", b, fc)])
                else:
                    dma("pool", Wg[b], wg_d[l, e].rearrange("(k p) f -> p k f", p=128), w=[("Wg", b, fc) for fc in range(4)])
                    dma("pool", Wu[b], wu_d[l, e].rearrange("(k p) f -> p k f", p=128), w=[("Wu", b, fc) for fc in range(4)])
                dma("pool", Wd[b], wd_d[l, e].rearrange("(k p) f -> p k f", p=128), w=[("Wd", b)])

            load_w(0)
            load_w(1)
            mark = A.off
            rms_stats(list(range(NT)))
            normT("ffn%d" % l, router_l=l, logits=logits, wr_s=wr_s)
            R = lambda shape: A.a(shape)
            rb = rowv("rb%d" % l)
            lg = R([128, NT, 20])
            tt("dve", lg, logits, rb.unsqueeze(1).to_broadcast([128, NT, 20]), ALU.add, r=["logits", "rowp"], w=["lg"])
            m4 = R([128, NT])
            S.add("dve", lambda e: e.tensor_reduce(out=m4, in_=lg[:, :, 0:4], axis=AX.X, op=ALU.max), r=["lg"], w=["m4"])
            d4 = R([128, NT, 4])
            tt("dve", d4, lg[:, :, 0:4], m4.unsqueeze(2).to_broadcast([128, NT, 4]), ALU.subtract, r=["lg", "m4"], w=["d4"])
            mg = R([128, NT, 4])
            ts("dve", mg, d4, 0.0, ALU.is_ge, r=["d4"], w=["mg"])
            e4 = R([128, NT, 4])
            act(e4, d4, AF.Exp, r=["d4"], w=["e4"])
            s4 = R([128, NT])
            S.add("dve", lambda e: e.tensor_reduce(out=s4, in_=e4, axis=AX.X, op=ALU.add), r=["e4"], w=["s4"])
            le = lg[:, :, 4:20].rearrange("p t (g j) -> p t g j", g=4)
            ml = R([128, NT, 4, 4])
            tt("dve", ml, le, mg.unsqueeze(3).to_broadcast([128, NT, 4, 4]), ALU.mult, r=["lg", "mg"], w=["ml"])
            sel = R([128, NT, 4])
            tt("dve", sel, ml[:, :, 0, :], ml[:, :, 1, :], ALU.add, r=["ml"], w=["sel"])
            tt("dve", sel, sel, ml[:, :, 2, :], ALU.add, r=["ml", "sel"], w=["sel"])
            tt("dve", sel, sel, ml[:, :, 3, :], ALU.add, r=["ml", "sel"], w=["sel"])
            m1 = R([128, NT])
            S.add("dve", lambda e: e.tensor_reduce(out=m1, in_=sel, axis=AX.X, op=ALU.max), r=["sel"], w=["m1"])
            k1 = R([128, NT, 4])
            tt("dve", k1, sel, m1.unsqueeze(2).to_broadcast([128, NT, 4]), ALU.is_ge, r=["sel", "m1"], w=["k1"])
            sel2 = R([128, NT, 4])
            stt(sel2, k1, -1e30, sel, ALU.mult, ALU.add, r=["k1", "sel"], w=["sel2"])
            m2 = R([128, NT])
            S.add("dve", lambda e: e.tensor_reduce(out=m2, in_=sel2, axis=AX.X, op=ALU.max), r=["sel2"], w=["m2"])
            k2 = R([128, NT, 4])
            tt("dve", k2, sel2, m2.unsqueeze(2).to_broadcast([128, NT, 4]), ALU.is_ge, r=["sel2", "m2"], w=["k2"])
            dd = R([128, NT])
            tt("dve", dd, m2, m1, ALU.subtract, r=["m1", "m2"], w=["dd"])
            w2 = R([128, NT])
            act(w2, dd, AF.Exp, r=["dd"], w=["w2"])
            den = R([128, NT])
            stt(den, w2, 1.0, s4, ALU.add, ALU.mult, r=["w2", "s4"], w=["den"])
            g1 = R([128, NT])
            S.add("dve", lambda e: e.reciprocal(g1, den), r=["den"], w=["g1"])
            g2 = R([128, NT])
            tt("dve", g2, g1, w2, ALU.mult, r=["g1", "w2"], w=["g2"])
            gs = R([128, NT, 4])
            tt("dve", gs, k1, g1.unsqueeze(2).to_broadcast([128, NT, 4]), ALU.mult, r=["k1", "g1"], w=["gs"])
            gs2 = R([128, NT, 4])
            tt("dve", gs2, k2, g2.unsqueeze(2).to_broadcast([128, NT, 4]), ALU.mult, r=["k2", "g2"], w=["gs2"])
            tt("dve", gs, gs, gs2, ALU.add, r=["gs", "gs2"], w=["gs"])
            g4 = gates.rearrange("p t (g j) -> p t g j", g=4)
            for g in range(4):
                tt("dve", g4[:, :, g, :], gs, mg[:, :, g:g + 1].to_broadcast([128, NT, 4]), ALU.mult, r=["gs", "mg"], w=["gates"])

            hid = [A.a([128, 4, 512], BF16) for _ in range(2)]
            sg = [A.a([128, 512]) for _ in range(2)]
            for hb_ in range(2):
                memset("dve", hid[hb_], 0.0, w=[("hid", hb_)])
            TBM = [(PADN, 128 - PADN), (128, 512), (640, 512), (1152, 512), (1664, 512)]
            cnt = 0
            cnt_o = [0]
            blk = 0
            pend = None
            for e in range(16):
                b = e % 2
                if e >= 2:
                    load_w(e)
                for (t0, n) in TBM:
                    hb = blk % 2
                    blk += 1
                    hk = [("hnT", tt_) for tt_ in range(t0 // 128, (t0 + n + 127) // 128)]
                    ho = t0 % 128
                    for fc in range(4):
                        i = cnt % 2
                        cnt += 1
                        pg, pu = ps[i], ps[2 + i]
                        for k in range(8):
                            mm(pg[:, 0:n], Wg[b][:, k, fc * 128:(fc + 1) * 128], hnT[:, k, t0:t0 + n], k == 0, k == 7,
                               r=[("Wg", b, fc)] + hk, w=[("ps", i)])
                        for k in range(8):
                            mm(pu[:, 0:n], Wu[b][:, k, fc * 128:(fc + 1) * 128], hnT[:, k, t0:t0 + n], k == 0, k == 7,
                               r=[("Wu", b, fc)] + hk, w=[("ps", 2 + i)])
                        act(sg[i][:, 0:n], pg[:, 0:n], AF.Silu, r=[("ps", i)], w=[("sg", i)])
                        tt("dve", hid[hb][:, fc, ho:ho + n], sg[i][:, 0:n], pu[:, 0:n], ALU.mult, r=[("sg", i), ("ps", 2 + i)], w=[("hid", hb)])
                    if pend is not None:
                        pend()

                    def mk(e=e, b=b, hb=hb, t0=t0, n=n):
                        def go():
                            for tl in range((n + 127) // 128):
                                t = t0 // 128 + tl
                                for dh in range(2):
                                    io = 4 + cnt_o[0] % 2
                                    cnt_o[0] += 1
                                    for fc in range(4):
                                        mm(ps[io][:, :], hid[hb][:, fc, tl * 128:(tl + 1) * 128], Wd[b][:, fc, dh * 512:(dh + 1) * 512],
                                           fc == 0, fc == 3, r=[("hid", hb), ("Wd", b)], w=[("ps", io)])
                                    hv = h[:, t, dh * 512:(dh + 1) * 512]
                                    stt(hv, ps[io][:, :], gates[:, t, e:e + 1], hv,
                                        ALU.mult, ALU.add, r=[("ps", io), "gates", ("h", t, dh)], w=[("h", t, dh)])
                        return go
                    pend = mk()
            pend()

        def mix1():
            A.reset()
            memset("pool", h[0:PADN, 0, :], 0.0, w=HK(0))
            rms_stats(list(range(NT)))
            pw = A.a([128, 4, 2, 256], BF16)
            for g in range(4):
                dma("pool", pw[:, g, :, :], pw_d[g].rearrange("(c p) j -> p c j", p=128), w=["pw"])
            pA = A.a([128, 12, 128], BF16)
            dma("pool", pA, pA_d, w=["pA"])
            pb_bc = A.a([128, D])
            sc_bc = A.a([128, D])
            gn_bc = A.a([128, D])
            dma("sp", pb_bc, big_d[:, 1, :].partition_broadcast(128), w=["pb_bc"])
            dma("sp", sc_bc, big_d[:, 2, :].partition_broadcast(128), w=["sc_bc"])
            dma("sp", gn_bc, big_d[:, 3, :].partition_broadcast(128), w=["gn_bc"])
            bs_bc = A.a([128, D])
            tt("dve", bs_bc, pb_bc, sc_bc, ALU.mult, r=["pb_bc", "sc_bc"], w=["bs_bc"])
            bs16 = A.a([128, D], BF16)
            cp("dve", bs16, bs_bc, r=["bs_bc"], w=["bs16"])
            pws = A.a([128, 4, 2, 256], BF16)
            for g in range(4):
                tt("dve", pws[:, g, :, :], pw[:, g, :, :], sc_bc[:, g * 256:(g + 1) * 256].unsqueeze(1).to_broadcast([128, 2, 256]), ALU.mult,
                   r=["pw", "sc_bc"], w=["pws"])
            xsn = A.a([128, NT, D], BF16)
            for t in range(NT):
                stt(xsn[:, t, :], h[:, t, :], rstd[:, t:t + 1], gn_bc, ALU.mult, ALU.mult, r=HK(t) + [("rstd", t), "gn_bc"], w=[("xsn", t)])
            pooledT = A.a([128, 8, T], BF16)
            ev = 0
            for k in range(8):
                g = k // 2
                for q in range(5):
                    tiles = list(range(4 * q, min(4 * q + 4, NT)))
                    bi_ = 6 + (k * 5 + q) % 2
                    bank = ps[bi_]
                    for i, t in enumerate(tiles):
                        o = bank[:, i * 128:(i + 1) * 128]
                        if t == 0:
                            mm(o, xsn[:, 0, k * 128:(k + 1) * 128], pA[:, 3 * g + 2, :], True, True, r=[("xsn", 0), "pA"], w=[("ps", bi_)])
                        else:
                            mm(o, xsn[:, t - 1, k * 128:(k + 1) * 128], pA[:, 3 * g + 1, :], True, False, r=[("xsn", t - 1), "pA"], w=[("ps", bi_)])
                            mm(o, xsn[:, t, k * 128:(k + 1) * 128], pA[:, 3 * g + 0, :], False, True, r=[("xsn", t), "pA"], w=[("ps", bi_)])
                    nn = len(tiles) * 128
                    cp("act" if ev % 2 == 0 else "dve", pooledT[:, k, q * 512:q * 512 + nn], bank[:, 0:nn], r=[("ps", bi_)], w=[("pooledT", k)])
                    ev += 1
            for t in range(NT):
                for dh in range(2):
                    bi_ = dh + 2 * (t % 2)
                    bank = ps[bi_]
                    for gg in range(2):
                        g = dh * 2 + gg
                        o = bank[:, gg * 256:(gg + 1) * 256]
                        for ic in range(2):
                            mm(o, pooledT[:, 2 * g + ic, t * 128:(t + 1) * 128], pws[:, g, ic, :], ic == 0, False,
                               r=[("pooledT", 2 * g + ic), "pws"], w=[("ps", bi_)])
                        mm(o, cst16[0:1, 3, :], bs16[0:1, g * 256:(g + 1) * 256], False, True, r=["cst16", "bs16"], w=[("ps", bi_)])
                    hv = h[:, t, dh * 512:(dh + 1) * 512]
                    tt("dve", hv, hv, bank[:, :], ALU.add, r=[("ps", bi_), ("h", t, dh)], w=[("h", t, dh)])

        def mix0():
            A.reset()
            rms_stats(list(range(NT)))
            normT("mix_even")
            A.reset()
            hnk = [("hnT", t) for t in range(NT)]
            wbuf = [A.a([128, 8, 128], BF16) for _ in range(3)]
            wcnt = [0]

            def load_wchunk(cc):
                i = wcnt[0] % 3
                wcnt[0] += 1
                dma("pool", wbuf[i], w_in_d[cc], w=[("wbuf", i)])
                return i

            def proj_blk(wi, bi, t0, n):
                bk = ("ps", bi % 2)
                bank = ps[bi % 2]
                for k in range(8):
                    mm(bank[:, 0:n], wbuf[wi][:, k, :], hnT[:, k, t0:t0 + n], k == 0, k == 7, r=[("wbuf", wi)] + hnk, w=[bk])
                return bank, bk

            xinb = [A.a([128, 3 + 512]) for _ in range(2)]
            ctb = [A.a([128, 512]) for _ in range(2)]
            cvc = [0]

            def conv_blk(bank, bk, bi, n, wname, bname, cidx, tap0_act=False, copy_eng="act"):
                i = cvc[0] % 2
                cvc[0] += 1
                xi, xk = xinb[i], ("xinb", i)
                if bi == 0:
                    memset("dve", xi[:, 0:3], 0.0, w=[xk])
                else:
                    pv = xinb[1 - i]
                    cp("dve", xi[:, 0:3], pv[:, 512:515], r=[("xinb", 1 - i)], w=[xk])
                cp(copy_eng, xi[:, 3:3 + n], bank[:, 0:n], r=[bk], w=[xk])
                ct, ck = ctb[i], ("ctb", i)
                o4 = COLP[wname][0] + 4 * cidx
                if tap0_act:
                    act(ct[:, 0:n], xi[:, 0:n], AF.Identity, r=[xk, "colp"], w=[ck], scale=colp[:, o4:o4 + 1], bias=colv(bname, cidx))
                else:
                    ts("dve", ct[:, 0:n], xi[:, 0:n], colp[:, o4:o4 + 1], ALU.mult, r=[xk, "colp"], w=[ck], s2=colv(bname, cidx), op1=ALU.add)
                for k in range(1, 4):
                    stt(ct[:, 0:n], xi[:, k:k + n], colp[:, o4 + k:o4 + k + 1], ct[:, 0:n], ALU.mult, ALU.add, r=[xk, ck, "colp"], w=[ck])
                return ct, ck

            wo = A.a([128, 4, D], BF16)
            ybuf = A.a([128, 4, T], BF16)

            def out_proj(kc0, scale_ap_fn):
                dma("pool", wo, w_out_d[kc0 * 128:(kc0 + 4) * 128, :].rearrange("(k p) d -> p k d", p=128), w=["wo"])
                cnt = 0
                for t in range(NT):
                    for dh in range(2):
                        io = 4 + cnt % 2
                        cnt += 1
                        for kk in range(4):
                            mm(ps[io][:, :], ybuf[:, kk, t * 128:(t + 1) * 128], wo[:, kk, dh * 512:(dh + 1) * 512], kk == 0, kk == 3,
                               r=[("ybuf", kk), "wo"], w=[("ps", io)])
                        hv = h[:, t, dh * 512:(dh + 1) * 512]
                        if scale_ap_fn is None:
                            tt("dve", hv, hv, ps[io][:, :], ALU.add, r=[("ps", io), ("h", t, dh)], w=[("h", t, dh)])
                        else:
                            stt(hv, ps[io][:, :], scale_ap_fn(t), hv, ALU.mult, ALU.add, r=[("ps", io), ("h", t, dh), "rstdg"], w=[("h", t, dh)])

            mark = A.off

            def lru():
                bdA = A.a([128, 8, 128], BF16)
                bdX = A.a([128, 8, 128], BF16)
                memset("dve", bdA, 0.0, w=["bdA"])
                memset("dve", bdX, 0.0, w=["bdX"])
                for src, dst, nm in ((lwa_d, bdA, "bdA"), (lwx_d, bdX, "bdX")):
                    v = src.rearrange("(j two) i o -> two i j o", two=2)
                    dma("pool", dst[0:64, :, 0:64], v[0], w=[nm])
                    dma("pool", dst[64:128, :, 64:128], v[1], w=[nm])
                c1 = A.a([128, 8])
                tmpc = A.a([128, 8])
                act(tmpc, colv("lru_lam", 0, 8), AF.Exp, r=["colp"], w=["tmpc"], scale=-1.0)
                act(tmpc, tmpc, AF.Ln, r=["tmpc"], w=["tmpc"], bias=1.0)
                ts("dve", c1, tmpc, -8.0, ALU.mult, r=["tmpc"], w=["c1"])
                Bn = lambda n_: [A.a([128, 512]) for _ in range(n_)]
                gl, rr, ii, av, uv, hl = Bn(4), Bn(4), Bn(4), Bn(4), Bn(2), Bn(2)
                xb16 = [A.a([128, 512], BF16) for _ in range(4)]
                NB = len(TB)
                items = [(j, bi) for j in range(8) for bi in range(NB)]
                st1 = {}

                def S1(idx):
                    j, bi = items[idx]
                    t0, n = TB[bi]
                    if bi == 0:
                        st1[j] = (load_wchunk(28 + j), load_wchunk(20 + j))
                    wi_in, wi_gt = st1[j]
                    bank, bk = proj_blk(wi_in, 2 * idx, t0, n)
                    xb, xbk = conv_blk(bank, bk, bi, n, "lru_cw", "lru_cb", j, copy_eng="dve")
                    bank2, bk2 = proj_blk(wi_gt, 2 * idx + 1, t0, n)
                    g3 = idx % 4
                    act(gl[g3][:, 0:n], bank2[:, 0:n], AF.Gelu_apprx_tanh, r=[bk2], w=[("gl", g3)])
                    cp("act", xb16[g3][:, 0:n], xb[:, 0:n], r=[xbk], w=[("xb16", g3)])
                    st1[idx, "xb"] = (xb, xbk)

                def S2(idx):
                    j, bi = items[idx]
                    t0, n = TB[bi]
                    ip = idx % 2
                    q = idx % 4
                    K = lambda nm: (nm, q)
                    xb, xbk = st1.pop((idx, "xb"))
                    pa, px = ps[2 + ip], ps[4 + ip]
                    mm(pa[:, 0:n], bdA[:, j, :], xb16[q][:, 0:n], True, True, r=["bdA", K("xb16")], w=[("ps", 2 + ip)])
                    mm(px[:, 0:n], bdX[:, j, :], xb16[q][:, 0:n], True, True, r=["bdX", K("xb16")], w=[("ps", 4 + ip)])
                    r_, i_ = rr[q], ii[q]
                    act(r_[:, 0:n], pa[:, 0:n], AF.Sigmoid, r=[("ps", 2 + ip), "colp"], w=[K("rr")], bias=colv("lru_ba", j))
                    act(i_[:, 0:n], px[:, 0:n], AF.Sigmoid, r=[("ps", 4 + ip), "colp"], w=[K("ii")], bias=colv("lru_bx", j))
                    act(av[q][:, 0:n], r_[:, 0:n], AF.Exp, r=[K("rr"), "c1"], w=[K("av")], scale=c1[:, j:j + 1])
                    stt(r_[:, 0:n], av[q][:, 0:n], 0.9999998, av[q][:, 0:n], ALU.min, ALU.mult, r=[K("av")], w=[K("rr")])
                    act(r_[:, 0:n], r_[:, 0:n], AF.Sqrt, r=[K("rr")], w=[K("rr")], scale=-1.0, bias=1.0)
                    tt("dve", i_[:, 0:n], i_[:, 0:n], xb[:, 0:n], ALU.mult, r=[K("ii"), xbk], w=[K("ii")])
                    if bi == 0:
                        memset("dve", r_[:, PADN:PADN + 1], 1.0, w=[K("rr")])

                def S3(idx):
                    j, bi = items[idx]
                    t0, n = TB[bi]
                    i = idx % 2
                    q = idx % 4
                    jj = j % 4
                    K = lambda nm: (nm, q)
                    tt("dve", uv[i][:, 0:n], rr[q][:, 0:n], ii[q][:, 0:n], ALU.mult, r=[K("rr"), K("ii")], w=[("uv", i)])
                    if bi == 0:
                        memset("dve", hl[i][:, 0:PADN], 0.0, w=[("hl", i)])
                        S.add("dve", lambda e, i=i, q=q, n=n: e.tensor_tensor_scan(out=hl[i][:, PADN:n], data0=av[q][:, PADN:n], data1=uv[i][:, PADN:n],
                                                                             initial=0.0, op0=ALU.mult, op1=ALU.add),
                              r=[K("av"), ("uv", i)], w=[("hl", i)], cost=1.2)
                    else:
                        S.add("dve", lambda e, i=i, q=q, n=n: e.tensor_tensor_scan(out=hl[i][:, 0:n], data0=av[q][:, 0:n], data1=uv[i][:, 0:n],
                                                                             initial=hl[1 - i][:, 511:512], op0=ALU.mult, op1=ALU.add),
                              r=[K("av"), ("uv", i), ("hl", 1 - i)], w=[("hl", i)], cost=1.2)
                    tt("dve", ybuf[:, jj, t0:t0 + n], hl[i][:, 0:n], gl[q][:, 0:n], ALU.mult, r=[("hl", i), ("gl", q)], w=[("ybuf", jj)])
                    if bi == NB - 1 and jj == 3:
                        out_proj(8 + (j // 4) * 4, None)

                NI = len(items)
                for step in range(NI + 2):
                    if step < NI:
                        S1(step)
                    if 0 <= step - 1 < NI:
                        S2(step - 1)
                    if 0 <= step - 2 < NI:
                        S3(step - 2)

            def ssd():
                dt = A.a([128, NT, 16])
                adt = A.a([128, NT, 16])
                ea = A.a([128, NT, 16])
                eatot = A.a([128, NT, 16])
                dte = A.a([128, NT, 16])
                Aneg = A.a([128, 16])
                wdt = A.a([128, 8, 16], BF16)
                dma("pool", wdt, w_dt_d, w=["wdt"])
                for t in range(NT):
                    for k in range(8):
                        mm(ps[7][:, t * 16:(t + 1) * 16], hnT[:, k, t * 128:(t + 1) * 128], wdt[:, k, :], k == 0, k == 7,
                           r=["wdt", ("hnT", t)], w=[("ps", 7)])
                p7 = ps[7][:, 0:NT * 16].rearrange("p (t h) -> p t h", t=NT)
                tt("dve", dt, p7, rowv("dt_bias").unsqueeze(1).to_broadcast([128, NT, 16]), ALU.add, r=[("ps", 7), "rowp"], w=["dt"])
                act(dte, dt, AF.Abs, r=["dt"], w=["dte"])
                act(dte, dte, AF.Exp, r=["dte"], w=["dte"], scale=-1.0)
                act(dte, dte, AF.Ln, r=["dte"], w=["dte"], bias=1.0)
                stt(dt, dt, 0.0, dte, ALU.max, ALU.add, r=["dt", "dte"], w=["dt"])
                memset("dve", dt[0:PADN, 0, :], 0.0, w=["dt"])
                act(Aneg, rowv("a_log"), AF.Exp, r=["rowp"], w=["Aneg"])
                stt(adt, dt, -1.0, Aneg.unsqueeze(1).to_broadcast([128, NT, 16]), ALU.mult, ALU.mult, r=["dt", "Aneg"], w=["adt"])
                for t in range(NT):
                    mm(ps[6][:, t * 16:(t + 1) * 16], tri_le, adt[:, t, :], True, True, r=["cst", "adt"], w=[("ps", 6)])
                for t in range(NT):
                    mm(ps[7][:, t * 16:(t + 1) * 16], ones32, adt[:, t, :], True, True, r=["cst", "adt"], w=[("ps", 7)])
                p6 = ps[6][:, 0:NT * 16].rearrange("p (t h) -> p t h", t=NT)
                cp("dve", ea, p6, r=[("ps", 6)], w=["ea"])
                cp("dve", eatot, p7, r=[("ps", 7)], w=["eatot"])
                tt("dve", dte, eatot, ea, ALU.subtract, r=["eatot", "ea"], w=["dte"])
                act(dte, dte, AF.Exp, r=["dte"], w=["dte"])
                act(ea, ea, AF.Exp, r=["ea", "dte"], w=["ea"])
                act(eatot, eatot, AF.Exp, r=["eatot", "dte"], w=["eatot"])
                Dbc = rowv("ssd_d")
                DI = A.a([128, 2, 128], BF16)
                if SSD_STOP == "dt":
                    return

                BT = A.a([128, T], BF16)
                CT = A.a([128, T], BF16)
                Btok = A.a([128, NT, 128], BF16)
                CBm = A.a([128, NT, 128], BF16)
                xT16 = [A.a([128, 512], BF16) for _ in range(2)]
                xtok = A.a([128, NT, 128], BF16)
                xdt = A.a([128, NT, 128], BF16)
                sz = A.a([128, NT, 128], BF16)
                MT_all = A.a([128, NT, 256], BF16)
                S_all = A.a([128, NT, 128], BF16)
                ssp = A.a([128, NT, 4])
                rstdg = A.a([128, NT])
                Sst = A.a([128, 128])
                Lb = [A.a([128, 256]) for _ in range(2)]
                Db = [A.a([128, 256]) for _ in range(2)]
                xdte = [A.a([128, 128], BF16) for _ in range(4)]
                ytmp = [A.a([128, 128]) for _ in range(4)]
                yg16 = [A.a([128, 128], BF16) for _ in range(4)]
                junk = A.a([128, 128], BF16)

                def bank16(i):
                    return ps[i][:, 0:256].bitcast(BF16)

                def fm_chunk(cc, cidx, sink):
                    wi = load_wchunk(cc)
                    for bi, (t0, n) in enumerate(TB):
                        bank, bk = proj_blk(wi, bi, t0, n)
                        ct, ck = conv_blk(bank, bk, bi, n, "ssd_cw", "ssd_cb", cidx, tap0_act=True)
                        sink(bi, t0, n, ct, ck)

                for g in range(2):
                    pass
                    fm_chunk(8 + 8 + g, 8 + g, lambda bi, t0, n, ct, ck: act(BT[:, t0:t0 + n], ct[:, 0:n], AF.Silu, r=[ck], w=["BT"]))
                    fm_chunk(8 + 10 + g, 10 + g, lambda bi, t0, n, ct, ck: act(CT[:, t0:t0 + n], ct[:, 0:n], AF.Silu, r=[ck], w=["CT"]))
                    for q in range(5):
                        tiles = list(range(4 * q, min(4 * q + 4, NT)))
                        nn = len(tiles)
                        bk = bank16(6 + q % 2)
                        for i, t in enumerate(tiles):
                            tr(bk[:, i * 128:(i + 1) * 128], BT[:, t * 128:(t + 1) * 128], ident16, r=["BT", "cst16"], w=[("ps", 6 + q % 2)])
                        cp("dve", Btok[:, 4 * q:4 * q + nn, :], bk[:, 0:nn * 128].rearrange("p (a b) -> p a b", a=nn), r=[("ps", 6 + q % 2)], w=["Btok"])
                        bank = ps[2 + q % 2]
                        for i, t in enumerate(tiles):
                            mm(bank[:, i * 128:(i + 1) * 128], BT[:, t * 128:(t + 1) * 128], CT[:, t * 128:(t + 1) * 128], True, True,
                               r=["BT", "CT"], w=[("ps", 2 + q % 2)])
                        tt("dve", CBm[:, 4 * q:4 * q + nn, :], bank[:, 0:nn * 128].rearrange("p (a b) -> p a b", a=nn),
                           tri_le.unsqueeze(1).to_broadcast([128, nn, 128]), ALU.mult, r=[("ps", 2 + q % 2), "cst"], w=["CBm"])
                    pass
                    if SSD_STOP == "bc":
                        return
                    for jj in range(4):
                        j = g * 4 + jj
                        hh0 = 2 * j

                        def xsink(bi, t0, n, ct, ck, hh0=hh0):
                            xt = xT16[bi % 2]
                            xk = ("xT16", bi % 2)
                            act(xt[:, 0:n], ct[:, 0:n], AF.Silu, r=[ck], w=[xk])
                            nn = n // 128
                            bk = bank16(6 + bi % 2)
                            for i in range(nn):
                                tr(bk[:, i * 128:(i + 1) * 128], xt[:, i * 128:(i + 1) * 128], ident16, r=[xk, "cst16"], w=[("ps", 6 + bi % 2)])
                            a0 = t0 // 128
                            cp("act", xtok.rearrange("p a b -> p (a b)")[:, a0 * 128:(a0 + nn) * 128], bk[:, 0:nn * 128], r=[("ps", 6 + bi % 2)], w=[("xtok", bi)])
                            for i in range(nn):
                                t = a0 + i
                                tt("dve", xdt[:, t, :].rearrange("p (h c) -> p h c", h=2), xtok[:, t, :].rearrange("p (h c) -> p h c", h=2),
                                   dt[:, t, hh0:hh0 + 2].unsqueeze(2).to_broadcast([128, 2, 64]), ALU.mult, r=[("xtok", bi), "dt"], w=[("xdt", t)])
                        fm_chunk(8 + j, j, xsink)
                        if SSD_STOP == "x":
                            return
                        wz = load_wchunk(j)
                        for bi, (t0, n) in enumerate(TB):
                            bank, bk_ = proj_blk(wz, bi, t0, n)
                            zt = xT16[bi % 2]
                            zk = ("xT16", bi % 2)
                            act(zt[:, 0:n], bank[:, 0:n], AF.Silu, r=[bk_], w=[zk])
                            nn = n // 128
                            bk = bank16(6 + bi % 2)
                            for i in range(nn):
                                tr(bk[:, i * 128:(i + 1) * 128], zt[:, i * 128:(i + 1) * 128], ident16, r=[zk, "cst16"], w=[("ps", 6 + bi % 2)])
                            a0 = t0 // 128
                            cp("dve", sz.rearrange("p a b -> p (a b)")[:, a0 * 128:(a0 + nn) * 128], bk[:, 0:nn * 128], r=[("ps", 6 + bi % 2)], w=[("sz", bi)])
                        if SSD_STOP == "z":
                            return
                        memset("dve", Sst, 0.0, w=["Sst"])
                        memset("dve", S_all[:, 0, :], 0.0, w=[("S_all", 0)])
                        for hh in range(2):
                            act(DI[:, hh, :], ident, AF.Identity, r=["cst", "rowp"], w=["DI"], scale=Dbc[:, hh0 + hh:hh0 + hh + 1])
                        for c in range(NT):
                            i2 = c % 2
                            i4 = c % 4
                            for hh in range(2):
                                act(Lb[i2][:, hh * 128:(hh + 1) * 128], u_gt, AF.Identity, r=["cst", "adt"], w=[("L", i2, hh)], scale=adt[:, c, hh0 + hh:hh0 + hh + 1])
                                mm(ps[i2][:, hh * 128:(hh + 1) * 128], Lb[i2][:, hh * 128:(hh + 1) * 128], tri_le, True, True, r=[("L", i2, hh), "cst"], w=[("ps", i2)])
                            act(Db[i2], ps[i2][:, 0:256], AF.Exp, r=[("ps", i2)], w=[("D", i2)])
                            tt("dve", MT_all[:, c, :].rearrange("p (h l) -> p h l", h=2), Db[i2].rearrange("p (h l) -> p h l", h=2),
                               CBm[:, c, :].unsqueeze(1).to_broadcast([128, 2, 128]), ALU.mult, r=[("D", i2), "CBm"], w=[("MT", c)])
                            if c + 1 < NT:
                                tt("dve", xdte[i4].rearrange("p (h c) -> p h c", h=2), xdt[:, c, :].rearrange("p (h c) -> p h c", h=2),
                                   dte[:, c, hh0:hh0 + 2].unsqueeze(2).to_broadcast([128, 2, 64]), ALU.mult, r=[("xdt", c), "dte"], w=[("xdte", i4)])
                                mm(ps[2 + i2][:, 0:128], Btok[:, c, :], xdte[i4], True, True, r=["Btok", ("xdte", i4)], w=[("ps", 2 + i2)])
                                tt("dve", Sst.rearrange("p (h c) -> p h c", h=2), Sst.rearrange("p (h c) -> p h c", h=2),
                                   eatot[:, c, hh0:hh0 + 2].unsqueeze(2).to_broadcast([128, 2, 64]), ALU.mult, r=["Sst", "eatot"], w=["Sst"])
                                tt("dve", Sst, Sst, ps[2 + i2][:, 0:128], ALU.add, r=[("ps", 2 + i2), "Sst"], w=["Sst"])
                                cp("act", S_all[:, c + 1, :], Sst, r=["Sst"], w=[("S_all", c + 1)])
                        def p3_mm(c):
                            b = 4 + c % 2
                            pa = ps[b]
                            mm(pa[:, 0:128], CT[:, c * 128:(c + 1) * 128], S_all[:, c, :], True, True, r=["CT", ("S_all", c)], w=[("ps", b)])
                            for hh in range(2):
                                o = pa[:, 128 + hh * 64:128 + (hh + 1) * 64]
                                mm(o, MT_all[:, c, hh * 128:(hh + 1) * 128], xdt[:, c, hh * 64:(hh + 1) * 64], True, False, r=[("MT", c), ("xdt", c)], w=[("ps", b)])
                                mm(o, DI[:, hh, :], xtok[:, c, hh * 64:(hh + 1) * 64], False, True, r=["DI"] + [("xtok", bb) for bb in range(5)], w=[("ps", b)])

                        def p3_ev(c, jj=jj, j=j, hh0=hh0):
                            b = 4 + c % 2
                            pa = ps[b]
                            i4 = c % 4
                            y, yk = ytmp[i4], ("ytmp", i4)
                            tt("dve", y.rearrange("p (h c) -> p h c", h=2), pa[:, 0:128].rearrange("p (h c) -> p h c", h=2),
                               ea[:, c, hh0:hh0 + 2].unsqueeze(2).to_broadcast([128, 2, 64]), ALU.mult, r=[("ps", b), "ea"], w=[yk])
                            tt("dve", y, y, pa[:, 128:256], ALU.add, r=[("ps", b), yk], w=[yk])
                            yt, ytk = yg16[i4], ("yg", i4)
                            tt("dve", yt, y, sz[:, c, :], ALU.mult, r=[yk] + [("sz", bb) for bb in range(5)], w=[ytk])
                            act(junk, yt, AF.Square, r=[ytk], w=[("ssp", c), "junk"], accum=ssp[:, c, jj:jj + 1])
                            bk = bank16(6 + c % 2)
                            tr(bk[:, 0:128], yt, ident16, r=[ytk, "cst16"], w=[("ps", 6 + c % 2)])
                            act(ybuf[:, jj, c * 128:(c + 1) * 128], bk[:, 0:128], AF.Identity, r=[("ps", 6 + c % 2), "colp"], w=[("ybuf", jj)],
                                scale=colv("ssd_norm", j))

                        p3_mm(0)
                        for c in range(NT):
                            if c + 1 < NT:
                                p3_mm(c + 1)
                            p3_ev(c)
                    if SSD_STOP == "rec":
                        return
                    pass
                    S.add("dve", lambda e: e.tensor_reduce(out=rstdg, in_=ssp, axis=AX.X, op=ALU.add), r=[("ssp", c) for c in range(NT)], w=["rstdg"])
                    act(rstdg, rstdg, AF.Sqrt, r=["rstdg"], w=["rstdg"], scale=1.0 / 512, bias=colv_eps)
                    S.add("dve", lambda e: e.reciprocal(rstdg, rstdg), r=["rstdg"], w=["rstdg"])
                    out_proj(g * 4, lambda t: rstdg[:, t:t + 1])

            if "lru" in MIX0_PARTS:
                lru()
            S.barrier()
            A.off = mark
            if "ssd" in MIX0_PARTS:
                ssd()

        for ph in phases:
            {"mix0": mix0, "moe0": lambda: moe(0), "mix1": mix1, "moe1": lambda: moe(1)}[ph]()

        if dbg or not phases or not phases[-1].startswith("moe"):
            A.reset()
        if dbg:
            for t in range(NT):
                dma("sp", out_d[t * 128:(t + 1) * 128, :], h[:, t, :], r=HK(t), is_output=True)
        else:
            rms_stats(list(range(1, NT)))
            nf = A.a([128, D])
            dma("sp", nf, big_d[:, 0, :].partition_broadcast(128), w=["nf"])
            ob = [A.a([128, D]) for _ in range(2)]
            for t in range(1, NT):
                o = ob[t % 2]
                stt(o, h[:, t, :], rstd[:, t:t + 1], nf, ALU.mult, ALU.mult, r=HK(t) + [("rstd", t), "nf"], w=[("ob", t % 2)])
                dma("sp", out_d[(t - 1) * 128:t * 128, :], o, r=[("ob", t % 2)], is_output=True)
        if REORDER:
            S.reorder()
        S.emit()
    return nc


_CACHE = {}


def kernel(**inputs):
    shared = pack_inputs(inputs)
    x = np.ascontiguousarray(np.asarray(inputs["x"], np.float32))
    nb = x.shape[0]
    if "nc" not in _CACHE:
        _CACHE["nc"] = build()
    nc = _CACHE["nc"]
    in_maps = [dict(shared, x=x[b]) for b in range(nb)]
    res = run_bass_kernel_spmd(nc, in_maps, core_ids=list(range(nb)))
    return np.stack([np.asarray(r["out"], np.float32) for r in res.results], axis=0)
```

```python
from contextlib import ExitStack
import numpy as np
import concourse.bass as bass
import concourse.mybir as mybir
from concourse.bass_utils import run_bass_kernel_spmd

F32 = mybir.dt.float32
BF16 = mybir.dt.bfloat16
AF = mybir.ActivationFunctionType
ALU = mybir.AluOpType
AX = mybir.AxisListType

COMPUTE = ("pe", "act", "dve", "pool")
NDSEM = 16
SAME_ENGINE_GAP = 1 << 30

T = 2176
NT = 17
PADN = 112
D = 1024
TB = [(0, 512), (512, 512), (1024, 512), (1536, 512), (2048, 128)]
EPS = 1e-6
MIX0_PARTS = ("lru", "ssd")
SSD_STOP = None
REORDER = True


class Sched:
    def __init__(self, nc):
        self.nc = nc
        self.ops = {e: [] for e in ("pe", "act", "dve", "pool", "sp")}
        self.ccount = {e: 0 for e in COMPUTE}
        self.dcount = {"sp": 0, "pool": 0}
        self.last_w = {}
        self.readers = {}
        self.sig = {e: set() for e in COMPUTE}
        self.out_dmas = []
        self.pending_bar = {}
        self.ps_last = {}
        self.epoch = 0

    def _deps(self, tok, r, w, eng):
        deps = set()
        for k in r:
            t = self.last_w.get(k)
            if t is not None:
                deps.add(t)
        for k in w:
            t = self.last_w.get(k)
            if t is not None:
                deps.add(t)
            for t in self.readers.get(k, ()):
                deps.add(t)
        for k in w:
            self.last_w[k] = tok
            self.readers[k] = []
        for k in r:
            if k in w:
                continue
            self.readers.setdefault(k, []).append(tok)
        for k in set(r) | set(w):
            if isinstance(k, tuple) and k[0] == "ps":
                d = self.ps_last.setdefault(k, {})
                for oe in list(d):
                    if oe != eng:
                        deps.update(d[oe])
                        d[oe] = []
                d.setdefault(eng, []).append(tok)
        bar = self.pending_bar.pop(eng) if eng in self.pending_bar else set()
        deps.discard(tok)
        return deps, bar

    def barrier(self):
        toks = set()
        for e in COMPUTE:
            if self.ccount[e] > 0:
                toks.add(("c", e, self.ccount[e] - 1))
        for q in ("sp", "pool"):
            for k in range(max(0, self.dcount[q] - NDSEM), self.dcount[q]):
                toks.add(("d", q, k))
        for e in ("pe", "act", "dve", "pool", "sp"):
            self.pending_bar[e] = set(toks) | self.pending_bar.get(e, set())
        self.epoch += 1

    def add(self, eng, fn, r=(), w=(), cost=0.3):
        seq = self.ccount[eng]
        self.ccount[eng] += 1
        tok = ("c", eng, seq)
        deps, bar = self._deps(tok, tuple(r), tuple(w), eng)
        self.ops[eng].append(["c", fn, deps, seq, cost, self.epoch, bar])
        return tok

    def reorder(self, window=256, lat=1.5):
        RE = ("pe", "act", "dve")
        fin = {}
        etime = {e: 0.0 for e in self.ops}
        new_order = {e: [] for e in RE}
        ptr = {e: 0 for e in self.ops}
        fence = 0.0
        tokof = lambda e, op: ("c", e, op[3]) if op[0] == "c" else ("d", e, op[3])
        for ep in range(self.epoch + 1):
            seg = {}
            for e in self.ops:
                lst = self.ops[e]
                i = ptr[e]
                j = i
                while j < len(lst) and lst[j][5] == ep:
                    j += 1
                seg[e] = lst[i:j]
                ptr[e] = j
            for e in seg:
                etime[e] = max(etime[e], fence)
            left = {e: list(seg[e]) for e in seg}
            total = sum(len(v) for v in left.values())
            while total:
                best = None
                for e, lst in left.items():
                    if not lst:
                        continue
                    cands = lst[:window] if e in RE else lst[:1]
                    for pos, op in enumerate(cands):
                        ready = 0.0
                        ok = True
                        for t in op[2]:
                            f = fin.get(t)
                            if f is None:
                                ok = False
                                break
                            if t[1] != e:
                                f += lat
                            if f > ready:
                                ready = f
                        if not ok:
                            continue
                        start = max(etime[e], ready)
                        key = (start, pos)
                        if best is None or key < best[0]:
                            best = (key, e, pos, op, start)
                        if start <= etime[e]:
                            break
                assert best is not None, "scheduler stuck"
                _, e, pos, op, start = best
                left[e].pop(pos)
                total -= 1
                if op[0] == "c":
                    etime[e] = start + op[4]
                    fin[tokof(e, op)] = etime[e]
                else:
                    etime[e] = start + 0.5
                    fin[tokof(e, op)] = start + op[4]
                if e in RE:
                    new_order[e].append(op)
            fence = max([fence] + list(etime.values()) + [fin[tokof(e, op)] for e in seg for op in seg[e]])
        remap = {}
        for e in RE:
            bars = {}
            for op in new_order[e]:
                if op[6]:
                    bars.setdefault(op[5], set()).update(op[6])
                    op[6] = set()
            seen_ep = set()
            for i, op in enumerate(new_order[e]):
                if op[5] not in seen_ep:
                    seen_ep.add(op[5])
                    op[6] = bars.get(op[5], set())
                remap[("c", e, op[3])] = ("c", e, i)
                op[3] = i
            self.ops[e] = new_order[e]
        for e, lst in self.ops.items():
            for op in lst:
                op[2] = {remap.get(t, t) for t in op[2]}
                op[6] = {remap.get(t, t) for t in op[6]}
        self.model_time = max(etime.values())

    def dma(self, q, fn, r=(), w=(), is_output=False, cost=3.0):
        k = self.dcount[q]
        self.dcount[q] += 1
        tok = ("d", q, k)
        deps, bar = self._deps(tok, tuple(r), tuple(w), q)
        self.ops[q].append(["d", fn, deps, k, cost, self.epoch, bar])
        if is_output:
            self.out_dmas.append(tok)
        return tok

    def emit(self):
        nc = self.nc
        for e, lst in self.ops.items():
            for op in lst:
                op[2] = set(op[2]) | set(op[6])
        for e, lst in self.ops.items():
            seen_c = {f: -1 for f in COMPUTE}
            for kind, fn, deps, seq, _c, _e, _b in lst:
                cw = {}
                for t in deps:
                    if t[0] != "c":
                        continue
                    f, s = t[1], t[2]
                    if f == e and kind == "c":
                        if e == "pe":
                            continue
                        if seq - s > SAME_ENGINE_GAP:
                            continue
                    if s <= seen_c[f]:
                        continue
                    cw[f] = max(cw.get(f, -1), s)
                for f, s in cw.items():
                    seen_c[f] = s
                    self.sig[f].add(s)
        sigidx = {}
        for e in COMPUTE:
            s = sorted(self.sig[e])
            sigidx[e] = {seq: i + 1 for i, seq in enumerate(s)}
        with ExitStack() as st:
            csem = {e: st.enter_context(nc.semaphore("c_" + e)) for e in COMPUTE}
            dsem = {q: [st.enter_context(nc.semaphore(f"d_{q}{i}")) for i in range(NDSEM)]
                    for q in ("sp", "pool")}
            block = st.enter_context(nc.Block())

            def run(e, eng):
                seen_c = {f: -1 for f in COMPUTE}
                seen_d = {}
                for kind, fn, deps, seq, _c, _e, _b in self.ops[e]:
                    cw = {}
                    for t in deps:
                        if t[0] == "c":
                            f, s = t[1], t[2]
                            if f == e and kind == "c":
                                if e == "pe":
                                    continue
                                if seq - s > SAME_ENGINE_GAP:
                                    continue
                            if s <= seen_c[f]:
                                continue
                            cw[f] = max(cw.get(f, -1), s)
                        else:
                            q, k = t[1], t[2]
                            key = (q, k % NDSEM)
                            val = 16 * (k // NDSEM + 1)
                            if seen_d.get(key, 0) >= val:
                                continue
                            seen_d[key] = val
                            eng.wait_ge(dsem[q][k % NDSEM], val)
                    for f, s in cw.items():
                        seen_c[f] = s
                        eng.wait_ge(csem[f], sigidx[f][s])
                    if kind == "c":
                        ins = fn(eng)
                        if seq in sigidx[e]:
                            ins.then_inc(csem[e], 1)
                    else:
                        k = seq
                        if k >= NDSEM:
                            key = (e, k % NDSEM)
                            val = 16 * (k // NDSEM)
                            if seen_d.get(key, 0) < val:
                                seen_d[key] = val
                                eng.wait_ge(dsem[e][k % NDSEM], val)
                        ins = fn(eng)
                        ins.then_inc(dsem[e][k % NDSEM], 16)
                if e == "sp":
                    for t in self.out_dmas:
                        q, k = t[1], t[2]
                        eng.wait_ge(dsem[q][k % NDSEM], 16 * (k // NDSEM + 1))

            @block.tensor
            def _(eng):
                run("pe", eng)

            @block.scalar
            def _(eng):
                run("act", eng)

            @block.vector
            def _(eng):
                run("dve", eng)

            @block.gpsimd
            def _(eng):
                run("pool", eng)

            @block.sync
            def _(eng):
                run("sp", eng)


COLP = {}
_o = 0
for _n, _c in [("mix_even", 8), ("ffn0", 8), ("ffn1", 8), ("mix_odd", 8), ("ssd_cw", 48), ("ssd_cb", 12),
               ("ssd_norm", 8), ("lru_cw", 32), ("lru_cb", 8), ("lru_ba", 8), ("lru_bx", 8), ("lru_lam", 8)]:
    COLP[_n] = (_o, _c)
    _o += _c
NCOL = _o
ROWP = {}
_o = 0
for _n, _c in [("dt_bias", 16), ("a_log", 16), ("ssd_d", 16), ("rb0", 20), ("rb1", 20), ("rc", 64)]:
    ROWP[_n] = (_o, _c)
    _o += _c
NROW = _o


def _fm(v):
    v = np.asarray(v, np.float32).reshape(-1, 128)
    return np.ascontiguousarray(v.T)


def _pool_band_consts():
    A = np.zeros((128, 12, 128), np.float32)
    s_ = np.arange(128)[:, None]
    t_ = np.arange(128)[None, :]
    for g, w in enumerate((2, 4, 8, 16)):
        inwin = ((t_ - s_) >= 0) & ((t_ - s_) < w)
        A[:, 3 * g + 0, :] = inwin / float(w) - (s_ == t_)
        A[:, 3 * g + 1, :] = (((t_ + 128 - s_) < w) & ((t_ + 128 - s_) >= 0)) / float(w)
        cnt = np.where(t_ >= PADN, np.minimum(t_ - PADN + 1, w), w).astype(np.float32)
        A[:, 3 * g + 2, :] = inwin / cnt - (s_ == t_)
    return A


def pack_inputs(inp):
    f = lambda a: np.ascontiguousarray(np.asarray(a, np.float32))
    colp = np.zeros((128, NCOL), np.float32)

    def put(name, arr):
        o, c = COLP[name]
        assert arr.shape == (128, c), (name, arr.shape)
        colp[:, o:o + c] = arr

    put("mix_even", _fm(inp["mix_norm_even"][0]))
    put("ffn0", _fm(inp["ffn_norm"][0]))
    put("ffn1", _fm(inp["ffn_norm"][1]))
    put("mix_odd", _fm(inp["mix_norm_odd"][0]))
    cw = np.asarray(inp["ssd_conv_w"][0], np.float32)
    put("ssd_cw", np.concatenate([_fm(cw[k]) for k in range(4)], axis=1).reshape(128, 4, 12).transpose(0, 2, 1).reshape(128, 48))
    put("ssd_cb", _fm(inp["ssd_conv_b"][0]))
    put("ssd_norm", _fm(inp["ssd_norm"][0]))
    lw = np.asarray(inp["lru_conv_w"][0], np.float32)
    put("lru_cw", np.concatenate([_fm(lw[k]) for k in range(4)], axis=1).reshape(128, 4, 8).transpose(0, 2, 1).reshape(128, 32))
    put("lru_cb", _fm(inp["lru_conv_b"][0]))
    put("lru_ba", _fm(inp["lru_b_a"][0]))
    put("lru_bx", _fm(inp["lru_b_x"][0]))
    put("lru_lam", _fm(inp["lru_lambda"][0]))

    rowp = np.zeros((1, NROW), np.float32)

    def putr(name, arr):
        o, c = ROWP[name]
        rowp[0, o:o + c] = np.asarray(arr, np.float32).reshape(-1)

    putr("dt_bias", inp["ssd_dt_bias"][0])
    putr("a_log", inp["ssd_a_log"][0])
    putr("ssd_d", inp["ssd_d"][0])
    putr("rb0", np.concatenate([np.asarray(inp["router_group_b"][0]), np.asarray(inp["router_expert_b"][0])]))
    putr("rb1", np.concatenate([np.asarray(inp["router_group_b"][1]), np.asarray(inp["router_expert_b"][1])]))
    rc = np.zeros((4, 16), np.float32)
    for g, w in enumerate((2, 4, 8, 16)):
        rc[g] = 1.0 / np.minimum(np.arange(16) + 1, w)
    putr("rc", rc)

    w_in = f(inp["w_in"][0])
    cols = np.concatenate([np.arange(0, 2560), np.arange(2576, 4624)])
    w_in_r = np.ascontiguousarray(w_in[:, cols].reshape(8, 128, 36, 128).transpose(2, 1, 0, 3))
    w_dt = np.ascontiguousarray(w_in[:, 2560:2576].reshape(8, 128, 16).transpose(1, 0, 2))
    wr = np.stack([np.concatenate([f(inp["router_group_w"][l]), f(inp["router_expert_w"][l])], axis=1)
                   .reshape(8, 128, 20).transpose(1, 0, 2) for l in range(2)])
    k_ = np.arange(128)[:, None]
    s_ = np.arange(128)[None, :]
    cst = np.stack([np.eye(128, dtype=np.float32), (k_ <= s_).astype(np.float32), (k_ > s_).astype(np.float32),
                    np.ones((128, 128), np.float32)], axis=1)
    shared = {
        "meta": f(inp["meta_tokens"]), "colp": colp, "rowp": rowp, "cst": np.ascontiguousarray(cst),
        "w_in_r": w_in_r, "w_dt": w_dt, "w_out": f(inp["w_out"][0]),
        "lru_wa": f(inp["lru_w_a"][0]), "lru_wx": f(inp["lru_w_x"][0]),
        "pool_w": f(inp["pool_w"][0]), "wr": np.ascontiguousarray(wr),
        "bigrow": np.ascontiguousarray(np.stack([f(inp["norm_final"]), f(inp["pool_b"][0]), f(inp["pool_scale"][0]),
                                                 f(inp["mix_norm_odd"][0])])[None]),
        "poolA": _pool_band_consts(),
        "wg": f(inp["expert_w_gate"]), "wu": f(inp["expert_w_up"]), "wd": f(inp["expert_w_down"]),
    }
    return shared


def build(phases=("mix0", "moe0", "mix1", "moe1"), dbg=False):
    nc = bass.Bass("TRN2", target_bir_lowering=False)
    dram = lambda n, s, kind="ExternalInput": nc.dram_tensor(n, list(s), F32, kind=kind).ap()
    x_d = dram("x", [2048, D])
    meta_d = dram("meta", [16, D])
    colp_d = dram("colp", [128, NCOL])
    rowp_d = dram("rowp", [1, NROW])
    cst_d = dram("cst", [128, 4, 128])
    w_in_d = dram("w_in_r", [36, 128, 8, 128])
    w_dt_d = dram("w_dt", [128, 8, 16])
    w_out_d = dram("w_out", [2048, D])
    lwa_d = dram("lru_wa", [16, 64, 64])
    lwx_d = dram("lru_wx", [16, 64, 64])
    pw_d = dram("pool_w", [4, 256, 256])
    wr_d = dram("wr", [2, 128, 8, 20])
    big_d = dram("bigrow", [1, 4, D])
    pA_d = dram("poolA", [128, 12, 128])
    wg_d = dram("wg", [2, 16, D, 512])
    wu_d = dram("wu", [2, 16, D, 512])
    wd_d = dram("wd", [2, 16, 512, D])
    if dbg:
        out_d = dram("out", [T, D], kind="ExternalOutput")
    else:
        out_d = dram("out", [2048, D], kind="ExternalOutput")

    S = Sched(nc)
    with ExitStack() as st:
        sb = lambda n, s, dt=F32: st.enter_context(nc.sbuf_tensor(n, list(s), dt))
        h = sb("h", [128, NT, D])
        hnT = sb("hnT", [128, 8, T], BF16)
        colp = sb("colp_s", [128, NCOL])
        rowp = sb("rowp_s", [128, NROW])
        cst = sb("cst_s", [128, 4, 128])
        cst16 = sb("cst16", [128, 4, 128], BF16)
        stat = sb("stat", [128, 64])
        AW = 25900
        arena = sb("arena", [128, AW])
        ps = [st.enter_context(nc.psum_tensor(f"ps{i}", [128, 512], F32)) for i in range(8)]
        ident = cst[:, 0, :]
        tri_le = cst[:, 1, :]
        u_gt = cst[:, 2, :]
        ones32 = cst[:, 3, :]
        ident16 = cst16[:, 0, :]
        ss = stat[:, 0:17]
        sq = stat[:, 17:34]
        rstd = stat[:, 34:51]

        class Arena:
            def __init__(self):
                self.off = 0

            def reset(self):
                self.off = 0
                S.barrier()

            def a(self, shape, dt=F32):
                n = int(np.prod(shape[1:]))
                words = n if dt == F32 else (n + 1) // 2
                assert self.off + words <= AW, ("arena overflow", self.off, words)
                v = arena[:, self.off:self.off + words]
                self.off += words
                if dt != F32:
                    v = v.bitcast(dt)
                    if v.shape[1] != n:
                        v = v[:, 0:n]
                if len(shape) == 3:
                    v = v.rearrange("p (a b) -> p a b", a=shape[1])
                elif len(shape) == 4:
                    v = v.rearrange("p (a b c) -> p a b c", a=shape[1], b=shape[2])
                return v

        A = Arena()
        colv = lambda name, i=0, n=None: colp[:, COLP[name][0] + i:COLP[name][0] + i + (n if n else 1)]
        rowv = lambda name: rowp[:, ROWP[name][0]:ROWP[name][0] + ROWP[name][1]]

        def fsz(ap):
            n = 1
            for d_ in ap.shape[1:]:
                n *= int(d_)
            return n

        def ecost(eng, n, mult=1.0):
            if eng == "dve":
                return 0.12 + mult * n / 960.0
            if eng == "act":
                return 0.25 + n / 1400.0
            return 2.0 + 0.015 * n

        def mm(out, lhsT, rhs, start, stop, r, w):
            c = max(fsz(rhs), 64) / 2400.0 + 0.03
            if lhsT.dtype == F32:
                c *= 4
            S.add("pe", lambda e: e.matmul(out, lhsT, rhs, start=start, stop=stop), r=r, w=w, cost=c)

        def tr(out, in_, idn, r, w):
            S.add("pe", lambda e: e.transpose(out, in_, idn), r=r, w=w, cost=0.3 if in_.dtype == F32 else 0.12)

        def act(out, in_, func, r, w, scale=1.0, bias=0.0, accum=None):
            c = ecost("act", fsz(in_)) + (0.1 if accum is not None else 0.0)
            if accum is None:
                S.add("act", lambda e: e.activation(out=out, in_=in_, func=func, scale=scale, bias=bias), r=r, w=w, cost=c)
            else:
                S.add("act", lambda e: e.activation(out=out, in_=in_, func=func, scale=scale, bias=bias, accum_out=accum), r=r, w=w, cost=c)

        def tt(eng, out, in0, in1, op, r, w):
            S.add(eng, lambda e: e.tensor_tensor(out=out, in0=in0, in1=in1, op=op), r=r, w=w, cost=ecost(eng, fsz(out)))

        def ts(eng, out, in0, s1, op0, r, w, s2=None, op1=None):
            c = ecost(eng, fsz(out))
            if op1 is None:
                S.add(eng, lambda e: e.tensor_scalar(out=out, in0=in0, scalar1=s1, scalar2=None, op0=op0), r=r, w=w, cost=c)
            else:
                S.add(eng, lambda e: e.tensor_scalar(out=out, in0=in0, scalar1=s1, scalar2=s2, op0=op0, op1=op1), r=r, w=w, cost=c)

        def stt(out, in0, scalar, in1, op0, op1, r, w):
            S.add("dve", lambda e: e.scalar_tensor_tensor(out=out, in0=in0, scalar=scalar, in1=in1, op0=op0, op1=op1), r=r, w=w,
                  cost=ecost("dve", fsz(out)))

        def cp(eng, out, in_, r, w):
            c = ecost(eng, fsz(out))
            if eng == "act":
                S.add("act", lambda e: e.copy(out, in_), r=r, w=w, cost=c)
            else:
                S.add(eng, lambda e: e.tensor_copy(out, in_), r=r, w=w, cost=c)

        def memset(eng, ap, val, w):
            S.add(eng, lambda e: e.memset(ap, val), w=w, cost=ecost(eng, fsz(ap)))

        def dma(q, out, in_, r=(), w=(), is_output=False):
            nbytes = fsz(out) * int(out.shape[0]) * 4
            S.dma(q, lambda e: e.dma_start(out=out, in_=in_), r=r, w=w, is_output=is_output, cost=2.5 + nbytes / 150e3)

        HK = lambda t: [("h", t, 0), ("h", t, 1)]

        dma("sp", colp[:], colp_d, w=["colp"])
        dma("sp", rowp[:], rowp_d.partition_broadcast(128), w=["rowp"])
        dma("sp", cst[:], cst_d, w=["cst"])
        dma("pool", cst16[:], cst_d, w=["cst16"])
        memset("dve", h[:, 0, :], 0.0, w=HK(0))
        dma("sp", h[PADN:128, 0, :], meta_d, w=HK(0))
        xr = x_d.rearrange("(t p) d -> p t d", p=128)
        for i in range(4):
            dma("sp", h[:, 1 + 4 * i:5 + 4 * i, :], xr[:, 4 * i:4 * i + 4, :], w=[k for t in range(1 + 4 * i, 5 + 4 * i) for k in HK(t)])

        def rms_stats(tiles):
            junk = A.a([128, D], BF16)
            for t in tiles:
                act(junk, h[:, t, :], AF.Square, r=HK(t), w=[("ss", t), "junk"], accum=ss[:, t:t + 1])
                act(sq[:, t:t + 1], ss[:, t:t + 1], AF.Sqrt, r=[("ss", t)], w=[("sq", t)], scale=1.0 / D, bias=colv_eps)
                S.add("dve", lambda e, t=t: e.reciprocal(rstd[:, t:t + 1], sq[:, t:t + 1]), r=[("sq", t)], w=[("rstd", t)], cost=0.15)

        def normT(gname, router_l=None, logits=None, wr_s=None, consume=None):
            xsb = [A.a([128, D]) for _ in range(2)]
            t32 = [A.a([128, 4, 128]) for _ in range(4)]
            pend = None
            for t in range(NT):
                xs = xsb[t % 2]
                act(xs, h[:, t, :], AF.Identity, r=HK(t) + [("rstd", t)], w=[("xs", t % 2)], scale=rstd[:, t:t + 1])
                for half in range(2):
                    i2 = (2 * t + half) % 2
                    i4 = (2 * t + half) % 4
                    bank = ps[6 + i2]
                    for kk in range(4):
                        k = half * 4 + kk
                        tr(bank[:, kk * 128:(kk + 1) * 128], xs[:, k * 128:(k + 1) * 128], ident, r=[("xs", t % 2), "cst"], w=[("ps", 6 + i2)])
                    g0 = COLP[gname][0] + half * 4
                    tt("dve", t32[i4], bank[:, :].rearrange("p (a b) -> p a b", a=4),
                       colp[:, g0:g0 + 4].unsqueeze(2).to_broadcast([128, 4, 128]), ALU.mult,
                       r=[("ps", 6 + i2), "colp"], w=[("t32", i4)])
                    cp("act", hnT[:, half * 4:half * 4 + 4, t * 128:(t + 1) * 128], t32[i4], r=[("t32", i4)], w=[("hnT", t)])
                if router_l is not None:
                    if pend is not None:
                        pend()

                    def mk(t=t):
                        def go():
                            for k in range(8):
                                i4 = (2 * t + k // 4) % 4
                                mm(ps[5][:, 0:20], t32[i4][:, k % 4, :], wr_s[:, k, :], k == 0, k == 7,
                                   r=[("t32", i4), "wr"], w=[("ps", 5)])
                            cp("act", logits[:, t, :], ps[5][:, 0:20], r=[("ps", 5)], w=["logits"])
                        return go
                    pend = mk()
            if pend is not None:
                pend()

        memset("pool", stat[:, 60:61], EPS, w=["eps"])
        colv_eps = stat[:, 60:61]

        def moe(l):
            A.reset()
            wr_s = A.a([128, 8, 20])
            logits = A.a([128, NT, 20])
            gates = A.a([128, NT, 16])
            dma("sp", wr_s, wr_d[l], w=["wr"])
            Wg = [A.a([128, 8, 512], BF16) for _ in range(2)]
            Wu = [A.a([128, 8, 512], BF16) for _ in range(2)]
            Wd = [A.a([128, 4, D], BF16) for _ in range(2)]

            def load_w(e):
                b = e % 2
                if e == 0:
                    for fc in range(4):
                        fs = slice(fc * 128, (fc + 1) * 128)
                        dma("pool", Wg[b][:, :, fs], wg_d[l, e][:, fs].rearrange("(k p) f -> p k f", p=128), w=[("Wg", b, fc)])
                        dma("pool", Wu[b][:, :, fs], wu_d[l, e][:, fs].rearrange("(k p) f -> p k f", p=128), w=[("Wu", b, fc)])
                else:
                    dma("pool", Wg[b], wg_d[l, e].rearrange("(k p) f -> p k f", p=128), w=[("Wg", b, fc) for fc in range(4)])
                    dma("pool", Wu[b], wu_d[l, e].rearrange("(k p) f -> p k f", p=128), w=[("Wu", b, fc) for fc in range(4)])
                dma("pool", Wd[b], wd_d[l, e].rearrange("(k p) f -> p k f", p=128), w=[("Wd", b)])

            load_w(0)
            load_w(1)
            mark = A.off
            rms_stats(list(range(NT)))
            normT("ffn%d" % l, router_l=l, logits=logits, wr_s=wr_s)
            R = lambda shape: A.a(shape)
            rb = rowv("rb%d" % l)
            lg = R([128, NT, 20])
            tt("dve", lg, logits, rb.unsqueeze(1).to_broadcast([128, NT, 20]), ALU.add, r=["logits", "rowp"], w=["lg"])
            m4 = R([128, NT])
            S.add("dve", lambda e: e.tensor_reduce(out=m4, in_=lg[:, :, 0:4], axis=AX.X, op=ALU.max), r=["lg"], w=["m4"])
            d4 = R([128, NT, 4])
            tt("dve", d4, lg[:, :, 0:4], m4.unsqueeze(2).to_broadcast([128, NT, 4]), ALU.subtract, r=["lg", "m4"], w=["d4"])
            mg = R([128, NT, 4])
            ts("dve", mg, d4, 0.0, ALU.is_ge, r=["d4"], w=["mg"])
            e4 = R([128, NT, 4])
            act(e4, d4, AF.Exp, r=["d4"], w=["e4"])
            s4 = R([128, NT])
            S.add("dve", lambda e: e.tensor_reduce(out=s4, in_=e4, axis=AX.X, op=ALU.add), r=["e4"], w=["s4"])
            le = lg[:, :, 4:20].rearrange("p t (g j) -> p t g j", g=4)
            ml = R([128, NT, 4, 4])
            tt("dve", ml, le, mg.unsqueeze(3).to_broadcast([128, NT, 4, 4]), ALU.mult, r=["lg", "mg"], w=["ml"])
            sel = R([128, NT, 4])
            tt("dve", sel, ml[:, :, 0, :], ml[:, :, 1, :], ALU.add, r=["ml"], w=["sel"])
            tt("dve", sel, sel, ml[:, :, 2, :], ALU.add, r=["ml", "sel"], w=["sel"])
            tt("dve", sel, sel, ml[:, :, 3, :], ALU.add, r=["ml", "sel"], w=["sel"])
            m1 = R([128, NT])
            S.add("dve", lambda e: e.tensor_reduce(out=m1, in_=sel, axis=AX.X, op=ALU.max), r=["sel"], w=["m1"])
            k1 = R([128, NT, 4])
            tt("dve", k1, sel, m1.unsqueeze(2).to_broadcast([128, NT, 4]), ALU.is_ge, r=["sel", "m1"], w=["k1"])
            sel2 = R([128, NT, 4])
            stt(sel2, k1, -1e30, sel, ALU.mult, ALU.add, r=["k1", "sel"], w=["sel2"])
            m2 = R([128, NT])
            S.add("dve", lambda e: e.tensor_reduce(out=m2, in_=sel2, axis=AX.X, op=ALU.max), r=["sel2"], w=["m2"])
            k2 = R([128, NT, 4])
            tt("dve", k2, sel2, m2.unsqueeze(2).to_broadcast([128, NT, 4]), ALU.is_ge, r=["sel2", "m2"], w=["k2"])
            dd = R([128, NT])
            tt("dve", dd, m2, m1, ALU.subtract, r=["m1", "m2"], w=["dd"])
            w2 = R([128, NT])
            act(w2, dd, AF.Exp, r=["dd"], w=["w2"])
            den = R([128, NT])
            stt(den, w2, 1.0, s4, ALU.add, ALU.mult, r=["w2", "s4"], w=["den"])
            g1 = R([128, NT])
            S.add("dve", lambda e: e.reciprocal(g1, den), r=["den"], w=["g1"])
            g2 = R([128, NT])
            tt("dve", g2, g1, w2, ALU.mult, r=["g1", "w2"], w=["g2"])
            gs = R([128, NT, 4])
            tt("dve", gs, k1, g1.unsqueeze(2).to_broadcast([128, NT, 4]), ALU.mult, r=["k1", "g1"], w=["gs"])
            gs2 = R([128, NT, 4])
            tt("dve", gs2, k2, g2.unsqueeze(2).to_broadcast([128, NT, 4]), ALU.mult, r=["k2", "g2"], w=["gs2"])
            tt("dve", gs, gs, gs2, ALU.add, r=["gs", "gs2"], w=["gs"])
            g4 = gates.rearrange("p t (g j) -> p t g j", g=4)
            for g in range(4):
                tt("dve", g4[:, :, g, :], gs, mg[:, :, g:g + 1].to_broadcast([128, NT, 4]), ALU.mult, r=["gs", "mg"], w=["gates"])

            hid = [A.a([128, 4, 512], BF16) for _ in range(2)]
            sg = [A.a([128, 512]) for _ in range(2)]
            for hb_ in range(2):
                memset("dve", hid[hb_], 0.0, w=[("hid", hb_)])
            TBM = [(PADN, 128 - PADN), (128, 512), (640, 512), (1152, 512), (1664, 512)]
            cnt = 0
            cnt_o = [0]
            blk = 0
            pend = None
            for e in range(16):
                b = e % 2
                if e >= 2:
                    load_w(e)
                for (t0, n) in TBM:
                    hb = blk % 2
                    blk += 1
                    hk = [("hnT", tt_) for tt_ in range(t0 // 128, (t0 + n + 127) // 128)]
                    ho = t0 % 128
                    for fc in range(4):
                        i = cnt % 2
                        cnt += 1
                        pg, pu = ps[i], ps[2 + i]
                        for k in range(8):
                            mm(pg[:, 0:n], Wg[b][:, k, fc * 128:(fc + 1) * 128], hnT[:, k, t0:t0 + n], k == 0, k == 7,
                               r=[("Wg", b, fc)] + hk, w=[("ps", i)])
                        for k in range(8):
                            mm(pu[:, 0:n], Wu[b][:, k, fc * 128:(fc + 1) * 128], hnT[:, k, t0:t0 + n], k == 0, k == 7,
                               r=[("Wu", b, fc)] + hk, w=[("ps", 2 + i)])
                        act(sg[i][:, 0:n], pg[:, 0:n], AF.Silu, r=[("ps", i)], w=[("sg", i)])
                        tt("dve", hid[hb][:, fc, ho:ho + n], sg[i][:, 0:n], pu[:, 0:n], ALU.mult, r=[("sg", i), ("ps", 2 + i)], w=[("hid", hb)])
                    if pend is not None:
                        pend()

                    def mk(e=e, b=b, hb=hb, t0=t0, n=n):
                        def go():
                            for tl in range((n + 127) // 128):
                                t = t0 // 128 + tl
                                for dh in range(2):
                                    io = 4 + cnt_o[0] % 2
                                    cnt_o[0] += 1
                                    for fc in range(4):
                                        mm(ps[io][:, :], hid[hb][:, fc, tl * 128:(tl + 1) * 128], Wd[b][:, fc, dh * 512:(dh + 1) * 512],
                                           fc == 0, fc == 3, r=[("hid", hb), ("Wd", b)], w=[("ps", io)])
                                    hv = h[:, t, dh * 512:(dh + 1) * 512]
                                    stt(hv, ps[io][:, :], gates[:, t, e:e + 1], hv,
                                        ALU.mult, ALU.add, r=[("ps", io), "gates", ("h", t, dh)], w=[("h", t, dh)])
                        return go
                    pend = mk()
            pend()

        def mix1():
            A.reset()
            memset("pool", h[0:PADN, 0, :], 0.0, w=HK(0))
            rms_stats(list(range(NT)))
            pw = A.a([128, 4, 2, 256], BF16)
            for g in range(4):
                dma("pool", pw[:, g, :, :], pw_d[g].rearrange("(c p) j -> p c j", p=128), w=["pw"])
            pA = A.a([128, 12, 128], BF16)
            dma("pool", pA, pA_d, w=["pA"])
            pb_bc = A.a([128, D])
            sc_bc = A.a([128, D])
            gn_bc = A.a([128, D])
            dma("sp", pb_bc, big_d[:, 1, :].partition_broadcast(128), w=["pb_bc"])
            dma("sp", sc_bc, big_d[:, 2, :].partition_broadcast(128), w=["sc_bc"])
            dma("sp", gn_bc, big_d[:, 3, :].partition_broadcast(128), w=["gn_bc"])
            bs_bc = A.a([128, D])
            tt("dve", bs_bc, pb_bc, sc_bc, ALU.mult, r=["pb_bc", "sc_bc"], w=["bs_bc"])
            bs16 = A.a([128, D], BF16)
            cp("dve", bs16, bs_bc, r=["bs_bc"], w=["bs16"])
            pws = A.a([128, 4, 2, 256], BF16)
            for g in range(4):
                tt("dve", pws[:, g, :, :], pw[:, g, :, :], sc_bc[:, g * 256:(g + 1) * 256].unsqueeze(1).to_broadcast([128, 2, 256]), ALU.mult,
                   r=["pw", "sc_bc"], w=["pws"])
            xsn = A.a([128, NT, D], BF16)
            for t in range(NT):
                stt(xsn[:, t, :], h[:, t, :], rstd[:, t:t + 1], gn_bc, ALU.mult, ALU.mult, r=HK(t) + [("rstd", t), "gn_bc"], w=[("xsn", t)])
            pooledT = A.a([128, 8, T], BF16)
            ev = 0
            for k in range(8):
                g = k // 2
                for q in range(5):
                    tiles = list(range(4 * q, min(4 * q + 4, NT)))
                    bi_ = 6 + (k * 5 + q) % 2
                    bank = ps[bi_]
                    for i, t in enumerate(tiles):
                        o = bank[:, i * 128:(i + 1) * 128]
                        if t == 0:
                            mm(o, xsn[:, 0, k * 128:(k + 1) * 128], pA[:, 3 * g + 2, :], True, True, r=[("xsn", 0), "pA"], w=[("ps", bi_)])
                        else:
                            mm(o, xsn[:, t - 1, k * 128:(k + 1) * 128], pA[:, 3 * g + 1, :], True, False, r=[("xsn", t - 1), "pA"], w=[("ps", bi_)])
                            mm(o, xsn[:, t, k * 128:(k + 1) * 128], pA[:, 3 * g + 0, :], False, True, r=[("xsn", t), "pA"], w=[("ps", bi_)])
                    nn = len(tiles) * 128
                    cp("act" if ev % 2 == 0 else "dve", pooledT[:, k, q * 512:q * 512 + nn], bank[:, 0:nn], r=[("ps", bi_)], w=[("pooledT", k)])
                    ev += 1
            for t in range(NT):
                for dh in range(2):
                    bi_ = dh + 2 * (t % 2)
                    bank = ps[bi_]
                    for gg in range(2):
                        g = dh * 2 + gg
                        o = bank[:, gg * 256:(gg + 1) * 256]
                        for ic in range(2):
                            mm(o, pooledT[:, 2 * g + ic, t * 128:(t + 1) * 128], pws[:, g, ic, :], ic == 0, False,
                               r=[("pooledT", 2 * g + ic), "pws"], w=[("ps", bi_)])
                        mm(o, cst16[0:1, 3, :], bs16[0:1, g * 256:(g + 1) * 256], False, True, r=["cst16", "bs16"], w=[("ps", bi_)])
                    hv = h[:, t, dh * 512:(dh + 1) * 512]
                    tt("dve", hv, hv, bank[:, :], ALU.add, r=[("ps", bi_), ("h", t, dh)], w=[("h", t, dh)])

        def mix0():
            A.reset()
            rms_stats(list(range(NT)))
            normT("mix_even")
            A.reset()
            hnk = [("hnT", t) for t in range(NT)]
            wbuf = [A.a([128, 8, 128], BF16) for _ in range(3)]
            wcnt = [0]

            def load_wchunk(cc):
                i = wcnt[0] % 3
                wcnt[0] += 1
                dma("pool", wbuf[i], w_in_d[cc], w=[("wbuf", i)])
                return i

            def proj_blk(wi, bi, t0, n):
                bk = ("ps", bi % 2)
                bank = ps[bi % 2]
                for k in range(8):
                    mm(bank[:, 0:n], wbuf[wi][:, k, :], hnT[:, k, t0:t0 + n], k == 0, k == 7, r=[("wbuf", wi)] + hnk, w=[bk])
                return bank, bk

            xinb = [A.a([128, 3 + 512]) for _ in range(2)]
            ctb = [A.a([128, 512]) for _ in range(2)]
            cvc = [0]

            def conv_blk(bank, bk, bi, n, wname, bname, cidx, tap0_act=False, copy_eng="act"):
                i = cvc[0] % 2
                cvc[0] += 1
                xi, xk = xinb[i], ("xinb", i)
                if bi == 0:
                    memset("dve", xi[:, 0:3], 0.0, w=[xk])
                else:
                    pv = xinb[1 - i]
                    cp("dve", xi[:, 0:3], pv[:, 512:515], r=[("xinb", 1 - i)], w=[xk])
                cp(copy_eng, xi[:, 3:3 + n], bank[:, 0:n], r=[bk], w=[xk])
                ct, ck = ctb[i], ("ctb", i)
                o4 = COLP[wname][0] + 4 * cidx
                if tap0_act:
                    act(ct[:, 0:n], xi[:, 0:n], AF.Identity, r=[xk, "colp"], w=[ck], scale=colp[:, o4:o4 + 1], bias=colv(bname, cidx))
                else:
                    ts("dve", ct[:, 0:n], xi[:, 0:n], colp[:, o4:o4 + 1], ALU.mult, r=[xk, "colp"], w=[ck], s2=colv(bname, cidx), op1=ALU.add)
                for k in range(1, 4):
                    stt(ct[:, 0:n], xi[:, k:k + n], colp[:, o4 + k:o4 + k + 1], ct[:, 0:n], ALU.mult, ALU.add, r=[xk, ck, "colp"], w=[ck])
                return ct, ck

            wo = A.a([128, 4, D], BF16)
            ybuf = A.a([128, 4, T], BF16)

            def out_proj(kc0, scale_ap_fn):
                dma("pool", wo, w_out_d[kc0 * 128:(kc0 + 4) * 128, :].rearrange("(k p) d -> p k d", p=128), w=["wo"])
                cnt = 0
                for t in range(NT):
                    for dh in range(2):
                        io = 4 + cnt % 2
                        cnt += 1
                        for kk in range(4):
                            mm(ps[io][:, :], ybuf[:, kk, t * 128:(t + 1) * 128], wo[:, kk, dh * 512:(dh + 1) * 512], kk == 0, kk == 3,
                               r=[("ybuf", kk), "wo"], w=[("ps", io)])
                        hv = h[:, t, dh * 512:(dh + 1) * 512]
                        if scale_ap_fn is None:
                            tt("dve", hv, hv, ps[io][:, :], ALU.add, r=[("ps", io), ("h", t, dh)], w=[("h", t, dh)])
                        else:
                            stt(hv, ps[io][:, :], scale_ap_fn(t), hv, ALU.mult, ALU.add, r=[("ps", io), ("h", t, dh), "rstdg"], w=[("h", t, dh)])

            mark = A.off

            def lru():
                bdA = A.a([128, 8, 128], BF16)
                bdX = A.a([128, 8, 128], BF16)
                memset("dve", bdA, 0.0, w=["bdA"])
                memset("dve", bdX, 0.0, w=["bdX"])
                for src, dst, nm in ((lwa_d, bdA, "bdA"), (lwx_d, bdX, "bdX")):
                    v = src.rearrange("(j two) i o -> two i j o", two=2)
                    dma("pool", dst[0:64, :, 0:64], v[0], w=[nm])
                    dma("pool", dst[64:128, :, 64:128], v[1], w=[nm])
                c1 = A.a([128, 8])
                tmpc = A.a([128, 8])
                act(tmpc, colv("lru_lam", 0, 8), AF.Exp, r=["colp"], w=["tmpc"], scale=-1.0)
                act(tmpc, tmpc, AF.Ln, r=["tmpc"], w=["tmpc"], bias=1.0)
                ts("dve", c1, tmpc, -8.0, ALU.mult, r=["tmpc"], w=["c1"])
                Bn = lambda n_: [A.a([128, 512]) for _ in range(n_)]
                gl, rr, ii, av, uv, hl = Bn(4), Bn(4), Bn(4), Bn(4), Bn(2), Bn(2)
                xb16 = [A.a([128, 512], BF16) for _ in range(4)]
                NB = len(TB)
                items = [(j, bi) for j in range(8) for bi in range(NB)]
                st1 = {}

                def S1(idx):
                    j, bi = items[idx]
                    t0, n = TB[bi]
                    if bi == 0:
                        st1[j] = (load_wchunk(28 + j), load_wchunk(20 + j))
                    wi_in, wi_gt = st1[j]
                    bank, bk = proj_blk(wi_in, 2 * idx, t0, n)
                    xb, xbk = conv_blk(bank, bk, bi, n, "lru_cw", "lru_cb", j, copy_eng="dve")
                    bank2, bk2 = proj_blk(wi_gt, 2 * idx + 1, t0, n)
                    g3 = idx % 4
                    act(gl[g3][:, 0:n], bank2[:, 0:n], AF.Gelu_apprx_tanh, r=[bk2], w=[("gl", g3)])
                    cp("act", xb16[g3][:, 0:n], xb[:, 0:n], r=[xbk], w=[("xb16", g3)])
                    st1[idx, "xb"] = (xb, xbk)

                def S2(idx):
                    j, bi = items[idx]
                    t0, n = TB[bi]
                    ip = idx % 2
                    q = idx % 4
                    K = lambda nm: (nm, q)
                    xb, xbk = st1.pop((idx, "xb"))
                    pa, px = ps[2 + ip], ps[4 + ip]
                    mm(pa[:, 0:n], bdA[:, j, :], xb16[q][:, 0:n], True, True, r=["bdA", K("xb16")], w=[("ps", 2 + ip)])
                    mm(px[:, 0:n], bdX[:, j, :], xb16[q][:, 0:n], True, True, r=["bdX", K("xb16")], w=[("ps", 4 + ip)])
                    r_, i_ = rr[q], ii[q]
                    act(r_[:, 0:n], pa[:, 0:n], AF.Sigmoid, r=[("ps", 2 + ip), "colp"], w=[K("rr")], bias=colv("lru_ba", j))
                    act(i_[:, 0:n], px[:, 0:n], AF.Sigmoid, r=[("ps", 4 + ip), "colp"], w=[K("ii")], bias=colv("lru_bx", j))
                    act(av[q][:, 0:n], r_[:, 0:n], AF.Exp, r=[K("rr"), "c1"], w=[K("av")], scale=c1[:, j:j + 1])
                    stt(r_[:, 0:n], av[q][:, 0:n], 0.9999998, av[q][:, 0:n], ALU.min, ALU.mult, r=[K("av")], w=[K("rr")])
                    act(r_[:, 0:n], r_[:, 0:n], AF.Sqrt, r=[K("rr")], w=[K("rr")], scale=-1.0, bias=1.0)
                    tt("dve", i_[:, 0:n], i_[:, 0:n], xb[:, 0:n], ALU.mult, r=[K("ii"), xbk], w=[K("ii")])
                    if bi == 0:
                        memset("dve", r_[:, PADN:PADN + 1], 1.0, w=[K("rr")])

                def S3(idx):
                    j, bi = items[idx]
                    t0, n = TB[bi]
                    i = idx % 2
                    q = idx % 4
                    jj = j % 4
                    K = lambda nm: (nm, q)
                    tt("dve", uv[i][:, 0:n], rr[q][:, 0:n], ii[q][:, 0:n], ALU.mult, r=[K("rr"), K("ii")], w=[("uv", i)])
                    if bi == 0:
                        memset("dve", hl[i][:, 0:PADN], 0.0, w=[("hl", i)])
                        S.add("dve", lambda e, i=i, q=q, n=n: e.tensor_tensor_scan(out=hl[i][:, PADN:n], data0=av[q][:, PADN:n], data1=uv[i][:, PADN:n],
                                                                             initial=0.0, op0=ALU.mult, op1=ALU.add),
                              r=[K("av"), ("uv", i)], w=[("hl", i)], cost=1.2)
                    else:
                        S.add("dve", lambda e, i=i, q=q, n=n: e.tensor_tensor_scan(out=hl[i][:, 0:n], data0=av[q][:, 0:n], data1=uv[i][:, 0:n],
                                                                             initial=hl[1 - i][:, 511:512], op0=ALU.mult, op1=ALU.add),
                              r=[K("av"), ("uv", i), ("hl", 1 - i)], w=[("hl", i)], cost=1.2)
                    tt("dve", ybuf[:, jj, t0:t0 + n], hl[i][:, 0:n], gl[q][:, 0:n], ALU.mult, r=[("hl", i), ("gl", q)], w=[("ybuf", jj)])
                    if bi == NB - 1 and jj == 3:
                        out_proj(8 + (j // 4) * 4, None)

                NI = len(items)
                for step in range(NI + 2):
                    if step < NI:
                        S1(step)
                    if 0 <= step - 1 < NI:
                        S2(step - 1)
                    if 0 <= step - 2 < NI:
                        S3(step - 2)

            def ssd():
                dt = A.a([128, NT, 16])
                adt = A.a([128, NT, 16])
                ea = A.a([128, NT, 16])
                eatot = A.a([128, NT, 16])
                dte = A.a([128, NT, 16])
                Aneg = A.a([128, 16])
                wdt = A.a([128, 8, 16], BF16)
                dma("pool", wdt, w_dt_d, w=["wdt"])
                for t in range(NT):
                    for k in range(8):
                        mm(ps[7][:, t * 16:(t + 1) * 16], hnT[:, k, t * 128:(t + 1) * 128], wdt[:, k, :], k == 0, k == 7,
                           r=["wdt", ("hnT", t)], w=[("ps", 7)])
                p7 = ps[7][:, 0:NT * 16].rearrange("p (t h) -> p t h", t=NT)
                tt("dve", dt, p7, rowv("dt_bias").unsqueeze(1).to_broadcast([128, NT, 16]), ALU.add, r=[("ps", 7), "rowp"], w=["dt"])
                act(dte, dt, AF.Abs, r=["dt"], w=["dte"])
                act(dte, dte, AF.Exp, r=["dte"], w=["dte"], scale=-1.0)
                act(dte, dte, AF.Ln, r=["dte"], w=["dte"], bias=1.0)
                stt(dt, dt, 0.0, dte, ALU.max, ALU.add, r=["dt", "dte"], w=["dt"])
                memset("dve", dt[0:PADN, 0, :], 0.0, w=["dt"])
                act(Aneg, rowv("a_log"), AF.Exp, r=["rowp"], w=["Aneg"])
                stt(adt, dt, -1.0, Aneg.unsqueeze(1).to_broadcast([128, NT, 16]), ALU.mult, ALU.mult, r=["dt", "Aneg"], w=["adt"])
                for t in range(NT):
                    mm(ps[6][:, t * 16:(t + 1) * 16], tri_le, adt[:, t, :], True, True, r=["cst", "adt"], w=[("ps", 6)])
                for t in range(NT):
                    mm(ps[7][:, t * 16:(t + 1) * 16], ones32, adt[:, t, :], True, True, r=["cst", "adt"], w=[("ps", 7)])
                p6 = ps[6][:, 0:NT * 16].rearrange("p (t h) -> p t h", t=NT)
                cp("dve", ea, p6, r=[("ps", 6)], w=["ea"])
                cp("dve", eatot, p7, r=[("ps", 7)], w=["eatot"])
                tt("dve", dte, eatot, ea, ALU.subtract, r=["eatot", "ea"], w=["dte"])
                act(dte, dte, AF.Exp, r=["dte"], w=["dte"])
                act(ea, ea, AF.Exp, r=["ea", "dte"], w=["ea"])
                act(eatot, eatot, AF.Exp, r=["eatot", "dte"], w=["eatot"])
                Dbc = rowv("ssd_d")
                DI = A.a([128, 2, 128], BF16)
                if SSD_STOP == "dt":
                    return

                BT = A.a([128, T], BF16)
                CT = A.a([128, T], BF16)
                Btok = A.a([128, NT, 128], BF16)
                CBm = A.a([128, NT, 128], BF16)
                xT16 = [A.a([128, 512], BF16) for _ in range(2)]
                xtok = A.a([128, NT, 128], BF16)
                xdt = A.a([128, NT, 128], BF16)
                sz = A.a([128, NT, 128], BF16)
                MT_all = A.a([128, NT, 256], BF16)
                S_all = A.a([128, NT, 128], BF16)
                ssp = A.a([128, NT, 4])
                rstdg = A.a([128, NT])
                Sst = A.a([128, 128])
                Lb = [A.a([128, 256]) for _ in range(2)]
                Db = [A.a([128, 256]) for _ in range(2)]
                xdte = [A.a([128, 128], BF16) for _ in range(4)]
                ytmp = [A.a([128, 128]) for _ in range(4)]
                yg16 = [A.a([128, 128], BF16) for _ in range(4)]
                junk = A.a([128, 128], BF16)

                def bank16(i):
                    return ps[i][:, 0:256].bitcast(BF16)

                def fm_chunk(cc, cidx, sink):
                    wi = load_wchunk(cc)
                    for bi, (t0, n) in enumerate(TB):
                        bank, bk = proj_blk(wi, bi, t0, n)
                        ct, ck = conv_blk(bank, bk, bi, n, "ssd_cw", "ssd_cb", cidx, tap0_act=True)
                        sink(bi, t0, n, ct, ck)

                for g in range(2):
                    pass
                    fm_chunk(8 + 8 + g, 8 + g, lambda bi, t0, n, ct, ck: act(BT[:, t0:t0 + n], ct[:, 0:n], AF.Silu, r=[ck], w=["BT"]))
                    fm_chunk(8 + 10 + g, 10 + g, lambda bi, t0, n, ct, ck: act(CT[:, t0:t0 + n], ct[:, 0:n], AF.Silu, r=[ck], w=["CT"]))
                    for q in range(5):
                        tiles = list(range(4 * q, min(4 * q + 4, NT)))
                        nn = len(tiles)
                        bk = bank16(6 + q % 2)
                        for i, t in enumerate(tiles):
                            tr(bk[:, i * 128:(i + 1) * 128], BT[:, t * 128:(t + 1) * 128], ident16, r=["BT", "cst16"], w=[("ps", 6 + q % 2)])
                        cp("dve", Btok[:, 4 * q:4 * q + nn, :], bk[:, 0:nn * 128].rearrange("p (a b) -> p a b", a=nn), r=[("ps", 6 + q % 2)], w=["Btok"])
                        bank = ps[2 + q % 2]
                        for i, t in enumerate(tiles):
                            mm(bank[:, i * 128:(i + 1) * 128], BT[:, t * 128:(t + 1) * 128], CT[:, t * 128:(t + 1) * 128], True, True,
                               r=["BT", "CT"], w=[("ps", 2 + q % 2)])
                        tt("dve", CBm[:, 4 * q:4 * q + nn, :], bank[:, 0:nn * 128].rearrange("p (a b) -> p a b", a=nn),
                           tri_le.unsqueeze(1).to_broadcast([128, nn, 128]), ALU.mult, r=[("ps", 2 + q % 2), "cst"], w=["CBm"])
                    pass
                    if SSD_STOP == "bc":
                        return
                    for jj in range(4):
                        j = g * 4 + jj
                        hh0 = 2 * j

                        def xsink(bi, t0, n, ct, ck, hh0=hh0):
                            xt = xT16[bi % 2]
                            xk = ("xT16", bi % 2)
                            act(xt[:, 0:n], ct[:, 0:n], AF.Silu, r=[ck], w=[xk])
                            nn = n // 128
                            bk = bank16(6 + bi % 2)
                            for i in range(nn):
                                tr(bk[:, i * 128:(i + 1) * 128], xt[:, i * 128:(i + 1) * 128], ident16, r=[xk, "cst16"], w=[("ps", 6 + bi % 2)])
                            a0 = t0 // 128
                            cp("act", xtok.rearrange("p a b -> p (a b)")[:, a0 * 128:(a0 + nn) * 128], bk[:, 0:nn * 128], r=[("ps", 6 + bi % 2)], w=[("xtok", bi)])
                            for i in range(nn):
                                t = a0 + i
                                tt("dve", xdt[:, t, :].rearrange("p (h c) -> p h c", h=2), xtok[:, t, :].rearrange("p (h c) -> p h c", h=2),
                                   dt[:, t, hh0:hh0 + 2].unsqueeze(2).to_broadcast([128, 2, 64]), ALU.mult, r=[("xtok", bi), "dt"], w=[("xdt", t)])
                        fm_chunk(8 + j, j, xsink)
                        if SSD_STOP == "x":
                            return
                        wz = load_wchunk(j)
                        for bi, (t0, n) in enumerate(TB):
                            bank, bk_ = proj_blk(wz, bi, t0, n)
                            zt = xT16[bi % 2]
                            zk = ("xT16", bi % 2)
                            act(zt[:, 0:n], bank[:, 0:n], AF.Silu, r=[bk_], w=[zk])
                            nn = n // 128
                            bk = bank16(6 + bi % 2)
                            for i in range(nn):
                                tr(bk[:, i * 128:(i + 1) * 128], zt[:, i * 128:(i + 1) * 128], ident16, r=[zk, "cst16"], w=[("ps", 6 + bi % 2)])
                            a0 = t0 // 128
                            cp("dve", sz.rearrange("p a b -> p (a b)")[:, a0 * 128:(a0 + nn) * 128], bk[:, 0:nn * 128], r=[("ps", 6 + bi % 2)], w=[("sz", bi)])
                        if SSD_STOP == "z":
                            return
                        memset("dve", Sst, 0.0, w=["Sst"])
                        memset("dve", S_all[:, 0, :], 0.0, w=[("S_all", 0)])
                        for hh in range(2):
                            act(DI[:, hh, :], ident, AF.Identity, r=["cst", "rowp"], w=["DI"], scale=Dbc[:, hh0 + hh:hh0 + hh + 1])
                        for c in range(NT):
                            i2 = c % 2
                            i4 = c % 4
                            for hh in range(2):
                                act(Lb[i2][:, hh * 128:(hh + 1) * 128], u_gt, AF.Identity, r=["cst", "adt"], w=[("L", i2, hh)], scale=adt[:, c, hh0 + hh:hh0 + hh + 1])
                                mm(ps[i2][:, hh * 128:(hh + 1) * 128], Lb[i2][:, hh * 128:(hh + 1) * 128], tri_le, True, True, r=[("L", i2, hh), "cst"], w=[("ps", i2)])
                            act(Db[i2], ps[i2][:, 0:256], AF.Exp, r=[("ps", i2)], w=[("D", i2)])
                            tt("dve", MT_all[:, c, :].rearrange("p (h l) -> p h l", h=2), Db[i2].rearrange("p (h l) -> p h l", h=2),
                               CBm[:, c, :].unsqueeze(1).to_broadcast([128, 2, 128]), ALU.mult, r=[("D", i2), "CBm"], w=[("MT", c)])
                            if c + 1 < NT:
                                tt("dve", xdte[i4].rearrange("p (h c) -> p h c", h=2), xdt[:, c, :].rearrange("p (h c) -> p h c", h=2),
                                   dte[:, c, hh0:hh0 + 2].unsqueeze(2).to_broadcast([128, 2, 64]), ALU.mult, r=[("xdt", c), "dte"], w=[("xdte", i4)])
                                mm(ps[2 + i2][:, 0:128], Btok[:, c, :], xdte[i4], True, True, r=["Btok", ("xdte", i4)], w=[("ps", 2 + i2)])
                                tt("dve", Sst.rearrange("p (h c) -> p h c", h=2), Sst.rearrange("p (h c) -> p h c", h=2),
                                   eatot[:, c, hh0:hh0 + 2].unsqueeze(2).to_broadcast([128, 2, 64]), ALU.mult, r=["Sst", "eatot"], w=["Sst"])
                                tt("dve", Sst, Sst, ps[2 + i2][:, 0:128], ALU.add, r=[("ps", 2 + i2), "Sst"], w=["Sst"])
                                cp("act", S_all[:, c + 1, :], Sst, r=["Sst"], w=[("S_all", c + 1)])
                        def p3_mm(c):
                            b = 4 + c % 2
                            pa = ps[b]
                            mm(pa[:, 0:128], CT[:, c * 128:(c + 1) * 128], S_all[:, c, :], True, True, r=["CT", ("S_all", c)], w=[("ps", b)])
                            for hh in range(2):
                                o = pa[:, 128 + hh * 64:128 + (hh + 1) * 64]
                                mm(o, MT_all[:, c, hh * 128:(hh + 1) * 128], xdt[:, c, hh * 64:(hh + 1) * 64], True, False, r=[("MT", c), ("xdt", c)], w=[("ps", b)])
                                mm(o, DI[:, hh, :], xtok[:, c, hh * 64:(hh + 1) * 64], False, True, r=["DI"] + [("xtok", bb) for bb in range(5)], w=[("ps", b)])

                        def p3_ev(c, jj=jj, j=j, hh0=hh0):
                            b = 4 + c % 2
                            pa = ps[b]
                            i4 = c % 4
                            y, yk = ytmp[i4], ("ytmp", i4)
                            tt("dve", y.rearrange("p (h c) -> p h c", h=2), pa[:, 0:128].rearrange("p (h c) -> p h c", h=2),
                               ea[:, c, hh0:hh0 + 2].unsqueeze(2).to_broadcast([128, 2, 64]), ALU.mult, r=[("ps", b), "ea"], w=[yk])
                            tt("dve", y, y, pa[:, 128:256], ALU.add, r=[("ps", b), yk], w=[yk])
                            yt, ytk = yg16[i4], ("yg", i4)
                            tt("dve", yt, y, sz[:, c, :], ALU.mult, r=[yk] + [("sz", bb) for bb in range(5)], w=[ytk])
                            act(junk, yt, AF.Square, r=[ytk], w=[("ssp", c), "junk"], accum=ssp[:, c, jj:jj + 1])
                            bk = bank16(6 + c % 2)
                            tr(bk[:, 0:128], yt, ident16, r=[ytk, "cst16"], w=[("ps", 6 + c % 2)])
                            act(ybuf[:, jj, c * 128:(c + 1) * 128], bk[:, 0:128], AF.Identity, r=[("ps", 6 + c % 2), "colp"], w=[("ybuf", jj)],
                                scale=colv("ssd_norm", j))

                        p3_mm(0)
                        for c in range(NT):
                            if c + 1 < NT:
                                p3_mm(c + 1)
                            p3_ev(c)
                    if SSD_STOP == "rec":
                        return
                    pass
                    S.add("dve", lambda e: e.tensor_reduce(out=rstdg, in_=ssp, axis=AX.X, op=ALU.add), r=[("ssp", c) for c in range(NT)], w=["rstdg"])
                    act(rstdg, rstdg, AF.Sqrt, r=["rstdg"], w=["rstdg"], scale=1.0 / 512, bias=colv_eps)
                    S.add("dve", lambda e: e.reciprocal(rstdg, rstdg), r=["rstdg"], w=["rstdg"])
                    out_proj(g * 4, lambda t: rstdg[:, t:t + 1])

            if "lru" in MIX0_PARTS:
                lru()
            S.barrier()
            A.off = mark
            if "ssd" in MIX0_PARTS:
                ssd()

        for ph in phases:
            {"mix0": mix0, "moe0": lambda: moe(0), "mix1": mix1, "moe1": lambda: moe(1)}[ph]()

        if dbg or not phases or not phases[-1].startswith("moe"):
            A.reset()
        if dbg:
            for t in range(NT):
                dma("sp", out_d[t * 128:(t + 1) * 128, :], h[:, t, :], r=HK(t), is_output=True)
        else:
            rms_stats(list(range(1, NT)))
            nf = A.a([128, D])
            dma("sp", nf, big_d[:, 0, :].partition_broadcast(128), w=["nf"])
            ob = [A.a([128, D]) for _ in range(2)]
            for t in range(1, NT):
                o = ob[t % 2]
                stt(o, h[:, t, :], rstd[:, t:t + 1], nf, ALU.mult, ALU.mult, r=HK(t) + [("rstd", t), "nf"], w=[("ob", t % 2)])
                dma("sp", out_d[(t - 1) * 128:t * 128, :], o, r=[("ob", t % 2)], is_output=True)
        if REORDER:
            S.reorder()
        S.emit()
    return nc


_CACHE = {}


def kernel(**inputs):
    shared = pack_inputs(inputs)
    x = np.ascontiguousarray(np.asarray(inputs["x"], np.float32))
    nb = x.shape[0]
    if "nc" not in _CACHE:
        _CACHE["nc"] = build()
    nc = _CACHE["nc"]
    in_maps = [dict(shared, x=x[b]) for b in range(nb)]
    res = run_bass_kernel_spmd(nc, in_maps, core_ids=list(range(nb)))
    return np.stack([np.asarray(r["out"], np.float32) for r in res.results], axis=0)
```
